# Optimizing a Trainium2 kernel written in Bass

```python
import jax, jax.numpy as jnp
from jax import lax
import numpy as np

D_MODEL = 2048
BATCH = 4
SEQ = 8192
DEPTH = 1

ATTN_WIDTH = D_MODEL // 2
N_HEADS = 8
HEAD_DIM = ATTN_WIDTH // N_HEADS
MOBA_BLOCK = 256
MOBA_TOPK = 3
Q_CHUNK = 32
POOL_WIDTH = D_MODEL - ATTN_WIDTH
POOL_WINDOWS = (2, 4, 8, 16)
N_POOL_GROUPS = len(POOL_WINDOWS)
POOL_GROUP = POOL_WIDTH // N_POOL_GROUPS
MIX_WIDTH = ATTN_WIDTH + POOL_WIDTH
IN_WIDTH = 3 * ATTN_WIDTH + POOL_WIDTH
N_GROUPS = 8
EXPERTS_PER_GROUP = 8
N_EXPERTS = N_GROUPS * EXPERTS_PER_GROUP
TOPK_INNER = 2
D_EXPERT = D_MODEL // 4
DISPATCH_BLOCK = 128
PLE_DIM = 256
EPS = 1e-6

kernel_name = 'hymba_moba_pool_hmoe_ple'


def rms_norm(x, gain):
    xf = x.astype(jnp.float32)
    y = xf * lax.rsqrt(jnp.mean(xf * xf, axis=-1, keepdims=True) + EPS)
    return (y * gain.astype(jnp.float32)).astype(x.dtype)


def alibi_slopes(n):
    return jnp.asarray([2.0 ** (-8.0 * (h + 1) / n) for h in range(n)], jnp.float32)


def moba_attention(q, k, v):
    B, S = q.shape[0], q.shape[1]
    nb = -(-S // MOBA_BLOCK)
    pad = nb * MOBA_BLOCK - S
    K = min(MOBA_TOPK, nb)
    q = q.transpose(0, 2, 1, 3)
    k = jnp.pad(k.transpose(0, 2, 1, 3), ((0, 0), (0, 0), (0, pad), (0, 0)))
    v = jnp.pad(v.transpose(0, 2, 1, 3), ((0, 0), (0, 0), (0, pad), (0, 0)))
    kb = k.reshape(B, N_HEADS, nb, MOBA_BLOCK, HEAD_DIM)
    vb = v.reshape(B, N_HEADS, nb, MOBA_BLOCK, HEAD_DIM)
    kmean = jnp.mean(kb.astype(jnp.float32), axis=3).astype(k.dtype)
    slopes = alibi_slopes(N_HEADS)[None, :, None, None]
    scale = HEAD_DIM ** -0.5
    bi = jnp.arange(B)[:, None, None, None]
    hi = jnp.arange(N_HEADS)[None, :, None, None]
    blk_ids = jnp.arange(nb)
    in_blk = jnp.arange(MOBA_BLOCK)
    rank = jnp.arange(K)

    def chunk(c):
        start = c * Q_CHUNK
        own = start // MOBA_BLOCK
        qc = lax.dynamic_slice_in_dim(q, start, Q_CHUNK, axis=2)
        qpos = start + jnp.arange(Q_CHUNK)
        gate = jnp.einsum('bhqd,bhnd->bhqn', qc, kmean).astype(jnp.float32)
        gate = jnp.where(blk_ids < own, gate, -jnp.inf)
        _, sel = lax.top_k(gate, K)
        sel_ok = rank < own
        k_sel = kb[bi, hi, sel]
        v_sel = vb[bi, hi, sel]
        s_sel = jnp.einsum('bhqd,bhqnkd->bhqnk', qc, k_sel).astype(jnp.float32) * scale
        kpos_sel = sel[..., None] * MOBA_BLOCK + in_blk
        dist_sel = (qpos[:, None, None] - kpos_sel).astype(jnp.float32)
        s_sel = s_sel - slopes[..., None] * dist_sel
        s_sel = jnp.where(sel_ok[:, None], s_sel, -jnp.inf)
        k_own = lax.dynamic_index_in_dim(kb, own, axis=2, keepdims=False)
        v_own = lax.dynamic_index_in_dim(vb, own, axis=2, keepdims=False)
        s_own = jnp.einsum('bhqd,bhkd->bhqk', qc, k_own).astype(jnp.float32) * scale
        dist_own = qpos[:, None] - (own * MOBA_BLOCK + in_blk)[None, :]
        s_own = jnp.where(dist_own >= 0, s_own - slopes * dist_own.astype(jnp.float32), -jnp.inf)
        s = jnp.concatenate([s_sel.reshape(B, N_HEADS, Q_CHUNK, K * MOBA_BLOCK), s_own], axis=-1)
        prob = jax.nn.softmax(s, axis=-1).astype(v.dtype)
        p_sel = prob[..., :K * MOBA_BLOCK].reshape(B, N_HEADS, Q_CHUNK, K, MOBA_BLOCK)
        p_own = prob[..., K * MOBA_BLOCK:]
        return (jnp.einsum('bhqnk,bhqnkd->bhqd', p_sel, v_sel)
                + jnp.einsum('bhqk,bhkd->bhqd', p_own, v_own))

    outs = lax.map(chunk, jnp.arange(S // Q_CHUNK))
    return outs.transpose(1, 0, 3, 2, 4).reshape(B, S, N_HEADS * HEAD_DIM)


def pool_mixer(u, w_pool):
    B, S, _ = u.shape
    uf = u.astype(jnp.float32).reshape(B, S, N_POOL_GROUPS, POOL_GROUP)
    csum = jnp.concatenate([jnp.zeros_like(uf[:, :1]), jnp.cumsum(uf, axis=1)], axis=1)
    t = jnp.arange(S)[:, None]
    win = jnp.asarray(POOL_WINDOWS, jnp.int32)[None, :]
    lo = jnp.maximum(t + 1 - win, 0)
    gi = jnp.arange(N_POOL_GROUPS)[None, :]
    window_sum = csum[:, 1:] - csum[:, lo, gi]
    count = jnp.minimum(t + 1, win).astype(jnp.float32)
    z = window_sum / count[None, :, :, None] - uf
    z = jnp.einsum('bsgc,gcd->bsgd', z.astype(u.dtype), w_pool)
    return z.reshape(B, S, POOL_WIDTH)


def hier_moe(xn, w_rg, b_rg, w_re, b_re, w_gate, w_up, w_down):
    B, S, D = xn.shape
    T = B * S
    xt = xn.reshape(T, D)
    g_prob = jax.nn.softmax((xt @ w_rg).astype(jnp.float32) + b_rg.astype(jnp.float32), axis=-1)
    g_val, g_idx = lax.top_k(g_prob, 1)
    g_val, g_idx = g_val[:, 0], g_idx[:, 0]
    e_logits = jnp.einsum('td,gde->tge', xt, w_re).astype(jnp.float32) + b_re.astype(jnp.float32)
    e_logits = jnp.take_along_axis(e_logits, g_idx[:, None, None], axis=1)[:, 0]
    e_val, e_idx = lax.top_k(jax.nn.softmax(e_logits, axis=-1), TOPK_INNER)
    e_val = e_val / jnp.sum(e_val, axis=-1, keepdims=True)
    weights = g_val[:, None] * e_val
    eid = g_idx[:, None] * EXPERTS_PER_GROUP + e_idx
    N = T * TOPK_INNER
    M = DISPATCH_BLOCK
    flat_e = eid.reshape(N)
    flat_w = weights.reshape(N)
    flat_t = jnp.repeat(jnp.arange(T, dtype=jnp.int32), TOPK_INNER)
    order = jnp.argsort(flat_e)
    se, st, sw = flat_e[order], flat_t[order], flat_w[order]
    counts = jnp.bincount(flat_e, length=N_EXPERTS)
    starts = jnp.cumsum(counts) - counts
    padded = (counts + M - 1) // M * M
    pend = jnp.cumsum(padded)
    pstarts = pend - padded
    dest = pstarts[se] + (jnp.arange(N) - starts[se])
    P = N + N_EXPERTS * M
    nblk = P // M
    tok_buf = jnp.zeros((P,), jnp.int32).at[dest].set(st)
    w_buf = jnp.zeros((P,), jnp.float32).at[dest].set(sw)
    blk_e = jnp.clip(jnp.searchsorted(pend, jnp.arange(nblk) * M, side='right'), 0, N_EXPERTS - 1)

    def run(args):
        toks, wts, e = args
        xb = xt[toks]
        hid = jax.nn.silu(xb @ w_gate[e]) * (xb @ w_up[e])
        return (hid @ w_down[e]) * wts[:, None].astype(xb.dtype)

    yb = lax.map(run, (tok_buf.reshape(nblk, M), w_buf.reshape(nblk, M), blk_e))
    y = jax.ops.segment_sum(yb.reshape(P, D), tok_buf, num_segments=T)
    return y.reshape(B, S, D)


def setup_inputs(seed: int = 0) -> dict:
    key = jax.random.key(seed)
    ks = jax.random.split(key, 24)
    f32 = jnp.float32
    nrm = lambda k, shape, s: jax.random.normal(k, shape, f32) * s
    gain = lambda k, shape: 1.0 + 0.01 * jax.random.normal(k, shape, f32)
    L = DEPTH
    return {
        'x': nrm(ks[0], (BATCH, SEQ, D_MODEL), 1.0),
        'p': nrm(ks[1], (DEPTH, BATCH, SEQ, PLE_DIM), 1.0),
        'g_mix': gain(ks[2], (L, D_MODEL)),
        'w_in': nrm(ks[3], (L, D_MODEL, IN_WIDTH), D_MODEL ** -0.5),
        'beta_attn': gain(ks[4], (L, ATTN_WIDTH)),
        'w_pool': nrm(ks[5], (L, N_POOL_GROUPS, POOL_GROUP, POOL_GROUP), POOL_GROUP ** -0.5),
        'pool_scale': gain(ks[6], (L, POOL_WIDTH)),
        'w_out': nrm(ks[7], (L, MIX_WIDTH, D_MODEL), MIX_WIDTH ** -0.5),
        'g_ffn': gain(ks[8], (L, D_MODEL)),
        'w_router_group': nrm(ks[9], (L, D_MODEL, N_GROUPS), D_MODEL ** -0.5),
        'b_router_group': nrm(ks[10], (L, N_GROUPS), 0.01),
        'w_router_expert': nrm(ks[11], (L, N_GROUPS, D_MODEL, EXPERTS_PER_GROUP), D_MODEL ** -0.5),
        'b_router_expert': nrm(ks[12], (L, N_GROUPS, EXPERTS_PER_GROUP), 0.01),
        'w_expert_gate': nrm(ks[13], (L, N_EXPERTS, D_MODEL, D_EXPERT), D_MODEL ** -0.5),
        'w_expert_up': nrm(ks[14], (L, N_EXPERTS, D_MODEL, D_EXPERT), D_MODEL ** -0.5),
        'w_expert_down': nrm(ks[15], (L, N_EXPERTS, D_EXPERT, D_MODEL), D_EXPERT ** -0.5),
        'g_ple': gain(ks[16], (L, D_MODEL)),
        'w_ple': nrm(ks[17], (L, PLE_DIM, D_MODEL), PLE_DIM ** -0.5),
        'w_ple_gate': nrm(ks[18], (L, D_MODEL, D_MODEL), D_MODEL ** -0.5),
        'b_ple_gate': nrm(ks[19], (L, D_MODEL), 0.01),
        'g_final': gain(ks[20], (D_MODEL,)),
    }


def reference(x, p, g_mix, w_in, beta_attn, w_pool, pool_scale, w_out, g_ffn,
              w_router_group, b_router_group, w_router_expert, b_router_expert,
              w_expert_gate, w_expert_up, w_expert_down, g_ple, w_ple, w_ple_gate,
              b_ple_gate, g_final):
    B, S, _ = x.shape
    A = ATTN_WIDTH
    h = x
    for i in range(DEPTH):
        a = rms_norm(h, g_mix[i])
        proj = a @ w_in[i]
        q = proj[..., :A].reshape(B, S, N_HEADS, HEAD_DIM)
        k = proj[..., A:2 * A].reshape(B, S, N_HEADS, HEAD_DIM)
        v = proj[..., 2 * A:3 * A].reshape(B, S, N_HEADS, HEAD_DIM)
        u = proj[..., 3 * A:]
        o_attn = moba_attention(q, k, v)
        o_pool = pool_mixer(u, w_pool[i])
        mixed = jnp.concatenate([rms_norm(o_attn, beta_attn[i]),
                                 rms_norm(o_pool, pool_scale[i])], axis=-1)
        h = h + mixed @ w_out[i]
        f = rms_norm(h, g_ffn[i])
        h = h + hier_moe(f, w_router_group[i], b_router_group[i], w_router_expert[i],
                         b_router_expert[i], w_expert_gate[i], w_expert_up[i], w_expert_down[i])
        gate = jax.nn.sigmoid(rms_norm(h, g_ple[i]) @ w_ple_gate[i] + b_ple_gate[i])
        h = h + gate * (p[i] @ w_ple[i])
    return rms_norm(h, g_final)
```

```python
import numpy as np
import concourse.bass as bass
import concourse.mybir as mybir
from concourse.bass_utils import run_bass_kernel_spmd

F32 = mybir.dt.float32
BF16 = mybir.dt.bfloat16
I32 = mybir.dt.int32
AF = mybir.ActivationFunctionType
ALU = mybir.AluOpType
AX = mybir.AxisListType

D = 2048
H = 8
HD = 128
AW = 1024
PW = 1024
INW = 4096
NE = 64
NG = 8
DE = 512
PLE = 256
EPS = 1e-6
import os
SKIP = os.environ.get('KSKIP', '')
NEG = -1.0e30


class Buf:
    __slots__ = ("name", "wev", "revs", "slot", "excl")

    def __init__(self, name, excl=False):
        self.name = name
        self.excl = excl
        self.wev = None
        self.revs = {}
        self.slot = None


class Sched:
    def __init__(self, nc):
        self.nc = nc
        self.eng = {"pe": nc.tensor, "act": nc.scalar, "dve": nc.vector, "pool": nc.gpsimd, "sp": nc.sync}
        self.esem = {k: nc.alloc_semaphore("es_" + k) for k in self.eng}
        self.ecnt = {k: 0 for k in self.eng}
        self.seen = {k: {} for k in self.eng}
        self.free_slots = []
        self.nslots = 0
        self.phase_bufs = []
        self.ninst = 0
        self.nwait = 0

    def buf(self, name, excl=False):
        b = Buf(name, excl)
        self.phase_bufs.append(b)
        return b

    def bufs(self, name, n, excl=False):
        return [self.buf("%s%d" % (name, i), excl) for i in range(n)]

    def _slot(self, b):
        if b.slot is None:
            if self.free_slots:
                b.slot = self.free_slots.pop()
            else:
                b.slot = [self.nc.alloc_semaphore("ds%d" % self.nslots), 0]
                self.nslots += 1
        return b.slot

    def _waits(self, e, r, w):
        need = {}

        def add(ev):
            s, v, src = ev
            if src == e and e == "pe":
                return
            k = id(s)
            if k not in need or need[k][1] < v:
                need[k] = (s, v)

        for b in r:
            if b.wev is not None:
                add(b.wev)
            if b.excl:
                for ev in b.revs.values():
                    if ev[2] != e:
                        add(ev)
        for b in w:
            if b.wev is not None:
                add(b.wev)
            for ev in b.revs.values():
                if ev[2] == e:
                    continue
                add(ev)
        seen = self.seen[e]
        for k, (s, v) in need.items():
            if seen.get(k, 0) >= v:
                continue
            self.eng[e].wait_ge(s, v)
            self.nwait += 1
            seen[k] = v

    def _record(self, ev, r, w):
        k = id(ev[0])
        for b in r:
            b.revs[k] = ev
        for b in w:
            b.wev = ev
            b.revs = {}

    def op(self, e, fn, r=(), w=()):
        self._waits(e, r, w)
        ins = fn(self.eng[e])
        self.ecnt[e] += 1
        ins.then_inc(self.esem[e], 1)
        self.ninst += 1
        self._record((self.esem[e], self.ecnt[e], e), r, w)
        return ins

    def dma(self, e, fn, sb, r=(), w=()):
        self._waits(e, r, w)
        slot = self._slot(sb)
        ins = fn(self.eng[e])
        slot[1] += 16
        ins.then_inc(slot[0], 16)
        self.ninst += 1
        self._record((slot[0], slot[1], "dma"), r, w)
        return ins

    def end_phase(self):
        for b in self.phase_bufs:
            if b.slot is not None:
                s, v = b.slot
                if self.seen["sp"].get(id(s), 0) < v:
                    self.eng["sp"].wait_ge(s, v)
                    self.seen["sp"][id(s)] = v
        self.ecnt["sp"] += 1
        self.eng["sp"].nop().then_inc(self.esem["sp"], 1)
        for e in self.eng:
            for f in self.eng:
                if f == e:
                    continue
                s, v = self.esem[f], self.ecnt[f]
                if v > 0 and self.seen[e].get(id(s), 0) < v:
                    self.eng[e].wait_ge(s, v)
                    self.seen[e][id(s)] = v
        for b in self.phase_bufs:
            if b.slot is not None:
                self.free_slots.append(b.slot)
                b.slot = None
        self.phase_bufs = []


class Ctx:
    pass


def alibi_slopes():
    return np.array([2.0 ** (-8.0 * (h + 1) / H) for h in range(H)], np.float64)


def host_tables(S_len, r):
    NB = S_len // 256
    NBo = NB // 2
    NBP = max(NB, 8)
    sl = alibi_slopes()
    seqblk = np.zeros(NB, np.int64)
    for i in range(NBo):
        seqblk[2 * i] = 2 * i + r
        seqblk[2 * i + 1] = 2 * i + 1 - r
    j = np.arange(256)
    vtab = np.exp(-sl[None, None, :] * (255 - (np.arange(2)[None, :, None] * 128 + np.arange(128)[:, None, None])))
    mtab = np.zeros((NBo, 2, 128, H, NBP), np.float64)
    gbias = np.full((NBo, 128, NBP), NEG, np.float64)
    for i in range(NBo):
        own = seqblk[2 * i]
        for s in range(NB):
            if seqblk[s] < own:
                gbias[i, :, s] = 0.0
                for t in range(2):
                    qpos = own * 256 + t * 128 + np.arange(128)
                    dist = qpos - (seqblk[s] * 256 + 255)
                    mtab[i, t, :, :, s] = np.exp(-sl[None, :] * dist[:, None])
    kj = np.arange(128)[:, None]
    qi = np.arange(128)[None, :]
    ctab = np.zeros((128, H, 2, 128), np.float64)
    for h in range(H):
        ctab[:, h, 0, :] = np.where(qi >= kj, np.exp(-sl[h] * np.maximum(qi - kj, 0)), 0.0)
        ctab[:, h, 1, :] = np.exp(-sl[h] * (128 + qi - kj))
    wins = np.array([2, 4, 8, 16])
    t = seqblk[0] * 256 + np.arange(256)
    cnt = np.minimum(t[None, :] + 1, wins[:, None]).astype(np.float64)
    ptab = np.broadcast_to((1.0 / cnt)[None], (128, 4, 256))
    halo = np.zeros((128, 2), np.float64)
    halo[:, 0] = 1.0 if r == 0 else 0.0
    halo[:, 1] = 0.0 if r == 0 else 1.0
    f = lambda a: np.ascontiguousarray(a, dtype=np.float32)
    lt = (np.arange(128)[:, None] < np.arange(128)[None, :]).astype(np.float32)
    return dict(ident=np.eye(128, dtype=np.float32), vtab=f(vtab), mtab=f(mtab), gbias=f(gbias),
                ctab=f(ctab), ptab=f(ptab), halo=f(halo), ltri=lt,
                bpos=f((np.arange(128) * 128.0).reshape(128, 1)),
                pidx=f(np.arange(128).reshape(128, 1)))


def build(S_len, debug=False, upto="E"):
    nc = bass.Bass("TRN2", target_bir_lowering=False)
    NB = S_len // 256
    NBo = NB // 2
    NBP = max(NB, 8)
    To = S_len // 2
    NTo = To // 128
    NT = S_len // 128
    PL = 2 * To + NE * 128
    NBLK = PL // 128
    skind = "ExternalOutput" if debug else "Internal"

    def din(name, shape, dt=F32):
        return nc.dram_tensor(name, list(shape), dt, kind="ExternalInput").ap()

    def dscr(name, shape, dt):
        return nc.dram_tensor(name, list(shape), dt, kind=skind).ap()

    c = Ctx()
    c.nc = nc
    in_shapes = dict(
        x_perm=[S_len, D], p_own=[To, PLE], g_mix=[1, D], w_in=[D, INW], beta_attn=[1, AW],
        w_pool=[4, 256, 256], pool_scale=[1, PW], w_out=[D, D], g_ffn=[1, D], w_rg=[D, NG], b_rg=[1, NG],
        w_re=[NG, D, 8], b_re=[1, NE], w_eg=[NE, D, DE], w_eu=[NE, D, DE], w_ed=[NE, DE, D], g_ple=[1, D],
        w_ple=[PLE, D], w_pg=[D, D], b_pg=[1, D], g_final=[1, D], ident=[128, 128], vtab=[128, 2, H],
        mtab=[NBo, 2, 128, H, NBP], gbias=[NBo, 128, NBP], ctab=[128, H, 2, 128], ptab=[128, 4, 256],
        halo=[128, 2], ltri=[128, 128], bpos=[128, 1], pidx=[128, 1])
    c.used = {}

    def IN(name):
        if name not in c.used:
            c.used[name] = din(name, in_shapes[name])
        return c.used[name]
    out = nc.dram_tensor("out", [To, D], F32, kind="ExternalOutput").ap()
    KT_d = dscr("KT_d", [H, 128, S_len], BF16)
    VP_d = dscr("VP_d", [H, 128, NT, 129], BF16)
    V1_d = dscr("V1_d", [H, 128, NTo, 129], BF16)
    QT_d = dscr("QT_d", [H, 128, To], BF16)
    UT_d = dscr("UT_d", [8, 128, NBo, 272], F32)
    KM_d = dscr("KM_d", [128, H, NBP], F32)
    OA_d = dscr("OA_d", [To, AW], F32)
    MP_d = dscr("MP_d", [To, PW], BF16)
    H1_d = dscr("H1_d", [To, D], F32)
    F_d = dscr("F_d", [To, D], BF16)
    SLOT_d = dscr("SLOT_d", [PL, 2], F32)
    Y_d = dscr("Y_d", [PL, D], F32)
    RT_d = dscr("RT_d", [To, 8], F32)

    S = Sched(nc)
    c.S = S
    import contextlib
    c.stack = contextlib.ExitStack()

    def sb(name, shape, dt):
        t = c.stack.enter_context(nc.sbuf_tensor(name, list(shape), dt))
        return t.ap() if hasattr(t, "ap") else t[:]
    pb = [nc.alloc_psum_tensor("pb%d" % i, [128, 512], F32).ap() for i in range(8)]
    PB = S.bufs("pbank", 8, excl=True)

    ident = nc.alloc_sbuf_tensor("identb", [128, 128], BF16).ap()
    B_ident = Buf("ident")
    S.dma("pool", lambda e: e.dma_start(out=ident, in_=IN('ident')), B_ident, w=[B_ident])

    def rmsnorm_rstd(e_xt, B_xt, junk, B_junk, st, B_st, width):
        S.op("act", lambda e: e.activation(out=junk, in_=e_xt, func=AF.Square, accum_out=st[:, 0:1]),
             r=[B_xt], w=[B_junk, B_st])
        S.op("act", lambda e: e.activation(out=st[:, 1:2], in_=st[:, 0:1], func=AF.Sqrt, scale=1.0 / width, bias=EPS),
             r=[B_st], w=[B_st])
        S.op("dve", lambda e: e.reciprocal(out=st[:, 2:3], in_=st[:, 1:2]), r=[B_st], w=[B_st])

    def transpose_to(src, B_src, nchunk, dst_fn, B_dst, tps, step=1):
        for g0 in range(0, nchunk, 8):
            n = min(8, nchunk - g0)
            bi = tps[(g0 // 8) % len(tps)]
            tpv = pb[bi].bitcast(BF16).rearrange("p (k n) -> p k n", n=128)
            for k in range(g0, g0 + n):
                if step == 1:
                    sv = src[:, k * 128:(k + 1) * 128]
                else:
                    sv = src[:, k:k + 127 * step + 1:step]
                S.op("pe", lambda e: e.transpose(tpv[:, k - g0, :], sv, ident), r=[B_src, B_ident], w=[PB[bi]])
            eng = "act" if (g0 // 8) % 2 == 0 else "dve"
            if eng == "act":
                S.op("act", lambda e: e.copy(out=dst_fn(g0, g0 + n), in_=tpv[:, 0:n, :]), r=[PB[bi]], w=[B_dst])
            else:
                S.op("dve", lambda e: e.tensor_copy(out=dst_fn(g0, g0 + n), in_=tpv[:, 0:n, :]), r=[PB[bi]], w=[B_dst])

    def phase_A():
        Wb = sb("A_Wb", [128, 16, INW], BF16)
        B_W = S.buf("A_Wb")
        for k in range(16):
            S.dma("pool", lambda e: e.dma_start(out=Wb[:, k, :], in_=IN('w_in')[k * 128:(k + 1) * 128, :]), B_W, w=[B_W])
        gmix = sb("A_gmix", [128, D], F32)
        B_g = S.buf("A_gmix")
        S.dma("sp", lambda e: e.dma_start(out=gmix, in_=IN('g_mix').partition_broadcast(128)), B_g, w=[B_g])
        vtab = sb("A_vtab", [128, 2, H], F32)
        B_vt = S.buf("A_vtab")
        S.dma("sp", lambda e: e.dma_start(out=vtab, in_=IN('vtab')), B_vt, w=[B_vt])
        xb = [sb("A_x%d" % i, [128, D], F32) for i in range(2)]
        B_x = S.bufs("A_x", 2)
        ab = [sb("A_a%d" % i, [128, D], BF16) for i in range(2)]
        B_a = S.bufs("A_a", 2)
        stt = [sb("A_st%d" % i, [128, 4], F32) for i in range(2)]
        B_st = S.bufs("A_st", 2)
        aT = sb("A_aT", [128, 16, 512], BF16)
        B_aT = S.buf("A_aT")
        NSTG = 3
        fst = [sb("A_fs%d" % i, [128, 512], BF16) for i in range(NSTG)]
        B_fs = S.bufs("A_fs", NSTG)
        ust = [sb("A_us%d" % i, [128, 272], F32) for i in range(2)]
        B_us = S.bufs("A_us", 2)
        vp = [sb("A_vp%d" % i, [128, H, 129], BF16) for i in range(2)]
        B_vp = S.bufs("A_vp", 2)
        v1 = [sb("A_v1%d" % i, [128, H, 129], BF16) for i in range(2)]
        B_v1 = S.bufs("A_v1", 2)
        km = sb("A_km", [128, H, NBP], F32)
        B_km = S.buf("A_km")
        kms = sb("A_kms", [128, 2], F32)
        B_kms = S.buf("A_kms")
        S.op("dve", lambda e: e.memset(km, 0.0), w=[B_km])
        for i in range(2):
            S.op("dve", lambda e: e.memset(v1[i][:, :, 128:129], 1.0), w=[B_v1[i]])
        mmb = [2, 3, 4, 5, 6, 7]
        mmi = [0]

        def nextbank():
            b = mmb[mmi[0] % len(mmb)]
            mmi[0] += 1
            return b

        fsi = [0]
        tcount = [0]
        for i in range(NBo):
            for t in range(4):
                j = tcount[0] % 2
                tcount[0] += 1
                row0 = (i * 4 + t) * 128
                S.dma("sp", lambda e: e.dma_start(out=xb[j], in_=IN('x_perm')[row0:row0 + 128, :]), B_x[j], w=[B_x[j]])
                rmsnorm_rstd(xb[j], B_x[j], ab[j], B_a[j], stt[j], B_st[j], D)
                S.op("dve", lambda e: e.scalar_tensor_tensor(out=ab[j], in0=xb[j], scalar=stt[j][:, 2:3], in1=gmix,
                                                             op0=ALU.mult, op1=ALU.mult),
                     r=[B_x[j], B_st[j], B_g], w=[B_a[j]])
                transpose_to(ab[j], B_a[j], 16, lambda k0, k1: aT[:, k0:k1, t * 128:(t + 1) * 128], B_aT, [0, 1])
            for ch in range(H if 'k' not in SKIP else 0):
                bk = nextbank()
                for k in range(16):
                    S.op("pe", lambda e: e.matmul(pb[bk], lhsT=Wb[:, k, AW + ch * 128:AW + (ch + 1) * 128], rhs=aT[:, k, :],
                                                  start=(k == 0), stop=(k == 15)), r=[B_W, B_aT], w=[PB[bk]])
                f = fsi[0] % NSTG
                fsi[0] += 1
                S.op("act", lambda e: e.copy(out=fst[f], in_=pb[bk]), r=[PB[bk]], w=[B_fs[f]])
                S.op("dve", lambda e: e.reduce_sum(out=kms, in_=pb[bk].rearrange("p (b n) -> p b n", n=256), axis=AX.X),
                     r=[PB[bk], B_fs[f]], w=[B_kms])
                S.op("dve", lambda e: e.tensor_scalar(out=km[:, ch, 2 * i:2 * i + 2], in0=kms, scalar1=1.0 / 256, scalar2=None,
                                                      op0=ALU.mult), r=[B_kms], w=[B_km])
                S.dma("sp", lambda e: e.dma_start(out=KT_d[ch, :, i * 512:(i + 1) * 512], in_=fst[f]), B_fs[f], r=[B_fs[f]])
            for ch in range(H if 'q' not in SKIP else 0):
                bk = nextbank()
                for k in range(16):
                    S.op("pe", lambda e: e.matmul(pb[bk][:, 0:256], lhsT=Wb[:, k, ch * 128:(ch + 1) * 128], rhs=aT[:, k, 0:256],
                                                  start=(k == 0), stop=(k == 15)), r=[B_W, B_aT], w=[PB[bk]])
                f = fsi[0] % NSTG
                fsi[0] += 1
                S.op("act", lambda e: e.copy(out=fst[f][:, 0:256], in_=pb[bk][:, 0:256]), r=[PB[bk]], w=[B_fs[f]])
                S.dma("sp", lambda e: e.dma_start(out=QT_d[ch, :, i * 256:(i + 1) * 256], in_=fst[f][:, 0:256]), B_fs[f], r=[B_fs[f]])
            for ch in range(8 if 'u' not in SKIP else 0):
                bk = nextbank()
                for k in range(16):
                    S.op("pe", lambda e: e.matmul(pb[bk], lhsT=Wb[:, k, 3 * AW + ch * 128:3 * AW + (ch + 1) * 128], rhs=aT[:, k, :],
                                                  start=(k == 0), stop=(k == 15)), r=[B_W, B_aT], w=[PB[bk]])
                f = ch % 2
                S.op("act", lambda e: e.copy(out=ust[f][:, 0:256], in_=pb[bk][:, 0:256]), r=[PB[bk]], w=[B_us[f]])
                S.op("dve", lambda e: e.tensor_copy(out=ust[f][:, 256:272], in_=pb[bk][:, 496:512]), r=[PB[bk]], w=[B_us[f]])
                S.dma("sp", lambda e: e.dma_start(out=UT_d[ch, :, i, :], in_=ust[f]), B_us[f], r=[B_us[f]])
            for t in range(4 if 'v' not in SKIP else 0):
                par = t % 2
                tile_g = i * 4 + t
                jj = tile_g % 2
                for half in range(2):
                    bk = nextbank()
                    for k in range(16):
                        S.op("pe", lambda e: e.matmul(pb[bk], lhsT=aT[:, k, t * 128:(t + 1) * 128],
                                                      rhs=Wb[:, k, 2 * AW + half * 512:2 * AW + (half + 1) * 512],
                                                      start=(k == 0), stop=(k == 15)), r=[B_W, B_aT], w=[PB[bk]])
                    pv = pb[bk].rearrange("p (h n) -> p h n", n=128)
                    hs = slice(half * 4, (half + 1) * 4)
                    S.op("dve", lambda e: e.tensor_tensor(out=vp[jj][:, hs, 0:128], in0=pv,
                                                          in1=vtab[:, par, hs].unsqueeze(2).to_broadcast([128, 4, 128]),
                                                          op=ALU.mult), r=[PB[bk], B_vt], w=[B_vp[jj]])
                    if t < 2:
                        S.op("act", lambda e: e.copy(out=v1[jj][:, hs, 0:128], in_=pv), r=[PB[bk], B_vp[jj]], w=[B_v1[jj]])
                S.op("dve", lambda e: e.tensor_copy(out=vp[jj][:, :, 128:129], in_=vtab[:, par, :].unsqueeze(2)),
                     r=[B_vt], w=[B_vp[jj]])
                S.dma("sp", lambda e: e.dma_start(out=VP_d[:, :, tile_g, :].rearrange("h p c -> p h c"), in_=vp[jj]),
                      B_vp[jj], r=[B_vp[jj]])
                if t < 2:
                    S.dma("sp", lambda e: e.dma_start(out=V1_d[:, :, i * 2 + t, :].rearrange("h p c -> p h c"), in_=v1[jj]),
                          B_v1[jj], r=[B_v1[jj]])
        S.dma("sp", lambda e: e.dma_start(out=KM_d, in_=km), B_km, r=[B_km])
        S.end_phase()


    def phase_A2():
        wpb = sb("P_wp", [128, 4, 2, 256], BF16)
        B_wp = S.buf("P_wp")
        S.dma("pool", lambda e: e.dma_start(out=wpb, in_=IN('w_pool').rearrange("g (cc p) d -> p g cc d", p=128)), B_wp, w=[B_wp])
        psc = sb("P_psc", [128, PW], F32)
        B_psc = S.buf("P_psc")
        S.dma("sp", lambda e: e.dma_start(out=psc, in_=IN('pool_scale').partition_broadcast(128)), B_psc, w=[B_psc])
        ptab = sb("P_ptab", [128, 4, 256], F32)
        B_pt = S.buf("P_ptab")
        S.dma("sp", lambda e: e.dma_start(out=ptab, in_=IN('ptab')), B_pt, w=[B_pt])
        halo = sb("P_halo", [128, 2], F32)
        B_ha = S.buf("P_halo")
        S.dma("sp", lambda e: e.dma_start(out=halo, in_=IN('halo')), B_ha, w=[B_ha])
        ub = [sb("P_ub%d" % i, [128, 8, 272], F32) for i in range(2)]
        B_ub = S.bufs("P_ub", 2)
        tl = [sb("P_tl%d" % i, [128, 8, 32], F32) for i in range(2)]
        B_tl = S.bufs("P_tl", 2)
        Pb = sb("P_P", [128, 8, 272], F32)
        B_P = S.buf("P_P")
        Qb = sb("P_Q", [128, 8, 272], F32)
        B_Q = S.buf("P_Q")
        zb = sb("P_z", [128, 8, 256], BF16)
        B_z = S.buf("P_z")
        zt = sb("P_zt", [128, 2, 256], F32)
        B_zt = S.buf("P_zt")
        mp = [sb("P_mp%d" % i, [128, PW], BF16) for i in range(2)]
        B_mp = S.bufs("P_mp", 2)
        tmpf = sb("P_tmpf", [128, PW], F32)
        B_tmpf = S.buf("P_tmpf")
        st = [sb("P_st%d" % i, [128, 8], F32) for i in range(2)]
        B_st = S.bufs("P_st", 2)
        wins = [2, 4, 8, 16]
        UTv = UT_d.rearrange("c p i n -> p c i n")
        for i in range(NBo):
            j = i % 2
            A = ub[j]
            S.dma("sp", lambda e: e.dma_start(out=A[:, :, 16:272], in_=UTv[:, :, i, 0:256]), B_ub[j], w=[B_ub[j]])
            S.dma("sp", lambda e: e.dma_start(out=tl[j][:, :, 16:32], in_=UTv[:, :, i, 256:272]), B_tl[j], w=[B_tl[j]])
            if i > 0:
                S.dma("sp", lambda e: e.dma_start(out=tl[j][:, :, 0:16], in_=UTv[:, :, i - 1, 256:272]), B_tl[j], w=[B_tl[j]])
            else:
                S.op("dve", lambda e: e.memset(tl[j][:, :, 0:16], 0.0), w=[B_tl[j]])
            S.op("dve", lambda e: e.tensor_scalar(out=A[:, :, 0:16], in0=tl[j][:, :, 0:16], scalar1=halo[:, 0:1], scalar2=None, op0=ALU.mult),
                 r=[B_tl[j], B_ha], w=[B_ub[j]])
            S.op("dve", lambda e: e.scalar_tensor_tensor(out=A[:, :, 0:16], in0=tl[j][:, :, 16:32], scalar=halo[:, 1:2], in1=A[:, :, 0:16],
                                                         op0=ALU.mult, op1=ALU.add), r=[B_tl[j], B_ha, B_ub[j]], w=[B_ub[j]])
            S.op("dve", lambda e: e.tensor_tensor(out=Pb[:, :, 1:272], in0=A[:, :, 1:272], in1=A[:, :, 0:271], op=ALU.add),
                 r=[B_ub[j]], w=[B_P])
            S.op("dve", lambda e: e.tensor_tensor(out=Qb[:, 2:8, 3:272], in0=Pb[:, 2:8, 3:272], in1=Pb[:, 2:8, 1:270], op=ALU.add),
                 r=[B_P], w=[B_Q])
            S.op("dve", lambda e: e.tensor_tensor(out=Pb[:, 4:8, 7:272], in0=Qb[:, 4:8, 7:272], in1=Qb[:, 4:8, 3:268], op=ALU.add),
                 r=[B_Q], w=[B_P])
            S.op("dve", lambda e: e.tensor_tensor(out=Qb[:, 6:8, 15:272], in0=Pb[:, 6:8, 15:272], in1=Pb[:, 6:8, 7:264], op=ALU.add),
                 r=[B_P], w=[B_Q])
            for g in range(4):
                W = Pb if g % 2 == 0 else Qb
                B_Wb = B_P if g % 2 == 0 else B_Q
                cs = slice(2 * g, 2 * g + 2)
                if i == 0:
                    S.op("dve", lambda e: e.tensor_tensor(out=zt, in0=W[:, cs, 16:272],
                                                          in1=ptab[:, g, :].unsqueeze(1).to_broadcast([128, 2, 256]), op=ALU.mult),
                         r=[B_Wb, B_pt], w=[B_zt])
                    S.op("dve", lambda e: e.tensor_tensor(out=zb[:, cs, :], in0=zt, in1=A[:, cs, 16:272], op=ALU.subtract),
                         r=[B_zt, B_ub[j]], w=[B_z])
                else:
                    S.op("dve", lambda e: e.scalar_tensor_tensor(out=zb[:, cs, :], in0=W[:, cs, 16:272], scalar=1.0 / wins[g],
                                                                 in1=A[:, cs, 16:272], op0=ALU.mult, op1=ALU.subtract),
                         r=[B_Wb, B_ub[j]], w=[B_z])
            for t in range(2):
                jt = (i * 2 + t) % 2
                banks = [2 + 2 * jt, 3 + 2 * jt]
                for g in range(4):
                    bk = banks[g // 2]
                    for cc in range(2):
                        S.op("pe", lambda e: e.matmul(pb[bk][:, (g % 2) * 256:(g % 2 + 1) * 256], lhsT=zb[:, 2 * g + cc, t * 128:(t + 1) * 128],
                                                      rhs=wpb[:, g, cc, :], start=(cc == 0), stop=(cc == 1)),
                             r=[B_z, B_wp], w=[PB[bk]])
                S.op("act", lambda e: e.activation(out=tmpf[:, 0:512], in_=pb[banks[0]], func=AF.Square, accum_out=st[jt][:, 0:1]),
                     r=[PB[banks[0]]], w=[B_tmpf, B_st[jt]])
                S.op("act", lambda e: e.activation(out=tmpf[:, 512:1024], in_=pb[banks[1]], func=AF.Square, accum_out=st[jt][:, 3:4]),
                     r=[PB[banks[1]]], w=[B_tmpf, B_st[jt]])
                S.op("dve", lambda e: e.tensor_tensor(out=st[jt][:, 0:1], in0=st[jt][:, 0:1], in1=st[jt][:, 3:4], op=ALU.add),
                     r=[B_st[jt]], w=[B_st[jt]])
                S.op("act", lambda e: e.activation(out=st[jt][:, 1:2], in_=st[jt][:, 0:1], func=AF.Sqrt, scale=1.0 / PW, bias=EPS),
                     r=[B_st[jt]], w=[B_st[jt]])
                S.op("dve", lambda e: e.reciprocal(out=st[jt][:, 2:3], in_=st[jt][:, 1:2]), r=[B_st[jt]], w=[B_st[jt]])
                for hh in range(2):
                    S.op("dve", lambda e: e.scalar_tensor_tensor(out=mp[jt][:, hh * 512:(hh + 1) * 512], in0=pb[banks[hh]], scalar=st[jt][:, 2:3],
                                                                 in1=psc[:, hh * 512:(hh + 1) * 512], op0=ALU.mult, op1=ALU.mult),
                         r=[PB[banks[hh]], B_st[jt], B_psc], w=[B_mp[jt]])
                row0 = (i * 2 + t) * 128
                S.dma("sp", lambda e: e.dma_start(out=MP_d[row0:row0 + 128, :], in_=mp[jt]), B_mp[jt], r=[B_mp[jt]])
        S.end_phase()

    def phase_B():
        scale = HD ** -0.5
        kmf = sb("B_kmf", [128, H, NBP], F32)
        B_kmf = S.buf("B_kmf")
        S.dma("sp", lambda e: e.dma_start(out=kmf, in_=KM_d), B_kmf, w=[B_kmf])
        kmb = sb("B_kmb", [128, H, NBP], BF16)
        B_kmb = S.buf("B_kmb")
        S.op("dve", lambda e: e.tensor_copy(out=kmb, in_=kmf), r=[B_kmf], w=[B_kmb])
        gbias = sb("B_gbias", [128, NBo, NBP], F32)
        B_gb = S.buf("B_gbias")
        S.dma("sp", lambda e: e.dma_start(out=gbias, in_=IN('gbias').rearrange("i p s -> p i s")), B_gb, w=[B_gb])
        mtab = sb("B_mtab", [128, NBo, 2, H, NBP], F32)
        B_mt = S.buf("B_mtab")
        for i in range(NBo):
            S.dma("sp", lambda e: e.dma_start(out=mtab[:, i], in_=IN('mtab')[i].rearrange("t p h s -> p t h s")), B_mt, w=[B_mt])
        ctab = sb("B_ctab", [128, H, 2, 128], F32)
        B_ct = S.buf("B_ctab")
        S.dma("sp", lambda e: e.dma_start(out=ctab, in_=IN('ctab')), B_ct, w=[B_ct])
        KT = [sb("B_KT%d" % i, [128, S_len], BF16) for i in range(2)]
        VP = [sb("B_VP%d" % i, [128, NT, 129], BF16) for i in range(2)]
        QT = [sb("B_QT%d" % i, [128, To], BF16) for i in range(2)]
        V1 = [sb("B_V1%d" % i, [128, NTo, 129], BF16) for i in range(2)]
        B_KT = S.bufs("B_KT", 2)
        B_VP = S.bufs("B_VP", 2)
        B_QT = S.bufs("B_QT", 2)
        B_V1 = S.bufs("B_V1", 2)
        gs = sb("B_gs", [128, 2, NBP], F32)
        B_gs = S.buf("B_gs")
        top8 = sb("B_top8", [128, 2, 8], F32)
        B_t8 = S.buf("B_top8")
        sel = sb("B_sel", [128, 2, NBP], F32)
        B_sel = S.buf("B_sel")
        mm = [sb("B_m%d" % i, [128, 2, NBP], F32) for i in range(2)]
        B_m = S.bufs("B_m", 2)
        pT = [sb("B_pT%d" % i, [128, 512], BF16) for i in range(2)]
        B_pT = S.bufs("B_pT", 2)
        acc = [sb("B_acc%d" % i, [128, 2, 129], F32) for i in range(2)]
        B_acc = S.bufs("B_acc", 2)
        rec = sb("B_rec", [128, 2], F32)
        B_rec = S.buf("B_rec")
        oa = [sb("B_oa%d" % i, [128, 2, 128], F32) for i in range(2)]
        B_oa = S.bufs("B_oa", 2)
        itc = [0]

        def load_head(h):
            j = h % 2
            S.dma("sp", lambda e: e.dma_start(out=KT[j], in_=KT_d[h]), B_KT[j], w=[B_KT[j]])
            S.dma("sp", lambda e: e.dma_start(out=QT[j], in_=QT_d[h]), B_QT[j], w=[B_QT[j]])
            S.dma("sp", lambda e: e.dma_start(out=VP[j], in_=VP_d[h]), B_VP[j], w=[B_VP[j]])
            S.dma("sp", lambda e: e.dma_start(out=V1[j], in_=V1_d[h]), B_V1[j], w=[B_V1[j]])

        load_head(0)
        for h in range(H):
            j = h % 2
            if h + 1 < H:
                load_head(h + 1)
            for i in range(NBo):
                a = (h * NBo + i) % 2
                for t in range(2):
                    S.op("pe", lambda e: e.matmul(pb[0][:, t * 256:t * 256 + NBP], lhsT=QT[j][:, i * 256 + t * 128:i * 256 + (t + 1) * 128],
                                                  rhs=kmb[:, h, :], start=True, stop=True), r=[B_QT[j], B_kmb], w=[PB[0]])
                gv = pb[0].rearrange("p (t n) -> p t n", n=256)[:, :, 0:NBP]
                S.op("dve", lambda e: e.tensor_tensor(out=gs, in0=gv, in1=gbias[:, i, :].unsqueeze(1).to_broadcast([128, 2, NBP]), op=ALU.add),
                     r=[PB[0], B_gb], w=[B_gs])
                for t in range(2):
                    S.op("dve", lambda e: e.max(out=top8[:, t, :], in_=gs[:, t, :]), r=[B_gs], w=[B_t8])
                for t in range(2):
                    S.op("dve", lambda e: e.tensor_scalar(out=sel[:, t, :], in0=gs[:, t, :], scalar1=top8[:, t, 2:3], scalar2=None, op0=ALU.is_ge),
                         r=[B_gs, B_t8], w=[B_sel])
                S.op("dve", lambda e: e.tensor_tensor(out=mm[a], in0=sel, in1=mtab[:, i, :, h, :], op=ALU.mult), r=[B_sel, B_mt], w=[B_m[a]])
                it = itc[0]
                itc[0] += 1
                sbk = 1 + it % 2
                obk = 3 + it % 2
                pt = pT[it % 2]
                B_pt_ = B_pT[it % 2]
                q0 = i * 256
                S.op("pe", lambda e: e.matmul(pb[sbk][:, 0:256], lhsT=KT[j][:, (2 * i) * 256:(2 * i) * 256 + 128], rhs=QT[j][:, q0:q0 + 256],
                                              start=True, stop=True), r=[B_KT[j], B_QT[j]], w=[PB[sbk]])
                S.op("pe", lambda e: e.matmul(pb[sbk][:, 384:512], lhsT=KT[j][:, (2 * i) * 256 + 128:(2 * i) * 256 + 256],
                                              rhs=QT[j][:, q0 + 128:q0 + 256], start=True, stop=True), r=[B_KT[j], B_QT[j]], w=[PB[sbk]])
                S.op("act", lambda e: e.activation(out=pt[:, 0:256], in_=pb[sbk][:, 0:256], func=AF.Exp, scale=scale), r=[PB[sbk]], w=[B_pt_])
                S.op("act", lambda e: e.activation(out=pt[:, 384:512], in_=pb[sbk][:, 384:512], func=AF.Exp, scale=scale), r=[PB[sbk]], w=[B_pt_])
                S.op("dve", lambda e: e.tensor_tensor(out=pt[:, 0:256], in0=pt[:, 0:256], in1=ctab[:, h, :, :].rearrange("p a b -> p (a b)"), op=ALU.mult),
                     r=[B_pt_, B_ct], w=[B_pt_])
                S.op("dve", lambda e: e.tensor_tensor(out=pt[:, 384:512], in0=pt[:, 384:512], in1=ctab[:, h, 0, :], op=ALU.mult),
                     r=[B_pt_, B_ct], w=[B_pt_])
                S.op("pe", lambda e: e.matmul(pb[obk][:, 0:129], lhsT=pt[:, 0:128], rhs=V1[j][:, 2 * i, :], start=True, stop=True),
                     r=[B_pt_, B_V1[j]], w=[PB[obk]])
                S.op("pe", lambda e: e.matmul(pb[obk][:, 256:385], lhsT=pt[:, 128:256], rhs=V1[j][:, 2 * i, :], start=True, stop=False),
                     r=[B_pt_, B_V1[j]], w=[PB[obk]])
                S.op("pe", lambda e: e.matmul(pb[obk][:, 256:385], lhsT=pt[:, 384:512], rhs=V1[j][:, 2 * i + 1, :], start=False, stop=True),
                     r=[B_pt_, B_V1[j]], w=[PB[obk]])
                ov = pb[obk].rearrange("p (t n) -> p t n", n=256)[:, :, 0:129]
                S.op("dve", lambda e: e.tensor_copy(out=acc[a], in_=ov), r=[PB[obk]], w=[B_acc[a]])
                cands = list(range(2 * i)) + [2 * i + 1]
                for s_ in cands:
                    it = itc[0]
                    itc[0] += 1
                    sbk = 1 + it % 2
                    obk = 3 + it % 2
                    pt = pT[it % 2]
                    B_pt_ = B_pT[it % 2]
                    for kh in range(2):
                        S.op("pe", lambda e: e.matmul(pb[sbk][:, kh * 256:(kh + 1) * 256], lhsT=KT[j][:, s_ * 256 + kh * 128:s_ * 256 + (kh + 1) * 128],
                                                      rhs=QT[j][:, q0:q0 + 256], start=True, stop=True), r=[B_KT[j], B_QT[j]], w=[PB[sbk]])
                    S.op("act", lambda e: e.activation(out=pt, in_=pb[sbk], func=AF.Exp, scale=scale), r=[PB[sbk]], w=[B_pt_])
                    for t in range(2):
                        for kh in range(2):
                            S.op("pe", lambda e: e.matmul(pb[obk][:, t * 256:t * 256 + 129], lhsT=pt[:, kh * 256 + t * 128:kh * 256 + (t + 1) * 128],
                                                          rhs=VP[j][:, s_ * 2 + kh, :], start=(kh == 0), stop=(kh == 1)),
                                 r=[B_pt_, B_VP[j]], w=[PB[obk]])
                    for t in range(2):
                        S.op("dve", lambda e: e.scalar_tensor_tensor(out=acc[a][:, t, :], in0=pb[obk][:, t * 256:t * 256 + 129],
                                                                     scalar=mm[a][:, t, s_:s_ + 1], in1=acc[a][:, t, :],
                                                                     op0=ALU.mult, op1=ALU.add), r=[PB[obk], B_m[a], B_acc[a]], w=[B_acc[a]])
                S.op("dve", lambda e: e.reciprocal(out=rec, in_=acc[a][:, :, 128]), r=[B_acc[a]], w=[B_rec])
                for t in range(2):
                    S.op("dve", lambda e: e.tensor_scalar(out=oa[a][:, t, :], in0=acc[a][:, t, 0:128], scalar1=rec[:, t:t + 1], scalar2=None, op0=ALU.mult),
                         r=[B_acc[a], B_rec], w=[B_oa[a]])
                S.dma("sp", lambda e: e.dma_start(out=OA_d[q0:q0 + 256, h * 128:(h + 1) * 128].rearrange("(t p) c -> p t c", p=128), in_=oa[a]),
                      B_oa[a], r=[B_oa[a]])
        S.end_phase()

    WIDX = nc.alloc_sbuf_tensor("G_widx", [128, 128], I32).ap()
    B_widx = Buf("G_widx")
    DESTI = nc.alloc_sbuf_tensor("G_desti", [128, NTo, 2], I32).ap()
    B_desti = Buf("G_desti")

    def phase_C():
        Wo = sb("C_Wo", [128, 16, D], BF16)
        B_Wo = S.buf("C_Wo")
        for k in range(16):
            S.dma("pool", lambda e: e.dma_start(out=Wo[:, k, :], in_=IN('w_out')[k * 128:(k + 1) * 128, :]), B_Wo, w=[B_Wo])
        beta = sb("C_beta", [128, AW], F32)
        B_beta = S.buf("C_beta")
        S.dma("sp", lambda e: e.dma_start(out=beta, in_=IN('beta_attn').partition_broadcast(128)), B_beta, w=[B_beta])
        gffn = sb("C_gffn", [128, D], F32)
        B_gffn = S.buf("C_gffn")
        S.dma("sp", lambda e: e.dma_start(out=gffn, in_=IN('g_ffn').partition_broadcast(128)), B_gffn, w=[B_gffn])
        Wr32 = sb("C_Wr32", [128, 16, 72], F32)
        B_Wr32 = S.buf("C_Wr32")
        S.dma("sp", lambda e: e.dma_start(out=Wr32[:, :, 0:8], in_=IN('w_rg').rearrange("(k p) e -> p k e", p=128)), B_Wr32, w=[B_Wr32])
        for g in range(NG):
            S.dma("sp", lambda e: e.dma_start(out=Wr32[:, :, 8 + g * 8:16 + g * 8], in_=IN('w_re')[g].rearrange("(k p) e -> p k e", p=128)),
                  B_Wr32, w=[B_Wr32])
        Wr = sb("C_Wr", [128, 16, 72], BF16)
        B_Wr = S.buf("C_Wr")
        S.op("dve", lambda e: e.tensor_copy(out=Wr, in_=Wr32), r=[B_Wr32], w=[B_Wr])
        brb = sb("C_brb", [128, 72], F32)
        B_brb = S.buf("C_brb")
        S.dma("sp", lambda e: e.dma_start(out=brb[:, 0:8], in_=IN('b_rg').partition_broadcast(128)), B_brb, w=[B_brb])
        S.dma("sp", lambda e: e.dma_start(out=brb[:, 8:72], in_=IN('b_re').partition_broadcast(128)), B_brb, w=[B_brb])
        ltb = sb("C_ltb", [128, 128], BF16)
        B_ltb = S.buf("C_ltb")
        S.dma("pool", lambda e: e.dma_start(out=ltb, in_=IN('ltri')), B_ltb, w=[B_ltb])
        onesb = sb("C_ones", [128, 128], BF16)
        B_ones = S.buf("C_ones")
        S.op("dve", lambda e: e.memset(onesb, 1.0), w=[B_ones])
        pidx = sb("C_pidx", [128, 1], F32)
        B_pidx = S.buf("C_pidx")
        S.dma("sp", lambda e: e.dma_start(out=pidx, in_=IN('pidx')), B_pidx, w=[B_pidx])
        bpos = sb("C_bpos", [128, 1], F32)
        B_bpos = S.buf("C_bpos")
        S.dma("sp", lambda e: e.dma_start(out=bpos, in_=IN('bpos')), B_bpos, w=[B_bpos])
        zer = sb("C_zer", [128, (PL // 128) * 2], F32)
        B_zer = S.buf("C_zer")
        S.op("dve", lambda e: e.memset(zer, 0.0), w=[B_zer])
        B_slotd = S.buf("C_slotd")
        S.dma("sp", lambda e: e.dma_start(out=SLOT_d.rearrange("(p n) c -> p (n c)", p=128), in_=zer), B_zer, r=[B_zer], w=[B_slotd])
        OHK = sb("C_OHK", [128, NTo, 2, 64], F32)
        B_OHK = S.buf("C_OHK")
        RK = sb("C_RK", [128, NTo, 2], F32)
        B_RK = S.buf("C_RK")
        WK = sb("C_WK", [128, NTo, 2], F32)
        B_WK = S.buf("C_WK")
        ohacc = sb("C_ohacc", [128, 64], BF16)
        B_ohacc = S.buf("C_ohacc")
        S.op("dve", lambda e: e.memset(ohacc, 0.0), w=[B_ohacc])
        oa = [sb("C_oa%d" % i, [128, AW], F32) for i in range(2)]
        B_oa = S.bufs("C_oa", 2)
        mx = [sb("C_mx%d" % i, [128, D], BF16) for i in range(2)]
        B_mx = S.bufs("C_mx", 2)
        xt = [sb("C_x%d" % i, [128, D], F32) for i in range(2)]
        B_xt = S.bufs("C_x", 2)
        mT = sb("C_mT", [128, 16, 128], BF16)
        B_mT = S.buf("C_mT")
        h1 = [sb("C_h1%d" % i, [128, D], F32) for i in range(2)]
        B_h1 = S.bufs("C_h1", 2)
        fb = [sb("C_f%d" % i, [128, D], BF16) for i in range(2)]
        B_fb = S.bufs("C_f", 2)
        fT = sb("C_fT", [128, 16, 128], BF16)
        B_fT = S.buf("C_fT")
        st = [sb("C_st%d" % i, [128, 8], F32) for i in range(2)]
        B_st = S.bufs("C_st", 2)
        st2 = [sb("C_su%d" % i, [128, 8], F32) for i in range(2)]
        B_st2 = S.bufs("C_su", 2)
        lg = sb("C_lg", [128, 72], F32)
        B_lg = S.buf("C_lg")
        rs = sb("C_rs", [128, 32], F32)
        B_rs = S.buf("C_rs")
        ohg = sb("C_ohg", [128, 8], F32)
        B_ohg = S.buf("C_ohg")
        tmp88 = sb("C_tmp88", [128, 8, 8], F32)
        B_t88 = S.buf("C_tmp88")
        le = sb("C_le", [128, 8], F32)
        B_le = S.buf("C_le")
        t8 = sb("C_t8", [128, 8], F32)
        B_t8 = S.buf("C_t8")
        ohk = sb("C_ohk", [128, 2, 8], F32)
        B_ohk = S.buf("C_ohk")
        ohs = sb("C_ohs", [128, 64], BF16)
        B_ohs = S.buf("C_ohs")
        cum = sb("C_cum", [128, 64], F32)
        B_cum = S.buf("C_cum")
        tmp64 = sb("C_tmp64", [128, 64], F32)
        B_t64 = S.buf("C_tmp64")

        def load(j):
            b = j % 2
            i, t = j // 2, j % 2
            xr = i * 512 + t * 128
            S.dma("sp", lambda e: e.dma_start(out=oa[b], in_=OA_d[j * 128:(j + 1) * 128, :]), B_oa[b], w=[B_oa[b]])
            S.dma("sp", lambda e: e.dma_start(out=xt[b], in_=IN('x_perm')[xr:xr + 128, :]), B_xt[b], w=[B_xt[b]])
            S.dma("sp", lambda e: e.dma_start(out=mx[b][:, AW:D], in_=MP_d[j * 128:(j + 1) * 128, :]), B_mx[b], w=[B_mx[b]])

        load(0)
        for j in range(NTo):
            b = j % 2
            if j + 1 < NTo:
                load(j + 1)
            rmsnorm_rstd(oa[b], B_oa[b], mx[b][:, 0:AW], B_mx[b], st[b], B_st[b], AW)
            S.op("dve", lambda e: e.scalar_tensor_tensor(out=mx[b][:, 0:AW], in0=oa[b], scalar=st[b][:, 2:3], in1=beta, op0=ALU.mult, op1=ALU.mult),
                 r=[B_oa[b], B_st[b], B_beta], w=[B_mx[b]])
            transpose_to(mx[b], B_mx[b], 16, lambda k0, k1: mT[:, k0:k1, :], B_mT, [0, 1])
            for n in range(4):
                bk = 2 + n
                for k in range(16):
                    S.op("pe", lambda e: e.matmul(pb[bk], lhsT=mT[:, k, :], rhs=Wo[:, k, n * 512:(n + 1) * 512], start=(k == 0), stop=(k == 15)),
                         r=[B_mT, B_Wo], w=[PB[bk]])
                S.op("dve", lambda e: e.tensor_tensor(out=h1[b][:, n * 512:(n + 1) * 512], in0=pb[bk], in1=xt[b][:, n * 512:(n + 1) * 512], op=ALU.add),
                     r=[PB[bk], B_xt[b]], w=[B_h1[b]])
            S.dma("sp", lambda e: e.dma_start(out=H1_d[j * 128:(j + 1) * 128, :], in_=h1[b]), B_h1[b], r=[B_h1[b]])
            rmsnorm_rstd(h1[b], B_h1[b], fb[b], B_fb[b], st2[b], B_st2[b], D)
            S.op("dve", lambda e: e.scalar_tensor_tensor(out=fb[b], in0=h1[b], scalar=st2[b][:, 2:3], in1=gffn, op0=ALU.mult, op1=ALU.mult),
                 r=[B_h1[b], B_st2[b], B_gffn], w=[B_fb[b]])
            S.dma("sp", lambda e: e.dma_start(out=F_d[j * 128:(j + 1) * 128, :], in_=fb[b]), B_fb[b], r=[B_fb[b]])
            transpose_to(fb[b], B_fb[b], 16, lambda k0, k1: fT[:, k0:k1, :], B_fT, [0, 1])
            bk = 6
            for k in range(16):
                S.op("pe", lambda e: e.matmul(pb[bk][:, 0:72], lhsT=fT[:, k, :], rhs=Wr[:, k, :], start=(k == 0), stop=(k == 15)),
                     r=[B_fT, B_Wr], w=[PB[bk]])
            S.op("dve", lambda e: e.tensor_tensor(out=lg, in0=pb[bk][:, 0:72], in1=brb, op=ALU.add), r=[PB[bk], B_brb], w=[B_lg])
            S.op("dve", lambda e: e.reduce_max(out=rs[:, 0:1], in_=lg[:, 0:8], axis=AX.X), r=[B_lg], w=[B_rs])
            S.op("dve", lambda e: e.tensor_scalar(out=ohg, in0=lg[:, 0:8], scalar1=rs[:, 0:1], scalar2=None, op0=ALU.is_equal), r=[B_lg, B_rs], w=[B_ohg])
            S.op("dve", lambda e: e.tensor_scalar(out=rs[:, 1:2], in0=rs[:, 0:1], scalar1=-1.0, scalar2=None, op0=ALU.mult), r=[B_rs], w=[B_rs])
            S.op("act", lambda e: e.activation(out=t8, in_=lg[:, 0:8], func=AF.Exp, bias=rs[:, 1:2], scale=1.0, accum_out=rs[:, 2:3]),
                 r=[B_lg, B_rs], w=[B_t8, B_rs])
            S.op("dve", lambda e: e.reciprocal(out=rs[:, 3:4], in_=rs[:, 2:3]), r=[B_rs], w=[B_rs])
            S.op("dve", lambda e: e.tensor_tensor(out=tmp88, in0=lg[:, 8:72].rearrange("p (g e) -> p g e", e=8),
                                                  in1=ohg.unsqueeze(2).to_broadcast([128, 8, 8]), op=ALU.mult), r=[B_lg, B_ohg], w=[B_t88])
            S.op("dve", lambda e: e.reduce_sum(out=le, in_=tmp88.rearrange("p g e -> p e g"), axis=AX.X), r=[B_t88], w=[B_le])
            S.op("dve", lambda e: e.max(out=t8, in_=le), r=[B_le], w=[B_t8])
            for k2 in range(2):
                S.op("dve", lambda e: e.tensor_scalar(out=ohk[:, k2, :], in0=le, scalar1=t8[:, k2:k2 + 1], scalar2=None, op0=ALU.is_equal),
                     r=[B_le, B_t8], w=[B_ohk])
            S.op("dve", lambda e: e.tensor_tensor(out=rs[:, 4:5], in0=t8[:, 1:2], in1=t8[:, 0:1], op=ALU.subtract), r=[B_t8], w=[B_rs])
            S.op("act", lambda e: e.activation(out=rs[:, 5:6], in_=rs[:, 4:5], func=AF.Exp), r=[B_rs], w=[B_rs])
            S.op("dve", lambda e: e.tensor_scalar(out=rs[:, 6:7], in0=rs[:, 5:6], scalar1=1.0, scalar2=None, op0=ALU.add), r=[B_rs], w=[B_rs])
            S.op("dve", lambda e: e.reciprocal(out=rs[:, 7:8], in_=rs[:, 6:7]), r=[B_rs], w=[B_rs])
            S.op("dve", lambda e: e.tensor_tensor(out=rs[:, 8:9], in0=rs[:, 5:6], in1=rs[:, 7:8], op=ALU.mult), r=[B_rs], w=[B_rs])
            S.op("dve", lambda e: e.tensor_tensor(out=WK[:, j, 0:1], in0=rs[:, 7:8], in1=rs[:, 3:4], op=ALU.mult), r=[B_rs], w=[B_WK])
            S.op("dve", lambda e: e.tensor_tensor(out=WK[:, j, 1:2], in0=rs[:, 8:9], in1=rs[:, 3:4], op=ALU.mult), r=[B_rs], w=[B_WK])
            for k2 in range(2):
                S.op("dve", lambda e: e.tensor_tensor(out=OHK[:, j, k2, :].rearrange("p (g e) -> p g e", e=8),
                                                      in0=ohg.unsqueeze(2).to_broadcast([128, 8, 8]),
                                                      in1=ohk[:, k2, :].unsqueeze(1).to_broadcast([128, 8, 8]), op=ALU.mult),
                     r=[B_ohg, B_ohk], w=[B_OHK])
            S.op("dve", lambda e: e.tensor_tensor(out=ohs, in0=OHK[:, j, 0, :], in1=OHK[:, j, 1, :], op=ALU.add), r=[B_OHK], w=[B_ohs])
            bk = 7
            S.op("pe", lambda e: e.matmul(pb[bk][:, 0:64], lhsT=ltb, rhs=ohs, start=True, stop=(j == 0)), r=[B_ltb, B_ohs], w=[PB[bk]])
            if j > 0:
                S.op("pe", lambda e: e.matmul(pb[bk][:, 0:64], lhsT=onesb, rhs=ohacc, start=False, stop=True), r=[B_ones, B_ohacc], w=[PB[bk]])
            S.op("dve", lambda e: e.tensor_copy(out=cum, in_=pb[bk][:, 0:64]), r=[PB[bk]], w=[B_cum])
            S.op("dve", lambda e: e.tensor_tensor(out=ohacc, in0=ohacc, in1=ohs, op=ALU.add), r=[B_ohacc, B_ohs], w=[B_ohacc])
            for k2 in range(2):
                S.op("dve", lambda e: e.tensor_tensor(out=tmp64, in0=OHK[:, j, k2, :], in1=cum, op=ALU.mult), r=[B_OHK, B_cum], w=[B_t64])
                S.op("dve", lambda e: e.reduce_sum(out=RK[:, j, k2:k2 + 1], in_=tmp64, axis=AX.X), r=[B_t64], w=[B_RK])
        bk = 7
        S.op("pe", lambda e: e.matmul(pb[bk][:, 0:64], lhsT=onesb, rhs=ohacc, start=True, stop=True), r=[B_ones, B_ohacc], w=[PB[bk]])
        cnt = sb("C_cnt", [128, 64], F32)
        B_cnt = S.buf("C_cnt")
        cnti = sb("C_cnti", [128, 64], I32)
        B_cnti = S.buf("C_cnti")
        padf = sb("C_padf", [128, 64], F32)
        B_padf = S.buf("C_padf")
        pend = sb("C_pend", [128, 64], F32)
        B_pend = S.buf("C_pend")
        pstart = sb("C_pstart", [128, 64], F32)
        B_pstart = S.buf("C_pstart")
        ones64 = sb("C_ones64", [128, 64], F32)
        B_o64 = S.buf("C_ones64")
        S.op("dve", lambda e: e.memset(ones64, 1.0), w=[B_o64])
        S.op("dve", lambda e: e.tensor_scalar(out=cnt, in0=pb[bk][:, 0:64], scalar1=127.0, scalar2=None, op0=ALU.add), r=[PB[bk]], w=[B_cnt])
        S.op("dve", lambda e: e.tensor_copy(out=cnti, in_=cnt), r=[B_cnt], w=[B_cnti])
        S.op("dve", lambda e: e.tensor_scalar(out=cnti, in0=cnti, scalar1=7, scalar2=7, op0=ALU.arith_shift_right, op1=ALU.logical_shift_left),
             r=[B_cnti], w=[B_cnti])
        S.op("dve", lambda e: e.tensor_copy(out=padf, in_=cnti), r=[B_cnti], w=[B_padf])
        S.op("dve", lambda e: e.tensor_tensor_scan(out=pend, data0=ones64, data1=padf, initial=0.0, op0=ALU.mult, op1=ALU.add),
             r=[B_o64, B_padf], w=[B_pend])
        S.op("dve", lambda e: e.tensor_tensor(out=pstart, in0=pend, in1=padf, op=ALU.subtract), r=[B_pend, B_padf], w=[B_pstart])
        S.op("dve", lambda e: e.tensor_scalar(out=tmp64, in0=pend, scalar1=bpos[:, 0:1], scalar2=None, op0=ALU.is_le), r=[B_pend, B_bpos], w=[B_t64])
        S.op("dve", lambda e: e.reduce_sum(out=rs[:, 10:11], in_=tmp64, axis=AX.X), r=[B_t64], w=[B_rs])
        S.op("dve", lambda e: e.tensor_scalar(out=rs[:, 11:12], in0=rs[:, 10:11], scalar1=63.0, scalar2=None, op0=ALU.min), r=[B_rs], w=[B_rs])
        diag = sb("C_diag", [128, 128], BF16)
        B_diag = S.buf("C_diag")
        S.op("dve", lambda e: e.tensor_scalar(out=diag, in0=ident, scalar1=rs[:, 11:12], scalar2=None, op0=ALU.mult), r=[B_ident, B_rs], w=[B_diag])
        bk = 6
        S.op("pe", lambda e: e.matmul(pb[bk][:, 0:128], lhsT=onesb, rhs=diag, start=True, stop=True), r=[B_ones, B_diag], w=[PB[bk]])
        widf = sb("C_widf", [128, 128], F32)
        B_widf = S.buf("C_widf")
        S.op("dve", lambda e: e.tensor_scalar(out=widf, in0=pb[bk][:, 0:128], scalar1=128.0, scalar2=pidx[:, 0:1], op0=ALU.mult, op1=ALU.add),
             r=[PB[bk], B_pidx], w=[B_widf])
        S.op("dve", lambda e: e.tensor_copy(out=WIDX, in_=widf), r=[B_widf], w=[B_widx])
        destf = sb("C_destf", [128, NTo, 2], F32)
        B_destf = S.buf("C_destf")
        slt = [sb("C_slt%d" % i, [128, 2], F32) for i in range(4)]
        B_slt = S.bufs("C_slt", 4)
        for j in range(NTo):
            for k2 in range(2):
                S.op("dve", lambda e: e.tensor_tensor(out=tmp64, in0=OHK[:, j, k2, :], in1=pstart, op=ALU.mult), r=[B_OHK, B_pstart], w=[B_t64])
                S.op("dve", lambda e: e.reduce_sum(out=rs[:, 12:13], in_=tmp64, axis=AX.X), r=[B_t64], w=[B_rs])
                S.op("dve", lambda e: e.tensor_tensor(out=destf[:, j, k2:k2 + 1], in0=rs[:, 12:13], in1=RK[:, j, k2:k2 + 1], op=ALU.add),
                     r=[B_rs, B_RK], w=[B_destf])
        S.op("dve", lambda e: e.tensor_copy(out=DESTI, in_=destf), r=[B_destf], w=[B_desti])
        for j in range(NTo):
            for k2 in range(2):
                q = (j * 2 + k2) % 4
                S.op("dve", lambda e: e.tensor_scalar(out=slt[q][:, 0:1], in0=pidx, scalar1=float(j * 128), scalar2=None, op0=ALU.add),
                     r=[B_pidx], w=[B_slt[q]])
                S.op("dve", lambda e: e.tensor_copy(out=slt[q][:, 1:2], in_=WK[:, j, k2:k2 + 1]), r=[B_WK], w=[B_slt[q]])
                S.dma("pool", lambda e: e.indirect_dma_start(out=SLOT_d, out_offset=bass.IndirectOffsetOnAxis(ap=DESTI[:, j, k2:k2 + 1], axis=0),
                                                             in_=slt[q], in_offset=None),
                      B_slt[q], r=[B_slt[q], B_desti, B_slotd], w=[])
        if debug:
            S.dma("sp", lambda e: e.dma_start(out=RT_d.rearrange("(j p) c -> p j c", p=128)[:, :, 0:2], in_=destf), B_destf, r=[B_destf])
            S.dma("sp", lambda e: e.dma_start(out=RT_d.rearrange("(j p) c -> p j c", p=128)[:, :, 2:4], in_=WK), B_WK, r=[B_WK])
            S.dma("sp", lambda e: e.dma_start(out=RT_d[0:128, 4:6], in_=rs[:, 10:12]), B_rs, r=[B_rs])
        S.end_phase()

    def phase_D():
        wg = [sb("D_wg%d" % i, [128, 16 * DE], BF16) for i in range(2)]
        wu = [sb("D_wu%d" % i, [128, 16 * DE], BF16) for i in range(2)]
        wd = [sb("D_wd%d" % i, [128, 4 * D], BF16) for i in range(2)]
        B_wg = S.bufs("D_wg", 2)
        B_wu = S.bufs("D_wu", 2)
        B_wd = S.bufs("D_wd", 2)
        sl = [sb("D_sl%d" % i, [128, 2], F32) for i in range(2)]
        B_sl = S.bufs("D_sl", 2)
        ti = [sb("D_ti%d" % i, [128, 1], I32) for i in range(2)]
        B_ti = S.bufs("D_ti", 2)
        xg = [sb("D_xg%d" % i, [128, D], BF16) for i in range(2)]
        B_xg = S.bufs("D_xg", 2)
        xT = sb("D_xT", [128, 16, 128], BF16)
        B_xT = S.buf("D_xT")
        sg = sb("D_sg", [128, DE], F32)
        B_sg = S.buf("D_sg")
        hid = sb("D_hid", [128, DE], BF16)
        B_hid = S.buf("D_hid")
        hT = sb("D_hT", [128, 4, 128], BF16)
        B_hT = S.buf("D_hT")
        yb = [sb("D_y%d" % i, [128, D], F32) for i in range(2)]
        B_yb = S.bufs("D_y", 2)
        wgv = IN('w_eg').rearrange("e (p k) n -> (e p) (k n)", k=16)
        wuv = IN('w_eu').rearrange("e (p k) n -> (e p) (k n)", k=16)
        wdv = IN('w_ed').rearrange("e (p k) n -> (e p) (k n)", k=4)

        def load(b):
            j = b % 2
            S.dma("sp", lambda e: e.dma_start(out=sl[j], in_=SLOT_d[b * 128:(b + 1) * 128, :]), B_sl[j], w=[B_sl[j]])
            S.op("dve", lambda e: e.tensor_copy(out=ti[j], in_=sl[j][:, 0:1]), r=[B_sl[j]], w=[B_ti[j]])
            S.dma("pool", lambda e: e.indirect_dma_start(out=xg[j], out_offset=None, in_=F_d,
                                                         in_offset=bass.IndirectOffsetOnAxis(ap=ti[j][:, 0:1], axis=0)),
                  B_xg[j], r=[B_ti[j]], w=[B_xg[j]])
            for (dst, B_dst, src) in ((wg[j], B_wg[j], wgv), (wu[j], B_wu[j], wuv), (wd[j], B_wd[j], wdv)):
                S.dma("pool", lambda e: e.indirect_dma_start(out=dst, out_offset=None, in_=src,
                                                             in_offset=bass.IndirectOffsetOnAxis(ap=WIDX[:, b:b + 1], axis=0)),
                      B_dst, r=[B_widx], w=[B_dst])

        load(0)
        for b in range(NBLK):
            j = b % 2
            if b + 1 < NBLK:
                load(b + 1)
            transpose_to(xg[j], B_xg[j], 16, lambda k0, k1: xT[:, k0:k1, :], B_xT, [0, 1], step=16)
            for (bk, wt, B_wt) in ((2, wg[j], B_wg[j]), (3, wu[j], B_wu[j])):
                for k in range(16):
                    S.op("pe", lambda e: e.matmul(pb[bk], lhsT=xT[:, k, :], rhs=wt[:, k * DE:(k + 1) * DE], start=(k == 0), stop=(k == 15)),
                         r=[B_xT, B_wt], w=[PB[bk]])
            S.op("act", lambda e: e.activation(out=sg, in_=pb[2], func=AF.Silu), r=[PB[2]], w=[B_sg])
            S.op("dve", lambda e: e.tensor_tensor(out=hid, in0=pb[3], in1=sg, op=ALU.mult), r=[PB[3], B_sg], w=[B_hid])
            transpose_to(hid, B_hid, 4, lambda k0, k1: hT[:, k0:k1, :], B_hT, [0, 1], step=4)
            for n in range(4):
                bk = 4 + n
                for k in range(4):
                    S.op("pe", lambda e: e.matmul(pb[bk], lhsT=hT[:, k, :], rhs=wd[j][:, k * D + n * 512:k * D + (n + 1) * 512],
                                                  start=(k == 0), stop=(k == 3)), r=[B_hT, B_wd[j]], w=[PB[bk]])
                if n % 2 == 0:
                    S.op("act", lambda e: e.activation(out=yb[j][:, n * 512:(n + 1) * 512], in_=pb[bk], func=AF.Copy, scale=sl[j][:, 1:2]),
                         r=[PB[bk], B_sl[j]], w=[B_yb[j]])
                else:
                    S.op("dve", lambda e: e.tensor_scalar(out=yb[j][:, n * 512:(n + 1) * 512], in0=pb[bk], scalar1=sl[j][:, 1:2], scalar2=None, op0=ALU.mult),
                         r=[PB[bk], B_sl[j]], w=[B_yb[j]])
            S.dma("sp", lambda e: e.dma_start(out=Y_d[b * 128:(b + 1) * 128, :], in_=yb[j]), B_yb[j], r=[B_yb[j]])
        S.end_phase()

    def phase_E():
        Wpg = sb("E_Wpg", [128, 16, D], BF16)
        B_Wpg = S.buf("E_Wpg")
        for k in range(16):
            S.dma("pool", lambda e: e.dma_start(out=Wpg[:, k, :], in_=IN('w_pg')[k * 128:(k + 1) * 128, :]), B_Wpg, w=[B_Wpg])
        Wpl = sb("E_Wpl", [128, 2, D], BF16)
        B_Wpl = S.buf("E_Wpl")
        for k in range(2):
            S.dma("pool", lambda e: e.dma_start(out=Wpl[:, k, :], in_=IN('w_ple')[k * 128:(k + 1) * 128, :]), B_Wpl, w=[B_Wpl])
        gple = sb("E_gple", [128, D], F32)
        bpg = sb("E_bpg", [128, D], F32)
        gfin = sb("E_gfin", [128, D], F32)
        B_gple = S.buf("E_gple")
        B_bpg = S.buf("E_bpg")
        B_gfin = S.buf("E_gfin")
        S.dma("sp", lambda e: e.dma_start(out=gple, in_=IN('g_ple').partition_broadcast(128)), B_gple, w=[B_gple])
        S.dma("sp", lambda e: e.dma_start(out=bpg, in_=IN('b_pg').partition_broadcast(128)), B_bpg, w=[B_bpg])
        S.dma("sp", lambda e: e.dma_start(out=gfin, in_=IN('g_final').partition_broadcast(128)), B_gfin, w=[B_gfin])
        h2 = [sb("E_h2%d" % i, [128, D], F32) for i in range(2)]
        y1 = [sb("E_y1%d" % i, [128, D], F32) for i in range(2)]
        y2 = [sb("E_y2%d" % i, [128, D], F32) for i in range(2)]
        pbf = [sb("E_p%d" % i, [128, PLE], BF16) for i in range(2)]
        B_h2 = S.bufs("E_h2", 2)
        B_y1 = S.bufs("E_y1", 2)
        B_y2 = S.bufs("E_y2", 2)
        B_pbf = S.bufs("E_p", 2)
        hn = sb("E_hn", [128, D], BF16)
        B_hn = S.buf("E_hn")
        hT = sb("E_hT", [128, 16, 128], BF16)
        B_hT = S.buf("E_hT")
        pT = sb("E_pT", [128, 2, 128], BF16)
        B_pT = S.buf("E_pT")
        gl = sb("E_gl", [128, 512], F32)
        B_gl = S.buf("E_gl")
        sgm = sb("E_sg", [128, 512], F32)
        B_sgm = S.buf("E_sg")
        h3 = sb("E_h3", [128, D], F32)
        B_h3 = S.buf("E_h3")
        ob = [sb("E_o%d" % i, [128, D], F32) for i in range(2)]
        B_ob = S.bufs("E_o", 2)
        st = [sb("E_st%d" % i, [128, 8], F32) for i in range(2)]
        B_st = S.bufs("E_st", 2)
        st2 = [sb("E_su%d" % i, [128, 8], F32) for i in range(2)]
        B_st2 = S.bufs("E_su", 2)

        def load(j):
            b = j % 2
            S.dma("sp", lambda e: e.dma_start(out=h2[b], in_=H1_d[j * 128:(j + 1) * 128, :]), B_h2[b], w=[B_h2[b]])
            S.dma("pool", lambda e: e.dma_start(out=pbf[b], in_=IN('p_own')[j * 128:(j + 1) * 128, :]), B_pbf[b], w=[B_pbf[b]])
            S.dma("pool", lambda e: e.indirect_dma_start(out=y1[b], out_offset=None, in_=Y_d,
                                                         in_offset=bass.IndirectOffsetOnAxis(ap=DESTI[:, j, 0:1], axis=0)),
                  B_y1[b], r=[B_desti], w=[B_y1[b]])
            S.dma("pool", lambda e: e.indirect_dma_start(out=y2[b], out_offset=None, in_=Y_d,
                                                         in_offset=bass.IndirectOffsetOnAxis(ap=DESTI[:, j, 1:2], axis=0)),
                  B_y2[b], r=[B_desti], w=[B_y2[b]])

        load(0)
        for j in range(NTo):
            b = j % 2
            if j + 1 < NTo:
                load(j + 1)
            S.op("dve", lambda e: e.tensor_tensor(out=h2[b], in0=h2[b], in1=y1[b], op=ALU.add), r=[B_h2[b], B_y1[b]], w=[B_h2[b]])
            S.op("dve", lambda e: e.tensor_tensor(out=h2[b], in0=h2[b], in1=y2[b], op=ALU.add), r=[B_h2[b], B_y2[b]], w=[B_h2[b]])
            rmsnorm_rstd(h2[b], B_h2[b], hn, B_hn, st[b], B_st[b], D)
            S.op("dve", lambda e: e.scalar_tensor_tensor(out=hn, in0=h2[b], scalar=st[b][:, 2:3], in1=gple, op0=ALU.mult, op1=ALU.mult),
                 r=[B_h2[b], B_st[b], B_gple], w=[B_hn])
            transpose_to(hn, B_hn, 16, lambda k0, k1: hT[:, k0:k1, :], B_hT, [0, 1])
            transpose_to(pbf[b], B_pbf[b], 2, lambda k0, k1: pT[:, k0:k1, :], B_pT, [0, 1])
            for n in range(4):
                bg = 2 + (n % 2) * 2
                bp_ = 3 + (n % 2) * 2
                for k in range(16):
                    S.op("pe", lambda e: e.matmul(pb[bg], lhsT=hT[:, k, :], rhs=Wpg[:, k, n * 512:(n + 1) * 512], start=(k == 0), stop=(k == 15)),
                         r=[B_hT, B_Wpg], w=[PB[bg]])
                for k in range(2):
                    S.op("pe", lambda e: e.matmul(pb[bp_], lhsT=pT[:, k, :], rhs=Wpl[:, k, n * 512:(n + 1) * 512], start=(k == 0), stop=(k == 1)),
                         r=[B_pT, B_Wpl], w=[PB[bp_]])
                cs = slice(n * 512, (n + 1) * 512)
                S.op("dve", lambda e: e.tensor_tensor(out=gl, in0=pb[bg], in1=bpg[:, cs], op=ALU.add), r=[PB[bg], B_bpg], w=[B_gl])
                S.op("act", lambda e: e.activation(out=sgm, in_=gl, func=AF.Sigmoid), r=[B_gl], w=[B_sgm])
                S.op("dve", lambda e: e.tensor_tensor(out=sgm, in0=pb[bp_], in1=sgm, op=ALU.mult), r=[PB[bp_], B_sgm], w=[B_sgm])
                S.op("dve", lambda e: e.tensor_tensor(out=h3[:, cs], in0=h2[b][:, cs], in1=sgm, op=ALU.add), r=[B_h2[b], B_sgm], w=[B_h3])
            rmsnorm_rstd(h3, B_h3, ob[b], B_ob[b], st2[b], B_st2[b], D)
            S.op("dve", lambda e: e.scalar_tensor_tensor(out=ob[b], in0=h3, scalar=st2[b][:, 2:3], in1=gfin, op0=ALU.mult, op1=ALU.mult),
                 r=[B_h3, B_st2[b], B_gfin], w=[B_ob[b]])
            S.dma("sp", lambda e: e.dma_start(out=out[j * 128:(j + 1) * 128, :], in_=ob[b]), B_ob[b], r=[B_ob[b]])
        S.end_phase()

    phases = [("A", phase_A), ("A2", phase_A2), ("B", phase_B), ("C", phase_C), ("D", phase_D), ("E", phase_E)]
    for name, fn in phases:
        fn()
        c.stack.close()
        c.stack = contextlib.ExitStack()
        if name == upto:
            break
    return nc, S, c


def make_in_maps(inputs, S_len, used=None):
    x = np.asarray(inputs["x"], np.float32)
    Bn = x.shape[0]
    NB = S_len // 256
    NBo = NB // 2
    p = np.asarray(inputs["p"], np.float32)[0]
    sq = lambda k: np.ascontiguousarray(np.asarray(inputs[k], np.float32)[0])
    row = lambda a: np.ascontiguousarray(a.reshape(1, -1))
    shared = dict(
        g_mix=row(sq("g_mix")), w_in=sq("w_in"), beta_attn=row(sq("beta_attn")), w_pool=sq("w_pool"),
        pool_scale=row(sq("pool_scale")), w_out=sq("w_out"), g_ffn=row(sq("g_ffn")),
        w_rg=sq("w_router_group"), b_rg=row(sq("b_router_group")), w_re=sq("w_router_expert"),
        b_re=row(sq("b_router_expert")), w_eg=sq("w_expert_gate"), w_eu=sq("w_expert_up"),
        w_ed=sq("w_expert_down"), g_ple=row(sq("g_ple")), w_ple=sq("w_ple"), w_pg=sq("w_ple_gate"),
        b_pg=row(sq("b_ple_gate")), g_final=row(np.asarray(inputs["g_final"], np.float32)),
    )
    tabs = [host_tables(S_len, r) for r in range(2)]
    in_maps = []
    orders = []
    for cidx in range(2 * Bn):
        b, r = cidx // 2, cidx % 2
        order = []
        for i in range(NBo):
            order += [2 * i + r, 2 * i + 1 - r]
        own = [2 * i + r for i in range(NBo)]
        xb = x[b].reshape(NB, 256, D)
        pb_ = p[b].reshape(NB, 256, PLE)
        m = dict(shared)
        m["x_perm"] = np.ascontiguousarray(xb[order].reshape(S_len, D))
        m["p_own"] = np.ascontiguousarray(pb_[own].reshape(-1, PLE))
        m.update(tabs[r])
        if used is not None:
            m = {k: v for k, v in m.items() if k in used}
        in_maps.append(m)
        orders.append(own)
    return in_maps, orders


def kernel(**inputs):
    x = np.asarray(inputs["x"])
    Bn, S_len, _ = x.shape
    nc, S, c = build(S_len, debug=False, upto="E")
    in_maps, orders = make_in_maps(inputs, S_len, used=set(c.used))
    ncores = 2 * Bn
    res = run_bass_kernel_spmd(nc, in_maps, core_ids=list(range(ncores)))
    outp = np.empty((Bn, S_len // 256, 256, D), np.float32)
    for cidx in range(ncores):
        b = cidx // 2
        o = np.asarray(res.results[cidx]["out"], np.float32).reshape(-1, 256, D)
        outp[b, orders[cidx]] = o
    return outp.reshape(Bn, S_len, D)
```

```python
import numpy as np
import concourse.bass as bass
import concourse.mybir as mybir
from concourse.bass_utils import run_bass_kernel_spmd

F32 = mybir.dt.float32
BF16 = mybir.dt.bfloat16
I32 = mybir.dt.int32
AF = mybir.ActivationFunctionType
ALU = mybir.AluOpType
AX = mybir.AxisListType

D = 2048
H = 8
HD = 128
AW = 1024
PW = 1024
INW = 4096
NE = 64
NG = 8
DE = 512
PLE = 256
EPS = 1e-6
import os
SKIP = os.environ.get('KSKIP', '')
NEG = -1.0e30


class Buf:
    __slots__ = ("name", "wev", "revs", "slot", "excl")

    def __init__(self, name, excl=False):
        self.name = name
        self.excl = excl
        self.wev = None
        self.revs = {}
        self.slot = None


class Sched:
    def __init__(self, nc):
        self.nc = nc
        self.eng = {"pe": nc.tensor, "act": nc.scalar, "dve": nc.vector, "pool": nc.gpsimd, "sp": nc.sync}
        self.esem = {k: nc.alloc_semaphore("es_" + k) for k in self.eng}
        self.ecnt = {k: 0 for k in self.eng}
        self.seen = {k: {} for k in self.eng}
        self.free_slots = []
        self.nslots = 0
        self.phase_bufs = []
        self.ninst = 0
        self.nwait = 0

    def buf(self, name, excl=False):
        b = Buf(name, excl)
        self.phase_bufs.append(b)
        return b

    def bufs(self, name, n, excl=False):
        return [self.buf("%s%d" % (name, i), excl) for i in range(n)]

    def _slot(self, b):
        if b.slot is None:
            if self.free_slots:
                b.slot = self.free_slots.pop()
            else:
                b.slot = [self.nc.alloc_semaphore("ds%d" % self.nslots), 0]
                self.nslots += 1
        return b.slot

    def _waits(self, e, r, w):
        need = {}

        def add(ev):
            s, v, src = ev
            if src == e and e == "pe":
                return
            k = id(s)
            if k not in need or need[k][1] < v:
                need[k] = (s, v)

        for b in r:
            if b.wev is not None:
                add(b.wev)
            if b.excl:
                for ev in b.revs.values():
                    if ev[2] != e:
                        add(ev)
        for b in w:
            if b.wev is not None:
                add(b.wev)
            for ev in b.revs.values():
                if ev[2] == e:
                    continue
                add(ev)
        seen = self.seen[e]
        for k, (s, v) in need.items():
            if seen.get(k, 0) >= v:
                continue
            self.eng[e].wait_ge(s, v)
            self.nwait += 1
            seen[k] = v

    def _record(self, ev, r, w):
        k = id(ev[0])
        for b in r:
            b.revs[k] = ev
        for b in w:
            b.wev = ev
            b.revs = {}

    def op(self, e, fn, r=(), w=()):
        self._waits(e, r, w)
        ins = fn(self.eng[e])
        self.ecnt[e] += 1
        ins.then_inc(self.esem[e], 1)
        self.ninst += 1
        self._record((self.esem[e], self.ecnt[e], e), r, w)
        return ins

    def dma(self, e, fn, sb, r=(), w=()):
        self._waits(e, r, w)
        slot = self._slot(sb)
        ins = fn(self.eng[e])
        slot[1] += 16
        ins.then_inc(slot[0], 16)
        self.ninst += 1
        self._record((slot[0], slot[1], "dma"), r, w)
        return ins

    def end_phase(self):
        for b in self.phase_bufs:
            if b.slot is not None:
                s, v = b.slot
                if self.seen["sp"].get(id(s), 0) < v:
                    self.eng["sp"].wait_ge(s, v)
                    self.seen["sp"][id(s)] = v
        self.ecnt["sp"] += 1
        self.eng["sp"].nop().then_inc(self.esem["sp"], 1)
        for e in self.eng:
            for f in self.eng:
                if f == e:
                    continue
                s, v = self.esem[f], self.ecnt[f]
                if v > 0 and self.seen[e].get(id(s), 0) < v:
                    self.eng[e].wait_ge(s, v)
                    self.seen[e][id(s)] = v
        for b in self.phase_bufs:
            if b.slot is not None:
                self.free_slots.append(b.slot)
                b.slot = None
        self.phase_bufs = []


class Ctx:
    pass


def alibi_slopes():
    return np.array([2.0 ** (-8.0 * (h + 1) / H) for h in range(H)], np.float64)


def host_tables(S_len, r):
    NB = S_len // 256
    NBo = NB // 2
    NBP = max(NB, 8)
    sl = alibi_slopes()
    seqblk = np.zeros(NB, np.int64)
    for i in range(NBo):
        seqblk[2 * i] = 2 * i + r
        seqblk[2 * i + 1] = 2 * i + 1 - r
    j = np.arange(256)
    vtab = np.exp(-sl[None, None, :] * (255 - (np.arange(2)[None, :, None] * 128 + np.arange(128)[:, None, None])))
    mtab = np.zeros((NBo, 2, 128, H, NBP), np.float64)
    gbias = np.full((NBo, 128, NBP), NEG, np.float64)
    for i in range(NBo):
        own = seqblk[2 * i]
        for s in range(NB):
            if seqblk[s] < own:
                gbias[i, :, s] = 0.0
                for t in range(2):
                    qpos = own * 256 + t * 128 + np.arange(128)
                    dist = qpos - (seqblk[s] * 256 + 255)
                    mtab[i, t, :, :, s] = np.exp(-sl[None, :] * dist[:, None])
    kj = np.arange(128)[:, None]
    qi = np.arange(128)[None, :]
    ctab = np.zeros((128, H, 2, 128), np.float64)
    for h in range(H):
        ctab[:, h, 0, :] = np.where(qi >= kj, np.exp(-sl[h] * np.maximum(qi - kj, 0)), 0.0)
        ctab[:, h, 1, :] = np.exp(-sl[h] * (128 + qi - kj))
    wins = np.array([2, 4, 8, 16])
    t = seqblk[0] * 256 + np.arange(256)
    cnt = np.minimum(t[None, :] + 1, wins[:, None]).astype(np.float64)
    ptab = np.broadcast_to((1.0 / cnt)[None], (128, 4, 256))
    halo = np.zeros((128, 2), np.float64)
    halo[:, 0] = 1.0 if r == 0 else 0.0
    halo[:, 1] = 0.0 if r == 0 else 1.0
    f = lambda a: np.ascontiguousarray(a, dtype=np.float32)
    lt = (np.arange(128)[:, None] < np.arange(128)[None, :]).astype(np.float32)
    return dict(ident=np.eye(128, dtype=np.float32), vtab=f(vtab), mtab=f(mtab), gbias=f(gbias),
                ctab=f(ctab), ptab=f(ptab), halo=f(halo), ltri=lt,
                bpos=f((np.arange(128) * 128.0).reshape(128, 1)),
                pidx=f(np.arange(128).reshape(128, 1)))


def build(S_len, debug=False, upto="E"):
    nc = bass.Bass("TRN2", target_bir_lowering=False)
    NB = S_len // 256
    NBo = NB // 2
    NBP = max(NB, 8)
    To = S_len // 2
    NTo = To // 128
    NT = S_len // 128
    PL = 2 * To + NE * 128
    NBLK = PL // 128
    skind = "ExternalOutput" if debug else "Internal"

    def din(name, shape, dt=F32):
        return nc.dram_tensor(name, list(shape), dt, kind="ExternalInput").ap()

    def dscr(name, shape, dt):
        return nc.dram_tensor(name, list(shape), dt, kind=skind).ap()

    c = Ctx()
    c.nc = nc
    in_shapes = dict(
        x_perm=[S_len, D], p_own=[To, PLE], g_mix=[1, D], w_in=[D, INW], beta_attn=[1, AW],
        w_pool=[4, 256, 256], pool_scale=[1, PW], w_out=[D, D], g_ffn=[1, D], w_rg=[D, NG], b_rg=[1, NG],
        w_re=[NG, D, 8], b_re=[1, NE], w_eg=[NE, D, DE], w_eu=[NE, D, DE], w_ed=[NE, DE, D], g_ple=[1, D],
        w_ple=[PLE, D], w_pg=[D, D], b_pg=[1, D], g_final=[1, D], ident=[128, 128], vtab=[128, 2, H],
        mtab=[NBo, 2, 128, H, NBP], gbias=[NBo, 128, NBP], ctab=[128, H, 2, 128], ptab=[128, 4, 256],
        halo=[128, 2], ltri=[128, 128], bpos=[128, 1], pidx=[128, 1])
    c.used = {}

    def IN(name):
        if name not in c.used:
            c.used[name] = din(name, in_shapes[name])
        return c.used[name]
    out = nc.dram_tensor("out", [To, D], F32, kind="ExternalOutput").ap()
    KT_d = dscr("KT_d", [H, 128, S_len], BF16)
    VP_d = dscr("VP_d", [H, 128, NT, 129], BF16)
    V1_d = dscr("V1_d", [H, 128, NTo, 129], BF16)
    QT_d = dscr("QT_d", [H, 128, To], BF16)
    UT_d = dscr("UT_d", [8, 128, NBo, 272], F32)
    KM_d = dscr("KM_d", [128, H, NBP], F32)
    OA_d = dscr("OA_d", [To, AW], F32)
    MP_d = dscr("MP_d", [To, PW], BF16)
    H1_d = dscr("H1_d", [To, D], F32)
    F_d = dscr("F_d", [To, D], BF16)
    SLOT_d = dscr("SLOT_d", [PL, 2], F32)
    Y_d = dscr("Y_d", [PL, D], F32)
    RT_d = dscr("RT_d", [To, 8], F32)

    S = Sched(nc)
    c.S = S
    import contextlib
    c.stack = contextlib.ExitStack()

    def sb(name, shape, dt):
        t = c.stack.enter_context(nc.sbuf_tensor(name, list(shape), dt))
        return t.ap() if hasattr(t, "ap") else t[:]
    pb = [nc.alloc_psum_tensor("pb%d" % i, [128, 512], F32).ap() for i in range(8)]
    PB = S.bufs("pbank", 8, excl=True)

    ident = nc.alloc_sbuf_tensor("identb", [128, 128], BF16).ap()
    B_ident = Buf("ident")
    S.dma("pool", lambda e: e.dma_start(out=ident, in_=IN('ident')), B_ident, w=[B_ident])

    def rmsnorm_rstd(e_xt, B_xt, junk, B_junk, st, B_st, width):
        S.op("act", lambda e: e.activation(out=junk, in_=e_xt, func=AF.Square, accum_out=st[:, 0:1]),
             r=[B_xt], w=[B_junk, B_st])
        S.op("act", lambda e: e.activation(out=st[:, 1:2], in_=st[:, 0:1], func=AF.Sqrt, scale=1.0 / width, bias=EPS),
             r=[B_st], w=[B_st])
        S.op("dve", lambda e: e.reciprocal(out=st[:, 2:3], in_=st[:, 1:2]), r=[B_st], w=[B_st])

    def transpose_to(src, B_src, nchunk, dst_fn, B_dst, tps, step=1):
        for g0 in range(0, nchunk, 8):
            n = min(8, nchunk - g0)
            bi = tps[(g0 // 8) % len(tps)]
            tpv = pb[bi].bitcast(BF16).rearrange("p (k n) -> p k n", n=128)
            for k in range(g0, g0 + n):
                if step == 1:
                    sv = src[:, k * 128:(k + 1) * 128]
                else:
                    sv = src[:, k:k + 127 * step + 1:step]
                S.op("pe", lambda e: e.transpose(tpv[:, k - g0, :], sv, ident), r=[B_src, B_ident], w=[PB[bi]])
            eng = "act" if (g0 // 8) % 2 == 0 else "dve"
            if eng == "act":
                S.op("act", lambda e: e.copy(out=dst_fn(g0, g0 + n), in_=tpv[:, 0:n, :]), r=[PB[bi]], w=[B_dst])
            else:
                S.op("dve", lambda e: e.tensor_copy(out=dst_fn(g0, g0 + n), in_=tpv[:, 0:n, :]), r=[PB[bi]], w=[B_dst])

    def phase_A():
        Wb = sb("A_Wb", [128, 16, INW], BF16)
        B_W = S.buf("A_Wb")
        for k in range(16):
            S.dma("pool", lambda e: e.dma_start(out=Wb[:, k, :], in_=IN('w_in')[k * 128:(k + 1) * 128, :]), B_W, w=[B_W])
        gmix = sb("A_gmix", [128, D], F32)
        B_g = S.buf("A_gmix")
        S.dma("sp", lambda e: e.dma_start(out=gmix, in_=IN('g_mix').partition_broadcast(128)), B_g, w=[B_g])
        vtab = sb("A_vtab", [128, 2, H], F32)
        B_vt = S.buf("A_vtab")
        S.dma("sp", lambda e: e.dma_start(out=vtab, in_=IN('vtab')), B_vt, w=[B_vt])
        xb = [sb("A_x%d" % i, [128, D], F32) for i in range(2)]
        B_x = S.bufs("A_x", 2)
        ab = [sb("A_a%d" % i, [128, D], BF16) for i in range(2)]
        B_a = S.bufs("A_a", 2)
        stt = [sb("A_st%d" % i, [128, 4], F32) for i in range(2)]
        B_st = S.bufs("A_st", 2)
        aT = sb("A_aT", [128, 16, 512], BF16)
        B_aT = S.buf("A_aT")
        NSTG = 3
        fst = [sb("A_fs%d" % i, [128, 512], BF16) for i in range(NSTG)]
        B_fs = S.bufs("A_fs", NSTG)
        ust = [sb("A_us%d" % i, [128, 272], F32) for i in range(2)]
        B_us = S.bufs("A_us", 2)
        vp = [sb("A_vp%d" % i, [128, H, 129], BF16) for i in range(2)]
        B_vp = S.bufs("A_vp", 2)
        v1 = [sb("A_v1%d" % i, [128, H, 129], BF16) for i in range(2)]
        B_v1 = S.bufs("A_v1", 2)
        km = sb("A_km", [128, H, NBP], F32)
        B_km = S.buf("A_km")
        kms = sb("A_kms", [128, 2], F32)
        B_kms = S.buf("A_kms")
        S.op("dve", lambda e: e.memset(km, 0.0), w=[B_km])
        for i in range(2):
            S.op("dve", lambda e: e.memset(v1[i][:, :, 128:129], 1.0), w=[B_v1[i]])
        mmb = [2, 3, 4, 5, 6, 7]
        mmi = [0]

        def nextbank():
            b = mmb[mmi[0] % len(mmb)]
            mmi[0] += 1
            return b

        fsi = [0]
        tcount = [0]
        for i in range(NBo):
            for t in range(4):
                j = tcount[0] % 2
                tcount[0] += 1
                row0 = (i * 4 + t) * 128
                S.dma("sp", lambda e: e.dma_start(out=xb[j], in_=IN('x_perm')[row0:row0 + 128, :]), B_x[j], w=[B_x[j]])
                rmsnorm_rstd(xb[j], B_x[j], ab[j], B_a[j], stt[j], B_st[j], D)
                S.op("dve", lambda e: e.scalar_tensor_tensor(out=ab[j], in0=xb[j], scalar=stt[j][:, 2:3], in1=gmix,
                                                             op0=ALU.mult, op1=ALU.mult),
                     r=[B_x[j], B_st[j], B_g], w=[B_a[j]])
                transpose_to(ab[j], B_a[j], 16, lambda k0, k1: aT[:, k0:k1, t * 128:(t + 1) * 128], B_aT, [0, 1])
            for ch in range(H if 'k' not in SKIP else 0):
                bk = nextbank()
                for k in range(16):
                    S.op("pe", lambda e: e.matmul(pb[bk], lhsT=Wb[:, k, AW + ch * 128:AW + (ch + 1) * 128], rhs=aT[:, k, :],
                                                  start=(k == 0), stop=(k == 15)), r=[B_W, B_aT], w=[PB[bk]])
                f = fsi[0] % NSTG
                fsi[0] += 1
                S.op("act", lambda e: e.copy(out=fst[f], in_=pb[bk]), r=[PB[bk]], w=[B_fs[f]])
                S.op("dve", lambda e: e.reduce_sum(out=kms, in_=pb[bk].rearrange("p (b n) -> p b n", n=256), axis=AX.X),
                     r=[PB[bk], B_fs[f]], w=[B_kms])
                S.op("dve", lambda e: e.tensor_scalar(out=km[:, ch, 2 * i:2 * i + 2], in0=kms, scalar1=1.0 / 256, scalar2=None,
                                                      op0=ALU.mult), r=[B_kms], w=[B_km])
                S.dma("sp", lambda e: e.dma_start(out=KT_d[ch, :, i * 512:(i + 1) * 512], in_=fst[f]), B_fs[f], r=[B_fs[f]])
            for ch in range(H if 'q' not in SKIP else 0):
                bk = nextbank()
                for k in range(16):
                    S.op("pe", lambda e: e.matmul(pb[bk][:, 0:256], lhsT=Wb[:, k, ch * 128:(ch + 1) * 128], rhs=aT[:, k, 0:256],
                                                  start=(k == 0), stop=(k == 15)), r=[B_W, B_aT], w=[PB[bk]])
                f = fsi[0] % NSTG
                fsi[0] += 1
                S.op("act", lambda e: e.copy(out=fst[f][:, 0:256], in_=pb[bk][:, 0:256]), r=[PB[bk]], w=[B_fs[f]])
                S.dma("sp", lambda e: e.dma_start(out=QT_d[ch, :, i * 256:(i + 1) * 256], in_=fst[f][:, 0:256]), B_fs[f], r=[B_fs[f]])
            for ch in range(8 if 'u' not in SKIP else 0):
                bk = nextbank()
                for k in range(16):
                    S.op("pe", lambda e: e.matmul(pb[bk], lhsT=Wb[:, k, 3 * AW + ch * 128:3 * AW + (ch + 1) * 128], rhs=aT[:, k, :],
                                                  start=(k == 0), stop=(k == 15)), r=[B_W, B_aT], w=[PB[bk]])
                f = ch % 2
                S.op("act", lambda e: e.copy(out=ust[f][:, 0:256], in_=pb[bk][:, 0:256]), r=[PB[bk]], w=[B_us[f]])
                S.op("dve", lambda e: e.tensor_copy(out=ust[f][:, 256:272], in_=pb[bk][:, 496:512]), r=[PB[bk]], w=[B_us[f]])
                S.dma("sp", lambda e: e.dma_start(out=UT_d[ch, :, i, :], in_=ust[f]), B_us[f], r=[B_us[f]])
            for t in range(4 if 'v' not in SKIP else 0):
                par = t % 2
                tile_g = i * 4 + t
                jj = tile_g % 2
                for half in range(2):
                    bk = nextbank()
                    for k in range(16):
                        S.op("pe", lambda e: e.matmul(pb[bk], lhsT=aT[:, k, t * 128:(t + 1) * 128],
                                                      rhs=Wb[:, k, 2 * AW + half * 512:2 * AW + (half + 1) * 512],
                                                      start=(k == 0), stop=(k == 15)), r=[B_W, B_aT], w=[PB[bk]])
                    pv = pb[bk].rearrange("p (h n) -> p h n", n=128)
                    hs = slice(half * 4, (half + 1) * 4)
                    S.op("dve", lambda e: e.tensor_tensor(out=vp[jj][:, hs, 0:128], in0=pv,
                                                          in1=vtab[:, par, hs].unsqueeze(2).to_broadcast([128, 4, 128]),
                                                          op=ALU.mult), r=[PB[bk], B_vt], w=[B_vp[jj]])
                    if t < 2:
                        S.op("act", lambda e: e.copy(out=v1[jj][:, hs, 0:128], in_=pv), r=[PB[bk], B_vp[jj]], w=[B_v1[jj]])
                S.op("dve", lambda e: e.tensor_copy(out=vp[jj][:, :, 128:129], in_=vtab[:, par, :].unsqueeze(2)),
                     r=[B_vt], w=[B_vp[jj]])
                S.dma("sp", lambda e: e.dma_start(out=VP_d[:, :, tile_g, :].rearrange("h p c -> p h c"), in_=vp[jj]),
                      B_vp[jj], r=[B_vp[jj]])
                if t < 2:
                    S.dma("sp", lambda e: e.dma_start(out=V1_d[:, :, i * 2 + t, :].rearrange("h p c -> p h c"), in_=v1[jj]),
                          B_v1[jj], r=[B_v1[jj]])
        S.dma("sp", lambda e: e.dma_start(out=KM_d, in_=km), B_km, r=[B_km])
        S.end_phase()


    def phase_A2():
        wpb = sb("P_wp", [128, 4, 2, 256], BF16)
        B_wp = S.buf("P_wp")
        S.dma("pool", lambda e: e.dma_start(out=wpb, in_=IN('w_pool').rearrange("g (cc p) d -> p g cc d", p=128)), B_wp, w=[B_wp])
        psc = sb("P_psc", [128, PW], F32)
        B_psc = S.buf("P_psc")
        S.dma("sp", lambda e: e.dma_start(out=psc, in_=IN('pool_scale').partition_broadcast(128)), B_psc, w=[B_psc])
        ptab = sb("P_ptab", [128, 4, 256], F32)
        B_pt = S.buf("P_ptab")
        S.dma("sp", lambda e: e.dma_start(out=ptab, in_=IN('ptab')), B_pt, w=[B_pt])
        halo = sb("P_halo", [128, 2], F32)
        B_ha = S.buf("P_halo")
        S.dma("sp", lambda e: e.dma_start(out=halo, in_=IN('halo')), B_ha, w=[B_ha])
        ub = [sb("P_ub%d" % i, [128, 8, 272], F32) for i in range(2)]
        B_ub = S.bufs("P_ub", 2)
        tl = [sb("P_tl%d" % i, [128, 8, 32], F32) for i in range(2)]
        B_tl = S.bufs("P_tl", 2)
        Pb = sb("P_P", [128, 8, 272], F32)
        B_P = S.buf("P_P")
        Qb = sb("P_Q", [128, 8, 272], F32)
        B_Q = S.buf("P_Q")
        zb = sb("P_z", [128, 8, 256], BF16)
        B_z = S.buf("P_z")
        zt = sb("P_zt", [128, 2, 256], F32)
        B_zt = S.buf("P_zt")
        mp = [sb("P_mp%d" % i, [128, PW], BF16) for i in range(2)]
        B_mp = S.bufs("P_mp", 2)
        tmpf = sb("P_tmpf", [128, PW], F32)
        B_tmpf = S.buf("P_tmpf")
        st = [sb("P_st%d" % i, [128, 8], F32) for i in range(2)]
        B_st = S.bufs("P_st", 2)
        wins = [2, 4, 8, 16]
        UTv = UT_d.rearrange("c p i n -> p c i n")
        for i in range(NBo):
            j = i % 2
            A = ub[j]
            S.dma("sp", lambda e: e.dma_start(out=A[:, :, 16:272], in_=UTv[:, :, i, 0:256]), B_ub[j], w=[B_ub[j]])
            S.dma("sp", lambda e: e.dma_start(out=tl[j][:, :, 16:32], in_=UTv[:, :, i, 256:272]), B_tl[j], w=[B_tl[j]])
            if i > 0:
                S.dma("sp", lambda e: e.dma_start(out=tl[j][:, :, 0:16], in_=UTv[:, :, i - 1, 256:272]), B_tl[j], w=[B_tl[j]])
            else:
                S.op("dve", lambda e: e.memset(tl[j][:, :, 0:16], 0.0), w=[B_tl[j]])
            S.op("dve", lambda e: e.tensor_scalar(out=A[:, :, 0:16], in0=tl[j][:, :, 0:16], scalar1=halo[:, 0:1], scalar2=None, op0=ALU.mult),
                 r=[B_tl[j], B_ha], w=[B_ub[j]])
            S.op("dve", lambda e: e.scalar_tensor_tensor(out=A[:, :, 0:16], in0=tl[j][:, :, 16:32], scalar=halo[:, 1:2], in1=A[:, :, 0:16],
                                                         op0=ALU.mult, op1=ALU.add), r=[B_tl[j], B_ha, B_ub[j]], w=[B_ub[j]])
            S.op("dve", lambda e: e.tensor_tensor(out=Pb[:, :, 1:272], in0=A[:, :, 1:272], in1=A[:, :, 0:271], op=ALU.add),
                 r=[B_ub[j]], w=[B_P])
            S.op("dve", lambda e: e.tensor_tensor(out=Qb[:, 2:8, 3:272], in0=Pb[:, 2:8, 3:272], in1=Pb[:, 2:8, 1:270], op=ALU.add),
                 r=[B_P], w=[B_Q])
            S.op("dve", lambda e: e.tensor_tensor(out=Pb[:, 4:8, 7:272], in0=Qb[:, 4:8, 7:272], in1=Qb[:, 4:8, 3:268], op=ALU.add),
                 r=[B_Q], w=[B_P])
            S.op("dve", lambda e: e.tensor_tensor(out=Qb[:, 6:8, 15:272], in0=Pb[:, 6:8, 15:272], in1=Pb[:, 6:8, 7:264], op=ALU.add),
                 r=[B_P], w=[B_Q])
            for g in range(4):
                W = Pb if g % 2 == 0 else Qb
                B_Wb = B_P if g % 2 == 0 else B_Q
                cs = slice(2 * g, 2 * g + 2)
                if i == 0:
                    S.op("dve", lambda e: e.tensor_tensor(out=zt, in0=W[:, cs, 16:272],
                                                          in1=ptab[:, g, :].unsqueeze(1).to_broadcast([128, 2, 256]), op=ALU.mult),
                         r=[B_Wb, B_pt], w=[B_zt])
                    S.op("dve", lambda e: e.tensor_tensor(out=zb[:, cs, :], in0=zt, in1=A[:, cs, 16:272], op=ALU.subtract),
                         r=[B_zt, B_ub[j]], w=[B_z])
                else:
                    S.op("dve", lambda e: e.scalar_tensor_tensor(out=zb[:, cs, :], in0=W[:, cs, 16:272], scalar=1.0 / wins[g],
                                                                 in1=A[:, cs, 16:272], op0=ALU.mult, op1=ALU.subtract),
                         r=[B_Wb, B_ub[j]], w=[B_z])
            for t in range(2):
                jt = (i * 2 + t) % 2
                banks = [2 + 2 * jt, 3 + 2 * jt]
                for g in range(4):
                    bk = banks[g // 2]
                    for cc in range(2):
                        S.op("pe", lambda e: e.matmul(pb[bk][:, (g % 2) * 256:(g % 2 + 1) * 256], lhsT=zb[:, 2 * g + cc, t * 128:(t + 1) * 128],
                                                      rhs=wpb[:, g, cc, :], start=(cc == 0), stop=(cc == 1)),
                             r=[B_z, B_wp], w=[PB[bk]])
                S.op("act", lambda e: e.activation(out=tmpf[:, 0:512], in_=pb[banks[0]], func=AF.Square, accum_out=st[jt][:, 0:1]),
                     r=[PB[banks[0]]], w=[B_tmpf, B_st[jt]])
                S.op("act", lambda e: e.activation(out=tmpf[:, 512:1024], in_=pb[banks[1]], func=AF.Square, accum_out=st[jt][:, 3:4]),
                     r=[PB[banks[1]]], w=[B_tmpf, B_st[jt]])
                S.op("dve", lambda e: e.tensor_tensor(out=st[jt][:, 0:1], in0=st[jt][:, 0:1], in1=st[jt][:, 3:4], op=ALU.add),
                     r=[B_st[jt]], w=[B_st[jt]])
                S.op("act", lambda e: e.activation(out=st[jt][:, 1:2], in_=st[jt][:, 0:1], func=AF.Sqrt, scale=1.0 / PW, bias=EPS),
                     r=[B_st[jt]], w=[B_st[jt]])
                S.op("dve", lambda e: e.reciprocal(out=st[jt][:, 2:3], in_=st[jt][:, 1:2]), r=[B_st[jt]], w=[B_st[jt]])
                for hh in range(2):
                    S.op("dve", lambda e: e.scalar_tensor_tensor(out=mp[jt][:, hh * 512:(hh + 1) * 512], in0=pb[banks[hh]], scalar=st[jt][:, 2:3],
                                                                 in1=psc[:, hh * 512:(hh + 1) * 512], op0=ALU.mult, op1=ALU.mult),
                         r=[PB[banks[hh]], B_st[jt], B_psc], w=[B_mp[jt]])
                row0 = (i * 2 + t) * 128
                S.dma("sp", lambda e: e.dma_start(out=MP_d[row0:row0 + 128, :], in_=mp[jt]), B_mp[jt], r=[B_mp[jt]])
        S.end_phase()

    def phase_B():
        scale = HD ** -0.5
        kmf = sb("B_kmf", [128, H, NBP], F32)
        B_kmf = S.buf("B_kmf")
        S.dma("sp", lambda e: e.dma_start(out=kmf, in_=KM_d), B_kmf, w=[B_kmf])
        kmb = sb("B_kmb", [128, H, NBP], BF16)
        B_kmb = S.buf("B_kmb")
        S.op("dve", lambda e: e.tensor_copy(out=kmb, in_=kmf), r=[B_kmf], w=[B_kmb])
        gbias = sb("B_gbias", [128, NBo, NBP], F32)
        B_gb = S.buf("B_gbias")
        S.dma("sp", lambda e: e.dma_start(out=gbias, in_=IN('gbias').rearrange("i p s -> p i s")), B_gb, w=[B_gb])
        mtab = sb("B_mtab", [128, NBo, 2, H, NBP], F32)
        B_mt = S.buf("B_mtab")
        for i in range(NBo):
            S.dma("sp", lambda e: e.dma_start(out=mtab[:, i], in_=IN('mtab')[i].rearrange("t p h s -> p t h s")), B_mt, w=[B_mt])
        ctab = sb("B_ctab", [128, H, 2, 128], F32)
        B_ct = S.buf("B_ctab")
        S.dma("sp", lambda e: e.dma_start(out=ctab, in_=IN('ctab')), B_ct, w=[B_ct])
        KT = [sb("B_KT%d" % i, [128, S_len], BF16) for i in range(2)]
        VP = [sb("B_VP%d" % i, [128, NT, 129], BF16) for i in range(2)]
        QT = [sb("B_QT%d" % i, [128, To], BF16) for i in range(2)]
        V1 = [sb("B_V1%d" % i, [128, NTo, 129], BF16) for i in range(2)]
        B_KT = S.bufs("B_KT", 2)
        B_VP = S.bufs("B_VP", 2)
        B_QT = S.bufs("B_QT", 2)
        B_V1 = S.bufs("B_V1", 2)
        gs = sb("B_gs", [128, 2, NBP], F32)
        B_gs = S.buf("B_gs")
        top8 = sb("B_top8", [128, 2, 8], F32)
        B_t8 = S.buf("B_top8")
        sel = sb("B_sel", [128, 2, NBP], F32)
        B_sel = S.buf("B_sel")
        mm = [sb("B_m%d" % i, [128, 2, NBP], F32) for i in range(2)]
        B_m = S.bufs("B_m", 2)
        pT = [sb("B_pT%d" % i, [128, 512], BF16) for i in range(2)]
        B_pT = S.bufs("B_pT", 2)
        acc = [sb("B_acc%d" % i, [128, 2, 129], F32) for i in range(2)]
        B_acc = S.bufs("B_acc", 2)
        rec = sb("B_rec", [128, 2], F32)
        B_rec = S.buf("B_rec")
        oa = [sb("B_oa%d" % i, [128, 2, 128], F32) for i in range(2)]
        B_oa = S.bufs("B_oa", 2)
        itc = [0]

        def load_head(h):
            j = h % 2
            S.dma("sp", lambda e: e.dma_start(out=KT[j], in_=KT_d[h]), B_KT[j], w=[B_KT[j]])
            S.dma("sp", lambda e: e.dma_start(out=QT[j], in_=QT_d[h]), B_QT[j], w=[B_QT[j]])
            S.dma("sp", lambda e: e.dma_start(out=VP[j], in_=VP_d[h]), B_VP[j], w=[B_VP[j]])
            S.dma("sp", lambda e: e.dma_start(out=V1[j], in_=V1_d[h]), B_V1[j], w=[B_V1[j]])

        load_head(0)
        for h in range(H):
            j = h % 2
            if h + 1 < H:
                load_head(h + 1)
            for i in range(NBo):
                a = (h * NBo + i) % 2
                for t in range(2):
                    S.op("pe", lambda e: e.matmul(pb[0][:, t * 256:t * 256 + NBP], lhsT=QT[j][:, i * 256 + t * 128:i * 256 + (t + 1) * 128],
                                                  rhs=kmb[:, h, :], start=True, stop=True), r=[B_QT[j], B_kmb], w=[PB[0]])
                gv = pb[0].rearrange("p (t n) -> p t n", n=256)[:, :, 0:NBP]
                S.op("dve", lambda e: e.tensor_tensor(out=gs, in0=gv, in1=gbias[:, i, :].unsqueeze(1).to_broadcast([128, 2, NBP]), op=ALU.add),
                     r=[PB[0], B_gb], w=[B_gs])
                for t in range(2):
                    S.op("dve", lambda e: e.max(out=top8[:, t, :], in_=gs[:, t, :]), r=[B_gs], w=[B_t8])
                for t in range(2):
                    S.op("dve", lambda e: e.tensor_scalar(out=sel[:, t, :], in0=gs[:, t, :], scalar1=top8[:, t, 2:3], scalar2=None, op0=ALU.is_ge),
                         r=[B_gs, B_t8], w=[B_sel])
                S.op("dve", lambda e: e.tensor_tensor(out=mm[a], in0=sel, in1=mtab[:, i, :, h, :], op=ALU.mult), r=[B_sel, B_mt], w=[B_m[a]])
                it = itc[0]
                itc[0] += 1
                sbk = 1 + it % 2
                obk = 3 + it % 2
                pt = pT[it % 2]
                B_pt_ = B_pT[it % 2]
                q0 = i * 256
                S.op("pe", lambda e: e.matmul(pb[sbk][:, 0:256], lhsT=KT[j][:, (2 * i) * 256:(2 * i) * 256 + 128], rhs=QT[j][:, q0:q0 + 256],
                                              start=True, stop=True), r=[B_KT[j], B_QT[j]], w=[PB[sbk]])
                S.op("pe", lambda e: e.matmul(pb[sbk][:, 384:512], lhsT=KT[j][:, (2 * i) * 256 + 128:(2 * i) * 256 + 256],
                                              rhs=QT[j][:, q0 + 128:q0 + 256], start=True, stop=True), r=[B_KT[j], B_QT[j]], w=[PB[sbk]])
                S.op("act", lambda e: e.activation(out=pt[:, 0:256], in_=pb[sbk][:, 0:256], func=AF.Exp, scale=scale), r=[PB[sbk]], w=[B_pt_])
                S.op("act", lambda e: e.activation(out=pt[:, 384:512], in_=pb[sbk][:, 384:512], func=AF.Exp, scale=scale), r=[PB[sbk]], w=[B_pt_])
                S.op("dve", lambda e: e.tensor_tensor(out=pt[:, 0:256], in0=pt[:, 0:256], in1=ctab[:, h, :, :].rearrange("p a b -> p (a b)"), op=ALU.mult),
                     r=[B_pt_, B_ct], w=[B_pt_])
                S.op("dve", lambda e: e.tensor_tensor(out=pt[:, 384:512], in0=pt[:, 384:512], in1=ctab[:, h, 0, :], op=ALU.mult),
                     r=[B_pt_, B_ct], w=[B_pt_])
                S.op("pe", lambda e: e.matmul(pb[obk][:, 0:129], lhsT=pt[:, 0:128], rhs=V1[j][:, 2 * i, :], start=True, stop=True),
                     r=[B_pt_, B_V1[j]], w=[PB[obk]])
                S.op("pe", lambda e: e.matmul(pb[obk][:, 256:385], lhsT=pt[:, 128:256], rhs=V1[j][:, 2 * i, :], start=True, stop=False),
                     r=[B_pt_, B_V1[j]], w=[PB[obk]])
                S.op("pe", lambda e: e.matmul(pb[obk][:, 256:385], lhsT=pt[:, 384:512], rhs=V1[j][:, 2 * i + 1, :], start=False, stop=True),
                     r=[B_pt_, B_V1[j]], w=[PB[obk]])
                ov = pb[obk].rearrange("p (t n) -> p t n", n=256)[:, :, 0:129]
                S.op("dve", lambda e: e.tensor_copy(out=acc[a], in_=ov), r=[PB[obk]], w=[B_acc[a]])
                cands = list(range(2 * i)) + [2 * i + 1]
                for s_ in cands:
                    it = itc[0]
                    itc[0] += 1
                    sbk = 1 + it % 2
                    obk = 3 + it % 2
                    pt = pT[it % 2]
                    B_pt_ = B_pT[it % 2]
                    for kh in range(2):
                        S.op("pe", lambda e: e.matmul(pb[sbk][:, kh * 256:(kh + 1) * 256], lhsT=KT[j][:, s_ * 256 + kh * 128:s_ * 256 + (kh + 1) * 128],
                                                      rhs=QT[j][:, q0:q0 + 256], start=True, stop=True), r=[B_KT[j], B_QT[j]], w=[PB[sbk]])
                    S.op("act", lambda e: e.activation(out=pt, in_=pb[sbk], func=AF.Exp, scale=scale), r=[PB[sbk]], w=[B_pt_])
                    for t in range(2):
                        for kh in range(2):
                            S.op("pe", lambda e: e.matmul(pb[obk][:, t * 256:t * 256 + 129], lhsT=pt[:, kh * 256 + t * 128:kh * 256 + (t + 1) * 128],
                                                          rhs=VP[j][:, s_ * 2 + kh, :], start=(kh == 0), stop=(kh == 1)),
                                 r=[B_pt_, B_VP[j]], w=[PB[obk]])
                    for t in range(2):
                        S.op("dve", lambda e: e.scalar_tensor_tensor(out=acc[a][:, t, :], in0=pb[obk][:, t * 256:t * 256 + 129],
                                                                     scalar=mm[a][:, t, s_:s_ + 1], in1=acc[a][:, t, :],
                                                                     op0=ALU.mult, op1=ALU.add), r=[PB[obk], B_m[a], B_acc[a]], w=[B_acc[a]])
                S.op("dve", lambda e: e.reciprocal(out=rec, in_=acc[a][:, :, 128]), r=[B_acc[a]], w=[B_rec])
                for t in range(2):
                    S.op("dve", lambda e: e.tensor_scalar(out=oa[a][:, t, :], in0=acc[a][:, t, 0:128], scalar1=rec[:, t:t + 1], scalar2=None, op0=ALU.mult),
                         r=[B_acc[a], B_rec], w=[B_oa[a]])
                S.dma("sp", lambda e: e.dma_start(out=OA_d[q0:q0 + 256, h * 128:(h + 1) * 128].rearrange("(t p) c -> p t c", p=128), in_=oa[a]),
                      B_oa[a], r=[B_oa[a]])
        S.end_phase()

    WIDX = nc.alloc_sbuf_tensor("G_widx", [128, 128], I32).ap()
    B_widx = Buf("G_widx")
    DESTI = nc.alloc_sbuf_tensor("G_desti", [128, NTo, 2], I32).ap()
    B_desti = Buf("G_desti")

    def phase_C():
        Wo = sb("C_Wo", [128, 16, D], BF16)
        B_Wo = S.buf("C_Wo")
        for k in range(16):
            S.dma("pool", lambda e: e.dma_start(out=Wo[:, k, :], in_=IN('w_out')[k * 128:(k + 1) * 128, :]), B_Wo, w=[B_Wo])
        beta = sb("C_beta", [128, AW], F32)
        B_beta = S.buf("C_beta")
        S.dma("sp", lambda e: e.dma_start(out=beta, in_=IN('beta_attn').partition_broadcast(128)), B_beta, w=[B_beta])
        gffn = sb("C_gffn", [128, D], F32)
        B_gffn = S.buf("C_gffn")
        S.dma("sp", lambda e: e.dma_start(out=gffn, in_=IN('g_ffn').partition_broadcast(128)), B_gffn, w=[B_gffn])
        Wr32 = sb("C_Wr32", [128, 16, 72], F32)
        B_Wr32 = S.buf("C_Wr32")
        S.dma("sp", lambda e: e.dma_start(out=Wr32[:, :, 0:8], in_=IN('w_rg').rearrange("(k p) e -> p k e", p=128)), B_Wr32, w=[B_Wr32])
        for g in range(NG):
            S.dma("sp", lambda e: e.dma_start(out=Wr32[:, :, 8 + g * 8:16 + g * 8], in_=IN('w_re')[g].rearrange("(k p) e -> p k e", p=128)),
                  B_Wr32, w=[B_Wr32])
        Wr = sb("C_Wr", [128, 16, 72], BF16)
        B_Wr = S.buf("C_Wr")
        S.op("dve", lambda e: e.tensor_copy(out=Wr, in_=Wr32), r=[B_Wr32], w=[B_Wr])
        brb = sb("C_brb", [128, 72], F32)
        B_brb = S.buf("C_brb")
        S.dma("sp", lambda e: e.dma_start(out=brb[:, 0:8], in_=IN('b_rg').partition_broadcast(128)), B_brb, w=[B_brb])
        S.dma("sp", lambda e: e.dma_start(out=brb[:, 8:72], in_=IN('b_re').partition_broadcast(128)), B_brb, w=[B_brb])
        ltb = sb("C_ltb", [128, 128], BF16)
        B_ltb = S.buf("C_ltb")
        S.dma("pool", lambda e: e.dma_start(out=ltb, in_=IN('ltri')), B_ltb, w=[B_ltb])
        onesb = sb("C_ones", [128, 128], BF16)
        B_ones = S.buf("C_ones")
        S.op("dve", lambda e: e.memset(onesb, 1.0), w=[B_ones])
        pidx = sb("C_pidx", [128, 1], F32)
        B_pidx = S.buf("C_pidx")
        S.dma("sp", lambda e: e.dma_start(out=pidx, in_=IN('pidx')), B_pidx, w=[B_pidx])
        bpos = sb("C_bpos", [128, 1], F32)
        B_bpos = S.buf("C_bpos")
        S.dma("sp", lambda e: e.dma_start(out=bpos, in_=IN('bpos')), B_bpos, w=[B_bpos])
        zer = sb("C_zer", [128, (PL // 128) * 2], F32)
        B_zer = S.buf("C_zer")
        S.op("dve", lambda e: e.memset(zer, 0.0), w=[B_zer])
        B_slotd = S.buf("C_slotd")
        S.dma("sp", lambda e: e.dma_start(out=SLOT_d.rearrange("(p n) c -> p (n c)", p=128), in_=zer), B_zer, r=[B_zer], w=[B_slotd])
        OHK = sb("C_OHK", [128, NTo, 2, 64], F32)
        B_OHK = S.buf("C_OHK")
        RK = sb("C_RK", [128, NTo, 2], F32)
        B_RK = S.buf("C_RK")
        WK = sb("C_WK", [128, NTo, 2], F32)
        B_WK = S.buf("C_WK")
        ohacc = sb("C_ohacc", [128, 64], BF16)
        B_ohacc = S.buf("C_ohacc")
        S.op("dve", lambda e: e.memset(ohacc, 0.0), w=[B_ohacc])
        oa = [sb("C_oa%d" % i, [128, AW], F32) for i in range(2)]
        B_oa = S.bufs("C_oa", 2)
        mx = [sb("C_mx%d" % i, [128, D], BF16) for i in range(2)]
        B_mx = S.bufs("C_mx", 2)
        xt = [sb("C_x%d" % i, [128, D], F32) for i in range(2)]
        B_xt = S.bufs("C_x", 2)
        mT = sb("C_mT", [128, 16, 128], BF16)
        B_mT = S.buf("C_mT")
        h1 = [sb("C_h1%d" % i, [128, D], F32) for i in range(2)]
        B_h1 = S.bufs("C_h1", 2)
        fb = [sb("C_f%d" % i, [128, D], BF16) for i in range(2)]
        B_fb = S.bufs("C_f", 2)
        fT = sb("C_fT", [128, 16, 128], BF16)
        B_fT = S.buf("C_fT")
        st = [sb("C_st%d" % i, [128, 8], F32) for i in range(2)]
        B_st = S.bufs("C_st", 2)
        st2 = [sb("C_su%d" % i, [128, 8], F32) for i in range(2)]
        B_st2 = S.bufs("C_su", 2)
        lg = sb("C_lg", [128, 72], F32)
        B_lg = S.buf("C_lg")
        rs = sb("C_rs", [128, 32], F32)
        B_rs = S.buf("C_rs")
        ohg = sb("C_ohg", [128, 8], F32)
        B_ohg = S.buf("C_ohg")
        tmp88 = sb("C_tmp88", [128, 8, 8], F32)
        B_t88 = S.buf("C_tmp88")
        le = sb("C_le", [128, 8], F32)
        B_le = S.buf("C_le")
        t8 = sb("C_t8", [128, 8], F32)
        B_t8 = S.buf("C_t8")
        ohk = sb("C_ohk", [128, 2, 8], F32)
        B_ohk = S.buf("C_ohk")
        ohs = sb("C_ohs", [128, 64], BF16)
        B_ohs = S.buf("C_ohs")
        cum = sb("C_cum", [128, 64], F32)
        B_cum = S.buf("C_cum")
        tmp64 = sb("C_tmp64", [128, 64], F32)
        B_t64 = S.buf("C_tmp64")

        def load(j):
            b = j % 2
            i, t = j // 2, j % 2
            xr = i * 512 + t * 128
            S.dma("sp", lambda e: e.dma_start(out=oa[b], in_=OA_d[j * 128:(j + 1) * 128, :]), B_oa[b], w=[B_oa[b]])
            S.dma("sp", lambda e: e.dma_start(out=xt[b], in_=IN('x_perm')[xr:xr + 128, :]), B_xt[b], w=[B_xt[b]])
            S.dma("sp", lambda e: e.dma_start(out=mx[b][:, AW:D], in_=MP_d[j * 128:(j + 1) * 128, :]), B_mx[b], w=[B_mx[b]])

        load(0)
        for j in range(NTo):
            b = j % 2
            if j + 1 < NTo:
                load(j + 1)
            rmsnorm_rstd(oa[b], B_oa[b], mx[b][:, 0:AW], B_mx[b], st[b], B_st[b], AW)
            S.op("dve", lambda e: e.scalar_tensor_tensor(out=mx[b][:, 0:AW], in0=oa[b], scalar=st[b][:, 2:3], in1=beta, op0=ALU.mult, op1=ALU.mult),
                 r=[B_oa[b], B_st[b], B_beta], w=[B_mx[b]])
            transpose_to(mx[b], B_mx[b], 16, lambda k0, k1: mT[:, k0:k1, :], B_mT, [0, 1])
            for n in range(4):
                bk = 2 + n
                for k in range(16):
                    S.op("pe", lambda e: e.matmul(pb[bk], lhsT=mT[:, k, :], rhs=Wo[:, k, n * 512:(n + 1) * 512], start=(k == 0), stop=(k == 15)),
                         r=[B_mT, B_Wo], w=[PB[bk]])
                S.op("dve", lambda e: e.tensor_tensor(out=h1[b][:, n * 512:(n + 1) * 512], in0=pb[bk], in1=xt[b][:, n * 512:(n + 1) * 512], op=ALU.add),
                     r=[PB[bk], B_xt[b]], w=[B_h1[b]])
            S.dma("sp", lambda e: e.dma_start(out=H1_d[j * 128:(j + 1) * 128, :], in_=h1[b]), B_h1[b], r=[B_h1[b]])
            rmsnorm_rstd(h1[b], B_h1[b], fb[b], B_fb[b], st2[b], B_st2[b], D)
            S.op("dve", lambda e: e.scalar_tensor_tensor(out=fb[b], in0=h1[b], scalar=st2[b][:, 2:3], in1=gffn, op0=ALU.mult, op1=ALU.mult),
                 r=[B_h1[b], B_st2[b], B_gffn], w=[B_fb[b]])
            S.dma("sp", lambda e: e.dma_start(out=F_d[j * 128:(j + 1) * 128, :], in_=fb[b]), B_fb[b], r=[B_fb[b]])
            transpose_to(fb[b], B_fb[b], 16, lambda k0, k1: fT[:, k0:k1, :], B_fT, [0, 1])
            bk = 6
            for k in range(16):
                S.op("pe", lambda e: e.matmul(pb[bk][:, 0:72], lhsT=fT[:, k, :], rhs=Wr[:, k, :], start=(k == 0), stop=(k == 15)),
                     r=[B_fT, B_Wr], w=[PB[bk]])
            S.op("dve", lambda e: e.tensor_tensor(out=lg, in0=pb[bk][:, 0:72], in1=brb, op=ALU.add), r=[PB[bk], B_brb], w=[B_lg])
            S.op("dve", lambda e: e.reduce_max(out=rs[:, 0:1], in_=lg[:, 0:8], axis=AX.X), r=[B_lg], w=[B_rs])
            S.op("dve", lambda e: e.tensor_scalar(out=ohg, in0=lg[:, 0:8], scalar1=rs[:, 0:1], scalar2=None, op0=ALU.is_equal), r=[B_lg, B_rs], w=[B_ohg])
            S.op("dve", lambda e: e.tensor_scalar(out=rs[:, 1:2], in0=rs[:, 0:1], scalar1=-1.0, scalar2=None, op0=ALU.mult), r=[B_rs], w=[B_rs])
            S.op("act", lambda e: e.activation(out=t8, in_=lg[:, 0:8], func=AF.Exp, bias=rs[:, 1:2], scale=1.0, accum_out=rs[:, 2:3]),
                 r=[B_lg, B_rs], w=[B_t8, B_rs])
            S.op("dve", lambda e: e.reciprocal(out=rs[:, 3:4], in_=rs[:, 2:3]), r=[B_rs], w=[B_rs])
            S.op("dve", lambda e: e.tensor_tensor(out=tmp88, in0=lg[:, 8:72].rearrange("p (g e) -> p g e", e=8),
                                                  in1=ohg.unsqueeze(2).to_broadcast([128, 8, 8]), op=ALU.mult), r=[B_lg, B_ohg], w=[B_t88])
            S.op("dve", lambda e: e.reduce_sum(out=le, in_=tmp88.rearrange("p g e -> p e g"), axis=AX.X), r=[B_t88], w=[B_le])
            S.op("dve", lambda e: e.max(out=t8, in_=le), r=[B_le], w=[B_t8])
            for k2 in range(2):
                S.op("dve", lambda e: e.tensor_scalar(out=ohk[:, k2, :], in0=le, scalar1=t8[:, k2:k2 + 1], scalar2=None, op0=ALU.is_equal),
                     r=[B_le, B_t8], w=[B_ohk])
            S.op("dve", lambda e: e.tensor_tensor(out=rs[:, 4:5], in0=t8[:, 1:2], in1=t8[:, 0:1], op=ALU.subtract), r=[B_t8], w=[B_rs])
            S.op("act", lambda e: e.activation(out=rs[:, 5:6], in_=rs[:, 4:5], func=AF.Exp), r=[B_rs], w=[B_rs])
            S.op("dve", lambda e: e.tensor_scalar(out=rs[:, 6:7], in0=rs[:, 5:6], scalar1=1.0, scalar2=None, op0=ALU.add), r=[B_rs], w=[B_rs])
            S.op("dve", lambda e: e.reciprocal(out=rs[:, 7:8], in_=rs[:, 6:7]), r=[B_rs], w=[B_rs])
            S.op("dve", lambda e: e.tensor_tensor(out=rs[:, 8:9], in0=rs[:, 5:6], in1=rs[:, 7:8], op=ALU.mult), r=[B_rs], w=[B_rs])
            S.op("dve", lambda e: e.tensor_tensor(out=WK[:, j, 0:1], in0=rs[:, 7:8], in1=rs[:, 3:4], op=ALU.mult), r=[B_rs], w=[B_WK])
            S.op("dve", lambda e: e.tensor_tensor(out=WK[:, j, 1:2], in0=rs[:, 8:9], in1=rs[:, 3:4], op=ALU.mult), r=[B_rs], w=[B_WK])
            for k2 in range(2):
                S.op("dve", lambda e: e.tensor_tensor(out=OHK[:, j, k2, :].rearrange("p (g e) -> p g e", e=8),
                                                      in0=ohg.unsqueeze(2).to_broadcast([128, 8, 8]),
                                                      in1=ohk[:, k2, :].unsqueeze(1).to_broadcast([128, 8, 8]), op=ALU.mult),
                     r=[B_ohg, B_ohk], w=[B_OHK])
            S.op("dve", lambda e: e.tensor_tensor(out=ohs, in0=OHK[:, j, 0, :], in1=OHK[:, j, 1, :], op=ALU.add), r=[B_OHK], w=[B_ohs])
            bk = 7
            S.op("pe", lambda e: e.matmul(pb[bk][:, 0:64], lhsT=ltb, rhs=ohs, start=True, stop=(j == 0)), r=[B_ltb, B_ohs], w=[PB[bk]])
            if j > 0:
                S.op("pe", lambda e: e.matmul(pb[bk][:, 0:64], lhsT=onesb, rhs=ohacc, start=False, stop=True), r=[B_ones, B_ohacc], w=[PB[bk]])
            S.op("dve", lambda e: e.tensor_copy(out=cum, in_=pb[bk][:, 0:64]), r=[PB[bk]], w=[B_cum])
            S.op("dve", lambda e: e.tensor_tensor(out=ohacc, in0=ohacc, in1=ohs, op=ALU.add), r=[B_ohacc, B_ohs], w=[B_ohacc])
            for k2 in range(2):
                S.op("dve", lambda e: e.tensor_tensor(out=tmp64, in0=OHK[:, j, k2, :], in1=cum, op=ALU.mult), r=[B_OHK, B_cum], w=[B_t64])
                S.op("dve", lambda e: e.reduce_sum(out=RK[:, j, k2:k2 + 1], in_=tmp64, axis=AX.X), r=[B_t64], w=[B_RK])
        bk = 7
        S.op("pe", lambda e: e.matmul(pb[bk][:, 0:64], lhsT=onesb, rhs=ohacc, start=True, stop=True), r=[B_ones, B_ohacc], w=[PB[bk]])
        cnt = sb("C_cnt", [128, 64], F32)
        B_cnt = S.buf("C_cnt")
        cnti = sb("C_cnti", [128, 64], I32)
        B_cnti = S.buf("C_cnti")
        padf = sb("C_padf", [128, 64], F32)
        B_padf = S.buf("C_padf")
        pend = sb("C_pend", [128, 64], F32)
        B_pend = S.buf("C_pend")
        pstart = sb("C_pstart", [128, 64], F32)
        B_pstart = S.buf("C_pstart")
        ones64 = sb("C_ones64", [128, 64], F32)
        B_o64 = S.buf("C_ones64")
        S.op("dve", lambda e: e.memset(ones64, 1.0), w=[B_o64])
        S.op("dve", lambda e: e.tensor_scalar(out=cnt, in0=pb[bk][:, 0:64], scalar1=127.0, scalar2=None, op0=ALU.add), r=[PB[bk]], w=[B_cnt])
        S.op("dve", lambda e: e.tensor_copy(out=cnti, in_=cnt), r=[B_cnt], w=[B_cnti])
        S.op("dve", lambda e: e.tensor_scalar(out=cnti, in0=cnti, scalar1=7, scalar2=7, op0=ALU.arith_shift_right, op1=ALU.logical_shift_left),
             r=[B_cnti], w=[B_cnti])
        S.op("dve", lambda e: e.tensor_copy(out=padf, in_=cnti), r=[B_cnti], w=[B_padf])
        S.op("dve", lambda e: e.tensor_tensor_scan(out=pend, data0=ones64, data1=padf, initial=0.0, op0=ALU.mult, op1=ALU.add),
             r=[B_o64, B_padf], w=[B_pend])
        S.op("dve", lambda e: e.tensor_tensor(out=pstart, in0=pend, in1=padf, op=ALU.subtract), r=[B_pend, B_padf], w=[B_pstart])
        S.op("dve", lambda e: e.tensor_scalar(out=tmp64, in0=pend, scalar1=bpos[:, 0:1], scalar2=None, op0=ALU.is_le), r=[B_pend, B_bpos], w=[B_t64])
        S.op("dve", lambda e: e.reduce_sum(out=rs[:, 10:11], in_=tmp64, axis=AX.X), r=[B_t64], w=[B_rs])
        S.op("dve", lambda e: e.tensor_scalar(out=rs[:, 11:12], in0=rs[:, 10:11], scalar1=63.0, scalar2=None, op0=ALU.min), r=[B_rs], w=[B_rs])
        S.op("dve", lambda e: e.tensor_scalar(out=rs[:, 13:14], in0=bpos, scalar1=-128.0, scalar2=None, op0=ALU.add), r=[B_bpos], w=[B_rs])
        S.op("dve", lambda e: e.tensor_scalar(out=tmp64, in0=pend, scalar1=rs[:, 13:14], scalar2=None, op0=ALU.is_le), r=[B_pend, B_rs], w=[B_t64])
        S.op("dve", lambda e: e.reduce_sum(out=rs[:, 14:15], in_=tmp64, axis=AX.X), r=[B_t64], w=[B_rs])
        S.op("dve", lambda e: e.tensor_scalar(out=rs[:, 15:16], in0=rs[:, 14:15], scalar1=63.0, scalar2=None, op0=ALU.min), r=[B_rs], w=[B_rs])
        S.op("dve", lambda e: e.tensor_tensor(out=rs[:, 16:17], in0=rs[:, 11:12], in1=rs[:, 15:16], op=ALU.not_equal), r=[B_rs], w=[B_rs])
        S.op("dve", lambda e: e.tensor_scalar(out=rs[:, 17:18], in0=pidx, scalar1=0.0, scalar2=None, op0=ALU.is_equal), r=[B_pidx], w=[B_rs])
        S.op("dve", lambda e: e.tensor_tensor(out=rs[:, 18:19], in0=rs[:, 16:17], in1=rs[:, 17:18], op=ALU.max), r=[B_rs], w=[B_rs])
        S.op("dve", lambda e: e.tensor_scalar(out=rs[:, 19:20], in0=rs[:, 11:12], scalar1=-64.0, scalar2=None, op0=ALU.add), r=[B_rs], w=[B_rs])
        S.op("dve", lambda e: e.tensor_tensor(out=rs[:, 20:21], in0=rs[:, 19:20], in1=rs[:, 18:19], op=ALU.mult), r=[B_rs], w=[B_rs])
        S.op("dve", lambda e: e.tensor_scalar(out=rs[:, 21:22], in0=rs[:, 20:21], scalar1=64.0, scalar2=None, op0=ALU.add), r=[B_rs], w=[B_rs])
        diag = sb("C_diag", [128, 128], BF16)
        B_diag = S.buf("C_diag")
        S.op("dve", lambda e: e.tensor_scalar(out=diag, in0=ident, scalar1=rs[:, 21:22], scalar2=None, op0=ALU.mult), r=[B_ident, B_rs], w=[B_diag])
        bk = 6
        S.op("pe", lambda e: e.matmul(pb[bk][:, 0:128], lhsT=onesb, rhs=diag, start=True, stop=True), r=[B_ones, B_diag], w=[PB[bk]])
        widf = sb("C_widf", [128, 128], F32)
        B_widf = S.buf("C_widf")
        S.op("dve", lambda e: e.tensor_scalar(out=widf, in0=pb[bk][:, 0:128], scalar1=128.0, scalar2=pidx[:, 0:1], op0=ALU.mult, op1=ALU.add),
             r=[PB[bk], B_pidx], w=[B_widf])
        S.op("dve", lambda e: e.tensor_copy(out=WIDX, in_=widf), r=[B_widf], w=[B_widx])
        destf = sb("C_destf", [128, NTo, 2], F32)
        B_destf = S.buf("C_destf")
        slt = [sb("C_slt%d" % i, [128, 2], F32) for i in range(4)]
        B_slt = S.bufs("C_slt", 4)
        for j in range(NTo):
            for k2 in range(2):
                S.op("dve", lambda e: e.tensor_tensor(out=tmp64, in0=OHK[:, j, k2, :], in1=pstart, op=ALU.mult), r=[B_OHK, B_pstart], w=[B_t64])
                S.op("dve", lambda e: e.reduce_sum(out=rs[:, 12:13], in_=tmp64, axis=AX.X), r=[B_t64], w=[B_rs])
                S.op("dve", lambda e: e.tensor_tensor(out=destf[:, j, k2:k2 + 1], in0=rs[:, 12:13], in1=RK[:, j, k2:k2 + 1], op=ALU.add),
                     r=[B_rs, B_RK], w=[B_destf])
        S.op("dve", lambda e: e.tensor_copy(out=DESTI, in_=destf), r=[B_destf], w=[B_desti])
        for j in range(NTo):
            for k2 in range(2):
                q = (j * 2 + k2) % 4
                S.op("dve", lambda e: e.tensor_scalar(out=slt[q][:, 0:1], in0=pidx, scalar1=float(j * 128), scalar2=None, op0=ALU.add),
                     r=[B_pidx], w=[B_slt[q]])
                S.op("dve", lambda e: e.tensor_copy(out=slt[q][:, 1:2], in_=WK[:, j, k2:k2 + 1]), r=[B_WK], w=[B_slt[q]])
                S.dma("pool", lambda e: e.indirect_dma_start(out=SLOT_d, out_offset=bass.IndirectOffsetOnAxis(ap=DESTI[:, j, k2:k2 + 1], axis=0),
                                                             in_=slt[q], in_offset=None),
                      B_slt[q], r=[B_slt[q], B_desti, B_slotd], w=[])
        if debug:
            S.dma("sp", lambda e: e.dma_start(out=RT_d.rearrange("(j p) c -> p j c", p=128)[:, :, 0:2], in_=destf), B_destf, r=[B_destf])
            S.dma("sp", lambda e: e.dma_start(out=RT_d.rearrange("(j p) c -> p j c", p=128)[:, :, 2:4], in_=WK), B_WK, r=[B_WK])
            S.dma("sp", lambda e: e.dma_start(out=RT_d[0:128, 4:6], in_=rs[:, 10:12]), B_rs, r=[B_rs])
        S.end_phase()

    def phase_D():
        wg = sb("D_wg", [128, 16 * DE], BF16)
        wu = sb("D_wu", [128, 16 * DE], BF16)
        wd = sb("D_wd", [128, 4 * D], BF16)
        B_wg = S.buf("D_wg")
        B_wu = S.buf("D_wu")
        B_wd = S.buf("D_wd")
        sl = [sb("D_sl%d" % i, [128, 2], F32) for i in range(2)]
        B_sl = S.bufs("D_sl", 2)
        ti = [sb("D_ti%d" % i, [128, 1], I32) for i in range(2)]
        B_ti = S.bufs("D_ti", 2)
        xg = [sb("D_xg%d" % i, [128, D], BF16) for i in range(2)]
        B_xg = S.bufs("D_xg", 2)
        xT = sb("D_xT", [128, 16, 128], BF16)
        B_xT = S.buf("D_xT")
        sg = sb("D_sg", [128, DE], F32)
        B_sg = S.buf("D_sg")
        hid = sb("D_hid", [128, DE], BF16)
        B_hid = S.buf("D_hid")
        hT = sb("D_hT", [128, 4, 128], BF16)
        B_hT = S.buf("D_hT")
        yb = [sb("D_y%d" % i, [128, D], F32) for i in range(2)]
        B_yb = S.bufs("D_y", 2)
        wgv = IN('w_eg').rearrange("e (p k) n -> (e p) (k n)", k=16)
        wuv = IN('w_eu').rearrange("e (p k) n -> (e p) (k n)", k=16)
        wdv = IN('w_ed').rearrange("e (p k) n -> (e p) (k n)", k=4)

        def load_x(b):
            j = b % 2
            S.dma("sp", lambda e: e.dma_start(out=sl[j], in_=SLOT_d[b * 128:(b + 1) * 128, :]), B_sl[j], w=[B_sl[j]])
            S.op("dve", lambda e: e.tensor_copy(out=ti[j], in_=sl[j][:, 0:1]), r=[B_sl[j]], w=[B_ti[j]])
            S.dma("pool", lambda e: e.indirect_dma_start(out=xg[j], out_offset=None, in_=F_d,
                                                         in_offset=bass.IndirectOffsetOnAxis(ap=ti[j][:, 0:1], axis=0)),
                  B_xg[j], r=[B_ti[j]], w=[B_xg[j]])

        def load_w(b, which):
            for (dst, B_dst, src) in which:
                S.dma("pool", lambda e: e.indirect_dma_start(out=dst, out_offset=None, in_=src,
                                                             in_offset=bass.IndirectOffsetOnAxis(ap=WIDX[:, b:b + 1], axis=0),
                                                             bounds_check=bc_reg, oob_is_err=False),
                      B_dst, r=[B_widx], w=[B_dst])

        bc_reg = nc.gpsimd.alloc_register("bc_reg")
        nc.gpsimd.reg_mov(bc_reg, NE * 128 - 1)
        GU = ((wg, B_wg, wgv), (wu, B_wu, wuv))
        DN = ((wd, B_wd, wdv),)
        load_x(0)
        load_w(0, GU)
        load_w(0, DN)
        for b in range(NBLK):
            j = b % 2
            if b + 1 < NBLK:
                load_x(b + 1)
            transpose_to(xg[j], B_xg[j], 16, lambda k0, k1: xT[:, k0:k1, :], B_xT, [0, 1], step=16)
            for (bk, wt, B_wt) in ((2, wg, B_wg), (3, wu, B_wu)):
                for k in range(16):
                    S.op("pe", lambda e: e.matmul(pb[bk], lhsT=xT[:, k, :], rhs=wt[:, k * DE:(k + 1) * DE], start=(k == 0), stop=(k == 15)),
                         r=[B_xT, B_wt], w=[PB[bk]])
            if b + 1 < NBLK:
                load_w(b + 1, GU)
            S.op("act", lambda e: e.activation(out=sg, in_=pb[2], func=AF.Silu), r=[PB[2]], w=[B_sg])
            S.op("dve", lambda e: e.tensor_tensor(out=hid, in0=pb[3], in1=sg, op=ALU.mult), r=[PB[3], B_sg], w=[B_hid])
            transpose_to(hid, B_hid, 4, lambda k0, k1: hT[:, k0:k1, :], B_hT, [0, 1], step=4)
            for n in range(4):
                bk = 4 + n
                for k in range(4):
                    S.op("pe", lambda e: e.matmul(pb[bk], lhsT=hT[:, k, :], rhs=wd[:, k * D + n * 512:k * D + (n + 1) * 512],
                                                  start=(k == 0), stop=(k == 3)), r=[B_hT, B_wd], w=[PB[bk]])
            if b + 1 < NBLK:
                load_w(b + 1, DN)
            for n in range(4):
                bk = 4 + n
                if n % 2 == 0:
                    S.op("act", lambda e: e.activation(out=yb[j][:, n * 512:(n + 1) * 512], in_=pb[bk], func=AF.Copy, scale=sl[j][:, 1:2]),
                         r=[PB[bk], B_sl[j]], w=[B_yb[j]])
                else:
                    S.op("dve", lambda e: e.tensor_scalar(out=yb[j][:, n * 512:(n + 1) * 512], in0=pb[bk], scalar1=sl[j][:, 1:2], scalar2=None, op0=ALU.mult),
                         r=[PB[bk], B_sl[j]], w=[B_yb[j]])
            S.dma("sp", lambda e: e.dma_start(out=Y_d[b * 128:(b + 1) * 128, :], in_=yb[j]), B_yb[j], r=[B_yb[j]])
        S.end_phase()

    def phase_E():
        Wpg = sb("E_Wpg", [128, 16, D], BF16)
        B_Wpg = S.buf("E_Wpg")
        for k in range(16):
            S.dma("pool", lambda e: e.dma_start(out=Wpg[:, k, :], in_=IN('w_pg')[k * 128:(k + 1) * 128, :]), B_Wpg, w=[B_Wpg])
        Wpl = sb("E_Wpl", [128, 2, D], BF16)
        B_Wpl = S.buf("E_Wpl")
        for k in range(2):
            S.dma("pool", lambda e: e.dma_start(out=Wpl[:, k, :], in_=IN('w_ple')[k * 128:(k + 1) * 128, :]), B_Wpl, w=[B_Wpl])
        gple = sb("E_gple", [128, D], F32)
        bpg = sb("E_bpg", [128, D], F32)
        gfin = sb("E_gfin", [128, D], F32)
        B_gple = S.buf("E_gple")
        B_bpg = S.buf("E_bpg")
        B_gfin = S.buf("E_gfin")
        S.dma("sp", lambda e: e.dma_start(out=gple, in_=IN('g_ple').partition_broadcast(128)), B_gple, w=[B_gple])
        S.dma("sp", lambda e: e.dma_start(out=bpg, in_=IN('b_pg').partition_broadcast(128)), B_bpg, w=[B_bpg])
        S.dma("sp", lambda e: e.dma_start(out=gfin, in_=IN('g_final').partition_broadcast(128)), B_gfin, w=[B_gfin])
        h2 = [sb("E_h2%d" % i, [128, D], F32) for i in range(2)]
        y1 = [sb("E_y1%d" % i, [128, D], F32) for i in range(2)]
        y2 = [sb("E_y2%d" % i, [128, D], F32) for i in range(2)]
        pbf = [sb("E_p%d" % i, [128, PLE], BF16) for i in range(2)]
        B_h2 = S.bufs("E_h2", 2)
        B_y1 = S.bufs("E_y1", 2)
        B_y2 = S.bufs("E_y2", 2)
        B_pbf = S.bufs("E_p", 2)
        hn = sb("E_hn", [128, D], BF16)
        B_hn = S.buf("E_hn")
        hT = sb("E_hT", [128, 16, 128], BF16)
        B_hT = S.buf("E_hT")
        pT = sb("E_pT", [128, 2, 128], BF16)
        B_pT = S.buf("E_pT")
        gl = sb("E_gl", [128, 512], F32)
        B_gl = S.buf("E_gl")
        sgm = sb("E_sg", [128, 512], F32)
        B_sgm = S.buf("E_sg")
        h3 = sb("E_h3", [128, D], F32)
        B_h3 = S.buf("E_h3")
        ob = [sb("E_o%d" % i, [128, D], F32) for i in range(2)]
        B_ob = S.bufs("E_o", 2)
        st = [sb("E_st%d" % i, [128, 8], F32) for i in range(2)]
        B_st = S.bufs("E_st", 2)
        st2 = [sb("E_su%d" % i, [128, 8], F32) for i in range(2)]
        B_st2 = S.bufs("E_su", 2)

        def load(j):
            b = j % 2
            S.dma("sp", lambda e: e.dma_start(out=h2[b], in_=H1_d[j * 128:(j + 1) * 128, :]), B_h2[b], w=[B_h2[b]])
            S.dma("pool", lambda e: e.dma_start(out=pbf[b], in_=IN('p_own')[j * 128:(j + 1) * 128, :]), B_pbf[b], w=[B_pbf[b]])
            S.dma("pool", lambda e: e.indirect_dma_start(out=y1[b], out_offset=None, in_=Y_d,
                                                         in_offset=bass.IndirectOffsetOnAxis(ap=DESTI[:, j, 0:1], axis=0)),
                  B_y1[b], r=[B_desti], w=[B_y1[b]])
            S.dma("pool", lambda e: e.indirect_dma_start(out=y2[b], out_offset=None, in_=Y_d,
                                                         in_offset=bass.IndirectOffsetOnAxis(ap=DESTI[:, j, 1:2], axis=0)),
                  B_y2[b], r=[B_desti], w=[B_y2[b]])

        load(0)
        for j in range(NTo):
            b = j % 2
            if j + 1 < NTo:
                load(j + 1)
            S.op("dve", lambda e: e.tensor_tensor(out=h2[b], in0=h2[b], in1=y1[b], op=ALU.add), r=[B_h2[b], B_y1[b]], w=[B_h2[b]])
            S.op("dve", lambda e: e.tensor_tensor(out=h2[b], in0=h2[b], in1=y2[b], op=ALU.add), r=[B_h2[b], B_y2[b]], w=[B_h2[b]])
            rmsnorm_rstd(h2[b], B_h2[b], hn, B_hn, st[b], B_st[b], D)
            S.op("dve", lambda e: e.scalar_tensor_tensor(out=hn, in0=h2[b], scalar=st[b][:, 2:3], in1=gple, op0=ALU.mult, op1=ALU.mult),
                 r=[B_h2[b], B_st[b], B_gple], w=[B_hn])
            transpose_to(hn, B_hn, 16, lambda k0, k1: hT[:, k0:k1, :], B_hT, [0, 1])
            transpose_to(pbf[b], B_pbf[b], 2, lambda k0, k1: pT[:, k0:k1, :], B_pT, [0, 1])
            for n in range(4):
                bg = 2 + (n % 2) * 2
                bp_ = 3 + (n % 2) * 2
                for k in range(16):
                    S.op("pe", lambda e: e.matmul(pb[bg], lhsT=hT[:, k, :], rhs=Wpg[:, k, n * 512:(n + 1) * 512], start=(k == 0), stop=(k == 15)),
                         r=[B_hT, B_Wpg], w=[PB[bg]])
                for k in range(2):
                    S.op("pe", lambda e: e.matmul(pb[bp_], lhsT=pT[:, k, :], rhs=Wpl[:, k, n * 512:(n + 1) * 512], start=(k == 0), stop=(k == 1)),
                         r=[B_pT, B_Wpl], w=[PB[bp_]])
                cs = slice(n * 512, (n + 1) * 512)
                S.op("dve", lambda e: e.tensor_tensor(out=gl, in0=pb[bg], in1=bpg[:, cs], op=ALU.add), r=[PB[bg], B_bpg], w=[B_gl])
                S.op("act", lambda e: e.activation(out=sgm, in_=gl, func=AF.Sigmoid), r=[B_gl], w=[B_sgm])
                S.op("dve", lambda e: e.tensor_tensor(out=sgm, in0=pb[bp_], in1=sgm, op=ALU.mult), r=[PB[bp_], B_sgm], w=[B_sgm])
                S.op("dve", lambda e: e.tensor_tensor(out=h3[:, cs], in0=h2[b][:, cs], in1=sgm, op=ALU.add), r=[B_h2[b], B_sgm], w=[B_h3])
            rmsnorm_rstd(h3, B_h3, ob[b], B_ob[b], st2[b], B_st2[b], D)
            S.op("dve", lambda e: e.scalar_tensor_tensor(out=ob[b], in0=h3, scalar=st2[b][:, 2:3], in1=gfin, op0=ALU.mult, op1=ALU.mult),
                 r=[B_h3, B_st2[b], B_gfin], w=[B_ob[b]])
            S.dma("sp", lambda e: e.dma_start(out=out[j * 128:(j + 1) * 128, :], in_=ob[b]), B_ob[b], r=[B_ob[b]])
        S.end_phase()

    phases = [("A", phase_A), ("A2", phase_A2), ("B", phase_B), ("C", phase_C), ("D", phase_D), ("E", phase_E)]
    for name, fn in phases:
        fn()
        c.stack.close()
        c.stack = contextlib.ExitStack()
        if name == upto:
            break
    return nc, S, c


def make_in_maps(inputs, S_len, used=None):
    x = np.asarray(inputs["x"], np.float32)
    Bn = x.shape[0]
    NB = S_len // 256
    NBo = NB // 2
    p = np.asarray(inputs["p"], np.float32)[0]
    sq = lambda k: np.ascontiguousarray(np.asarray(inputs[k], np.float32)[0])
    row = lambda a: np.ascontiguousarray(a.reshape(1, -1))
    shared = dict(
        g_mix=row(sq("g_mix")), w_in=sq("w_in"), beta_attn=row(sq("beta_attn")), w_pool=sq("w_pool"),
        pool_scale=row(sq("pool_scale")), w_out=sq("w_out"), g_ffn=row(sq("g_ffn")),
        w_rg=sq("w_router_group"), b_rg=row(sq("b_router_group")), w_re=sq("w_router_expert"),
        b_re=row(sq("b_router_expert")), w_eg=sq("w_expert_gate"), w_eu=sq("w_expert_up"),
        w_ed=sq("w_expert_down"), g_ple=row(sq("g_ple")), w_ple=sq("w_ple"), w_pg=sq("w_ple_gate"),
        b_pg=row(sq("b_ple_gate")), g_final=row(np.asarray(inputs["g_final"], np.float32)),
    )
    tabs = [host_tables(S_len, r) for r in range(2)]
    in_maps = []
    orders = []
    for cidx in range(2 * Bn):
        b, r = cidx // 2, cidx % 2
        order = []
        for i in range(NBo):
            order += [2 * i + r, 2 * i + 1 - r]
        own = [2 * i + r for i in range(NBo)]
        xb = x[b].reshape(NB, 256, D)
        pb_ = p[b].reshape(NB, 256, PLE)
        m = dict(shared)
        m["x_perm"] = np.ascontiguousarray(xb[order].reshape(S_len, D))
        m["p_own"] = np.ascontiguousarray(pb_[own].reshape(-1, PLE))
        m.update(tabs[r])
        if used is not None:
            m = {k: v for k, v in m.items() if k in used}
        in_maps.append(m)
        orders.append(own)
    return in_maps, orders


def kernel(**inputs):
    x = np.asarray(inputs["x"])
    Bn, S_len, _ = x.shape
    nc, S, c = build(S_len, debug=False, upto="E")
    in_maps, orders = make_in_maps(inputs, S_len, used=set(c.used))
    ncores = 2 * Bn
    res = run_bass_kernel_spmd(nc, in_maps, core_ids=list(range(ncores)))
    outp = np.empty((Bn, S_len // 256, 256, D), np.float32)
    for cidx in range(ncores):
        b = cidx // 2
        o = np.asarray(res.results[cidx]["out"], np.float32).reshape(-1, 256, D)
        outp[b, orders[cidx]] = o
    return outp.reshape(Bn, S_len, D)
```

```python
import numpy as np
import concourse.bass as bass
import concourse.mybir as mybir
from concourse.bass_utils import run_bass_kernel_spmd

F32 = mybir.dt.float32
BF16 = mybir.dt.bfloat16
I32 = mybir.dt.int32
AF = mybir.ActivationFunctionType
ALU = mybir.AluOpType
AX = mybir.AxisListType

D = 2048
H = 8
HD = 128
AW = 1024
PW = 1024
INW = 4096
NE = 64
NG = 8
DE = 512
PLE = 256
EPS = 1e-6
import os
SKIP = os.environ.get('KSKIP', '')
NEG = -1.0e30


class Buf:
    __slots__ = ("name", "wev", "revs", "slot", "excl")

    def __init__(self, name, excl=False):
        self.name = name
        self.excl = excl
        self.wev = None
        self.revs = {}
        self.slot = None


class Sched:
    def __init__(self, nc):
        self.nc = nc
        self.eng = {"pe": nc.tensor, "act": nc.scalar, "dve": nc.vector, "pool": nc.gpsimd, "sp": nc.sync}
        self.esem = {k: nc.alloc_semaphore("es_" + k) for k in self.eng}
        self.ecnt = {k: 0 for k in self.eng}
        self.seen = {k: {} for k in self.eng}
        self.free_slots = []
        self.nslots = 0
        self.phase_bufs = []
        self.ninst = 0
        self.nwait = 0

    def buf(self, name, excl=False):
        b = Buf(name, excl)
        self.phase_bufs.append(b)
        return b

    def bufs(self, name, n, excl=False):
        return [self.buf("%s%d" % (name, i), excl) for i in range(n)]

    def _slot(self, b):
        if b.slot is None:
            if self.free_slots:
                b.slot = self.free_slots.pop()
            else:
                b.slot = [self.nc.alloc_semaphore("ds%d" % self.nslots), 0]
                self.nslots += 1
        return b.slot

    def _waits(self, e, r, w):
        need = {}

        def add(ev):
            s, v, src = ev
            if src == e and e == "pe":
                return
            k = id(s)
            if k not in need or need[k][1] < v:
                need[k] = (s, v)

        for b in r:
            if b.wev is not None:
                add(b.wev)
            if b.excl:
                for ev in b.revs.values():
                    if ev[2] != e:
                        add(ev)
        for b in w:
            if b.wev is not None:
                add(b.wev)
            for ev in b.revs.values():
                if ev[2] == e:
                    continue
                add(ev)
        seen = self.seen[e]
        for k, (s, v) in need.items():
            if seen.get(k, 0) >= v:
                continue
            self.eng[e].wait_ge(s, v)
            self.nwait += 1
            seen[k] = v

    def _record(self, ev, r, w):
        k = id(ev[0])
        for b in r:
            b.revs[k] = ev
        for b in w:
            b.wev = ev
            b.revs = {}

    def op(self, e, fn, r=(), w=()):
        self._waits(e, r, w)
        ins = fn(self.eng[e])
        self.ecnt[e] += 1
        ins.then_inc(self.esem[e], 1)
        self.ninst += 1
        self._record((self.esem[e], self.ecnt[e], e), r, w)
        return ins

    def dma(self, e, fn, sb, r=(), w=()):
        self._waits(e, r, w)
        slot = self._slot(sb)
        ins = fn(self.eng[e])
        slot[1] += 16
        ins.then_inc(slot[0], 16)
        self.ninst += 1
        self._record((slot[0], slot[1], "dma"), r, w)
        return ins

    def end_phase(self):
        for b in self.phase_bufs:
            if b.slot is not None:
                s, v = b.slot
                if self.seen["sp"].get(id(s), 0) < v:
                    self.eng["sp"].wait_ge(s, v)
                    self.seen["sp"][id(s)] = v
        self.ecnt["sp"] += 1
        self.eng["sp"].nop().then_inc(self.esem["sp"], 1)
        for e in self.eng:
            for f in self.eng:
                if f == e:
                    continue
                s, v = self.esem[f], self.ecnt[f]
                if v > 0 and self.seen[e].get(id(s), 0) < v:
                    self.eng[e].wait_ge(s, v)
                    self.seen[e][id(s)] = v
        for b in self.phase_bufs:
            if b.slot is not None:
                self.free_slots.append(b.slot)
                b.slot = None
        self.phase_bufs = []


class Ctx:
    pass


def alibi_slopes():
    return np.array([2.0 ** (-8.0 * (h + 1) / H) for h in range(H)], np.float64)


def host_tables(S_len, r):
    NB = S_len // 256
    NBo = NB // 2
    NBP = max(NB, 8)
    sl = alibi_slopes()
    seqblk = np.zeros(NB, np.int64)
    for i in range(NBo):
        seqblk[2 * i] = 2 * i + r
        seqblk[2 * i + 1] = 2 * i + 1 - r
    j = np.arange(256)
    vtab = np.exp(-sl[None, None, :] * (255 - (np.arange(2)[None, :, None] * 128 + np.arange(128)[:, None, None])))
    mtab = np.zeros((NBo, 2, 128, H, NBP), np.float64)
    gbias = np.full((NBo, 128, NBP), NEG, np.float64)
    for i in range(NBo):
        own = seqblk[2 * i]
        for s in range(NB):
            if seqblk[s] < own:
                gbias[i, :, s] = 0.0
                for t in range(2):
                    qpos = own * 256 + t * 128 + np.arange(128)
                    dist = qpos - (seqblk[s] * 256 + 255)
                    mtab[i, t, :, :, s] = np.exp(-sl[None, :] * dist[:, None])
    kj = np.arange(128)[:, None]
    qi = np.arange(128)[None, :]
    ctab = np.zeros((128, H, 2, 128), np.float64)
    for h in range(H):
        ctab[:, h, 0, :] = np.where(qi >= kj, np.exp(-sl[h] * np.maximum(qi - kj, 0)), 0.0)
        ctab[:, h, 1, :] = np.exp(-sl[h] * (128 + qi - kj))
    wins = np.array([2, 4, 8, 16])
    t = seqblk[0] * 256 + np.arange(256)
    cnt = np.minimum(t[None, :] + 1, wins[:, None]).astype(np.float64)
    ptab = np.broadcast_to((1.0 / cnt)[None], (128, 4, 256))
    halo = np.zeros((128, 2), np.float64)
    halo[:, 0] = 1.0 if r == 0 else 0.0
    halo[:, 1] = 0.0 if r == 0 else 1.0
    f = lambda a: np.ascontiguousarray(a, dtype=np.float32)
    lt = (np.arange(128)[:, None] < np.arange(128)[None, :]).astype(np.float32)
    return dict(ident=np.eye(128, dtype=np.float32), vtab=f(vtab), mtab=f(mtab), gbias=f(gbias),
                ctab=f(ctab), ptab=f(ptab), halo=f(halo), ltri=lt,
                bpos=f((np.arange(128) * 128.0).reshape(128, 1)),
                pidx=f(np.arange(128).reshape(128, 1)))


def build(S_len, debug=False, upto="E"):
    nc = bass.Bass("TRN2", target_bir_lowering=False)
    NB = S_len // 256
    NBo = NB // 2
    NBP = max(NB, 8)
    To = S_len // 2
    NTo = To // 128
    NT = S_len // 128
    PL = 2 * To + NE * 128
    NBLK = PL // 128
    skind = "ExternalOutput" if debug else "Internal"

    def din(name, shape, dt=F32):
        return nc.dram_tensor(name, list(shape), dt, kind="ExternalInput").ap()

    def dscr(name, shape, dt):
        return nc.dram_tensor(name, list(shape), dt, kind=skind).ap()

    c = Ctx()
    c.nc = nc
    in_shapes = dict(
        x_perm=[S_len, D], p_own=[To, PLE], g_mix=[1, D], w_in=[D, INW], beta_attn=[1, AW],
        w_pool=[4, 256, 256], pool_scale=[1, PW], w_out=[D, D], g_ffn=[1, D], w_rg=[D, NG], b_rg=[1, NG],
        w_re=[NG, D, 8], b_re=[1, NE], w_eg=[NE, D, DE], w_eu=[NE, D, DE], w_ed=[NE, DE, D], g_ple=[1, D],
        w_ple=[PLE, D], w_pg=[D, D], b_pg=[1, D], g_final=[1, D], ident=[128, 128], vtab=[128, 2, H],
        mtab=[NBo, 2, 128, H, NBP], gbias=[NBo, 128, NBP], ctab=[128, H, 2, 128], ptab=[128, 4, 256],
        halo=[128, 2], ltri=[128, 128], bpos=[128, 1], pidx=[128, 1])
    c.used = {}

    def IN(name):
        if name not in c.used:
            c.used[name] = din(name, in_shapes[name])
        return c.used[name]
    out = nc.dram_tensor("out", [To, D], F32, kind="ExternalOutput").ap()
    KT_d = dscr("KT_d", [H, 128, S_len], BF16)
    VP_d = dscr("VP_d", [H, 128, NT, 129], BF16)
    V1_d = dscr("V1_d", [H, 128, NTo, 129], BF16)
    QT_d = dscr("QT_d", [H, 128, To], BF16)
    UT_d = dscr("UT_d", [8, 128, NBo, 272], F32)
    KM_d = dscr("KM_d", [128, H, NBP], F32)
    OA_d = dscr("OA_d", [To, AW], F32)
    MP_d = dscr("MP_d", [To, PW], BF16)
    H1_d = dscr("H1_d", [To, D], F32)
    F_d = dscr("F_d", [To, D], BF16)
    SLOT_d = dscr("SLOT_d", [PL, 2], F32)
    Y_d = dscr("Y_d", [PL, D], F32)
    RT_d = dscr("RT_d", [To, 8], F32)

    S = Sched(nc)
    c.S = S
    import contextlib
    c.stack = contextlib.ExitStack()

    def sb(name, shape, dt):
        t = c.stack.enter_context(nc.sbuf_tensor(name, list(shape), dt))
        return t.ap() if hasattr(t, "ap") else t[:]
    pb = [nc.alloc_psum_tensor("pb%d" % i, [128, 512], F32).ap() for i in range(8)]
    PB = S.bufs("pbank", 8, excl=True)

    ident = nc.alloc_sbuf_tensor("identb", [128, 128], BF16).ap()
    B_ident = Buf("ident")
    S.dma("pool", lambda e: e.dma_start(out=ident, in_=IN('ident')), B_ident, w=[B_ident])

    def rmsnorm_rstd(e_xt, B_xt, junk, B_junk, st, B_st, width):
        S.op("act", lambda e: e.activation(out=junk, in_=e_xt, func=AF.Square, accum_out=st[:, 0:1]),
             r=[B_xt], w=[B_junk, B_st])
        S.op("act", lambda e: e.activation(out=st[:, 1:2], in_=st[:, 0:1], func=AF.Sqrt, scale=1.0 / width, bias=EPS),
             r=[B_st], w=[B_st])
        S.op("dve", lambda e: e.reciprocal(out=st[:, 2:3], in_=st[:, 1:2]), r=[B_st], w=[B_st])

    def transpose_to(src, B_src, nchunk, dst_fn, B_dst, tps, step=1):
        for g0 in range(0, nchunk, 8):
            n = min(8, nchunk - g0)
            bi = tps[(g0 // 8) % len(tps)]
            tpv = pb[bi].bitcast(BF16).rearrange("p (k n) -> p k n", n=128)
            for k in range(g0, g0 + n):
                if step == 1:
                    sv = src[:, k * 128:(k + 1) * 128]
                else:
                    sv = src[:, k:k + 127 * step + 1:step]
                S.op("pe", lambda e: e.transpose(tpv[:, k - g0, :], sv, ident), r=[B_src, B_ident], w=[PB[bi]])
            eng = "act" if (g0 // 8) % 2 == 0 else "dve"
            if eng == "act":
                S.op("act", lambda e: e.copy(out=dst_fn(g0, g0 + n), in_=tpv[:, 0:n, :]), r=[PB[bi]], w=[B_dst])
            else:
                S.op("dve", lambda e: e.tensor_copy(out=dst_fn(g0, g0 + n), in_=tpv[:, 0:n, :]), r=[PB[bi]], w=[B_dst])

    def phase_A():
        Wb = sb("A_Wb", [128, 16, INW], BF16)
        B_W = S.buf("A_Wb")
        for k in range(16):
            S.dma("pool", lambda e: e.dma_start(out=Wb[:, k, :], in_=IN('w_in')[k * 128:(k + 1) * 128, :]), B_W, w=[B_W])
        gmix = sb("A_gmix", [128, D], F32)
        B_g = S.buf("A_gmix")
        S.dma("sp", lambda e: e.dma_start(out=gmix, in_=IN('g_mix').partition_broadcast(128)), B_g, w=[B_g])
        vtab = sb("A_vtab", [128, 2, H], F32)
        B_vt = S.buf("A_vtab")
        S.dma("sp", lambda e: e.dma_start(out=vtab, in_=IN('vtab')), B_vt, w=[B_vt])
        xb = [sb("A_x%d" % i, [128, D], F32) for i in range(2)]
        B_x = S.bufs("A_x", 2)
        ab = [sb("A_a%d" % i, [128, D], BF16) for i in range(4)]
        B_a = S.bufs("A_a", 4)
        stt = [sb("A_st%d" % i, [128, 4], F32) for i in range(2)]
        B_st = S.bufs("A_st", 2)
        aT = sb("A_aT", [128, 16, 512], BF16)
        B_aT = S.buf("A_aT")
        NSTG = 3
        fst = [sb("A_fs%d" % i, [128, 512], BF16) for i in range(NSTG)]
        B_fs = S.bufs("A_fs", NSTG)
        ust = [sb("A_us%d" % i, [128, 272], F32) for i in range(2)]
        B_us = S.bufs("A_us", 2)
        vp = [sb("A_vp%d" % i, [128, H, 129], BF16) for i in range(2)]
        B_vp = S.bufs("A_vp", 2)
        v1 = [sb("A_v1%d" % i, [128, H, 129], BF16) for i in range(2)]
        B_v1 = S.bufs("A_v1", 2)
        km = sb("A_km", [128, H, NBP], F32)
        B_km = S.buf("A_km")
        kms = sb("A_kms", [128, 2], F32)
        B_kms = S.buf("A_kms")
        S.op("dve", lambda e: e.memset(km, 0.0), w=[B_km])
        for i in range(2):
            S.op("dve", lambda e: e.memset(v1[i][:, :, 128:129], 1.0), w=[B_v1[i]])
        mmb = [2, 3, 4, 5, 6, 7]
        mmi = [0]

        def nextbank():
            b = mmb[mmi[0] % len(mmb)]
            mmi[0] += 1
            return b

        fsi = [0]
        tcount = [0]

        def norm_tile(i, t):
            j = tcount[0] % 2
            tcount[0] += 1
            row0 = (i * 4 + t) * 128
            S.dma("sp", lambda e: e.dma_start(out=xb[j], in_=IN('x_perm')[row0:row0 + 128, :]), B_x[j], w=[B_x[j]])
            rmsnorm_rstd(xb[j], B_x[j], ab[t], B_a[t], stt[j], B_st[j], D)
            S.op("dve", lambda e: e.scalar_tensor_tensor(out=ab[t], in0=xb[j], scalar=stt[j][:, 2:3], in1=gmix,
                                                         op0=ALU.mult, op1=ALU.mult),
                 r=[B_x[j], B_st[j], B_g], w=[B_a[t]])

        def transposes(i):
            for t in range(4):
                transpose_to(ab[t], B_a[t], 16, lambda k0, k1: aT[:, k0:k1, t * 128:(t + 1) * 128], B_aT, [0, 1])

        for t in range(4):
            norm_tile(0, t)
        transposes(0)
        for i in range(NBo):
            for ch in range(H if 'k' not in SKIP else 0):
                bk = nextbank()
                for k in range(16):
                    S.op("pe", lambda e: e.matmul(pb[bk], lhsT=Wb[:, k, AW + ch * 128:AW + (ch + 1) * 128], rhs=aT[:, k, :],
                                                  start=(k == 0), stop=(k == 15)), r=[B_W, B_aT], w=[PB[bk]])
                f = fsi[0] % NSTG
                fsi[0] += 1
                S.op("act", lambda e: e.copy(out=fst[f], in_=pb[bk]), r=[PB[bk]], w=[B_fs[f]])
                S.op("dve", lambda e: e.reduce_sum(out=kms, in_=pb[bk].rearrange("p (b n) -> p b n", n=256), axis=AX.X),
                     r=[PB[bk], B_fs[f]], w=[B_kms])
                S.op("dve", lambda e: e.tensor_scalar(out=km[:, ch, 2 * i:2 * i + 2], in0=kms, scalar1=1.0 / 256, scalar2=None,
                                                      op0=ALU.mult), r=[B_kms], w=[B_km])
                S.dma("sp", lambda e: e.dma_start(out=KT_d[ch, :, i * 512:(i + 1) * 512], in_=fst[f]), B_fs[f], r=[B_fs[f]])
            if i + 1 < NBo:
                norm_tile(i + 1, 0)
            for ch in range(H if 'q' not in SKIP else 0):
                bk = nextbank()
                for k in range(16):
                    S.op("pe", lambda e: e.matmul(pb[bk][:, 0:256], lhsT=Wb[:, k, ch * 128:(ch + 1) * 128], rhs=aT[:, k, 0:256],
                                                  start=(k == 0), stop=(k == 15)), r=[B_W, B_aT], w=[PB[bk]])
                f = fsi[0] % NSTG
                fsi[0] += 1
                S.op("act", lambda e: e.copy(out=fst[f][:, 0:256], in_=pb[bk][:, 0:256]), r=[PB[bk]], w=[B_fs[f]])
                S.dma("sp", lambda e: e.dma_start(out=QT_d[ch, :, i * 256:(i + 1) * 256], in_=fst[f][:, 0:256]), B_fs[f], r=[B_fs[f]])
            if i + 1 < NBo:
                norm_tile(i + 1, 1)
            for ch in range(8 if 'u' not in SKIP else 0):
                bk = nextbank()
                for k in range(16):
                    S.op("pe", lambda e: e.matmul(pb[bk], lhsT=Wb[:, k, 3 * AW + ch * 128:3 * AW + (ch + 1) * 128], rhs=aT[:, k, :],
                                                  start=(k == 0), stop=(k == 15)), r=[B_W, B_aT], w=[PB[bk]])
                f = ch % 2
                S.op("act", lambda e: e.copy(out=ust[f][:, 0:256], in_=pb[bk][:, 0:256]), r=[PB[bk]], w=[B_us[f]])
                S.op("dve", lambda e: e.tensor_copy(out=ust[f][:, 256:272], in_=pb[bk][:, 496:512]), r=[PB[bk]], w=[B_us[f]])
                S.dma("sp", lambda e: e.dma_start(out=UT_d[ch, :, i, :], in_=ust[f]), B_us[f], r=[B_us[f]])
            if i + 1 < NBo:
                norm_tile(i + 1, 2)
            for t in range(4 if 'v' not in SKIP else 0):
                par = t % 2
                tile_g = i * 4 + t
                jj = tile_g % 2
                for half in range(2):
                    bk = nextbank()
                    for k in range(16):
                        S.op("pe", lambda e: e.matmul(pb[bk], lhsT=aT[:, k, t * 128:(t + 1) * 128],
                                                      rhs=Wb[:, k, 2 * AW + half * 512:2 * AW + (half + 1) * 512],
                                                      start=(k == 0), stop=(k == 15)), r=[B_W, B_aT], w=[PB[bk]])
                    pv = pb[bk].rearrange("p (h n) -> p h n", n=128)
                    hs = slice(half * 4, (half + 1) * 4)
                    S.op("dve", lambda e: e.tensor_tensor(out=vp[jj][:, hs, 0:128], in0=pv,
                                                          in1=vtab[:, par, hs].unsqueeze(2).to_broadcast([128, 4, 128]),
                                                          op=ALU.mult), r=[PB[bk], B_vt], w=[B_vp[jj]])
                    if t < 2:
                        S.op("act", lambda e: e.copy(out=v1[jj][:, hs, 0:128], in_=pv), r=[PB[bk], B_vp[jj]], w=[B_v1[jj]])
                S.op("dve", lambda e: e.tensor_copy(out=vp[jj][:, :, 128:129], in_=vtab[:, par, :].unsqueeze(2)),
                     r=[B_vt], w=[B_vp[jj]])
                S.dma("sp", lambda e: e.dma_start(out=VP_d[:, :, tile_g, :].rearrange("h p c -> p h c"), in_=vp[jj]),
                      B_vp[jj], r=[B_vp[jj]])
                if t < 2:
                    S.dma("sp", lambda e: e.dma_start(out=V1_d[:, :, i * 2 + t, :].rearrange("h p c -> p h c"), in_=v1[jj]),
                          B_v1[jj], r=[B_v1[jj]])
            if i + 1 < NBo:
                norm_tile(i + 1, 3)
                transposes(i + 1)
        S.dma("sp", lambda e: e.dma_start(out=KM_d, in_=km), B_km, r=[B_km])
        S.end_phase()


    def phase_A2():
        wpb = sb("P_wp", [128, 4, 2, 256], BF16)
        B_wp = S.buf("P_wp")
        S.dma("pool", lambda e: e.dma_start(out=wpb, in_=IN('w_pool').rearrange("g (cc p) d -> p g cc d", p=128)), B_wp, w=[B_wp])
        psc = sb("P_psc", [128, PW], F32)
        B_psc = S.buf("P_psc")
        S.dma("sp", lambda e: e.dma_start(out=psc, in_=IN('pool_scale').partition_broadcast(128)), B_psc, w=[B_psc])
        ptab = sb("P_ptab", [128, 4, 256], F32)
        B_pt = S.buf("P_ptab")
        S.dma("sp", lambda e: e.dma_start(out=ptab, in_=IN('ptab')), B_pt, w=[B_pt])
        halo = sb("P_halo", [128, 2], F32)
        B_ha = S.buf("P_halo")
        S.dma("sp", lambda e: e.dma_start(out=halo, in_=IN('halo')), B_ha, w=[B_ha])
        ub = [sb("P_ub%d" % i, [128, 8, 272], F32) for i in range(2)]
        B_ub = S.bufs("P_ub", 2)
        tl = [sb("P_tl%d" % i, [128, 8, 32], F32) for i in range(2)]
        B_tl = S.bufs("P_tl", 2)
        Pb = sb("P_P", [128, 8, 272], F32)
        B_P = S.buf("P_P")
        Qb = sb("P_Q", [128, 8, 272], F32)
        B_Q = S.buf("P_Q")
        zb = sb("P_z", [128, 8, 256], BF16)
        B_z = S.buf("P_z")
        zt = sb("P_zt", [128, 2, 256], F32)
        B_zt = S.buf("P_zt")
        mp = [sb("P_mp%d" % i, [128, PW], BF16) for i in range(2)]
        B_mp = S.bufs("P_mp", 2)
        tmpf = sb("P_tmpf", [128, PW], F32)
        B_tmpf = S.buf("P_tmpf")
        st = [sb("P_st%d" % i, [128, 8], F32) for i in range(2)]
        B_st = S.bufs("P_st", 2)
        wins = [2, 4, 8, 16]
        UTv = UT_d.rearrange("c p i n -> p c i n")
        for i in range(NBo):
            j = i % 2
            A = ub[j]
            S.dma("sp", lambda e: e.dma_start(out=A[:, :, 16:272], in_=UTv[:, :, i, 0:256]), B_ub[j], w=[B_ub[j]])
            S.dma("sp", lambda e: e.dma_start(out=tl[j][:, :, 16:32], in_=UTv[:, :, i, 256:272]), B_tl[j], w=[B_tl[j]])
            if i > 0:
                S.dma("sp", lambda e: e.dma_start(out=tl[j][:, :, 0:16], in_=UTv[:, :, i - 1, 256:272]), B_tl[j], w=[B_tl[j]])
            else:
                S.op("dve", lambda e: e.memset(tl[j][:, :, 0:16], 0.0), w=[B_tl[j]])
            S.op("dve", lambda e: e.tensor_scalar(out=A[:, :, 0:16], in0=tl[j][:, :, 0:16], scalar1=halo[:, 0:1], scalar2=None, op0=ALU.mult),
                 r=[B_tl[j], B_ha], w=[B_ub[j]])
            S.op("dve", lambda e: e.scalar_tensor_tensor(out=A[:, :, 0:16], in0=tl[j][:, :, 16:32], scalar=halo[:, 1:2], in1=A[:, :, 0:16],
                                                         op0=ALU.mult, op1=ALU.add), r=[B_tl[j], B_ha, B_ub[j]], w=[B_ub[j]])
            S.op("dve", lambda e: e.tensor_tensor(out=Pb[:, :, 1:272], in0=A[:, :, 1:272], in1=A[:, :, 0:271], op=ALU.add),
                 r=[B_ub[j]], w=[B_P])
            S.op("dve", lambda e: e.tensor_tensor(out=Qb[:, 2:8, 3:272], in0=Pb[:, 2:8, 3:272], in1=Pb[:, 2:8, 1:270], op=ALU.add),
                 r=[B_P], w=[B_Q])
            S.op("dve", lambda e: e.tensor_tensor(out=Pb[:, 4:8, 7:272], in0=Qb[:, 4:8, 7:272], in1=Qb[:, 4:8, 3:268], op=ALU.add),
                 r=[B_Q], w=[B_P])
            S.op("dve", lambda e: e.tensor_tensor(out=Qb[:, 6:8, 15:272], in0=Pb[:, 6:8, 15:272], in1=Pb[:, 6:8, 7:264], op=ALU.add),
                 r=[B_P], w=[B_Q])
            for g in range(4):
                W = Pb if g % 2 == 0 else Qb
                B_Wb = B_P if g % 2 == 0 else B_Q
                cs = slice(2 * g, 2 * g + 2)
                if i == 0:
                    S.op("dve", lambda e: e.tensor_tensor(out=zt, in0=W[:, cs, 16:272],
                                                          in1=ptab[:, g, :].unsqueeze(1).to_broadcast([128, 2, 256]), op=ALU.mult),
                         r=[B_Wb, B_pt], w=[B_zt])
                    S.op("dve", lambda e: e.tensor_tensor(out=zb[:, cs, :], in0=zt, in1=A[:, cs, 16:272], op=ALU.subtract),
                         r=[B_zt, B_ub[j]], w=[B_z])
                else:
                    S.op("dve", lambda e: e.scalar_tensor_tensor(out=zb[:, cs, :], in0=W[:, cs, 16:272], scalar=1.0 / wins[g],
                                                                 in1=A[:, cs, 16:272], op0=ALU.mult, op1=ALU.subtract),
                         r=[B_Wb, B_ub[j]], w=[B_z])
            for t in range(2):
                jt = (i * 2 + t) % 2
                banks = [2 + 2 * jt, 3 + 2 * jt]
                for g in range(4):
                    bk = banks[g // 2]
                    for cc in range(2):
                        S.op("pe", lambda e: e.matmul(pb[bk][:, (g % 2) * 256:(g % 2 + 1) * 256], lhsT=zb[:, 2 * g + cc, t * 128:(t + 1) * 128],
                                                      rhs=wpb[:, g, cc, :], start=(cc == 0), stop=(cc == 1)),
                             r=[B_z, B_wp], w=[PB[bk]])
                S.op("act", lambda e: e.activation(out=tmpf[:, 0:512], in_=pb[banks[0]], func=AF.Square, accum_out=st[jt][:, 0:1]),
                     r=[PB[banks[0]]], w=[B_tmpf, B_st[jt]])
                S.op("act", lambda e: e.activation(out=tmpf[:, 512:1024], in_=pb[banks[1]], func=AF.Square, accum_out=st[jt][:, 3:4]),
                     r=[PB[banks[1]]], w=[B_tmpf, B_st[jt]])
                S.op("dve", lambda e: e.tensor_tensor(out=st[jt][:, 0:1], in0=st[jt][:, 0:1], in1=st[jt][:, 3:4], op=ALU.add),
                     r=[B_st[jt]], w=[B_st[jt]])
                S.op("act", lambda e: e.activation(out=st[jt][:, 1:2], in_=st[jt][:, 0:1], func=AF.Sqrt, scale=1.0 / PW, bias=EPS),
                     r=[B_st[jt]], w=[B_st[jt]])
                S.op("dve", lambda e: e.reciprocal(out=st[jt][:, 2:3], in_=st[jt][:, 1:2]), r=[B_st[jt]], w=[B_st[jt]])
                for hh in range(2):
                    S.op("dve", lambda e: e.scalar_tensor_tensor(out=mp[jt][:, hh * 512:(hh + 1) * 512], in0=pb[banks[hh]], scalar=st[jt][:, 2:3],
                                                                 in1=psc[:, hh * 512:(hh + 1) * 512], op0=ALU.mult, op1=ALU.mult),
                         r=[PB[banks[hh]], B_st[jt], B_psc], w=[B_mp[jt]])
                row0 = (i * 2 + t) * 128
                S.dma("sp", lambda e: e.dma_start(out=MP_d[row0:row0 + 128, :], in_=mp[jt]), B_mp[jt], r=[B_mp[jt]])
        S.end_phase()

    def phase_B():
        scale = HD ** -0.5
        kmf = sb("B_kmf", [128, H, NBP], F32)
        B_kmf = S.buf("B_kmf")
        S.dma("sp", lambda e: e.dma_start(out=kmf, in_=KM_d), B_kmf, w=[B_kmf])
        kmb = sb("B_kmb", [128, H, NBP], BF16)
        B_kmb = S.buf("B_kmb")
        S.op("dve", lambda e: e.tensor_copy(out=kmb, in_=kmf), r=[B_kmf], w=[B_kmb])
        gbias = sb("B_gbias", [128, NBo, NBP], F32)
        B_gb = S.buf("B_gbias")
        S.dma("sp", lambda e: e.dma_start(out=gbias, in_=IN('gbias').rearrange("i p s -> p i s")), B_gb, w=[B_gb])
        mtab = sb("B_mtab", [128, NBo, 2, H, NBP], F32)
        B_mt = S.buf("B_mtab")
        for i in range(NBo):
            S.dma("sp", lambda e: e.dma_start(out=mtab[:, i], in_=IN('mtab')[i].rearrange("t p h s -> p t h s")), B_mt, w=[B_mt])
        ctab = sb("B_ctab", [128, H, 2, 128], F32)
        B_ct = S.buf("B_ctab")
        S.dma("sp", lambda e: e.dma_start(out=ctab, in_=IN('ctab')), B_ct, w=[B_ct])
        KT = [sb("B_KT%d" % i, [128, S_len], BF16) for i in range(2)]
        VP = [sb("B_VP%d" % i, [128, NT, 129], BF16) for i in range(2)]
        QT = [sb("B_QT%d" % i, [128, To], BF16) for i in range(2)]
        V1 = [sb("B_V1%d" % i, [128, NTo, 129], BF16) for i in range(2)]
        B_KT = S.bufs("B_KT", 2)
        B_VP = S.bufs("B_VP", 2)
        B_QT = S.bufs("B_QT", 2)
        B_V1 = S.bufs("B_V1", 2)
        gs = sb("B_gs", [128, 2, NBP], F32)
        B_gs = S.buf("B_gs")
        top8 = sb("B_top8", [128, 2, 8], F32)
        B_t8 = S.buf("B_top8")
        sel = sb("B_sel", [128, 2, NBP], F32)
        B_sel = S.buf("B_sel")
        mm = [sb("B_m%d" % i, [128, 2, NBP], F32) for i in range(2)]
        B_m = S.bufs("B_m", 2)
        pT = [sb("B_pT%d" % i, [128, 512], BF16) for i in range(2)]
        B_pT = S.bufs("B_pT", 2)
        acc = [sb("B_acc%d" % i, [128, 2, 129], F32) for i in range(2)]
        B_acc = S.bufs("B_acc", 2)
        rec = sb("B_rec", [128, 2], F32)
        B_rec = S.buf("B_rec")
        oa = [sb("B_oa%d" % i, [128, 2, 128], F32) for i in range(2)]
        B_oa = S.bufs("B_oa", 2)
        itc = [0]

        def load_head(h):
            j = h % 2
            S.dma("sp", lambda e: e.dma_start(out=KT[j], in_=KT_d[h]), B_KT[j], w=[B_KT[j]])
            S.dma("sp", lambda e: e.dma_start(out=QT[j], in_=QT_d[h]), B_QT[j], w=[B_QT[j]])
            S.dma("sp", lambda e: e.dma_start(out=VP[j], in_=VP_d[h]), B_VP[j], w=[B_VP[j]])
            S.dma("sp", lambda e: e.dma_start(out=V1[j], in_=V1_d[h]), B_V1[j], w=[B_V1[j]])

        items = []
        for h in range(H):
            for i in range(NBo):
                a = (h * NBo + i) % 2
                cands = list(range(2 * i)) + [2 * i + 1]
                items.append(dict(h=h, i=i, a=a, own=True, s=2 * i, last=False))
                for ci, s_ in enumerate(cands):
                    items.append(dict(h=h, i=i, a=a, own=False, s=s_, last=(ci == len(cands) - 1)))

        def emit_S(k):
            it = items[k]
            h, i, j = it["h"], it["i"], it["h"] % 2
            sbk = 1 + k % 2
            q0 = i * 256
            if it["own"]:
                for t in range(2):
                    S.op("pe", lambda e: e.matmul(pb[0][:, t * 256:t * 256 + NBP], lhsT=QT[j][:, q0 + t * 128:q0 + (t + 1) * 128],
                                                  rhs=kmb[:, h, :], start=True, stop=True), r=[B_QT[j], B_kmb], w=[PB[0]])
                S.op("pe", lambda e: e.matmul(pb[sbk][:, 0:256], lhsT=KT[j][:, (2 * i) * 256:(2 * i) * 256 + 128], rhs=QT[j][:, q0:q0 + 256],
                                              start=True, stop=True), r=[B_KT[j], B_QT[j]], w=[PB[sbk]])
                S.op("pe", lambda e: e.matmul(pb[sbk][:, 384:512], lhsT=KT[j][:, (2 * i) * 256 + 128:(2 * i) * 256 + 256],
                                              rhs=QT[j][:, q0 + 128:q0 + 256], start=True, stop=True), r=[B_KT[j], B_QT[j]], w=[PB[sbk]])
            else:
                s_ = it["s"]
                for kh in range(2):
                    S.op("pe", lambda e: e.matmul(pb[sbk][:, kh * 256:(kh + 1) * 256], lhsT=KT[j][:, s_ * 256 + kh * 128:s_ * 256 + (kh + 1) * 128],
                                                  rhs=QT[j][:, q0:q0 + 256], start=True, stop=True), r=[B_KT[j], B_QT[j]], w=[PB[sbk]])

        def emit_exp(k):
            it = items[k]
            h = it["h"]
            sbk = 1 + k % 2
            pt = pT[k % 2]
            B_pt_ = B_pT[k % 2]
            if it["own"]:
                S.op("act", lambda e: e.activation(out=pt[:, 0:256], in_=pb[sbk][:, 0:256], func=AF.Exp, scale=scale), r=[PB[sbk]], w=[B_pt_])
                S.op("act", lambda e: e.activation(out=pt[:, 384:512], in_=pb[sbk][:, 384:512], func=AF.Exp, scale=scale), r=[PB[sbk]], w=[B_pt_])
                S.op("dve", lambda e: e.tensor_tensor(out=pt[:, 0:256], in0=pt[:, 0:256], in1=ctab[:, h, :, :].rearrange("p a b -> p (a b)"), op=ALU.mult),
                     r=[B_pt_, B_ct], w=[B_pt_])
                S.op("dve", lambda e: e.tensor_tensor(out=pt[:, 384:512], in0=pt[:, 384:512], in1=ctab[:, h, 0, :], op=ALU.mult),
                     r=[B_pt_, B_ct], w=[B_pt_])
            else:
                S.op("act", lambda e: e.activation(out=pt, in_=pb[sbk], func=AF.Exp, scale=scale), r=[PB[sbk]], w=[B_pt_])

        def emit_rest(k):
            it = items[k]
            h, i, a, j = it["h"], it["i"], it["a"], it["h"] % 2
            obk = 3 + k % 2
            pt = pT[k % 2]
            B_pt_ = B_pT[k % 2]
            q0 = i * 256
            if it["own"]:
                if i == 0 and h + 1 < H:
                    load_head(h + 1)
                gv = pb[0].rearrange("p (t n) -> p t n", n=256)[:, :, 0:NBP]
                S.op("dve", lambda e: e.tensor_tensor(out=gs, in0=gv, in1=gbias[:, i, :].unsqueeze(1).to_broadcast([128, 2, NBP]), op=ALU.add),
                     r=[PB[0], B_gb], w=[B_gs])
                for t in range(2):
                    S.op("dve", lambda e: e.max(out=top8[:, t, :], in_=gs[:, t, :]), r=[B_gs], w=[B_t8])
                for t in range(2):
                    S.op("dve", lambda e: e.tensor_scalar(out=sel[:, t, :], in0=gs[:, t, :], scalar1=top8[:, t, 2:3], scalar2=None, op0=ALU.is_ge),
                         r=[B_gs, B_t8], w=[B_sel])
                S.op("dve", lambda e: e.tensor_tensor(out=mm[a], in0=sel, in1=mtab[:, i, :, h, :], op=ALU.mult), r=[B_sel, B_mt], w=[B_m[a]])
                S.op("pe", lambda e: e.matmul(pb[obk][:, 0:129], lhsT=pt[:, 0:128], rhs=V1[j][:, 2 * i, :], start=True, stop=True),
                     r=[B_pt_, B_V1[j]], w=[PB[obk]])
                S.op("pe", lambda e: e.matmul(pb[obk][:, 256:385], lhsT=pt[:, 128:256], rhs=V1[j][:, 2 * i, :], start=True, stop=False),
                     r=[B_pt_, B_V1[j]], w=[PB[obk]])
                S.op("pe", lambda e: e.matmul(pb[obk][:, 256:385], lhsT=pt[:, 384:512], rhs=V1[j][:, 2 * i + 1, :], start=False, stop=True),
                     r=[B_pt_, B_V1[j]], w=[PB[obk]])
                ov = pb[obk].rearrange("p (t n) -> p t n", n=256)[:, :, 0:129]
                S.op("dve", lambda e: e.tensor_copy(out=acc[a], in_=ov), r=[PB[obk]], w=[B_acc[a]])
            else:
                s_ = it["s"]
                for t in range(2):
                    for kh in range(2):
                        S.op("pe", lambda e: e.matmul(pb[obk][:, t * 256:t * 256 + 129], lhsT=pt[:, kh * 256 + t * 128:kh * 256 + (t + 1) * 128],
                                                      rhs=VP[j][:, s_ * 2 + kh, :], start=(kh == 0), stop=(kh == 1)),
                             r=[B_pt_, B_VP[j]], w=[PB[obk]])
                for t in range(2):
                    S.op("dve", lambda e: e.scalar_tensor_tensor(out=acc[a][:, t, :], in0=pb[obk][:, t * 256:t * 256 + 129],
                                                                 scalar=mm[a][:, t, s_:s_ + 1], in1=acc[a][:, t, :],
                                                                 op0=ALU.mult, op1=ALU.add), r=[PB[obk], B_m[a], B_acc[a]], w=[B_acc[a]])
            if it["last"]:
                S.op("dve", lambda e: e.reciprocal(out=rec, in_=acc[a][:, :, 128]), r=[B_acc[a]], w=[B_rec])
                for t in range(2):
                    S.op("dve", lambda e: e.tensor_scalar(out=oa[a][:, t, :], in0=acc[a][:, t, 0:128], scalar1=rec[:, t:t + 1], scalar2=None, op0=ALU.mult),
                         r=[B_acc[a], B_rec], w=[B_oa[a]])
                S.dma("sp", lambda e: e.dma_start(out=OA_d[q0:q0 + 256, h * 128:(h + 1) * 128].rearrange("(t p) c -> p t c", p=128), in_=oa[a]),
                      B_oa[a], r=[B_oa[a]])

        load_head(0)
        emit_S(0)
        for k in range(len(items)):
            emit_exp(k)
            if k + 1 < len(items):
                emit_S(k + 1)
            emit_rest(k)
        S.end_phase()

    WIDX = nc.alloc_sbuf_tensor("G_widx", [128, 128], I32).ap()
    B_widx = Buf("G_widx")
    DESTI = nc.alloc_sbuf_tensor("G_desti", [128, NTo, 2], I32).ap()
    B_desti = Buf("G_desti")

    def phase_C():
        Wo = sb("C_Wo", [128, 16, D], BF16)
        B_Wo = S.buf("C_Wo")
        for k in range(16):
            S.dma("pool", lambda e: e.dma_start(out=Wo[:, k, :], in_=IN('w_out')[k * 128:(k + 1) * 128, :]), B_Wo, w=[B_Wo])
        beta = sb("C_beta", [128, AW], F32)
        B_beta = S.buf("C_beta")
        S.dma("sp", lambda e: e.dma_start(out=beta, in_=IN('beta_attn').partition_broadcast(128)), B_beta, w=[B_beta])
        gffn = sb("C_gffn", [128, D], F32)
        B_gffn = S.buf("C_gffn")
        S.dma("sp", lambda e: e.dma_start(out=gffn, in_=IN('g_ffn').partition_broadcast(128)), B_gffn, w=[B_gffn])
        Wr32 = sb("C_Wr32", [128, 16, 72], F32)
        B_Wr32 = S.buf("C_Wr32")
        S.dma("sp", lambda e: e.dma_start(out=Wr32[:, :, 0:8], in_=IN('w_rg').rearrange("(k p) e -> p k e", p=128)), B_Wr32, w=[B_Wr32])
        for g in range(NG):
            S.dma("sp", lambda e: e.dma_start(out=Wr32[:, :, 8 + g * 8:16 + g * 8], in_=IN('w_re')[g].rearrange("(k p) e -> p k e", p=128)),
                  B_Wr32, w=[B_Wr32])
        Wr = sb("C_Wr", [128, 16, 72], BF16)
        B_Wr = S.buf("C_Wr")
        S.op("dve", lambda e: e.tensor_copy(out=Wr, in_=Wr32), r=[B_Wr32], w=[B_Wr])
        brb = sb("C_brb", [128, 72], F32)
        B_brb = S.buf("C_brb")
        S.dma("sp", lambda e: e.dma_start(out=brb[:, 0:8], in_=IN('b_rg').partition_broadcast(128)), B_brb, w=[B_brb])
        S.dma("sp", lambda e: e.dma_start(out=brb[:, 8:72], in_=IN('b_re').partition_broadcast(128)), B_brb, w=[B_brb])
        ltb = sb("C_ltb", [128, 128], BF16)
        B_ltb = S.buf("C_ltb")
        S.dma("pool", lambda e: e.dma_start(out=ltb, in_=IN('ltri')), B_ltb, w=[B_ltb])
        onesb = sb("C_ones", [128, 128], BF16)
        B_ones = S.buf("C_ones")
        S.op("dve", lambda e: e.memset(onesb, 1.0), w=[B_ones])
        pidx = sb("C_pidx", [128, 1], F32)
        B_pidx = S.buf("C_pidx")
        S.dma("sp", lambda e: e.dma_start(out=pidx, in_=IN('pidx')), B_pidx, w=[B_pidx])
        bpos = sb("C_bpos", [128, 1], F32)
        B_bpos = S.buf("C_bpos")
        S.dma("sp", lambda e: e.dma_start(out=bpos, in_=IN('bpos')), B_bpos, w=[B_bpos])
        zer = sb("C_zer", [128, (PL // 128) * 2], F32)
        B_zer = S.buf("C_zer")
        S.op("dve", lambda e: e.memset(zer, 0.0), w=[B_zer])
        B_slotd = S.buf("C_slotd")
        S.dma("sp", lambda e: e.dma_start(out=SLOT_d.rearrange("(p n) c -> p (n c)", p=128), in_=zer), B_zer, r=[B_zer], w=[B_slotd])
        OHK = sb("C_OHK", [128, NTo, 2, 64], F32)
        B_OHK = S.buf("C_OHK")
        RK = sb("C_RK", [128, NTo, 2], F32)
        B_RK = S.buf("C_RK")
        WK = sb("C_WK", [128, NTo, 2], F32)
        B_WK = S.buf("C_WK")
        ohacc = sb("C_ohacc", [128, 64], BF16)
        B_ohacc = S.buf("C_ohacc")
        S.op("dve", lambda e: e.memset(ohacc, 0.0), w=[B_ohacc])
        oa = [sb("C_oa%d" % i, [128, AW], F32) for i in range(2)]
        B_oa = S.bufs("C_oa", 2)
        mx = [sb("C_mx%d" % i, [128, D], BF16) for i in range(2)]
        B_mx = S.bufs("C_mx", 2)
        xt = [sb("C_x%d" % i, [128, D], F32) for i in range(2)]
        B_xt = S.bufs("C_x", 2)
        mT = sb("C_mT", [128, 16, 128], BF16)
        B_mT = S.buf("C_mT")
        h1 = [sb("C_h1%d" % i, [128, D], F32) for i in range(2)]
        B_h1 = S.bufs("C_h1", 2)
        fb = [sb("C_f%d" % i, [128, D], BF16) for i in range(2)]
        B_fb = S.bufs("C_f", 2)
        fT = sb("C_fT", [128, 16, 128], BF16)
        B_fT = S.buf("C_fT")
        st = [sb("C_st%d" % i, [128, 8], F32) for i in range(2)]
        B_st = S.bufs("C_st", 2)
        st2 = [sb("C_su%d" % i, [128, 8], F32) for i in range(2)]
        B_st2 = S.bufs("C_su", 2)
        lg = sb("C_lg", [128, 72], F32)
        B_lg = S.buf("C_lg")
        rs = sb("C_rs", [128, 32], F32)
        B_rs = S.buf("C_rs")
        ohg = sb("C_ohg", [128, 8], F32)
        B_ohg = S.buf("C_ohg")
        tmp88 = sb("C_tmp88", [128, 8, 8], F32)
        B_t88 = S.buf("C_tmp88")
        le = sb("C_le", [128, 8], F32)
        B_le = S.buf("C_le")
        t8 = sb("C_t8", [128, 8], F32)
        B_t8 = S.buf("C_t8")
        ohk = sb("C_ohk", [128, 2, 8], F32)
        B_ohk = S.buf("C_ohk")
        ohs = sb("C_ohs", [128, 64], BF16)
        B_ohs = S.buf("C_ohs")
        cum = sb("C_cum", [128, 64], F32)
        B_cum = S.buf("C_cum")
        tmp64 = sb("C_tmp64", [128, 64], F32)
        B_t64 = S.buf("C_tmp64")

        def load(j):
            b = j % 2
            i, t = j // 2, j % 2
            xr = i * 512 + t * 128
            S.dma("sp", lambda e: e.dma_start(out=oa[b], in_=OA_d[j * 128:(j + 1) * 128, :]), B_oa[b], w=[B_oa[b]])
            S.dma("sp", lambda e: e.dma_start(out=xt[b], in_=IN('x_perm')[xr:xr + 128, :]), B_xt[b], w=[B_xt[b]])
            S.dma("sp", lambda e: e.dma_start(out=mx[b][:, AW:D], in_=MP_d[j * 128:(j + 1) * 128, :]), B_mx[b], w=[B_mx[b]])

        load(0)
        for j in range(NTo):
            b = j % 2
            if j + 1 < NTo:
                load(j + 1)
            rmsnorm_rstd(oa[b], B_oa[b], mx[b][:, 0:AW], B_mx[b], st[b], B_st[b], AW)
            S.op("dve", lambda e: e.scalar_tensor_tensor(out=mx[b][:, 0:AW], in0=oa[b], scalar=st[b][:, 2:3], in1=beta, op0=ALU.mult, op1=ALU.mult),
                 r=[B_oa[b], B_st[b], B_beta], w=[B_mx[b]])
            transpose_to(mx[b], B_mx[b], 16, lambda k0, k1: mT[:, k0:k1, :], B_mT, [0, 1])
            for n in range(4):
                bk = 2 + n
                for k in range(16):
                    S.op("pe", lambda e: e.matmul(pb[bk], lhsT=mT[:, k, :], rhs=Wo[:, k, n * 512:(n + 1) * 512], start=(k == 0), stop=(k == 15)),
                         r=[B_mT, B_Wo], w=[PB[bk]])
                S.op("dve", lambda e: e.tensor_tensor(out=h1[b][:, n * 512:(n + 1) * 512], in0=pb[bk], in1=xt[b][:, n * 512:(n + 1) * 512], op=ALU.add),
                     r=[PB[bk], B_xt[b]], w=[B_h1[b]])
            S.dma("sp", lambda e: e.dma_start(out=H1_d[j * 128:(j + 1) * 128, :], in_=h1[b]), B_h1[b], r=[B_h1[b]])
            rmsnorm_rstd(h1[b], B_h1[b], fb[b], B_fb[b], st2[b], B_st2[b], D)
            S.op("dve", lambda e: e.scalar_tensor_tensor(out=fb[b], in0=h1[b], scalar=st2[b][:, 2:3], in1=gffn, op0=ALU.mult, op1=ALU.mult),
                 r=[B_h1[b], B_st2[b], B_gffn], w=[B_fb[b]])
            S.dma("sp", lambda e: e.dma_start(out=F_d[j * 128:(j + 1) * 128, :], in_=fb[b]), B_fb[b], r=[B_fb[b]])
            transpose_to(fb[b], B_fb[b], 16, lambda k0, k1: fT[:, k0:k1, :], B_fT, [0, 1])
            bk = 6
            for k in range(16):
                S.op("pe", lambda e: e.matmul(pb[bk][:, 0:72], lhsT=fT[:, k, :], rhs=Wr[:, k, :], start=(k == 0), stop=(k == 15)),
                     r=[B_fT, B_Wr], w=[PB[bk]])
            S.op("dve", lambda e: e.tensor_tensor(out=lg, in0=pb[bk][:, 0:72], in1=brb, op=ALU.add), r=[PB[bk], B_brb], w=[B_lg])
            S.op("dve", lambda e: e.reduce_max(out=rs[:, 0:1], in_=lg[:, 0:8], axis=AX.X), r=[B_lg], w=[B_rs])
            S.op("dve", lambda e: e.tensor_scalar(out=ohg, in0=lg[:, 0:8], scalar1=rs[:, 0:1], scalar2=None, op0=ALU.is_equal), r=[B_lg, B_rs], w=[B_ohg])
            S.op("dve", lambda e: e.tensor_scalar(out=rs[:, 1:2], in0=rs[:, 0:1], scalar1=-1.0, scalar2=None, op0=ALU.mult), r=[B_rs], w=[B_rs])
            S.op("act", lambda e: e.activation(out=t8, in_=lg[:, 0:8], func=AF.Exp, bias=rs[:, 1:2], scale=1.0, accum_out=rs[:, 2:3]),
                 r=[B_lg, B_rs], w=[B_t8, B_rs])
            S.op("dve", lambda e: e.reciprocal(out=rs[:, 3:4], in_=rs[:, 2:3]), r=[B_rs], w=[B_rs])
            S.op("dve", lambda e: e.tensor_tensor(out=tmp88, in0=lg[:, 8:72].rearrange("p (g e) -> p g e", e=8),
                                                  in1=ohg.unsqueeze(2).to_broadcast([128, 8, 8]), op=ALU.mult), r=[B_lg, B_ohg], w=[B_t88])
            S.op("dve", lambda e: e.reduce_sum(out=le, in_=tmp88.rearrange("p g e -> p e g"), axis=AX.X), r=[B_t88], w=[B_le])
            S.op("dve", lambda e: e.max(out=t8, in_=le), r=[B_le], w=[B_t8])
            for k2 in range(2):
                S.op("dve", lambda e: e.tensor_scalar(out=ohk[:, k2, :], in0=le, scalar1=t8[:, k2:k2 + 1], scalar2=None, op0=ALU.is_equal),
                     r=[B_le, B_t8], w=[B_ohk])
            S.op("dve", lambda e: e.tensor_tensor(out=rs[:, 4:5], in0=t8[:, 1:2], in1=t8[:, 0:1], op=ALU.subtract), r=[B_t8], w=[B_rs])
            S.op("act", lambda e: e.activation(out=rs[:, 5:6], in_=rs[:, 4:5], func=AF.Exp), r=[B_rs], w=[B_rs])
            S.op("dve", lambda e: e.tensor_scalar(out=rs[:, 6:7], in0=rs[:, 5:6], scalar1=1.0, scalar2=None, op0=ALU.add), r=[B_rs], w=[B_rs])
            S.op("dve", lambda e: e.reciprocal(out=rs[:, 7:8], in_=rs[:, 6:7]), r=[B_rs], w=[B_rs])
            S.op("dve", lambda e: e.tensor_tensor(out=rs[:, 8:9], in0=rs[:, 5:6], in1=rs[:, 7:8], op=ALU.mult), r=[B_rs], w=[B_rs])
            S.op("dve", lambda e: e.tensor_tensor(out=WK[:, j, 0:1], in0=rs[:, 7:8], in1=rs[:, 3:4], op=ALU.mult), r=[B_rs], w=[B_WK])
            S.op("dve", lambda e: e.tensor_tensor(out=WK[:, j, 1:2], in0=rs[:, 8:9], in1=rs[:, 3:4], op=ALU.mult), r=[B_rs], w=[B_WK])
            for k2 in range(2):
                S.op("dve", lambda e: e.tensor_tensor(out=OHK[:, j, k2, :].rearrange("p (g e) -> p g e", e=8),
                                                      in0=ohg.unsqueeze(2).to_broadcast([128, 8, 8]),
                                                      in1=ohk[:, k2, :].unsqueeze(1).to_broadcast([128, 8, 8]), op=ALU.mult),
                     r=[B_ohg, B_ohk], w=[B_OHK])
            S.op("dve", lambda e: e.tensor_tensor(out=ohs, in0=OHK[:, j, 0, :], in1=OHK[:, j, 1, :], op=ALU.add), r=[B_OHK], w=[B_ohs])
            bk = 7
            S.op("pe", lambda e: e.matmul(pb[bk][:, 0:64], lhsT=ltb, rhs=ohs, start=True, stop=(j == 0)), r=[B_ltb, B_ohs], w=[PB[bk]])
            if j > 0:
                S.op("pe", lambda e: e.matmul(pb[bk][:, 0:64], lhsT=onesb, rhs=ohacc, start=False, stop=True), r=[B_ones, B_ohacc], w=[PB[bk]])
            S.op("dve", lambda e: e.tensor_copy(out=cum, in_=pb[bk][:, 0:64]), r=[PB[bk]], w=[B_cum])
            S.op("dve", lambda e: e.tensor_tensor(out=ohacc, in0=ohacc, in1=ohs, op=ALU.add), r=[B_ohacc, B_ohs], w=[B_ohacc])
            for k2 in range(2):
                S.op("dve", lambda e: e.tensor_tensor(out=tmp64, in0=OHK[:, j, k2, :], in1=cum, op=ALU.mult), r=[B_OHK, B_cum], w=[B_t64])
                S.op("dve", lambda e: e.reduce_sum(out=RK[:, j, k2:k2 + 1], in_=tmp64, axis=AX.X), r=[B_t64], w=[B_RK])
        bk = 7
        S.op("pe", lambda e: e.matmul(pb[bk][:, 0:64], lhsT=onesb, rhs=ohacc, start=True, stop=True), r=[B_ones, B_ohacc], w=[PB[bk]])
        cnt = sb("C_cnt", [128, 64], F32)
        B_cnt = S.buf("C_cnt")
        cnti = sb("C_cnti", [128, 64], I32)
        B_cnti = S.buf("C_cnti")
        padf = sb("C_padf", [128, 64], F32)
        B_padf = S.buf("C_padf")
        pend = sb("C_pend", [128, 64], F32)
        B_pend = S.buf("C_pend")
        pstart = sb("C_pstart", [128, 64], F32)
        B_pstart = S.buf("C_pstart")
        ones64 = sb("C_ones64", [128, 64], F32)
        B_o64 = S.buf("C_ones64")
        S.op("dve", lambda e: e.memset(ones64, 1.0), w=[B_o64])
        S.op("dve", lambda e: e.tensor_scalar(out=cnt, in0=pb[bk][:, 0:64], scalar1=127.0, scalar2=None, op0=ALU.add), r=[PB[bk]], w=[B_cnt])
        S.op("dve", lambda e: e.tensor_copy(out=cnti, in_=cnt), r=[B_cnt], w=[B_cnti])
        S.op("dve", lambda e: e.tensor_scalar(out=cnti, in0=cnti, scalar1=7, scalar2=7, op0=ALU.arith_shift_right, op1=ALU.logical_shift_left),
             r=[B_cnti], w=[B_cnti])
        S.op("dve", lambda e: e.tensor_copy(out=padf, in_=cnti), r=[B_cnti], w=[B_padf])
        S.op("dve", lambda e: e.tensor_tensor_scan(out=pend, data0=ones64, data1=padf, initial=0.0, op0=ALU.mult, op1=ALU.add),
             r=[B_o64, B_padf], w=[B_pend])
        S.op("dve", lambda e: e.tensor_tensor(out=pstart, in0=pend, in1=padf, op=ALU.subtract), r=[B_pend, B_padf], w=[B_pstart])
        S.op("dve", lambda e: e.tensor_scalar(out=tmp64, in0=pend, scalar1=bpos[:, 0:1], scalar2=None, op0=ALU.is_le), r=[B_pend, B_bpos], w=[B_t64])
        S.op("dve", lambda e: e.reduce_sum(out=rs[:, 10:11], in_=tmp64, axis=AX.X), r=[B_t64], w=[B_rs])
        S.op("dve", lambda e: e.tensor_scalar(out=rs[:, 11:12], in0=rs[:, 10:11], scalar1=63.0, scalar2=None, op0=ALU.min), r=[B_rs], w=[B_rs])
        S.op("dve", lambda e: e.tensor_scalar(out=rs[:, 13:14], in0=bpos, scalar1=-128.0, scalar2=None, op0=ALU.add), r=[B_bpos], w=[B_rs])
        S.op("dve", lambda e: e.tensor_scalar(out=tmp64, in0=pend, scalar1=rs[:, 13:14], scalar2=None, op0=ALU.is_le), r=[B_pend, B_rs], w=[B_t64])
        S.op("dve", lambda e: e.reduce_sum(out=rs[:, 14:15], in_=tmp64, axis=AX.X), r=[B_t64], w=[B_rs])
        S.op("dve", lambda e: e.tensor_scalar(out=rs[:, 15:16], in0=rs[:, 14:15], scalar1=63.0, scalar2=None, op0=ALU.min), r=[B_rs], w=[B_rs])
        S.op("dve", lambda e: e.tensor_tensor(out=rs[:, 16:17], in0=rs[:, 11:12], in1=rs[:, 15:16], op=ALU.not_equal), r=[B_rs], w=[B_rs])
        S.op("dve", lambda e: e.tensor_scalar(out=rs[:, 17:18], in0=pidx, scalar1=0.0, scalar2=None, op0=ALU.is_equal), r=[B_pidx], w=[B_rs])
        S.op("dve", lambda e: e.tensor_tensor(out=rs[:, 18:19], in0=rs[:, 16:17], in1=rs[:, 17:18], op=ALU.max), r=[B_rs], w=[B_rs])
        S.op("dve", lambda e: e.tensor_scalar(out=rs[:, 19:20], in0=rs[:, 11:12], scalar1=-64.0, scalar2=None, op0=ALU.add), r=[B_rs], w=[B_rs])
        S.op("dve", lambda e: e.tensor_tensor(out=rs[:, 20:21], in0=rs[:, 19:20], in1=rs[:, 18:19], op=ALU.mult), r=[B_rs], w=[B_rs])
        S.op("dve", lambda e: e.tensor_scalar(out=rs[:, 21:22], in0=rs[:, 20:21], scalar1=64.0, scalar2=None, op0=ALU.add), r=[B_rs], w=[B_rs])
        diag = sb("C_diag", [128, 128], BF16)
        B_diag = S.buf("C_diag")
        S.op("dve", lambda e: e.tensor_scalar(out=diag, in0=ident, scalar1=rs[:, 21:22], scalar2=None, op0=ALU.mult), r=[B_ident, B_rs], w=[B_diag])
        bk = 6
        S.op("pe", lambda e: e.matmul(pb[bk][:, 0:128], lhsT=onesb, rhs=diag, start=True, stop=True), r=[B_ones, B_diag], w=[PB[bk]])
        widf = sb("C_widf", [128, 128], F32)
        B_widf = S.buf("C_widf")
        S.op("dve", lambda e: e.tensor_scalar(out=widf, in0=pb[bk][:, 0:128], scalar1=128.0, scalar2=pidx[:, 0:1], op0=ALU.mult, op1=ALU.add),
             r=[PB[bk], B_pidx], w=[B_widf])
        S.op("dve", lambda e: e.tensor_copy(out=WIDX, in_=widf), r=[B_widf], w=[B_widx])
        destf = sb("C_destf", [128, NTo, 2], F32)
        B_destf = S.buf("C_destf")
        slt = [sb("C_slt%d" % i, [128, 2], F32) for i in range(4)]
        B_slt = S.bufs("C_slt", 4)
        for j in range(NTo):
            for k2 in range(2):
                S.op("dve", lambda e: e.tensor_tensor(out=tmp64, in0=OHK[:, j, k2, :], in1=pstart, op=ALU.mult), r=[B_OHK, B_pstart], w=[B_t64])
                S.op("dve", lambda e: e.reduce_sum(out=rs[:, 12:13], in_=tmp64, axis=AX.X), r=[B_t64], w=[B_rs])
                S.op("dve", lambda e: e.tensor_tensor(out=destf[:, j, k2:k2 + 1], in0=rs[:, 12:13], in1=RK[:, j, k2:k2 + 1], op=ALU.add),
                     r=[B_rs, B_RK], w=[B_destf])
        S.op("dve", lambda e: e.tensor_copy(out=DESTI, in_=destf), r=[B_destf], w=[B_desti])
        for j in range(NTo):
            for k2 in range(2):
                q = (j * 2 + k2) % 4
                S.op("dve", lambda e: e.tensor_scalar(out=slt[q][:, 0:1], in0=pidx, scalar1=float(j * 128), scalar2=None, op0=ALU.add),
                     r=[B_pidx], w=[B_slt[q]])
                S.op("dve", lambda e: e.tensor_copy(out=slt[q][:, 1:2], in_=WK[:, j, k2:k2 + 1]), r=[B_WK], w=[B_slt[q]])
                S.dma("pool", lambda e: e.indirect_dma_start(out=SLOT_d, out_offset=bass.IndirectOffsetOnAxis(ap=DESTI[:, j, k2:k2 + 1], axis=0),
                                                             in_=slt[q], in_offset=None),
                      B_slt[q], r=[B_slt[q], B_desti, B_slotd], w=[])
        if debug:
            S.dma("sp", lambda e: e.dma_start(out=RT_d.rearrange("(j p) c -> p j c", p=128)[:, :, 0:2], in_=destf), B_destf, r=[B_destf])
            S.dma("sp", lambda e: e.dma_start(out=RT_d.rearrange("(j p) c -> p j c", p=128)[:, :, 2:4], in_=WK), B_WK, r=[B_WK])
            S.dma("sp", lambda e: e.dma_start(out=RT_d[0:128, 4:6], in_=rs[:, 10:12]), B_rs, r=[B_rs])
        S.end_phase()

    def phase_D():
        wg = sb("D_wg", [128, 16 * DE], BF16)
        wu = sb("D_wu", [128, 16 * DE], BF16)
        wd = sb("D_wd", [128, 4 * D], BF16)
        B_wg = S.buf("D_wg")
        B_wu = S.buf("D_wu")
        B_wd = S.buf("D_wd")
        sl = [sb("D_sl%d" % i, [128, 2], F32) for i in range(2)]
        B_sl = S.bufs("D_sl", 2)
        ti = [sb("D_ti%d" % i, [128, 1], I32) for i in range(2)]
        B_ti = S.bufs("D_ti", 2)
        xg = [sb("D_xg%d" % i, [128, D], BF16) for i in range(2)]
        B_xg = S.bufs("D_xg", 2)
        xT = sb("D_xT", [128, 16, 128], BF16)
        B_xT = S.buf("D_xT")
        sg = sb("D_sg", [128, DE], F32)
        B_sg = S.buf("D_sg")
        hid = sb("D_hid", [128, DE], BF16)
        B_hid = S.buf("D_hid")
        hT = sb("D_hT", [128, 4, 128], BF16)
        B_hT = S.buf("D_hT")
        yb = [sb("D_y%d" % i, [128, D], F32) for i in range(2)]
        B_yb = S.bufs("D_y", 2)
        wgv = IN('w_eg').rearrange("e (p k) n -> (e p) (k n)", k=16)
        wuv = IN('w_eu').rearrange("e (p k) n -> (e p) (k n)", k=16)
        wdv = IN('w_ed').rearrange("e (p k) n -> (e p) (k n)", k=4)

        def load_x(b):
            j = b % 2
            S.dma("sp", lambda e: e.dma_start(out=sl[j], in_=SLOT_d[b * 128:(b + 1) * 128, :]), B_sl[j], w=[B_sl[j]])
            S.op("dve", lambda e: e.tensor_copy(out=ti[j], in_=sl[j][:, 0:1]), r=[B_sl[j]], w=[B_ti[j]])
            S.dma("pool", lambda e: e.indirect_dma_start(out=xg[j], out_offset=None, in_=F_d,
                                                         in_offset=bass.IndirectOffsetOnAxis(ap=ti[j][:, 0:1], axis=0)),
                  B_xg[j], r=[B_ti[j]], w=[B_xg[j]])

        def load_w(b, which):
            for (dst, B_dst, src) in which:
                S.dma("pool", lambda e: e.indirect_dma_start(out=dst, out_offset=None, in_=src,
                                                             in_offset=bass.IndirectOffsetOnAxis(ap=WIDX[:, b:b + 1], axis=0),
                                                             bounds_check=bc_reg, oob_is_err=False),
                      B_dst, r=[B_widx], w=[B_dst])

        bc_reg = nc.gpsimd.alloc_register("bc_reg")
        nc.gpsimd.reg_mov(bc_reg, NE * 128 - 1)
        GU = ((wg, B_wg, wgv), (wu, B_wu, wuv))
        DN = ((wd, B_wd, wdv),)
        load_x(0)
        load_w(0, GU)
        load_w(0, DN)
        for b in range(NBLK):
            j = b % 2
            if b + 1 < NBLK:
                load_x(b + 1)
            transpose_to(xg[j], B_xg[j], 16, lambda k0, k1: xT[:, k0:k1, :], B_xT, [0, 1], step=16)
            for (bk, wt, B_wt) in ((2, wg, B_wg), (3, wu, B_wu)):
                for k in range(16):
                    S.op("pe", lambda e: e.matmul(pb[bk], lhsT=xT[:, k, :], rhs=wt[:, k * DE:(k + 1) * DE], start=(k == 0), stop=(k == 15)),
                         r=[B_xT, B_wt], w=[PB[bk]])
            if b + 1 < NBLK:
                load_w(b + 1, GU)
            S.op("act", lambda e: e.activation(out=sg, in_=pb[2], func=AF.Silu), r=[PB[2]], w=[B_sg])
            S.op("dve", lambda e: e.tensor_tensor(out=hid, in0=pb[3], in1=sg, op=ALU.mult), r=[PB[3], B_sg], w=[B_hid])
            transpose_to(hid, B_hid, 4, lambda k0, k1: hT[:, k0:k1, :], B_hT, [0, 1], step=4)
            for n in range(4):
                bk = 4 + n
                for k in range(4):
                    S.op("pe", lambda e: e.matmul(pb[bk], lhsT=hT[:, k, :], rhs=wd[:, k * D + n * 512:k * D + (n + 1) * 512],
                                                  start=(k == 0), stop=(k == 3)), r=[B_hT, B_wd], w=[PB[bk]])
            if b + 1 < NBLK:
                load_w(b + 1, DN)
            for n in range(4):
                bk = 4 + n
                if n % 2 == 0:
                    S.op("act", lambda e: e.activation(out=yb[j][:, n * 512:(n + 1) * 512], in_=pb[bk], func=AF.Copy, scale=sl[j][:, 1:2]),
                         r=[PB[bk], B_sl[j]], w=[B_yb[j]])
                else:
                    S.op("dve", lambda e: e.tensor_scalar(out=yb[j][:, n * 512:(n + 1) * 512], in0=pb[bk], scalar1=sl[j][:, 1:2], scalar2=None, op0=ALU.mult),
                         r=[PB[bk], B_sl[j]], w=[B_yb[j]])
            S.dma("sp", lambda e: e.dma_start(out=Y_d[b * 128:(b + 1) * 128, :], in_=yb[j]), B_yb[j], r=[B_yb[j]])
        S.end_phase()

    def phase_E():
        Wpg = sb("E_Wpg", [128, 16, D], BF16)
        B_Wpg = S.buf("E_Wpg")
        for k in range(16):
            S.dma("pool", lambda e: e.dma_start(out=Wpg[:, k, :], in_=IN('w_pg')[k * 128:(k + 1) * 128, :]), B_Wpg, w=[B_Wpg])
        Wpl = sb("E_Wpl", [128, 2, D], BF16)
        B_Wpl = S.buf("E_Wpl")
        for k in range(2):
            S.dma("pool", lambda e: e.dma_start(out=Wpl[:, k, :], in_=IN('w_ple')[k * 128:(k + 1) * 128, :]), B_Wpl, w=[B_Wpl])
        gple = sb("E_gple", [128, D], F32)
        bpg = sb("E_bpg", [128, D], F32)
        gfin = sb("E_gfin", [128, D], F32)
        B_gple = S.buf("E_gple")
        B_bpg = S.buf("E_bpg")
        B_gfin = S.buf("E_gfin")
        S.dma("sp", lambda e: e.dma_start(out=gple, in_=IN('g_ple').partition_broadcast(128)), B_gple, w=[B_gple])
        S.dma("sp", lambda e: e.dma_start(out=bpg, in_=IN('b_pg').partition_broadcast(128)), B_bpg, w=[B_bpg])
        S.dma("sp", lambda e: e.dma_start(out=gfin, in_=IN('g_final').partition_broadcast(128)), B_gfin, w=[B_gfin])
        h2 = [sb("E_h2%d" % i, [128, D], F32) for i in range(2)]
        y1 = [sb("E_y1%d" % i, [128, D], F32) for i in range(2)]
        y2 = [sb("E_y2%d" % i, [128, D], F32) for i in range(2)]
        pbf = [sb("E_p%d" % i, [128, PLE], BF16) for i in range(2)]
        B_h2 = S.bufs("E_h2", 2)
        B_y1 = S.bufs("E_y1", 2)
        B_y2 = S.bufs("E_y2", 2)
        B_pbf = S.bufs("E_p", 2)
        hn = sb("E_hn", [128, D], BF16)
        B_hn = S.buf("E_hn")
        hT = sb("E_hT", [128, 16, 128], BF16)
        B_hT = S.buf("E_hT")
        pT = sb("E_pT", [128, 2, 128], BF16)
        B_pT = S.buf("E_pT")
        gl = sb("E_gl", [128, 512], F32)
        B_gl = S.buf("E_gl")
        sgm = sb("E_sg", [128, 512], F32)
        B_sgm = S.buf("E_sg")
        h3 = sb("E_h3", [128, D], F32)
        B_h3 = S.buf("E_h3")
        ob = [sb("E_o%d" % i, [128, D], F32) for i in range(2)]
        B_ob = S.bufs("E_o", 2)
        st = [sb("E_st%d" % i, [128, 8], F32) for i in range(2)]
        B_st = S.bufs("E_st", 2)
        st2 = [sb("E_su%d" % i, [128, 8], F32) for i in range(2)]
        B_st2 = S.bufs("E_su", 2)

        def load(j):
            b = j % 2
            S.dma("sp", lambda e: e.dma_start(out=h2[b], in_=H1_d[j * 128:(j + 1) * 128, :]), B_h2[b], w=[B_h2[b]])
            S.dma("pool", lambda e: e.dma_start(out=pbf[b], in_=IN('p_own')[j * 128:(j + 1) * 128, :]), B_pbf[b], w=[B_pbf[b]])
            S.dma("pool", lambda e: e.indirect_dma_start(out=y1[b], out_offset=None, in_=Y_d,
                                                         in_offset=bass.IndirectOffsetOnAxis(ap=DESTI[:, j, 0:1], axis=0)),
                  B_y1[b], r=[B_desti], w=[B_y1[b]])
            S.dma("pool", lambda e: e.indirect_dma_start(out=y2[b], out_offset=None, in_=Y_d,
                                                         in_offset=bass.IndirectOffsetOnAxis(ap=DESTI[:, j, 1:2], axis=0)),
                  B_y2[b], r=[B_desti], w=[B_y2[b]])

        load(0)
        for j in range(NTo):
            b = j % 2
            if j + 1 < NTo:
                load(j + 1)
            S.op("dve", lambda e: e.tensor_tensor(out=h2[b], in0=h2[b], in1=y1[b], op=ALU.add), r=[B_h2[b], B_y1[b]], w=[B_h2[b]])
            S.op("dve", lambda e: e.tensor_tensor(out=h2[b], in0=h2[b], in1=y2[b], op=ALU.add), r=[B_h2[b], B_y2[b]], w=[B_h2[b]])
            rmsnorm_rstd(h2[b], B_h2[b], hn, B_hn, st[b], B_st[b], D)
            S.op("dve", lambda e: e.scalar_tensor_tensor(out=hn, in0=h2[b], scalar=st[b][:, 2:3], in1=gple, op0=ALU.mult, op1=ALU.mult),
                 r=[B_h2[b], B_st[b], B_gple], w=[B_hn])
            transpose_to(hn, B_hn, 16, lambda k0, k1: hT[:, k0:k1, :], B_hT, [0, 1])
            transpose_to(pbf[b], B_pbf[b], 2, lambda k0, k1: pT[:, k0:k1, :], B_pT, [0, 1])
            for n in range(4):
                bg = 2 + (n % 2) * 2
                bp_ = 3 + (n % 2) * 2
                for k in range(16):
                    S.op("pe", lambda e: e.matmul(pb[bg], lhsT=hT[:, k, :], rhs=Wpg[:, k, n * 512:(n + 1) * 512], start=(k == 0), stop=(k == 15)),
                         r=[B_hT, B_Wpg], w=[PB[bg]])
                for k in range(2):
                    S.op("pe", lambda e: e.matmul(pb[bp_], lhsT=pT[:, k, :], rhs=Wpl[:, k, n * 512:(n + 1) * 512], start=(k == 0), stop=(k == 1)),
                         r=[B_pT, B_Wpl], w=[PB[bp_]])
                cs = slice(n * 512, (n + 1) * 512)
                S.op("dve", lambda e: e.tensor_tensor(out=gl, in0=pb[bg], in1=bpg[:, cs], op=ALU.add), r=[PB[bg], B_bpg], w=[B_gl])
                S.op("act", lambda e: e.activation(out=sgm, in_=gl, func=AF.Sigmoid), r=[B_gl], w=[B_sgm])
                S.op("dve", lambda e: e.tensor_tensor(out=sgm, in0=pb[bp_], in1=sgm, op=ALU.mult), r=[PB[bp_], B_sgm], w=[B_sgm])
                S.op("dve", lambda e: e.tensor_tensor(out=h3[:, cs], in0=h2[b][:, cs], in1=sgm, op=ALU.add), r=[B_h2[b], B_sgm], w=[B_h3])
            rmsnorm_rstd(h3, B_h3, ob[b], B_ob[b], st2[b], B_st2[b], D)
            S.op("dve", lambda e: e.scalar_tensor_tensor(out=ob[b], in0=h3, scalar=st2[b][:, 2:3], in1=gfin, op0=ALU.mult, op1=ALU.mult),
                 r=[B_h3, B_st2[b], B_gfin], w=[B_ob[b]])
            S.dma("sp", lambda e: e.dma_start(out=out[j * 128:(j + 1) * 128, :], in_=ob[b]), B_ob[b], r=[B_ob[b]])
        S.end_phase()

    phases = [("A", phase_A), ("A2", phase_A2), ("B", phase_B), ("C", phase_C), ("D", phase_D), ("E", phase_E)]
    for name, fn in phases:
        fn()
        c.stack.close()
        c.stack = contextlib.ExitStack()
        if name == upto:
            break
    return nc, S, c


def make_in_maps(inputs, S_len, used=None):
    x = np.asarray(inputs["x"], np.float32)
    Bn = x.shape[0]
    NB = S_len // 256
    NBo = NB // 2
    p = np.asarray(inputs["p"], np.float32)[0]
    sq = lambda k: np.ascontiguousarray(np.asarray(inputs[k], np.float32)[0])
    row = lambda a: np.ascontiguousarray(a.reshape(1, -1))
    shared = dict(
        g_mix=row(sq("g_mix")), w_in=sq("w_in"), beta_attn=row(sq("beta_attn")), w_pool=sq("w_pool"),
        pool_scale=row(sq("pool_scale")), w_out=sq("w_out"), g_ffn=row(sq("g_ffn")),
        w_rg=sq("w_router_group"), b_rg=row(sq("b_router_group")), w_re=sq("w_router_expert"),
        b_re=row(sq("b_router_expert")), w_eg=sq("w_expert_gate"), w_eu=sq("w_expert_up"),
        w_ed=sq("w_expert_down"), g_ple=row(sq("g_ple")), w_ple=sq("w_ple"), w_pg=sq("w_ple_gate"),
        b_pg=row(sq("b_ple_gate")), g_final=row(np.asarray(inputs["g_final"], np.float32)),
    )
    tabs = [host_tables(S_len, r) for r in range(2)]
    in_maps = []
    orders = []
    for cidx in range(2 * Bn):
        b, r = cidx // 2, cidx % 2
        order = []
        for i in range(NBo):
            order += [2 * i + r, 2 * i + 1 - r]
        own = [2 * i + r for i in range(NBo)]
        xb = x[b].reshape(NB, 256, D)
        pb_ = p[b].reshape(NB, 256, PLE)
        m = dict(shared)
        m["x_perm"] = np.ascontiguousarray(xb[order].reshape(S_len, D))
        m["p_own"] = np.ascontiguousarray(pb_[own].reshape(-1, PLE))
        m.update(tabs[r])
        if used is not None:
            m = {k: v for k, v in m.items() if k in used}
        in_maps.append(m)
        orders.append(own)
    return in_maps, orders


def kernel(**inputs):
    x = np.asarray(inputs["x"])
    Bn, S_len, _ = x.shape
    nc, S, c = build(S_len, debug=False, upto="E")
    in_maps, orders = make_in_maps(inputs, S_len, used=set(c.used))
    ncores = 2 * Bn
    res = run_bass_kernel_spmd(nc, in_maps, core_ids=list(range(ncores)))
    outp = np.empty((Bn, S_len // 256, 256, D), np.float32)
    for cidx in range(ncores):
        b = cidx // 2
        o = np.asarray(res.results[cidx]["out"], np.float32).reshape(-1, 256, D)
        outp[b, orders[cidx]] = o
    return outp.reshape(Bn, S_len, D)
```

```python
import numpy as np
import concourse.bass as bass
import concourse.mybir as mybir
from concourse.bass_utils import run_bass_kernel_spmd

F32 = mybir.dt.float32
BF16 = mybir.dt.bfloat16
I32 = mybir.dt.int32
AF = mybir.ActivationFunctionType
ALU = mybir.AluOpType
AX = mybir.AxisListType

D = 2048
H = 8
HD = 128
AW = 1024
PW = 1024
INW = 4096
NE = 64
NG = 8
DE = 512
PLE = 256
EPS = 1e-6
import os
SKIP = os.environ.get('KSKIP', '')
NEG = -1.0e30


class Buf:
    __slots__ = ("name", "wev", "revs", "slot", "excl")

    def __init__(self, name, excl=False):
        self.name = name
        self.excl = excl
        self.wev = None
        self.revs = {}
        self.slot = None


class Sched:
    def __init__(self, nc):
        self.nc = nc
        self.eng = {"pe": nc.tensor, "act": nc.scalar, "dve": nc.vector, "pool": nc.gpsimd, "sp": nc.sync}
        self.esem = {k: nc.alloc_semaphore("es_" + k) for k in self.eng}
        self.ecnt = {k: 0 for k in self.eng}
        self.seen = {k: {} for k in self.eng}
        self.free_slots = []
        self.nslots = 0
        self.phase_bufs = []
        self.ninst = 0
        self.nwait = 0

    def buf(self, name, excl=False):
        b = Buf(name, excl)
        self.phase_bufs.append(b)
        return b

    def bufs(self, name, n, excl=False):
        return [self.buf("%s%d" % (name, i), excl) for i in range(n)]

    def _slot(self, b):
        if b.slot is None:
            if self.free_slots:
                b.slot = self.free_slots.pop()
            else:
                b.slot = [self.nc.alloc_semaphore("ds%d" % self.nslots), 0]
                self.nslots += 1
        return b.slot

    def _waits(self, e, r, w):
        need = {}

        def add(ev):
            s, v, src = ev
            if src == e and e == "pe":
                return
            k = id(s)
            if k not in need or need[k][1] < v:
                need[k] = (s, v)

        for b in r:
            if b.wev is not None:
                add(b.wev)
            if b.excl:
                for ev in b.revs.values():
                    if ev[2] != e:
                        add(ev)
        for b in w:
            if b.wev is not None:
                add(b.wev)
            for ev in b.revs.values():
                if ev[2] == e:
                    continue
                add(ev)
        seen = self.seen[e]
        for k, (s, v) in need.items():
            if seen.get(k, 0) >= v:
                continue
            self.eng[e].wait_ge(s, v)
            self.nwait += 1
            seen[k] = v

    def _record(self, ev, r, w):
        k = id(ev[0])
        for b in r:
            b.revs[k] = ev
        for b in w:
            b.wev = ev
            b.revs = {}

    def op(self, e, fn, r=(), w=()):
        self._waits(e, r, w)
        ins = fn(self.eng[e])
        self.ecnt[e] += 1
        ins.then_inc(self.esem[e], 1)
        self.ninst += 1
        self._record((self.esem[e], self.ecnt[e], e), r, w)
        return ins

    def dma(self, e, fn, sb, r=(), w=()):
        self._waits(e, r, w)
        slot = self._slot(sb)
        ins = fn(self.eng[e])
        slot[1] += 16
        ins.then_inc(slot[0], 16)
        self.ninst += 1
        self._record((slot[0], slot[1], "dma"), r, w)
        return ins

    def end_phase(self):
        for b in self.phase_bufs:
            if b.slot is not None:
                s, v = b.slot
                if self.seen["sp"].get(id(s), 0) < v:
                    self.eng["sp"].wait_ge(s, v)
                    self.seen["sp"][id(s)] = v
        self.ecnt["sp"] += 1
        self.eng["sp"].nop().then_inc(self.esem["sp"], 1)
        for e in self.eng:
            for f in self.eng:
                if f == e:
                    continue
                s, v = self.esem[f], self.ecnt[f]
                if v > 0 and self.seen[e].get(id(s), 0) < v:
                    self.eng[e].wait_ge(s, v)
                    self.seen[e][id(s)] = v
        for b in self.phase_bufs:
            if b.slot is not None:
                self.free_slots.append(b.slot)
                b.slot = None
        self.phase_bufs = []


class Ctx:
    pass


def alibi_slopes():
    return np.array([2.0 ** (-8.0 * (h + 1) / H) for h in range(H)], np.float64)


def host_tables(S_len, r):
    NB = S_len // 256
    NBo = NB // 2
    NBP = max(NB, 8)
    sl = alibi_slopes()
    seqblk = np.zeros(NB, np.int64)
    for i in range(NBo):
        seqblk[2 * i] = 2 * i + r
        seqblk[2 * i + 1] = 2 * i + 1 - r
    j = np.arange(256)
    vtab = np.exp(-sl[None, None, :] * (255 - (np.arange(2)[None, :, None] * 128 + np.arange(128)[:, None, None])))
    mtab = np.zeros((NBo, 2, 128, H, NBP), np.float64)
    gbias = np.full((NBo, 128, NBP), NEG, np.float64)
    for i in range(NBo):
        own = seqblk[2 * i]
        for s in range(NB):
            if seqblk[s] < own:
                gbias[i, :, s] = 0.0
                for t in range(2):
                    qpos = own * 256 + t * 128 + np.arange(128)
                    dist = qpos - (seqblk[s] * 256 + 255)
                    mtab[i, t, :, :, s] = np.exp(-sl[None, :] * dist[:, None])
    kj = np.arange(128)[:, None]
    qi = np.arange(128)[None, :]
    ctab = np.zeros((128, H, 2, 128), np.float64)
    for h in range(H):
        ctab[:, h, 0, :] = np.where(qi >= kj, np.exp(-sl[h] * np.maximum(qi - kj, 0)), 0.0)
        ctab[:, h, 1, :] = np.exp(-sl[h] * (128 + qi - kj))
    wins = np.array([2, 4, 8, 16])
    t = seqblk[0] * 256 + np.arange(256)
    cnt = np.minimum(t[None, :] + 1, wins[:, None]).astype(np.float64)
    ptab = np.broadcast_to((1.0 / cnt)[None], (128, 4, 256))
    halo = np.zeros((128, 2), np.float64)
    halo[:, 0] = 1.0 if r == 0 else 0.0
    halo[:, 1] = 0.0 if r == 0 else 1.0
    f = lambda a: np.ascontiguousarray(a, dtype=np.float32)
    lt = (np.arange(128)[:, None] < np.arange(128)[None, :]).astype(np.float32)
    return dict(ident=np.eye(128, dtype=np.float32), vtab=f(vtab), mtab=f(mtab), gbias=f(gbias),
                ctab=f(ctab), ptab=f(ptab), halo=f(halo), ltri=lt,
                bpos=f((np.arange(128) * 128.0).reshape(128, 1)),
                pidx=f(np.arange(128).reshape(128, 1)))


def build(S_len, debug=False, upto="E"):
    nc = bass.Bass("TRN2", target_bir_lowering=False)
    NB = S_len // 256
    NBo = NB // 2
    NBP = max(NB, 8)
    To = S_len // 2
    NTo = To // 128
    NT = S_len // 128
    PL = 2 * To + NE * 128
    NBLK = PL // 128
    skind = "ExternalOutput" if debug else "Internal"

    def din(name, shape, dt=F32):
        return nc.dram_tensor(name, list(shape), dt, kind="ExternalInput").ap()

    def dscr(name, shape, dt):
        return nc.dram_tensor(name, list(shape), dt, kind=skind).ap()

    c = Ctx()
    c.nc = nc
    in_shapes = dict(
        x_perm=[S_len, D], p_own=[To, PLE], g_mix=[1, D], w_in=[D, INW], beta_attn=[1, AW],
        w_pool=[4, 256, 256], pool_scale=[1, PW], w_out=[D, D], g_ffn=[1, D], w_rg=[D, NG], b_rg=[1, NG],
        w_re=[NG, D, 8], b_re=[1, NE], w_eg=[NE, D, DE], w_eu=[NE, D, DE], w_ed=[NE, DE, D], g_ple=[1, D],
        w_ple=[PLE, D], w_pg=[D, D], b_pg=[1, D], g_final=[1, D], ident=[128, 128], vtab=[128, 2, H],
        mtab=[NBo, 2, 128, H, NBP], gbias=[NBo, 128, NBP], ctab=[128, H, 2, 128], ptab=[128, 4, 256],
        halo=[128, 2], ltri=[128, 128], bpos=[128, 1], pidx=[128, 1])
    c.used = {}

    def IN(name):
        if name not in c.used:
            c.used[name] = din(name, in_shapes[name])
        return c.used[name]
    out = nc.dram_tensor("out", [To, D], F32, kind="ExternalOutput").ap()
    KT_d = dscr("KT_d", [H, 128, S_len], BF16)
    VP_d = dscr("VP_d", [H, 128, NT, 129], BF16)
    V1_d = dscr("V1_d", [H, 128, NTo, 129], BF16)
    QT_d = dscr("QT_d", [H, 128, To], BF16)
    UT_d = dscr("UT_d", [8, 128, NBo, 272], F32)
    KM_d = dscr("KM_d", [128, H, NBP], F32)
    OA_d = dscr("OA_d", [To, AW], F32)
    MP_d = dscr("MP_d", [To, PW], BF16)
    H1_d = dscr("H1_d", [To, D], F32)
    F_d = dscr("F_d", [To, D], BF16)
    SLOT_d = dscr("SLOT_d", [PL, 2], F32)
    Y_d = dscr("Y_d", [PL, D], F32)
    RT_d = dscr("RT_d", [To, 8], F32)

    S = Sched(nc)
    c.S = S
    import contextlib
    c.stack = contextlib.ExitStack()

    def sb(name, shape, dt):
        t = c.stack.enter_context(nc.sbuf_tensor(name, list(shape), dt))
        return t.ap() if hasattr(t, "ap") else t[:]
    pb = [nc.alloc_psum_tensor("pb%d" % i, [128, 512], F32).ap() for i in range(8)]
    PB = S.bufs("pbank", 8, excl=True)

    ident = nc.alloc_sbuf_tensor("identb", [128, 128], BF16).ap()
    B_ident = Buf("ident")
    S.dma("pool", lambda e: e.dma_start(out=ident, in_=IN('ident')), B_ident, w=[B_ident])

    def rmsnorm_rstd(e_xt, B_xt, junk, B_junk, st, B_st, width):
        S.op("act", lambda e: e.activation(out=junk, in_=e_xt, func=AF.Square, accum_out=st[:, 0:1]),
             r=[B_xt], w=[B_junk, B_st])
        S.op("act", lambda e: e.activation(out=st[:, 1:2], in_=st[:, 0:1], func=AF.Sqrt, scale=1.0 / width, bias=EPS),
             r=[B_st], w=[B_st])
        S.op("dve", lambda e: e.reciprocal(out=st[:, 2:3], in_=st[:, 1:2]), r=[B_st], w=[B_st])

    def transpose_to(src, B_src, nchunk, dst_fn, B_dst, tps, step=1):
        for g0 in range(0, nchunk, 8):
            n = min(8, nchunk - g0)
            bi = tps[(g0 // 8) % len(tps)]
            tpv = pb[bi].bitcast(BF16).rearrange("p (k n) -> p k n", n=128)
            for k in range(g0, g0 + n):
                if step == 1:
                    sv = src[:, k * 128:(k + 1) * 128]
                else:
                    sv = src[:, k:k + 127 * step + 1:step]
                S.op("pe", lambda e: e.transpose(tpv[:, k - g0, :], sv, ident), r=[B_src, B_ident], w=[PB[bi]])
            eng = "act" if (g0 // 8) % 2 == 0 else "dve"
            if eng == "act":
                S.op("act", lambda e: e.copy(out=dst_fn(g0, g0 + n), in_=tpv[:, 0:n, :]), r=[PB[bi]], w=[B_dst])
            else:
                S.op("dve", lambda e: e.tensor_copy(out=dst_fn(g0, g0 + n), in_=tpv[:, 0:n, :]), r=[PB[bi]], w=[B_dst])

    def phase_A():
        Wb = sb("A_Wb", [128, 16, INW], BF16)
        B_W = S.buf("A_Wb")
        for k in range(16):
            S.dma("pool", lambda e: e.dma_start(out=Wb[:, k, :], in_=IN('w_in')[k * 128:(k + 1) * 128, :]), B_W, w=[B_W])
        gmix = sb("A_gmix", [128, D], F32)
        B_g = S.buf("A_gmix")
        S.dma("sp", lambda e: e.dma_start(out=gmix, in_=IN('g_mix').partition_broadcast(128)), B_g, w=[B_g])
        vtab = sb("A_vtab", [128, 2, H], F32)
        B_vt = S.buf("A_vtab")
        S.dma("sp", lambda e: e.dma_start(out=vtab, in_=IN('vtab')), B_vt, w=[B_vt])
        xb = [sb("A_x%d" % i, [128, D], F32) for i in range(2)]
        B_x = S.bufs("A_x", 2)
        ab = [sb("A_a%d" % i, [128, D], BF16) for i in range(4)]
        B_a = S.bufs("A_a", 4)
        stt = [sb("A_st%d" % i, [128, 4], F32) for i in range(2)]
        B_st = S.bufs("A_st", 2)
        aT = sb("A_aT", [128, 16, 512], BF16)
        B_aT = S.buf("A_aT")
        NSTG = 3
        fst = [sb("A_fs%d" % i, [128, 512], BF16) for i in range(NSTG)]
        B_fs = S.bufs("A_fs", NSTG)
        ust = [sb("A_us%d" % i, [128, 272], F32) for i in range(2)]
        B_us = S.bufs("A_us", 2)
        vp = [sb("A_vp%d" % i, [128, H, 129], BF16) for i in range(2)]
        B_vp = S.bufs("A_vp", 2)
        v1 = [sb("A_v1%d" % i, [128, H, 129], BF16) for i in range(2)]
        B_v1 = S.bufs("A_v1", 2)
        km = sb("A_km", [128, H, NBP], F32)
        B_km = S.buf("A_km")
        kms = sb("A_kms", [128, 2], F32)
        B_kms = S.buf("A_kms")
        S.op("dve", lambda e: e.memset(km, 0.0), w=[B_km])
        for i in range(2):
            S.op("dve", lambda e: e.memset(v1[i][:, :, 128:129], 1.0), w=[B_v1[i]])
        mmb = [2, 3, 4, 5, 6, 7]
        mmi = [0]

        def nextbank():
            b = mmb[mmi[0] % len(mmb)]
            mmi[0] += 1
            return b

        fsi = [0]
        tcount = [0]

        def norm_tile(i, t):
            j = tcount[0] % 2
            tcount[0] += 1
            row0 = (i * 4 + t) * 128
            S.dma("sp", lambda e: e.dma_start(out=xb[j], in_=IN('x_perm')[row0:row0 + 128, :]), B_x[j], w=[B_x[j]])
            rmsnorm_rstd(xb[j], B_x[j], ab[t], B_a[t], stt[j], B_st[j], D)
            S.op("dve", lambda e: e.scalar_tensor_tensor(out=ab[t], in0=xb[j], scalar=stt[j][:, 2:3], in1=gmix,
                                                         op0=ALU.mult, op1=ALU.mult),
                 r=[B_x[j], B_st[j], B_g], w=[B_a[t]])

        def transposes(i):
            for t in range(4):
                transpose_to(ab[t], B_a[t], 16, lambda k0, k1: aT[:, k0:k1, t * 128:(t + 1) * 128], B_aT, [0, 1])

        for t in range(4):
            norm_tile(0, t)
        transposes(0)
        for i in range(NBo):
            for ch in range(H if 'k' not in SKIP else 0):
                bk = nextbank()
                for k in range(16):
                    S.op("pe", lambda e: e.matmul(pb[bk], lhsT=Wb[:, k, AW + ch * 128:AW + (ch + 1) * 128], rhs=aT[:, k, :],
                                                  start=(k == 0), stop=(k == 15)), r=[B_W, B_aT], w=[PB[bk]])
                f = fsi[0] % NSTG
                fsi[0] += 1
                S.op("act", lambda e: e.copy(out=fst[f], in_=pb[bk]), r=[PB[bk]], w=[B_fs[f]])
                S.op("dve", lambda e: e.reduce_sum(out=kms, in_=pb[bk].rearrange("p (b n) -> p b n", n=256), axis=AX.X),
                     r=[PB[bk], B_fs[f]], w=[B_kms])
                S.op("dve", lambda e: e.tensor_scalar(out=km[:, ch, 2 * i:2 * i + 2], in0=kms, scalar1=1.0 / 256, scalar2=None,
                                                      op0=ALU.mult), r=[B_kms], w=[B_km])
                S.dma("sp", lambda e: e.dma_start(out=KT_d[ch, :, i * 512:(i + 1) * 512], in_=fst[f]), B_fs[f], r=[B_fs[f]])
            if i + 1 < NBo:
                norm_tile(i + 1, 0)
            for ch in range(H if 'q' not in SKIP else 0):
                bk = nextbank()
                for k in range(16):
                    S.op("pe", lambda e: e.matmul(pb[bk][:, 0:256], lhsT=Wb[:, k, ch * 128:(ch + 1) * 128], rhs=aT[:, k, 0:256],
                                                  start=(k == 0), stop=(k == 15)), r=[B_W, B_aT], w=[PB[bk]])
                f = fsi[0] % NSTG
                fsi[0] += 1
                S.op("act", lambda e: e.copy(out=fst[f][:, 0:256], in_=pb[bk][:, 0:256]), r=[PB[bk]], w=[B_fs[f]])
                S.dma("sp", lambda e: e.dma_start(out=QT_d[ch, :, i * 256:(i + 1) * 256], in_=fst[f][:, 0:256]), B_fs[f], r=[B_fs[f]])
            if i + 1 < NBo:
                norm_tile(i + 1, 1)
            for ch in range(8 if 'u' not in SKIP else 0):
                bk = nextbank()
                for k in range(16):
                    S.op("pe", lambda e: e.matmul(pb[bk], lhsT=Wb[:, k, 3 * AW + ch * 128:3 * AW + (ch + 1) * 128], rhs=aT[:, k, :],
                                                  start=(k == 0), stop=(k == 15)), r=[B_W, B_aT], w=[PB[bk]])
                f = ch % 2
                S.op("act", lambda e: e.copy(out=ust[f][:, 0:256], in_=pb[bk][:, 0:256]), r=[PB[bk]], w=[B_us[f]])
                S.op("dve", lambda e: e.tensor_copy(out=ust[f][:, 256:272], in_=pb[bk][:, 496:512]), r=[PB[bk]], w=[B_us[f]])
                S.dma("sp", lambda e: e.dma_start(out=UT_d[ch, :, i, :], in_=ust[f]), B_us[f], r=[B_us[f]])
            if i + 1 < NBo:
                norm_tile(i + 1, 2)
            for t in range(4 if 'v' not in SKIP else 0):
                par = t % 2
                tile_g = i * 4 + t
                jj = tile_g % 2
                for half in range(2):
                    bk = nextbank()
                    for k in range(16):
                        S.op("pe", lambda e: e.matmul(pb[bk], lhsT=aT[:, k, t * 128:(t + 1) * 128],
                                                      rhs=Wb[:, k, 2 * AW + half * 512:2 * AW + (half + 1) * 512],
                                                      start=(k == 0), stop=(k == 15)), r=[B_W, B_aT], w=[PB[bk]])
                    pv = pb[bk].rearrange("p (h n) -> p h n", n=128)
                    hs = slice(half * 4, (half + 1) * 4)
                    S.op("dve", lambda e: e.tensor_tensor(out=vp[jj][:, hs, 0:128], in0=pv,
                                                          in1=vtab[:, par, hs].unsqueeze(2).to_broadcast([128, 4, 128]),
                                                          op=ALU.mult), r=[PB[bk], B_vt], w=[B_vp[jj]])
                    if t < 2:
                        S.op("act", lambda e: e.copy(out=v1[jj][:, hs, 0:128], in_=pv), r=[PB[bk], B_vp[jj]], w=[B_v1[jj]])
                S.op("dve", lambda e: e.tensor_copy(out=vp[jj][:, :, 128:129], in_=vtab[:, par, :].unsqueeze(2)),
                     r=[B_vt], w=[B_vp[jj]])
                S.dma("sp", lambda e: e.dma_start(out=VP_d[:, :, tile_g, :].rearrange("h p c -> p h c"), in_=vp[jj]),
                      B_vp[jj], r=[B_vp[jj]])
                if t < 2:
                    S.dma("sp", lambda e: e.dma_start(out=V1_d[:, :, i * 2 + t, :].rearrange("h p c -> p h c"), in_=v1[jj]),
                          B_v1[jj], r=[B_v1[jj]])
            if i + 1 < NBo:
                norm_tile(i + 1, 3)
                transposes(i + 1)
        S.dma("sp", lambda e: e.dma_start(out=KM_d, in_=km), B_km, r=[B_km])
        S.end_phase()


    def phase_A2():
        wpb = sb("P_wp", [128, 4, 2, 256], BF16)
        B_wp = S.buf("P_wp")
        S.dma("pool", lambda e: e.dma_start(out=wpb, in_=IN('w_pool').rearrange("g (cc p) d -> p g cc d", p=128)), B_wp, w=[B_wp])
        psc = sb("P_psc", [128, PW], F32)
        B_psc = S.buf("P_psc")
        S.dma("sp", lambda e: e.dma_start(out=psc, in_=IN('pool_scale').partition_broadcast(128)), B_psc, w=[B_psc])
        ptab = sb("P_ptab", [128, 4, 256], F32)
        B_pt = S.buf("P_ptab")
        S.dma("sp", lambda e: e.dma_start(out=ptab, in_=IN('ptab')), B_pt, w=[B_pt])
        halo = sb("P_halo", [128, 2], F32)
        B_ha = S.buf("P_halo")
        S.dma("sp", lambda e: e.dma_start(out=halo, in_=IN('halo')), B_ha, w=[B_ha])
        ub = [sb("P_ub%d" % i, [128, 8, 272], F32) for i in range(2)]
        B_ub = S.bufs("P_ub", 2)
        tl = [sb("P_tl%d" % i, [128, 8, 32], F32) for i in range(2)]
        B_tl = S.bufs("P_tl", 2)
        Pb = sb("P_P", [128, 8, 272], F32)
        B_P = S.buf("P_P")
        Qb = sb("P_Q", [128, 8, 272], F32)
        B_Q = S.buf("P_Q")
        zb = sb("P_z", [128, 8, 256], BF16)
        B_z = S.buf("P_z")
        zt = sb("P_zt", [128, 2, 256], F32)
        B_zt = S.buf("P_zt")
        mp = [sb("P_mp%d" % i, [128, PW], BF16) for i in range(2)]
        B_mp = S.bufs("P_mp", 2)
        tmpf = sb("P_tmpf", [128, PW], F32)
        B_tmpf = S.buf("P_tmpf")
        st = [sb("P_st%d" % i, [128, 8], F32) for i in range(2)]
        B_st = S.bufs("P_st", 2)
        wins = [2, 4, 8, 16]
        UTv = UT_d.rearrange("c p i n -> p c i n")
        for i in range(NBo):
            j = i % 2
            A = ub[j]
            S.dma("sp", lambda e: e.dma_start(out=A[:, :, 16:272], in_=UTv[:, :, i, 0:256]), B_ub[j], w=[B_ub[j]])
            S.dma("sp", lambda e: e.dma_start(out=tl[j][:, :, 16:32], in_=UTv[:, :, i, 256:272]), B_tl[j], w=[B_tl[j]])
            if i > 0:
                S.dma("sp", lambda e: e.dma_start(out=tl[j][:, :, 0:16], in_=UTv[:, :, i - 1, 256:272]), B_tl[j], w=[B_tl[j]])
            else:
                S.op("dve", lambda e: e.memset(tl[j][:, :, 0:16], 0.0), w=[B_tl[j]])
            S.op("dve", lambda e: e.tensor_scalar(out=A[:, :, 0:16], in0=tl[j][:, :, 0:16], scalar1=halo[:, 0:1], scalar2=None, op0=ALU.mult),
                 r=[B_tl[j], B_ha], w=[B_ub[j]])
            S.op("dve", lambda e: e.scalar_tensor_tensor(out=A[:, :, 0:16], in0=tl[j][:, :, 16:32], scalar=halo[:, 1:2], in1=A[:, :, 0:16],
                                                         op0=ALU.mult, op1=ALU.add), r=[B_tl[j], B_ha, B_ub[j]], w=[B_ub[j]])
            S.op("dve", lambda e: e.tensor_tensor(out=Pb[:, :, 1:272], in0=A[:, :, 1:272], in1=A[:, :, 0:271], op=ALU.add),
                 r=[B_ub[j]], w=[B_P])
            S.op("dve", lambda e: e.tensor_tensor(out=Qb[:, 2:8, 3:272], in0=Pb[:, 2:8, 3:272], in1=Pb[:, 2:8, 1:270], op=ALU.add),
                 r=[B_P], w=[B_Q])
            S.op("dve", lambda e: e.tensor_tensor(out=Pb[:, 4:8, 7:272], in0=Qb[:, 4:8, 7:272], in1=Qb[:, 4:8, 3:268], op=ALU.add),
                 r=[B_Q], w=[B_P])
            S.op("dve", lambda e: e.tensor_tensor(out=Qb[:, 6:8, 15:272], in0=Pb[:, 6:8, 15:272], in1=Pb[:, 6:8, 7:264], op=ALU.add),
                 r=[B_P], w=[B_Q])
            for g in range(4):
                W = Pb if g % 2 == 0 else Qb
                B_Wb = B_P if g % 2 == 0 else B_Q
                cs = slice(2 * g, 2 * g + 2)
                if i == 0:
                    S.op("dve", lambda e: e.tensor_tensor(out=zt, in0=W[:, cs, 16:272],
                                                          in1=ptab[:, g, :].unsqueeze(1).to_broadcast([128, 2, 256]), op=ALU.mult),
                         r=[B_Wb, B_pt], w=[B_zt])
                    S.op("dve", lambda e: e.tensor_tensor(out=zb[:, cs, :], in0=zt, in1=A[:, cs, 16:272], op=ALU.subtract),
                         r=[B_zt, B_ub[j]], w=[B_z])
                else:
                    S.op("dve", lambda e: e.scalar_tensor_tensor(out=zb[:, cs, :], in0=W[:, cs, 16:272], scalar=1.0 / wins[g],
                                                                 in1=A[:, cs, 16:272], op0=ALU.mult, op1=ALU.subtract),
                         r=[B_Wb, B_ub[j]], w=[B_z])
            for t in range(2):
                jt = (i * 2 + t) % 2
                banks = [2 + 2 * jt, 3 + 2 * jt]
                for g in range(4):
                    bk = banks[g // 2]
                    for cc in range(2):
                        S.op("pe", lambda e: e.matmul(pb[bk][:, (g % 2) * 256:(g % 2 + 1) * 256], lhsT=zb[:, 2 * g + cc, t * 128:(t + 1) * 128],
                                                      rhs=wpb[:, g, cc, :], start=(cc == 0), stop=(cc == 1)),
                             r=[B_z, B_wp], w=[PB[bk]])
                S.op("act", lambda e: e.activation(out=tmpf[:, 0:512], in_=pb[banks[0]], func=AF.Square, accum_out=st[jt][:, 0:1]),
                     r=[PB[banks[0]]], w=[B_tmpf, B_st[jt]])
                S.op("act", lambda e: e.activation(out=tmpf[:, 512:1024], in_=pb[banks[1]], func=AF.Square, accum_out=st[jt][:, 3:4]),
                     r=[PB[banks[1]]], w=[B_tmpf, B_st[jt]])
                S.op("dve", lambda e: e.tensor_tensor(out=st[jt][:, 0:1], in0=st[jt][:, 0:1], in1=st[jt][:, 3:4], op=ALU.add),
                     r=[B_st[jt]], w=[B_st[jt]])
                S.op("act", lambda e: e.activation(out=st[jt][:, 1:2], in_=st[jt][:, 0:1], func=AF.Sqrt, scale=1.0 / PW, bias=EPS),
                     r=[B_st[jt]], w=[B_st[jt]])
                S.op("dve", lambda e: e.reciprocal(out=st[jt][:, 2:3], in_=st[jt][:, 1:2]), r=[B_st[jt]], w=[B_st[jt]])
                for hh in range(2):
                    S.op("dve", lambda e: e.scalar_tensor_tensor(out=mp[jt][:, hh * 512:(hh + 1) * 512], in0=pb[banks[hh]], scalar=st[jt][:, 2:3],
                                                                 in1=psc[:, hh * 512:(hh + 1) * 512], op0=ALU.mult, op1=ALU.mult),
                         r=[PB[banks[hh]], B_st[jt], B_psc], w=[B_mp[jt]])
                row0 = (i * 2 + t) * 128
                S.dma("sp", lambda e: e.dma_start(out=MP_d[row0:row0 + 128, :], in_=mp[jt]), B_mp[jt], r=[B_mp[jt]])
        S.end_phase()

    def phase_B():
        scale = HD ** -0.5
        kmf = sb("B_kmf", [128, H, NBP], F32)
        B_kmf = S.buf("B_kmf")
        S.dma("sp", lambda e: e.dma_start(out=kmf, in_=KM_d), B_kmf, w=[B_kmf])
        kmb = sb("B_kmb", [128, H, NBP], BF16)
        B_kmb = S.buf("B_kmb")
        S.op("dve", lambda e: e.tensor_copy(out=kmb, in_=kmf), r=[B_kmf], w=[B_kmb])
        gbias = sb("B_gbias", [128, NBo, NBP], F32)
        B_gb = S.buf("B_gbias")
        S.dma("sp", lambda e: e.dma_start(out=gbias, in_=IN('gbias').rearrange("i p s -> p i s")), B_gb, w=[B_gb])
        mtab = sb("B_mtab", [128, NBo, 2, H, NBP], F32)
        B_mt = S.buf("B_mtab")
        for i in range(NBo):
            S.dma("sp", lambda e: e.dma_start(out=mtab[:, i], in_=IN('mtab')[i].rearrange("t p h s -> p t h s")), B_mt, w=[B_mt])
        ctab = sb("B_ctab", [128, H, 2, 128], F32)
        B_ct = S.buf("B_ctab")
        S.dma("sp", lambda e: e.dma_start(out=ctab, in_=IN('ctab')), B_ct, w=[B_ct])
        KT = [sb("B_KT%d" % i, [128, S_len], BF16) for i in range(2)]
        VP = [sb("B_VP%d" % i, [128, NT, 129], BF16) for i in range(2)]
        QT = [sb("B_QT%d" % i, [128, To], BF16) for i in range(2)]
        V1 = [sb("B_V1%d" % i, [128, NTo, 129], BF16) for i in range(2)]
        B_KT = S.bufs("B_KT", 2)
        B_VP = S.bufs("B_VP", 2)
        B_QT = S.bufs("B_QT", 2)
        B_V1 = S.bufs("B_V1", 2)
        gs = sb("B_gs", [128, 2, NBP], F32)
        B_gs = S.buf("B_gs")
        top8 = sb("B_top8", [128, 2, 8], F32)
        B_t8 = S.buf("B_top8")
        sel = sb("B_sel", [128, 2, NBP], F32)
        B_sel = S.buf("B_sel")
        mm = [sb("B_m%d" % i, [128, 2, NBP], F32) for i in range(2)]
        B_m = S.bufs("B_m", 2)
        pT = [sb("B_pT%d" % i, [128, 512], BF16) for i in range(2)]
        B_pT = S.bufs("B_pT", 2)
        acc = [sb("B_acc%d" % i, [128, 2, 129], F32) for i in range(2)]
        B_acc = [S.bufs("B_acc%d_" % i, 2) for i in range(2)]
        rec = sb("B_rec", [128, 2], F32)
        B_rec = S.buf("B_rec")
        oa = [sb("B_oa%d" % i, [128, 2, 128], F32) for i in range(2)]
        B_oa = S.bufs("B_oa", 2)
        itc = [0]

        def load_head(h):
            j = h % 2
            S.dma("sp", lambda e: e.dma_start(out=KT[j], in_=KT_d[h]), B_KT[j], w=[B_KT[j]])
            S.dma("sp", lambda e: e.dma_start(out=QT[j], in_=QT_d[h]), B_QT[j], w=[B_QT[j]])
            S.dma("sp", lambda e: e.dma_start(out=VP[j], in_=VP_d[h]), B_VP[j], w=[B_VP[j]])
            S.dma("sp", lambda e: e.dma_start(out=V1[j], in_=V1_d[h]), B_V1[j], w=[B_V1[j]])

        items = []
        for h in range(H):
            for i in range(NBo):
                a = (h * NBo + i) % 2
                cands = list(range(2 * i)) + [2 * i + 1]
                items.append(dict(h=h, i=i, a=a, own=True, s=2 * i, last=False))
                for ci, s_ in enumerate(cands):
                    items.append(dict(h=h, i=i, a=a, own=False, s=s_, last=(ci == len(cands) - 1)))

        def emit_S(k):
            it = items[k]
            h, i, j = it["h"], it["i"], it["h"] % 2
            sbk = 1 + k % 2
            q0 = i * 256
            if it["own"]:
                for t in range(2):
                    S.op("pe", lambda e: e.matmul(pb[0][:, t * 256:t * 256 + NBP], lhsT=QT[j][:, q0 + t * 128:q0 + (t + 1) * 128],
                                                  rhs=kmb[:, h, :], start=True, stop=True), r=[B_QT[j], B_kmb], w=[PB[0]])
                S.op("pe", lambda e: e.matmul(pb[sbk][:, 0:256], lhsT=KT[j][:, (2 * i) * 256:(2 * i) * 256 + 128], rhs=QT[j][:, q0:q0 + 256],
                                              start=True, stop=True), r=[B_KT[j], B_QT[j]], w=[PB[sbk]])
                S.op("pe", lambda e: e.matmul(pb[sbk][:, 384:512], lhsT=KT[j][:, (2 * i) * 256 + 128:(2 * i) * 256 + 256],
                                              rhs=QT[j][:, q0 + 128:q0 + 256], start=True, stop=True), r=[B_KT[j], B_QT[j]], w=[PB[sbk]])
            else:
                s_ = it["s"]
                for kh in range(2):
                    S.op("pe", lambda e: e.matmul(pb[sbk][:, kh * 256:(kh + 1) * 256], lhsT=KT[j][:, s_ * 256 + kh * 128:s_ * 256 + (kh + 1) * 128],
                                                  rhs=QT[j][:, q0:q0 + 256], start=True, stop=True), r=[B_KT[j], B_QT[j]], w=[PB[sbk]])

        def emit_exp(k):
            it = items[k]
            h = it["h"]
            sbk = 1 + k % 2
            pt = pT[k % 2]
            B_pt_ = B_pT[k % 2]
            if it["own"]:
                S.op("act", lambda e: e.activation(out=pt[:, 0:256], in_=pb[sbk][:, 0:256], func=AF.Exp, scale=scale), r=[PB[sbk]], w=[B_pt_])
                S.op("act", lambda e: e.activation(out=pt[:, 384:512], in_=pb[sbk][:, 384:512], func=AF.Exp, scale=scale), r=[PB[sbk]], w=[B_pt_])
                S.op("dve", lambda e: e.tensor_tensor(out=pt[:, 0:256], in0=pt[:, 0:256], in1=ctab[:, h, :, :].rearrange("p a b -> p (a b)"), op=ALU.mult),
                     r=[B_pt_, B_ct], w=[B_pt_])
                S.op("dve", lambda e: e.tensor_tensor(out=pt[:, 384:512], in0=pt[:, 384:512], in1=ctab[:, h, 0, :], op=ALU.mult),
                     r=[B_pt_, B_ct], w=[B_pt_])
            else:
                S.op("act", lambda e: e.activation(out=pt, in_=pb[sbk], func=AF.Exp, scale=scale), r=[PB[sbk]], w=[B_pt_])

        def emit_rest(k):
            it = items[k]
            h, i, a, j = it["h"], it["i"], it["a"], it["h"] % 2
            obk = 3 + k % 2
            pt = pT[k % 2]
            B_pt_ = B_pT[k % 2]
            q0 = i * 256
            if it["own"]:
                if i == 0 and h + 1 < H:
                    load_head(h + 1)
                gv = pb[0].rearrange("p (t n) -> p t n", n=256)[:, :, 0:NBP]
                S.op("dve", lambda e: e.tensor_tensor(out=gs, in0=gv, in1=gbias[:, i, :].unsqueeze(1).to_broadcast([128, 2, NBP]), op=ALU.add),
                     r=[PB[0], B_gb], w=[B_gs])
                for t in range(2):
                    S.op("dve", lambda e: e.max(out=top8[:, t, :], in_=gs[:, t, :]), r=[B_gs], w=[B_t8])
                for t in range(2):
                    S.op("dve", lambda e: e.tensor_scalar(out=sel[:, t, :], in0=gs[:, t, :], scalar1=top8[:, t, 2:3], scalar2=None, op0=ALU.is_ge),
                         r=[B_gs, B_t8], w=[B_sel])
                S.op("dve", lambda e: e.tensor_tensor(out=mm[a], in0=sel, in1=mtab[:, i, :, h, :], op=ALU.mult), r=[B_sel, B_mt], w=[B_m[a]])
                S.op("pe", lambda e: e.matmul(pb[obk][:, 0:129], lhsT=pt[:, 0:128], rhs=V1[j][:, 2 * i, :], start=True, stop=True),
                     r=[B_pt_, B_V1[j]], w=[PB[obk]])
                S.op("pe", lambda e: e.matmul(pb[obk][:, 256:385], lhsT=pt[:, 128:256], rhs=V1[j][:, 2 * i, :], start=True, stop=False),
                     r=[B_pt_, B_V1[j]], w=[PB[obk]])
                S.op("pe", lambda e: e.matmul(pb[obk][:, 256:385], lhsT=pt[:, 384:512], rhs=V1[j][:, 2 * i + 1, :], start=False, stop=True),
                     r=[B_pt_, B_V1[j]], w=[PB[obk]])
                ov = pb[obk].rearrange("p (t n) -> p t n", n=256)[:, :, 0:129]
                S.op("dve", lambda e: e.tensor_copy(out=acc[a], in_=ov), r=[PB[obk]], w=[B_acc[a][0], B_acc[a][1]])
            else:
                s_ = it["s"]
                for t in range(2):
                    for kh in range(2):
                        S.op("pe", lambda e: e.matmul(pb[obk][:, t * 256:t * 256 + 129], lhsT=pt[:, kh * 256 + t * 128:kh * 256 + (t + 1) * 128],
                                                      rhs=VP[j][:, s_ * 2 + kh, :], start=(kh == 0), stop=(kh == 1)),
                             r=[B_pt_, B_VP[j]], w=[PB[obk]])
                for t in range(2):
                    S.op("dve", lambda e: e.scalar_tensor_tensor(out=acc[a][:, t, :], in0=pb[obk][:, t * 256:t * 256 + 129],
                                                                 scalar=mm[a][:, t, s_:s_ + 1], in1=acc[a][:, t, :],
                                                                 op0=ALU.mult, op1=ALU.add), r=[PB[obk], B_m[a], B_acc[a][t]], w=[B_acc[a][t]])
            if it["last"]:
                S.op("dve", lambda e: e.reciprocal(out=rec, in_=acc[a][:, :, 128]), r=[B_acc[a][0], B_acc[a][1]], w=[B_rec])
                for t in range(2):
                    S.op("dve", lambda e: e.tensor_scalar(out=oa[a][:, t, :], in0=acc[a][:, t, 0:128], scalar1=rec[:, t:t + 1], scalar2=None, op0=ALU.mult),
                         r=[B_acc[a][t], B_rec], w=[B_oa[a]])
                S.dma("sp", lambda e: e.dma_start(out=OA_d[q0:q0 + 256, h * 128:(h + 1) * 128].rearrange("(t p) c -> p t c", p=128), in_=oa[a]),
                      B_oa[a], r=[B_oa[a]])

        load_head(0)
        emit_S(0)
        for k in range(len(items)):
            emit_exp(k)
            if k + 1 < len(items):
                emit_S(k + 1)
            emit_rest(k)
        S.end_phase()

    WIDX = nc.alloc_sbuf_tensor("G_widx", [128, 128], I32).ap()
    B_widx = Buf("G_widx")
    DESTI = nc.alloc_sbuf_tensor("G_desti", [128, NTo, 2], I32).ap()
    B_desti = Buf("G_desti")

    def phase_C():
        Wo = sb("C_Wo", [128, 16, D], BF16)
        B_Wo = S.buf("C_Wo")
        for k in range(16):
            S.dma("pool", lambda e: e.dma_start(out=Wo[:, k, :], in_=IN('w_out')[k * 128:(k + 1) * 128, :]), B_Wo, w=[B_Wo])
        beta = sb("C_beta", [128, AW], F32)
        B_beta = S.buf("C_beta")
        S.dma("sp", lambda e: e.dma_start(out=beta, in_=IN('beta_attn').partition_broadcast(128)), B_beta, w=[B_beta])
        gffn = sb("C_gffn", [128, D], F32)
        B_gffn = S.buf("C_gffn")
        S.dma("sp", lambda e: e.dma_start(out=gffn, in_=IN('g_ffn').partition_broadcast(128)), B_gffn, w=[B_gffn])
        Wr32 = sb("C_Wr32", [128, 16, 72], F32)
        B_Wr32 = S.buf("C_Wr32")
        S.dma("sp", lambda e: e.dma_start(out=Wr32[:, :, 0:8], in_=IN('w_rg').rearrange("(k p) e -> p k e", p=128)), B_Wr32, w=[B_Wr32])
        for g in range(NG):
            S.dma("sp", lambda e: e.dma_start(out=Wr32[:, :, 8 + g * 8:16 + g * 8], in_=IN('w_re')[g].rearrange("(k p) e -> p k e", p=128)),
                  B_Wr32, w=[B_Wr32])
        Wr = sb("C_Wr", [128, 16, 72], BF16)
        B_Wr = S.buf("C_Wr")
        S.op("dve", lambda e: e.tensor_copy(out=Wr, in_=Wr32), r=[B_Wr32], w=[B_Wr])
        brb = sb("C_brb", [128, 72], F32)
        B_brb = S.buf("C_brb")
        S.dma("sp", lambda e: e.dma_start(out=brb[:, 0:8], in_=IN('b_rg').partition_broadcast(128)), B_brb, w=[B_brb])
        S.dma("sp", lambda e: e.dma_start(out=brb[:, 8:72], in_=IN('b_re').partition_broadcast(128)), B_brb, w=[B_brb])
        ltb = sb("C_ltb", [128, 128], BF16)
        B_ltb = S.buf("C_ltb")
        S.dma("pool", lambda e: e.dma_start(out=ltb, in_=IN('ltri')), B_ltb, w=[B_ltb])
        onesb = sb("C_ones", [128, 128], BF16)
        B_ones = S.buf("C_ones")
        S.op("dve", lambda e: e.memset(onesb, 1.0), w=[B_ones])
        pidx = sb("C_pidx", [128, 1], F32)
        B_pidx = S.buf("C_pidx")
        S.dma("sp", lambda e: e.dma_start(out=pidx, in_=IN('pidx')), B_pidx, w=[B_pidx])
        bpos = sb("C_bpos", [128, 1], F32)
        B_bpos = S.buf("C_bpos")
        S.dma("sp", lambda e: e.dma_start(out=bpos, in_=IN('bpos')), B_bpos, w=[B_bpos])
        zer = sb("C_zer", [128, (PL // 128) * 2], F32)
        B_zer = S.buf("C_zer")
        S.op("dve", lambda e: e.memset(zer, 0.0), w=[B_zer])
        B_slotd = S.buf("C_slotd")
        S.dma("sp", lambda e: e.dma_start(out=SLOT_d.rearrange("(p n) c -> p (n c)", p=128), in_=zer), B_zer, r=[B_zer], w=[B_slotd])
        OHK = sb("C_OHK", [128, NTo, 2, 64], F32)
        B_OHK = S.buf("C_OHK")
        RK = sb("C_RK", [128, NTo, 2], F32)
        B_RK = S.buf("C_RK")
        WK = sb("C_WK", [128, NTo, 2], F32)
        B_WK = S.buf("C_WK")
        ohacc = sb("C_ohacc", [128, 64], BF16)
        B_ohacc = S.buf("C_ohacc")
        S.op("dve", lambda e: e.memset(ohacc, 0.0), w=[B_ohacc])
        oa = [sb("C_oa%d" % i, [128, AW], F32) for i in range(2)]
        B_oa = S.bufs("C_oa", 2)
        mx = [sb("C_mx%d" % i, [128, D], BF16) for i in range(2)]
        B_mx = S.bufs("C_mx", 2)
        xt = [sb("C_x%d" % i, [128, D], F32) for i in range(2)]
        B_xt = S.bufs("C_x", 2)
        mTs = [sb("C_mT%d" % i, [128, 16, 128], BF16) for i in range(2)]
        B_mTs = S.bufs("C_mT", 2)
        h1 = [sb("C_h1%d" % i, [128, D], F32) for i in range(2)]
        B_h1 = S.bufs("C_h1", 2)
        fb = [sb("C_f%d" % i, [128, D], BF16) for i in range(2)]
        B_fb = S.bufs("C_f", 2)
        fT = sb("C_fT", [128, 16, 128], BF16)
        B_fT = S.buf("C_fT")
        st = [sb("C_st%d" % i, [128, 8], F32) for i in range(2)]
        B_st = S.bufs("C_st", 2)
        st2 = [sb("C_su%d" % i, [128, 8], F32) for i in range(2)]
        B_st2 = S.bufs("C_su", 2)
        lg = sb("C_lg", [128, 72], F32)
        B_lg = S.buf("C_lg")
        rs = sb("C_rs", [128, 32], F32)
        B_rs = S.buf("C_rs")
        ohg = sb("C_ohg", [128, 8], F32)
        B_ohg = S.buf("C_ohg")
        tmp88 = sb("C_tmp88", [128, 8, 8], F32)
        B_t88 = S.buf("C_tmp88")
        le = sb("C_le", [128, 8], F32)
        B_le = S.buf("C_le")
        t8 = sb("C_t8", [128, 8], F32)
        B_t8 = S.buf("C_t8")
        ohk = sb("C_ohk", [128, 2, 8], F32)
        B_ohk = S.buf("C_ohk")
        ohs = sb("C_ohs", [128, 64], BF16)
        B_ohs = S.buf("C_ohs")
        cum = sb("C_cum", [128, 64], F32)
        B_cum = S.buf("C_cum")
        tmp64 = sb("C_tmp64", [128, 64], F32)
        B_t64 = S.buf("C_tmp64")

        def load(j):
            b = j % 2
            i, t = j // 2, j % 2
            xr = i * 512 + t * 128
            S.dma("sp", lambda e: e.dma_start(out=oa[b], in_=OA_d[j * 128:(j + 1) * 128, :]), B_oa[b], w=[B_oa[b]])
            S.dma("sp", lambda e: e.dma_start(out=xt[b], in_=IN('x_perm')[xr:xr + 128, :]), B_xt[b], w=[B_xt[b]])
            S.dma("sp", lambda e: e.dma_start(out=mx[b][:, AW:D], in_=MP_d[j * 128:(j + 1) * 128, :]), B_mx[b], w=[B_mx[b]])

        def stageA(j):
            b = j % 2
            mT = mTs[b]
            B_mT = B_mTs[b]
            rmsnorm_rstd(oa[b], B_oa[b], mx[b][:, 0:AW], B_mx[b], st[b], B_st[b], AW)
            S.op("dve", lambda e: e.scalar_tensor_tensor(out=mx[b][:, 0:AW], in0=oa[b], scalar=st[b][:, 2:3], in1=beta, op0=ALU.mult, op1=ALU.mult),
                 r=[B_oa[b], B_st[b], B_beta], w=[B_mx[b]])
            transpose_to(mx[b], B_mx[b], 16, lambda k0, k1: mT[:, k0:k1, :], B_mT, [0, 1])

        load(0)
        if NTo > 1:
            load(1)
        stageA(0)
        for j in range(NTo):
            b = j % 2
            mT = mTs[b]
            B_mT = B_mTs[b]
            if j + 1 < NTo:
                stageA(j + 1)
            for n in range(4):
                bk = 2 + n
                for k in range(16):
                    S.op("pe", lambda e: e.matmul(pb[bk], lhsT=mT[:, k, :], rhs=Wo[:, k, n * 512:(n + 1) * 512], start=(k == 0), stop=(k == 15)),
                         r=[B_mT, B_Wo], w=[PB[bk]])
                S.op("dve", lambda e: e.tensor_tensor(out=h1[b][:, n * 512:(n + 1) * 512], in0=pb[bk], in1=xt[b][:, n * 512:(n + 1) * 512], op=ALU.add),
                     r=[PB[bk], B_xt[b]], w=[B_h1[b]])
            S.dma("sp", lambda e: e.dma_start(out=H1_d[j * 128:(j + 1) * 128, :], in_=h1[b]), B_h1[b], r=[B_h1[b]])
            if j + 2 < NTo:
                load(j + 2)
            rmsnorm_rstd(h1[b], B_h1[b], fb[b], B_fb[b], st2[b], B_st2[b], D)
            S.op("dve", lambda e: e.scalar_tensor_tensor(out=fb[b], in0=h1[b], scalar=st2[b][:, 2:3], in1=gffn, op0=ALU.mult, op1=ALU.mult),
                 r=[B_h1[b], B_st2[b], B_gffn], w=[B_fb[b]])
            S.dma("sp", lambda e: e.dma_start(out=F_d[j * 128:(j + 1) * 128, :], in_=fb[b]), B_fb[b], r=[B_fb[b]])
            transpose_to(fb[b], B_fb[b], 16, lambda k0, k1: fT[:, k0:k1, :], B_fT, [0, 1])
            bk = 6
            for k in range(16):
                S.op("pe", lambda e: e.matmul(pb[bk][:, 0:72], lhsT=fT[:, k, :], rhs=Wr[:, k, :], start=(k == 0), stop=(k == 15)),
                     r=[B_fT, B_Wr], w=[PB[bk]])
            S.op("dve", lambda e: e.tensor_tensor(out=lg, in0=pb[bk][:, 0:72], in1=brb, op=ALU.add), r=[PB[bk], B_brb], w=[B_lg])
            S.op("dve", lambda e: e.reduce_max(out=rs[:, 0:1], in_=lg[:, 0:8], axis=AX.X), r=[B_lg], w=[B_rs])
            S.op("dve", lambda e: e.tensor_scalar(out=ohg, in0=lg[:, 0:8], scalar1=rs[:, 0:1], scalar2=None, op0=ALU.is_equal), r=[B_lg, B_rs], w=[B_ohg])
            S.op("dve", lambda e: e.tensor_scalar(out=rs[:, 1:2], in0=rs[:, 0:1], scalar1=-1.0, scalar2=None, op0=ALU.mult), r=[B_rs], w=[B_rs])
            S.op("act", lambda e: e.activation(out=t8, in_=lg[:, 0:8], func=AF.Exp, bias=rs[:, 1:2], scale=1.0, accum_out=rs[:, 2:3]),
                 r=[B_lg, B_rs], w=[B_t8, B_rs])
            S.op("dve", lambda e: e.reciprocal(out=rs[:, 3:4], in_=rs[:, 2:3]), r=[B_rs], w=[B_rs])
            S.op("dve", lambda e: e.tensor_tensor(out=tmp88, in0=lg[:, 8:72].rearrange("p (g e) -> p g e", e=8),
                                                  in1=ohg.unsqueeze(2).to_broadcast([128, 8, 8]), op=ALU.mult), r=[B_lg, B_ohg], w=[B_t88])
            S.op("dve", lambda e: e.reduce_sum(out=le, in_=tmp88.rearrange("p g e -> p e g"), axis=AX.X), r=[B_t88], w=[B_le])
            S.op("dve", lambda e: e.max(out=t8, in_=le), r=[B_le], w=[B_t8])
            for k2 in range(2):
                S.op("dve", lambda e: e.tensor_scalar(out=ohk[:, k2, :], in0=le, scalar1=t8[:, k2:k2 + 1], scalar2=None, op0=ALU.is_equal),
                     r=[B_le, B_t8], w=[B_ohk])
            S.op("dve", lambda e: e.tensor_tensor(out=rs[:, 4:5], in0=t8[:, 1:2], in1=t8[:, 0:1], op=ALU.subtract), r=[B_t8], w=[B_rs])
            S.op("act", lambda e: e.activation(out=rs[:, 5:6], in_=rs[:, 4:5], func=AF.Exp), r=[B_rs], w=[B_rs])
            S.op("dve", lambda e: e.tensor_scalar(out=rs[:, 6:7], in0=rs[:, 5:6], scalar1=1.0, scalar2=None, op0=ALU.add), r=[B_rs], w=[B_rs])
            S.op("dve", lambda e: e.reciprocal(out=rs[:, 7:8], in_=rs[:, 6:7]), r=[B_rs], w=[B_rs])
            S.op("dve", lambda e: e.tensor_tensor(out=rs[:, 8:9], in0=rs[:, 5:6], in1=rs[:, 7:8], op=ALU.mult), r=[B_rs], w=[B_rs])
            S.op("dve", lambda e: e.tensor_tensor(out=WK[:, j, 0:1], in0=rs[:, 7:8], in1=rs[:, 3:4], op=ALU.mult), r=[B_rs], w=[B_WK])
            S.op("dve", lambda e: e.tensor_tensor(out=WK[:, j, 1:2], in0=rs[:, 8:9], in1=rs[:, 3:4], op=ALU.mult), r=[B_rs], w=[B_WK])
            for k2 in range(2):
                S.op("dve", lambda e: e.tensor_tensor(out=OHK[:, j, k2, :].rearrange("p (g e) -> p g e", e=8),
                                                      in0=ohg.unsqueeze(2).to_broadcast([128, 8, 8]),
                                                      in1=ohk[:, k2, :].unsqueeze(1).to_broadcast([128, 8, 8]), op=ALU.mult),
                     r=[B_ohg, B_ohk], w=[B_OHK])
            S.op("dve", lambda e: e.tensor_tensor(out=ohs, in0=OHK[:, j, 0, :], in1=OHK[:, j, 1, :], op=ALU.add), r=[B_OHK], w=[B_ohs])
            bk = 7
            S.op("pe", lambda e: e.matmul(pb[bk][:, 0:64], lhsT=ltb, rhs=ohs, start=True, stop=(j == 0)), r=[B_ltb, B_ohs], w=[PB[bk]])
            if j > 0:
                S.op("pe", lambda e: e.matmul(pb[bk][:, 0:64], lhsT=onesb, rhs=ohacc, start=False, stop=True), r=[B_ones, B_ohacc], w=[PB[bk]])
            S.op("dve", lambda e: e.tensor_copy(out=cum, in_=pb[bk][:, 0:64]), r=[PB[bk]], w=[B_cum])
            S.op("dve", lambda e: e.tensor_tensor(out=ohacc, in0=ohacc, in1=ohs, op=ALU.add), r=[B_ohacc, B_ohs], w=[B_ohacc])
            for k2 in range(2):
                S.op("dve", lambda e: e.tensor_tensor(out=tmp64, in0=OHK[:, j, k2, :], in1=cum, op=ALU.mult), r=[B_OHK, B_cum], w=[B_t64])
                S.op("dve", lambda e: e.reduce_sum(out=RK[:, j, k2:k2 + 1], in_=tmp64, axis=AX.X), r=[B_t64], w=[B_RK])
        bk = 7
        S.op("pe", lambda e: e.matmul(pb[bk][:, 0:64], lhsT=onesb, rhs=ohacc, start=True, stop=True), r=[B_ones, B_ohacc], w=[PB[bk]])
        cnt = sb("C_cnt", [128, 64], F32)
        B_cnt = S.buf("C_cnt")
        cnti = sb("C_cnti", [128, 64], I32)
        B_cnti = S.buf("C_cnti")
        padf = sb("C_padf", [128, 64], F32)
        B_padf = S.buf("C_padf")
        pend = sb("C_pend", [128, 64], F32)
        B_pend = S.buf("C_pend")
        pstart = sb("C_pstart", [128, 64], F32)
        B_pstart = S.buf("C_pstart")
        ones64 = sb("C_ones64", [128, 64], F32)
        B_o64 = S.buf("C_ones64")
        S.op("dve", lambda e: e.memset(ones64, 1.0), w=[B_o64])
        S.op("dve", lambda e: e.tensor_scalar(out=cnt, in0=pb[bk][:, 0:64], scalar1=127.0, scalar2=None, op0=ALU.add), r=[PB[bk]], w=[B_cnt])
        S.op("dve", lambda e: e.tensor_copy(out=cnti, in_=cnt), r=[B_cnt], w=[B_cnti])
        S.op("dve", lambda e: e.tensor_scalar(out=cnti, in0=cnti, scalar1=7, scalar2=7, op0=ALU.arith_shift_right, op1=ALU.logical_shift_left),
             r=[B_cnti], w=[B_cnti])
        S.op("dve", lambda e: e.tensor_copy(out=padf, in_=cnti), r=[B_cnti], w=[B_padf])
        S.op("dve", lambda e: e.tensor_tensor_scan(out=pend, data0=ones64, data1=padf, initial=0.0, op0=ALU.mult, op1=ALU.add),
             r=[B_o64, B_padf], w=[B_pend])
        S.op("dve", lambda e: e.tensor_tensor(out=pstart, in0=pend, in1=padf, op=ALU.subtract), r=[B_pend, B_padf], w=[B_pstart])
        S.op("dve", lambda e: e.tensor_scalar(out=tmp64, in0=pend, scalar1=bpos[:, 0:1], scalar2=None, op0=ALU.is_le), r=[B_pend, B_bpos], w=[B_t64])
        S.op("dve", lambda e: e.reduce_sum(out=rs[:, 10:11], in_=tmp64, axis=AX.X), r=[B_t64], w=[B_rs])
        S.op("dve", lambda e: e.tensor_scalar(out=rs[:, 11:12], in0=rs[:, 10:11], scalar1=63.0, scalar2=None, op0=ALU.min), r=[B_rs], w=[B_rs])
        S.op("dve", lambda e: e.tensor_scalar(out=rs[:, 13:14], in0=bpos, scalar1=-128.0, scalar2=None, op0=ALU.add), r=[B_bpos], w=[B_rs])
        S.op("dve", lambda e: e.tensor_scalar(out=tmp64, in0=pend, scalar1=rs[:, 13:14], scalar2=None, op0=ALU.is_le), r=[B_pend, B_rs], w=[B_t64])
        S.op("dve", lambda e: e.reduce_sum(out=rs[:, 14:15], in_=tmp64, axis=AX.X), r=[B_t64], w=[B_rs])
        S.op("dve", lambda e: e.tensor_scalar(out=rs[:, 15:16], in0=rs[:, 14:15], scalar1=63.0, scalar2=None, op0=ALU.min), r=[B_rs], w=[B_rs])
        S.op("dve", lambda e: e.tensor_tensor(out=rs[:, 16:17], in0=rs[:, 11:12], in1=rs[:, 15:16], op=ALU.not_equal), r=[B_rs], w=[B_rs])
        S.op("dve", lambda e: e.tensor_scalar(out=rs[:, 17:18], in0=pidx, scalar1=0.0, scalar2=None, op0=ALU.is_equal), r=[B_pidx], w=[B_rs])
        S.op("dve", lambda e: e.tensor_tensor(out=rs[:, 18:19], in0=rs[:, 16:17], in1=rs[:, 17:18], op=ALU.max), r=[B_rs], w=[B_rs])
        S.op("dve", lambda e: e.tensor_scalar(out=rs[:, 19:20], in0=rs[:, 11:12], scalar1=-64.0, scalar2=None, op0=ALU.add), r=[B_rs], w=[B_rs])
        S.op("dve", lambda e: e.tensor_tensor(out=rs[:, 20:21], in0=rs[:, 19:20], in1=rs[:, 18:19], op=ALU.mult), r=[B_rs], w=[B_rs])
        S.op("dve", lambda e: e.tensor_scalar(out=rs[:, 21:22], in0=rs[:, 20:21], scalar1=64.0, scalar2=None, op0=ALU.add), r=[B_rs], w=[B_rs])
        diag = sb("C_diag", [128, 128], BF16)
        B_diag = S.buf("C_diag")
        S.op("dve", lambda e: e.tensor_scalar(out=diag, in0=ident, scalar1=rs[:, 21:22], scalar2=None, op0=ALU.mult), r=[B_ident, B_rs], w=[B_diag])
        bk = 6
        S.op("pe", lambda e: e.matmul(pb[bk][:, 0:128], lhsT=onesb, rhs=diag, start=True, stop=True), r=[B_ones, B_diag], w=[PB[bk]])
        widf = sb("C_widf", [128, 128], F32)
        B_widf = S.buf("C_widf")
        S.op("dve", lambda e: e.tensor_scalar(out=widf, in0=pb[bk][:, 0:128], scalar1=128.0, scalar2=pidx[:, 0:1], op0=ALU.mult, op1=ALU.add),
             r=[PB[bk], B_pidx], w=[B_widf])
        S.op("dve", lambda e: e.tensor_copy(out=WIDX, in_=widf), r=[B_widf], w=[B_widx])
        destf = sb("C_destf", [128, NTo, 2], F32)
        B_destf = S.buf("C_destf")
        slt = [sb("C_slt%d" % i, [128, 2], F32) for i in range(4)]
        B_slt = S.bufs("C_slt", 4)
        for j in range(NTo):
            for k2 in range(2):
                S.op("dve", lambda e: e.tensor_tensor(out=tmp64, in0=OHK[:, j, k2, :], in1=pstart, op=ALU.mult), r=[B_OHK, B_pstart], w=[B_t64])
                S.op("dve", lambda e: e.reduce_sum(out=rs[:, 12:13], in_=tmp64, axis=AX.X), r=[B_t64], w=[B_rs])
                S.op("dve", lambda e: e.tensor_tensor(out=destf[:, j, k2:k2 + 1], in0=rs[:, 12:13], in1=RK[:, j, k2:k2 + 1], op=ALU.add),
                     r=[B_rs, B_RK], w=[B_destf])
        S.op("dve", lambda e: e.tensor_copy(out=DESTI, in_=destf), r=[B_destf], w=[B_desti])
        for j in range(NTo):
            for k2 in range(2):
                q = (j * 2 + k2) % 4
                S.op("dve", lambda e: e.tensor_scalar(out=slt[q][:, 0:1], in0=pidx, scalar1=float(j * 128), scalar2=None, op0=ALU.add),
                     r=[B_pidx], w=[B_slt[q]])
                S.op("dve", lambda e: e.tensor_copy(out=slt[q][:, 1:2], in_=WK[:, j, k2:k2 + 1]), r=[B_WK], w=[B_slt[q]])
                S.dma("pool", lambda e: e.indirect_dma_start(out=SLOT_d, out_offset=bass.IndirectOffsetOnAxis(ap=DESTI[:, j, k2:k2 + 1], axis=0),
                                                             in_=slt[q], in_offset=None),
                      B_slt[q], r=[B_slt[q], B_desti, B_slotd], w=[])
        if debug:
            S.dma("sp", lambda e: e.dma_start(out=RT_d.rearrange("(j p) c -> p j c", p=128)[:, :, 0:2], in_=destf), B_destf, r=[B_destf])
            S.dma("sp", lambda e: e.dma_start(out=RT_d.rearrange("(j p) c -> p j c", p=128)[:, :, 2:4], in_=WK), B_WK, r=[B_WK])
            S.dma("sp", lambda e: e.dma_start(out=RT_d[0:128, 4:6], in_=rs[:, 10:12]), B_rs, r=[B_rs])
        S.end_phase()

    def phase_D():
        wg = sb("D_wg", [128, 16 * DE], BF16)
        wu = sb("D_wu", [128, 16 * DE], BF16)
        wd = sb("D_wd", [128, 4 * D], BF16)
        B_wg = S.buf("D_wg")
        B_wu = S.buf("D_wu")
        B_wd = S.buf("D_wd")
        sl = [sb("D_sl%d" % i, [128, 2], F32) for i in range(2)]
        B_sl = S.bufs("D_sl", 2)
        ti = [sb("D_ti%d" % i, [128, 1], I32) for i in range(2)]
        B_ti = S.bufs("D_ti", 2)
        xg = [sb("D_xg%d" % i, [128, D], BF16) for i in range(2)]
        B_xg = S.bufs("D_xg", 2)
        xT = sb("D_xT", [128, 16, 128], BF16)
        B_xT = S.buf("D_xT")
        sg = sb("D_sg", [128, DE], F32)
        B_sg = S.buf("D_sg")
        hid = sb("D_hid", [128, DE], BF16)
        B_hid = S.buf("D_hid")
        hT = sb("D_hT", [128, 4, 128], BF16)
        B_hT = S.buf("D_hT")
        yb = [sb("D_y%d" % i, [128, D], F32) for i in range(2)]
        B_yb = S.bufs("D_y", 2)
        wgv = IN('w_eg').rearrange("e (p k) n -> (e p) (k n)", k=16)
        wuv = IN('w_eu').rearrange("e (p k) n -> (e p) (k n)", k=16)
        wdv = IN('w_ed').rearrange("e (p k) n -> (e p) (k n)", k=4)

        def load_x(b):
            j = b % 2
            S.dma("sp", lambda e: e.dma_start(out=sl[j], in_=SLOT_d[b * 128:(b + 1) * 128, :]), B_sl[j], w=[B_sl[j]])
            S.op("dve", lambda e: e.tensor_copy(out=ti[j], in_=sl[j][:, 0:1]), r=[B_sl[j]], w=[B_ti[j]])
            S.dma("pool", lambda e: e.indirect_dma_start(out=xg[j], out_offset=None, in_=F_d,
                                                         in_offset=bass.IndirectOffsetOnAxis(ap=ti[j][:, 0:1], axis=0)),
                  B_xg[j], r=[B_ti[j]], w=[B_xg[j]])

        def load_w(b, which):
            for (dst, B_dst, src) in which:
                S.dma("pool", lambda e: e.indirect_dma_start(out=dst, out_offset=None, in_=src,
                                                             in_offset=bass.IndirectOffsetOnAxis(ap=WIDX[:, b:b + 1], axis=0),
                                                             bounds_check=bc_reg, oob_is_err=False),
                      B_dst, r=[B_widx], w=[B_dst])

        bc_reg = nc.gpsimd.alloc_register("bc_reg")
        nc.gpsimd.reg_mov(bc_reg, NE * 128 - 1)
        GU = ((wg, B_wg, wgv), (wu, B_wu, wuv))
        DN = ((wd, B_wd, wdv),)
        load_x(0)
        load_w(0, GU)
        load_w(0, DN)
        for b in range(NBLK):
            j = b % 2
            if b + 1 < NBLK:
                load_x(b + 1)
            transpose_to(xg[j], B_xg[j], 16, lambda k0, k1: xT[:, k0:k1, :], B_xT, [0, 1], step=16)
            for (bk, wt, B_wt) in ((2, wg, B_wg), (3, wu, B_wu)):
                for k in range(16):
                    S.op("pe", lambda e: e.matmul(pb[bk], lhsT=xT[:, k, :], rhs=wt[:, k * DE:(k + 1) * DE], start=(k == 0), stop=(k == 15)),
                         r=[B_xT, B_wt], w=[PB[bk]])
            if b + 1 < NBLK:
                load_w(b + 1, GU)
            S.op("act", lambda e: e.activation(out=sg, in_=pb[2], func=AF.Silu), r=[PB[2]], w=[B_sg])
            S.op("dve", lambda e: e.tensor_tensor(out=hid, in0=pb[3], in1=sg, op=ALU.mult), r=[PB[3], B_sg], w=[B_hid])
            transpose_to(hid, B_hid, 4, lambda k0, k1: hT[:, k0:k1, :], B_hT, [0, 1], step=4)
            for n in range(4):
                bk = 4 + n
                for k in range(4):
                    S.op("pe", lambda e: e.matmul(pb[bk], lhsT=hT[:, k, :], rhs=wd[:, k * D + n * 512:k * D + (n + 1) * 512],
                                                  start=(k == 0), stop=(k == 3)), r=[B_hT, B_wd], w=[PB[bk]])
            if b + 1 < NBLK:
                load_w(b + 1, DN)
            for n in range(4):
                bk = 4 + n
                if n % 2 == 0:
                    S.op("act", lambda e: e.activation(out=yb[j][:, n * 512:(n + 1) * 512], in_=pb[bk], func=AF.Copy, scale=sl[j][:, 1:2]),
                         r=[PB[bk], B_sl[j]], w=[B_yb[j]])
                else:
                    S.op("dve", lambda e: e.tensor_scalar(out=yb[j][:, n * 512:(n + 1) * 512], in0=pb[bk], scalar1=sl[j][:, 1:2], scalar2=None, op0=ALU.mult),
                         r=[PB[bk], B_sl[j]], w=[B_yb[j]])
            S.dma("sp", lambda e: e.dma_start(out=Y_d[b * 128:(b + 1) * 128, :], in_=yb[j]), B_yb[j], r=[B_yb[j]])
        S.end_phase()

    def phase_E():
        Wpg = sb("E_Wpg", [128, 16, D], BF16)
        B_Wpg = S.buf("E_Wpg")
        for k in range(16):
            S.dma("pool", lambda e: e.dma_start(out=Wpg[:, k, :], in_=IN('w_pg')[k * 128:(k + 1) * 128, :]), B_Wpg, w=[B_Wpg])
        Wpl = sb("E_Wpl", [128, 2, D], BF16)
        B_Wpl = S.buf("E_Wpl")
        for k in range(2):
            S.dma("pool", lambda e: e.dma_start(out=Wpl[:, k, :], in_=IN('w_ple')[k * 128:(k + 1) * 128, :]), B_Wpl, w=[B_Wpl])
        gple = sb("E_gple", [128, D], F32)
        bpg = sb("E_bpg", [128, D], F32)
        gfin = sb("E_gfin", [128, D], F32)
        B_gple = S.buf("E_gple")
        B_bpg = S.buf("E_bpg")
        B_gfin = S.buf("E_gfin")
        S.dma("sp", lambda e: e.dma_start(out=gple, in_=IN('g_ple').partition_broadcast(128)), B_gple, w=[B_gple])
        S.dma("sp", lambda e: e.dma_start(out=bpg, in_=IN('b_pg').partition_broadcast(128)), B_bpg, w=[B_bpg])
        S.dma("sp", lambda e: e.dma_start(out=gfin, in_=IN('g_final').partition_broadcast(128)), B_gfin, w=[B_gfin])
        h2 = [sb("E_h2%d" % i, [128, D], F32) for i in range(2)]
        y1 = [sb("E_y1%d" % i, [128, D], F32) for i in range(2)]
        y2 = [sb("E_y2%d" % i, [128, D], F32) for i in range(2)]
        pbf = [sb("E_p%d" % i, [128, PLE], BF16) for i in range(2)]
        B_h2 = S.bufs("E_h2", 2)
        B_y1 = S.bufs("E_y1", 2)
        B_y2 = S.bufs("E_y2", 2)
        B_pbf = S.bufs("E_p", 2)
        hn = sb("E_hn", [128, D], BF16)
        B_hn = S.buf("E_hn")
        hTs = [sb("E_hT%d" % i, [128, 16, 128], BF16) for i in range(2)]
        B_hTs = S.bufs("E_hT", 2)
        pTs = [sb("E_pT%d" % i, [128, 2, 128], BF16) for i in range(2)]
        B_pTs = S.bufs("E_pT", 2)
        gl = sb("E_gl", [128, 512], F32)
        B_gl = S.buf("E_gl")
        sgm = sb("E_sg", [128, 512], F32)
        B_sgm = S.buf("E_sg")
        h3 = sb("E_h3", [128, D], F32)
        B_h3 = S.buf("E_h3")
        ob = [sb("E_o%d" % i, [128, D], F32) for i in range(2)]
        B_ob = S.bufs("E_o", 2)
        st = [sb("E_st%d" % i, [128, 8], F32) for i in range(2)]
        B_st = S.bufs("E_st", 2)
        st2 = [sb("E_su%d" % i, [128, 8], F32) for i in range(2)]
        B_st2 = S.bufs("E_su", 2)

        def load(j):
            b = j % 2
            S.dma("sp", lambda e: e.dma_start(out=h2[b], in_=H1_d[j * 128:(j + 1) * 128, :]), B_h2[b], w=[B_h2[b]])
            S.dma("pool", lambda e: e.dma_start(out=pbf[b], in_=IN('p_own')[j * 128:(j + 1) * 128, :]), B_pbf[b], w=[B_pbf[b]])
            S.dma("pool", lambda e: e.indirect_dma_start(out=y1[b], out_offset=None, in_=Y_d,
                                                         in_offset=bass.IndirectOffsetOnAxis(ap=DESTI[:, j, 0:1], axis=0)),
                  B_y1[b], r=[B_desti], w=[B_y1[b]])
            S.dma("pool", lambda e: e.indirect_dma_start(out=y2[b], out_offset=None, in_=Y_d,
                                                         in_offset=bass.IndirectOffsetOnAxis(ap=DESTI[:, j, 1:2], axis=0)),
                  B_y2[b], r=[B_desti], w=[B_y2[b]])

        def stage1(j):
            b = j % 2
            S.op("dve", lambda e: e.tensor_tensor(out=h2[b], in0=h2[b], in1=y1[b], op=ALU.add), r=[B_h2[b], B_y1[b]], w=[B_h2[b]])
            S.op("dve", lambda e: e.tensor_tensor(out=h2[b], in0=h2[b], in1=y2[b], op=ALU.add), r=[B_h2[b], B_y2[b]], w=[B_h2[b]])
            rmsnorm_rstd(h2[b], B_h2[b], hn, B_hn, st[b], B_st[b], D)
            S.op("dve", lambda e: e.scalar_tensor_tensor(out=hn, in0=h2[b], scalar=st[b][:, 2:3], in1=gple, op0=ALU.mult, op1=ALU.mult),
                 r=[B_h2[b], B_st[b], B_gple], w=[B_hn])
            transpose_to(hn, B_hn, 16, lambda k0, k1: hTs[b][:, k0:k1, :], B_hTs[b], [0, 1])
            transpose_to(pbf[b], B_pbf[b], 2, lambda k0, k1: pTs[b][:, k0:k1, :], B_pTs[b], [0, 1])

        load(0)
        stage1(0)
        for j in range(NTo):
            b = j % 2
            hT, B_hT, pT, B_pT = hTs[b], B_hTs[b], pTs[b], B_pTs[b]
            if j + 1 < NTo:
                load(j + 1)
                stage1(j + 1)
            for n in range(4):
                bg = 2 + (n % 2) * 2
                bp_ = 3 + (n % 2) * 2
                for k in range(16):
                    S.op("pe", lambda e: e.matmul(pb[bg], lhsT=hT[:, k, :], rhs=Wpg[:, k, n * 512:(n + 1) * 512], start=(k == 0), stop=(k == 15)),
                         r=[B_hT, B_Wpg], w=[PB[bg]])
                for k in range(2):
                    S.op("pe", lambda e: e.matmul(pb[bp_], lhsT=pT[:, k, :], rhs=Wpl[:, k, n * 512:(n + 1) * 512], start=(k == 0), stop=(k == 1)),
                         r=[B_pT, B_Wpl], w=[PB[bp_]])
                cs = slice(n * 512, (n + 1) * 512)
                S.op("dve", lambda e: e.tensor_tensor(out=gl, in0=pb[bg], in1=bpg[:, cs], op=ALU.add), r=[PB[bg], B_bpg], w=[B_gl])
                S.op("act", lambda e: e.activation(out=sgm, in_=gl, func=AF.Sigmoid), r=[B_gl], w=[B_sgm])
                S.op("dve", lambda e: e.tensor_tensor(out=sgm, in0=pb[bp_], in1=sgm, op=ALU.mult), r=[PB[bp_], B_sgm], w=[B_sgm])
                S.op("dve", lambda e: e.tensor_tensor(out=h3[:, cs], in0=h2[b][:, cs], in1=sgm, op=ALU.add), r=[B_h2[b], B_sgm], w=[B_h3])
            rmsnorm_rstd(h3, B_h3, ob[b], B_ob[b], st2[b], B_st2[b], D)
            S.op("dve", lambda e: e.scalar_tensor_tensor(out=ob[b], in0=h3, scalar=st2[b][:, 2:3], in1=gfin, op0=ALU.mult, op1=ALU.mult),
                 r=[B_h3, B_st2[b], B_gfin], w=[B_ob[b]])
            S.dma("sp", lambda e: e.dma_start(out=out[j * 128:(j + 1) * 128, :], in_=ob[b]), B_ob[b], r=[B_ob[b]])
        S.end_phase()

    phases = [("A", phase_A), ("A2", phase_A2), ("B", phase_B), ("C", phase_C), ("D", phase_D), ("E", phase_E)]
    for name, fn in phases:
        fn()
        c.stack.close()
        c.stack = contextlib.ExitStack()
        if name == upto:
            break
    return nc, S, c


def make_in_maps(inputs, S_len, used=None):
    x = np.asarray(inputs["x"], np.float32)
    Bn = x.shape[0]
    NB = S_len // 256
    NBo = NB // 2
    p = np.asarray(inputs["p"], np.float32)[0]
    sq = lambda k: np.ascontiguousarray(np.asarray(inputs[k], np.float32)[0])
    row = lambda a: np.ascontiguousarray(a.reshape(1, -1))
    shared = dict(
        g_mix=row(sq("g_mix")), w_in=sq("w_in"), beta_attn=row(sq("beta_attn")), w_pool=sq("w_pool"),
        pool_scale=row(sq("pool_scale")), w_out=sq("w_out"), g_ffn=row(sq("g_ffn")),
        w_rg=sq("w_router_group"), b_rg=row(sq("b_router_group")), w_re=sq("w_router_expert"),
        b_re=row(sq("b_router_expert")), w_eg=sq("w_expert_gate"), w_eu=sq("w_expert_up"),
        w_ed=sq("w_expert_down"), g_ple=row(sq("g_ple")), w_ple=sq("w_ple"), w_pg=sq("w_ple_gate"),
        b_pg=row(sq("b_ple_gate")), g_final=row(np.asarray(inputs["g_final"], np.float32)),
    )
    tabs = [host_tables(S_len, r) for r in range(2)]
    in_maps = []
    orders = []
    for cidx in range(2 * Bn):
        b, r = cidx // 2, cidx % 2
        order = []
        for i in range(NBo):
            order += [2 * i + r, 2 * i + 1 - r]
        own = [2 * i + r for i in range(NBo)]
        xb = x[b].reshape(NB, 256, D)
        pb_ = p[b].reshape(NB, 256, PLE)
        m = dict(shared)
        m["x_perm"] = np.ascontiguousarray(xb[order].reshape(S_len, D))
        m["p_own"] = np.ascontiguousarray(pb_[own].reshape(-1, PLE))
        m.update(tabs[r])
        if used is not None:
            m = {k: v for k, v in m.items() if k in used}
        in_maps.append(m)
        orders.append(own)
    return in_maps, orders


def kernel(**inputs):
    x = np.asarray(inputs["x"])
    Bn, S_len, _ = x.shape
    nc, S, c = build(S_len, debug=False, upto="E")
    in_maps, orders = make_in_maps(inputs, S_len, used=set(c.used))
    ncores = 2 * Bn
    res = run_bass_kernel_spmd(nc, in_maps, core_ids=list(range(ncores)))
    outp = np.empty((Bn, S_len // 256, 256, D), np.float32)
    for cidx in range(ncores):
        b = cidx // 2
        o = np.asarray(res.results[cidx]["out"], np.float32).reshape(-1, 256, D)
        outp[b, orders[cidx]] = o
    return outp.reshape(Bn, S_len, D)
```

```python
import numpy as np
import concourse.bass as bass
import concourse.mybir as mybir
from concourse.bass_utils import run_bass_kernel_spmd

F32 = mybir.dt.float32
BF16 = mybir.dt.bfloat16
I32 = mybir.dt.int32
AF = mybir.ActivationFunctionType
ALU = mybir.AluOpType
AX = mybir.AxisListType

D = 2048
H = 8
HD = 128
AW = 1024
PW = 1024
INW = 4096
NE = 64
NG = 8
DE = 512
PLE = 256
EPS = 1e-6
import os
SKIP = os.environ.get('KSKIP', '')
NEG = -1.0e30


class Buf:
    __slots__ = ("name", "wev", "revs", "slot", "excl")

    def __init__(self, name, excl=False):
        self.name = name
        self.excl = excl
        self.wev = None
        self.revs = {}
        self.slot = None


class Sched:
    def __init__(self, nc):
        self.nc = nc
        self.eng = {"pe": nc.tensor, "act": nc.scalar, "dve": nc.vector, "pool": nc.gpsimd, "sp": nc.sync}
        self.esem = {k: nc.alloc_semaphore("es_" + k) for k in self.eng}
        self.ecnt = {k: 0 for k in self.eng}
        self.seen = {k: {} for k in self.eng}
        self.free_slots = []
        self.nslots = 0
        self.phase_bufs = []
        self.ninst = 0
        self.nwait = 0

    def buf(self, name, excl=False):
        b = Buf(name, excl)
        self.phase_bufs.append(b)
        return b

    def bufs(self, name, n, excl=False):
        return [self.buf("%s%d" % (name, i), excl) for i in range(n)]

    def _slot(self, b):
        if b.slot is None:
            if self.free_slots:
                b.slot = self.free_slots.pop()
            else:
                b.slot = [self.nc.alloc_semaphore("ds%d" % self.nslots), 0]
                self.nslots += 1
        return b.slot

    def _waits(self, e, r, w):
        need = {}

        def add(ev):
            s, v, src = ev
            if src == e and e == "pe":
                return
            k = id(s)
            if k not in need or need[k][1] < v:
                need[k] = (s, v)

        for b in r:
            if b.wev is not None:
                add(b.wev)
            if b.excl:
                for ev in b.revs.values():
                    if ev[2] != e:
                        add(ev)
        for b in w:
            if b.wev is not None:
                add(b.wev)
            for ev in b.revs.values():
                if ev[2] == e:
                    continue
                add(ev)
        seen = self.seen[e]
        for k, (s, v) in need.items():
            if seen.get(k, 0) >= v:
                continue
            self.eng[e].wait_ge(s, v)
            self.nwait += 1
            seen[k] = v

    def _record(self, ev, r, w):
        k = id(ev[0])
        for b in r:
            b.revs[k] = ev
        for b in w:
            b.wev = ev
            b.revs = {}

    def op(self, e, fn, r=(), w=()):
        self._waits(e, r, w)
        ins = fn(self.eng[e])
        self.ecnt[e] += 1
        ins.then_inc(self.esem[e], 1)
        self.ninst += 1
        self._record((self.esem[e], self.ecnt[e], e), r, w)
        return ins

    def dma(self, e, fn, sb, r=(), w=()):
        self._waits(e, r, w)
        slot = self._slot(sb)
        ins = fn(self.eng[e])
        slot[1] += 16
        ins.then_inc(slot[0], 16)
        self.ninst += 1
        self._record((slot[0], slot[1], "dma"), r, w)
        return ins

    def end_phase(self):
        for b in self.phase_bufs:
            if b.slot is not None:
                s, v = b.slot
                if self.seen["sp"].get(id(s), 0) < v:
                    self.eng["sp"].wait_ge(s, v)
                    self.seen["sp"][id(s)] = v
        self.ecnt["sp"] += 1
        self.eng["sp"].nop().then_inc(self.esem["sp"], 1)
        for e in self.eng:
            for f in self.eng:
                if f == e:
                    continue
                s, v = self.esem[f], self.ecnt[f]
                if v > 0 and self.seen[e].get(id(s), 0) < v:
                    self.eng[e].wait_ge(s, v)
                    self.seen[e][id(s)] = v
        for b in self.phase_bufs:
            if b.slot is not None:
                self.free_slots.append(b.slot)
                b.slot = None
        self.phase_bufs = []


class Ctx:
    pass


def alibi_slopes():
    return np.array([2.0 ** (-8.0 * (h + 1) / H) for h in range(H)], np.float64)


def host_tables(S_len, r):
    NB = S_len // 256
    NBo = NB // 2
    NBP = max(NB, 8)
    sl = alibi_slopes()
    seqblk = np.zeros(NB, np.int64)
    for i in range(NBo):
        seqblk[2 * i] = 2 * i + r
        seqblk[2 * i + 1] = 2 * i + 1 - r
    j = np.arange(256)
    vtab = np.exp(-sl[None, None, :] * (255 - (np.arange(2)[None, :, None] * 128 + np.arange(128)[:, None, None])))
    mtab = np.zeros((NBo, 2, 128, H, NBP), np.float64)
    gbias = np.full((NBo, 128, NBP), NEG, np.float64)
    for i in range(NBo):
        own = seqblk[2 * i]
        for s in range(NB):
            if seqblk[s] < own:
                gbias[i, :, s] = 0.0
                for t in range(2):
                    qpos = own * 256 + t * 128 + np.arange(128)
                    dist = qpos - (seqblk[s] * 256 + 255)
                    mtab[i, t, :, :, s] = np.exp(-sl[None, :] * dist[:, None])
    kj = np.arange(128)[:, None]
    qi = np.arange(128)[None, :]
    ctab = np.zeros((128, H, 2, 128), np.float64)
    for h in range(H):
        ctab[:, h, 0, :] = np.where(qi >= kj, np.exp(-sl[h] * np.maximum(qi - kj, 0)), 0.0)
        ctab[:, h, 1, :] = np.exp(-sl[h] * (128 + qi - kj))
    wins = np.array([2, 4, 8, 16])
    t = seqblk[0] * 256 + np.arange(256)
    cnt = np.minimum(t[None, :] + 1, wins[:, None]).astype(np.float64)
    ptab = np.broadcast_to((1.0 / cnt)[None], (128, 4, 256))
    halo = np.zeros((128, 2), np.float64)
    halo[:, 0] = 1.0 if r == 0 else 0.0
    halo[:, 1] = 0.0 if r == 0 else 1.0
    f = lambda a: np.ascontiguousarray(a, dtype=np.float32)
    lt = (np.arange(128)[:, None] < np.arange(128)[None, :]).astype(np.float32)
    return dict(ident=np.eye(128, dtype=np.float32), vtab=f(vtab), mtab=f(mtab), gbias=f(gbias),
                ctab=f(ctab), ptab=f(ptab), halo=f(halo), ltri=lt,
                bpos=f((np.arange(128) * 128.0).reshape(128, 1)),
                pidx=f(np.arange(128).reshape(128, 1)))


def build(S_len, debug=False, upto="E"):
    nc = bass.Bass("TRN2", target_bir_lowering=False)
    NB = S_len // 256
    NBo = NB // 2
    NBP = max(NB, 8)
    To = S_len // 2
    NTo = To // 128
    NT = S_len // 128
    PL = 2 * To + NE * 128
    NBLK = PL // 128
    skind = "ExternalOutput" if debug else "Internal"

    def din(name, shape, dt=F32):
        return nc.dram_tensor(name, list(shape), dt, kind="ExternalInput").ap()

    def dscr(name, shape, dt):
        return nc.dram_tensor(name, list(shape), dt, kind=skind).ap()

    c = Ctx()
    c.nc = nc
    in_shapes = dict(
        x_perm=[S_len, D], p_own=[To, PLE], g_mix=[1, D], w_in=[D, INW], beta_attn=[1, AW],
        w_pool=[4, 256, 256], pool_scale=[1, PW], w_out=[D, D], g_ffn=[1, D], w_rg=[D, NG], b_rg=[1, NG],
        w_re=[NG, D, 8], b_re=[1, NE], w_eg=[NE, D, DE], w_eu=[NE, D, DE], w_ed=[NE, DE, D], g_ple=[1, D],
        w_ple=[PLE, D], w_pg=[D, D], b_pg=[1, D], g_final=[1, D], ident=[128, 128], vtab=[128, 2, H],
        mtab=[NBo, 2, 128, H, NBP], gbias=[NBo, 128, NBP], ctab=[128, H, 2, 128], ptab=[128, 4, 256],
        halo=[128, 2], ltri=[128, 128], bpos=[128, 1], pidx=[128, 1])
    c.used = {}

    def IN(name):
        if name not in c.used:
            c.used[name] = din(name, in_shapes[name])
        return c.used[name]
    out = nc.dram_tensor("out", [To, D], F32, kind="ExternalOutput").ap()
    KT_d = dscr("KT_d", [H, 128, S_len], BF16)
    VP_d = dscr("VP_d", [H, 128, NT, 129], BF16)
    V1_d = dscr("V1_d", [H, 128, NTo, 129], BF16)
    QT_d = dscr("QT_d", [H, 128, To], BF16)
    UT_d = dscr("UT_d", [8, 128, NBo, 272], F32)
    KM_d = dscr("KM_d", [128, H, NBP], F32)
    OA_d = dscr("OA_d", [To, AW], F32)
    MP_d = dscr("MP_d", [To, PW], BF16)
    H1_d = dscr("H1_d", [To, D], F32)
    F_d = dscr("F_d", [To, D], BF16)
    SLOT_d = dscr("SLOT_d", [PL, 2], F32)
    Y_d = dscr("Y_d", [PL, D], F32)
    RT_d = dscr("RT_d", [To, 8], F32)

    S = Sched(nc)
    c.S = S
    import contextlib
    c.stack = contextlib.ExitStack()

    def sb(name, shape, dt):
        t = c.stack.enter_context(nc.sbuf_tensor(name, list(shape), dt))
        return t.ap() if hasattr(t, "ap") else t[:]
    pb = [nc.alloc_psum_tensor("pb%d" % i, [128, 512], F32).ap() for i in range(8)]
    PB = S.bufs("pbank", 8, excl=True)

    ident = nc.alloc_sbuf_tensor("identb", [128, 128], BF16).ap()
    B_ident = Buf("ident")
    S.dma("pool", lambda e: e.dma_start(out=ident, in_=IN('ident')), B_ident, w=[B_ident])

    def rmsnorm_rstd(e_xt, B_xt, junk, B_junk, st, B_st, width):
        S.op("act", lambda e: e.activation(out=junk, in_=e_xt, func=AF.Square, accum_out=st[:, 0:1]),
             r=[B_xt], w=[B_junk, B_st])
        S.op("act", lambda e: e.activation(out=st[:, 1:2], in_=st[:, 0:1], func=AF.Sqrt, scale=1.0 / width, bias=EPS),
             r=[B_st], w=[B_st])
        S.op("dve", lambda e: e.reciprocal(out=st[:, 2:3], in_=st[:, 1:2]), r=[B_st], w=[B_st])

    def transpose_to(src, B_src, nchunk, dst_fn, B_dst, tps, step=1):
        for g0 in range(0, nchunk, 8):
            n = min(8, nchunk - g0)
            bi = tps[(g0 // 8) % len(tps)]
            tpv = pb[bi].bitcast(BF16).rearrange("p (k n) -> p k n", n=128)
            for k in range(g0, g0 + n):
                if step == 1:
                    sv = src[:, k * 128:(k + 1) * 128]
                else:
                    sv = src[:, k:k + 127 * step + 1:step]
                S.op("pe", lambda e: e.transpose(tpv[:, k - g0, :], sv, ident), r=[B_src, B_ident], w=[PB[bi]])
            eng = "act" if (g0 // 8) % 2 == 0 else "dve"
            if eng == "act":
                S.op("act", lambda e: e.copy(out=dst_fn(g0, g0 + n), in_=tpv[:, 0:n, :]), r=[PB[bi]], w=[B_dst])
            else:
                S.op("dve", lambda e: e.tensor_copy(out=dst_fn(g0, g0 + n), in_=tpv[:, 0:n, :]), r=[PB[bi]], w=[B_dst])

    def phase_A():
        Wb = sb("A_Wb", [128, 16, INW], BF16)
        B_W = S.buf("A_Wb")
        for k in range(16):
            S.dma("pool", lambda e: e.dma_start(out=Wb[:, k, :], in_=IN('w_in')[k * 128:(k + 1) * 128, :]), B_W, w=[B_W])
        gmix = sb("A_gmix", [128, D], F32)
        B_g = S.buf("A_gmix")
        S.dma("sp", lambda e: e.dma_start(out=gmix, in_=IN('g_mix').partition_broadcast(128)), B_g, w=[B_g])
        vtab = sb("A_vtab", [128, 2, H], F32)
        B_vt = S.buf("A_vtab")
        S.dma("sp", lambda e: e.dma_start(out=vtab, in_=IN('vtab')), B_vt, w=[B_vt])
        xb = [sb("A_x%d" % i, [128, D], F32) for i in range(2)]
        B_x = S.bufs("A_x", 2)
        ab = [sb("A_a%d" % i, [128, D], BF16) for i in range(4)]
        B_a = S.bufs("A_a", 4)
        stt = [sb("A_st%d" % i, [128, 4], F32) for i in range(2)]
        B_st = S.bufs("A_st", 2)
        aT = sb("A_aT", [128, 16, 512], BF16)
        B_aT = S.buf("A_aT")
        NSTG = 3
        fst = [sb("A_fs%d" % i, [128, 512], BF16) for i in range(NSTG)]
        B_fs = S.bufs("A_fs", NSTG)
        ust = [sb("A_us%d" % i, [128, 272], F32) for i in range(2)]
        B_us = S.bufs("A_us", 2)
        vp = [sb("A_vp%d" % i, [128, H, 129], BF16) for i in range(2)]
        B_vp = S.bufs("A_vp", 2)
        v1 = [sb("A_v1%d" % i, [128, H, 129], BF16) for i in range(2)]
        B_v1 = S.bufs("A_v1", 2)
        km = sb("A_km", [128, H, NBP], F32)
        B_km = S.buf("A_km")
        kms = sb("A_kms", [128, 2], F32)
        B_kms = S.buf("A_kms")
        S.op("dve", lambda e: e.memset(km, 0.0), w=[B_km])
        for i in range(2):
            S.op("dve", lambda e: e.memset(v1[i][:, :, 128:129], 1.0), w=[B_v1[i]])
        mmb = [2, 3, 4, 5, 6, 7]
        mmi = [0]

        def nextbank():
            b = mmb[mmi[0] % len(mmb)]
            mmi[0] += 1
            return b

        fsi = [0]
        tcount = [0]

        def norm_tile(i, t):
            j = tcount[0] % 2
            tcount[0] += 1
            row0 = (i * 4 + t) * 128
            S.dma("sp", lambda e: e.dma_start(out=xb[j], in_=IN('x_perm')[row0:row0 + 128, :]), B_x[j], w=[B_x[j]])
            rmsnorm_rstd(xb[j], B_x[j], ab[t], B_a[t], stt[j], B_st[j], D)
            S.op("dve", lambda e: e.scalar_tensor_tensor(out=ab[t], in0=xb[j], scalar=stt[j][:, 2:3], in1=gmix,
                                                         op0=ALU.mult, op1=ALU.mult),
                 r=[B_x[j], B_st[j], B_g], w=[B_a[t]])

        def transposes(i):
            for t in range(4):
                transpose_to(ab[t], B_a[t], 16, lambda k0, k1: aT[:, k0:k1, t * 128:(t + 1) * 128], B_aT, [0, 1])

        for t in range(4):
            norm_tile(0, t)
        transposes(0)
        for i in range(NBo):
            for ch in range(H if 'k' not in SKIP else 0):
                bk = nextbank()
                for k in range(16):
                    S.op("pe", lambda e: e.matmul(pb[bk], lhsT=Wb[:, k, AW + ch * 128:AW + (ch + 1) * 128], rhs=aT[:, k, :],
                                                  start=(k == 0), stop=(k == 15)), r=[B_W, B_aT], w=[PB[bk]])
                f = fsi[0] % NSTG
                fsi[0] += 1
                S.op("act", lambda e: e.copy(out=fst[f], in_=pb[bk]), r=[PB[bk]], w=[B_fs[f]])
                S.op("dve", lambda e: e.reduce_sum(out=kms, in_=pb[bk].rearrange("p (b n) -> p b n", n=256), axis=AX.X),
                     r=[PB[bk], B_fs[f]], w=[B_kms])
                S.op("dve", lambda e: e.tensor_scalar(out=km[:, ch, 2 * i:2 * i + 2], in0=kms, scalar1=1.0 / 256, scalar2=None,
                                                      op0=ALU.mult), r=[B_kms], w=[B_km])
                S.dma("sp", lambda e: e.dma_start(out=KT_d[ch, :, i * 512:(i + 1) * 512], in_=fst[f]), B_fs[f], r=[B_fs[f]])
            if i + 1 < NBo:
                norm_tile(i + 1, 0)
            for ch in range(H if 'q' not in SKIP else 0):
                bk = nextbank()
                for k in range(16):
                    S.op("pe", lambda e: e.matmul(pb[bk][:, 0:256], lhsT=Wb[:, k, ch * 128:(ch + 1) * 128], rhs=aT[:, k, 0:256],
                                                  start=(k == 0), stop=(k == 15)), r=[B_W, B_aT], w=[PB[bk]])
                f = fsi[0] % NSTG
                fsi[0] += 1
                S.op("act", lambda e: e.copy(out=fst[f][:, 0:256], in_=pb[bk][:, 0:256]), r=[PB[bk]], w=[B_fs[f]])
                S.dma("sp", lambda e: e.dma_start(out=QT_d[ch, :, i * 256:(i + 1) * 256], in_=fst[f][:, 0:256]), B_fs[f], r=[B_fs[f]])
            if i + 1 < NBo:
                norm_tile(i + 1, 1)
            for ch in range(8 if 'u' not in SKIP else 0):
                bk = nextbank()
                for k in range(16):
                    S.op("pe", lambda e: e.matmul(pb[bk], lhsT=Wb[:, k, 3 * AW + ch * 128:3 * AW + (ch + 1) * 128], rhs=aT[:, k, :],
                                                  start=(k == 0), stop=(k == 15)), r=[B_W, B_aT], w=[PB[bk]])
                f = ch % 2
                S.op("act", lambda e: e.copy(out=ust[f][:, 0:256], in_=pb[bk][:, 0:256]), r=[PB[bk]], w=[B_us[f]])
                S.op("dve", lambda e: e.tensor_copy(out=ust[f][:, 256:272], in_=pb[bk][:, 496:512]), r=[PB[bk]], w=[B_us[f]])
                S.dma("sp", lambda e: e.dma_start(out=UT_d[ch, :, i, :], in_=ust[f]), B_us[f], r=[B_us[f]])
            if i + 1 < NBo:
                norm_tile(i + 1, 2)
            for t in range(4 if 'v' not in SKIP else 0):
                par = t % 2
                tile_g = i * 4 + t
                jj = tile_g % 2
                for half in range(2):
                    bk = nextbank()
                    for k in range(16):
                        S.op("pe", lambda e: e.matmul(pb[bk], lhsT=aT[:, k, t * 128:(t + 1) * 128],
                                                      rhs=Wb[:, k, 2 * AW + half * 512:2 * AW + (half + 1) * 512],
                                                      start=(k == 0), stop=(k == 15)), r=[B_W, B_aT], w=[PB[bk]])
                    pv = pb[bk].rearrange("p (h n) -> p h n", n=128)
                    hs = slice(half * 4, (half + 1) * 4)
                    S.op("dve", lambda e: e.tensor_tensor(out=vp[jj][:, hs, 0:128], in0=pv,
                                                          in1=vtab[:, par, hs].unsqueeze(2).to_broadcast([128, 4, 128]),
                                                          op=ALU.mult), r=[PB[bk], B_vt], w=[B_vp[jj]])
                    if t < 2:
                        S.op("act", lambda e: e.copy(out=v1[jj][:, hs, 0:128], in_=pv), r=[PB[bk], B_vp[jj]], w=[B_v1[jj]])
                S.op("dve", lambda e: e.tensor_copy(out=vp[jj][:, :, 128:129], in_=vtab[:, par, :].unsqueeze(2)),
                     r=[B_vt], w=[B_vp[jj]])
                S.dma("sp", lambda e: e.dma_start(out=VP_d[:, :, tile_g, :].rearrange("h p c -> p h c"), in_=vp[jj]),
                      B_vp[jj], r=[B_vp[jj]])
                if t < 2:
                    S.dma("sp", lambda e: e.dma_start(out=V1_d[:, :, i * 2 + t, :].rearrange("h p c -> p h c"), in_=v1[jj]),
                          B_v1[jj], r=[B_v1[jj]])
            if i + 1 < NBo:
                norm_tile(i + 1, 3)
                transposes(i + 1)
        S.dma("sp", lambda e: e.dma_start(out=KM_d, in_=km), B_km, r=[B_km])
        S.end_phase()


    def phase_A2():
        wpb = sb("P_wp", [128, 4, 2, 256], BF16)
        B_wp = S.buf("P_wp")
        S.dma("pool", lambda e: e.dma_start(out=wpb, in_=IN('w_pool').rearrange("g (cc p) d -> p g cc d", p=128)), B_wp, w=[B_wp])
        psc = sb("P_psc", [128, PW], F32)
        B_psc = S.buf("P_psc")
        S.dma("sp", lambda e: e.dma_start(out=psc, in_=IN('pool_scale').partition_broadcast(128)), B_psc, w=[B_psc])
        ptab = sb("P_ptab", [128, 4, 256], F32)
        B_pt = S.buf("P_ptab")
        S.dma("sp", lambda e: e.dma_start(out=ptab, in_=IN('ptab')), B_pt, w=[B_pt])
        halo = sb("P_halo", [128, 2], F32)
        B_ha = S.buf("P_halo")
        S.dma("sp", lambda e: e.dma_start(out=halo, in_=IN('halo')), B_ha, w=[B_ha])
        ub = [sb("P_ub%d" % i, [128, 8, 272], F32) for i in range(2)]
        B_ub = S.bufs("P_ub", 2)
        tl = [sb("P_tl%d" % i, [128, 8, 32], F32) for i in range(2)]
        B_tl = S.bufs("P_tl", 2)
        Pb = sb("P_P", [128, 8, 272], F32)
        B_P = S.buf("P_P")
        Qb = sb("P_Q", [128, 8, 272], F32)
        B_Q = S.buf("P_Q")
        zb = sb("P_z", [128, 8, 256], BF16)
        B_z = S.buf("P_z")
        zt = sb("P_zt", [128, 2, 256], F32)
        B_zt = S.buf("P_zt")
        mp = [sb("P_mp%d" % i, [128, PW], BF16) for i in range(2)]
        B_mp = S.bufs("P_mp", 2)
        tmpf = sb("P_tmpf", [128, PW], F32)
        B_tmpf = S.buf("P_tmpf")
        st = [sb("P_st%d" % i, [128, 8], F32) for i in range(2)]
        B_st = S.bufs("P_st", 2)
        wins = [2, 4, 8, 16]
        UTv = UT_d.rearrange("c p i n -> p c i n")
        for i in range(NBo):
            j = i % 2
            A = ub[j]
            S.dma("sp", lambda e: e.dma_start(out=A[:, :, 16:272], in_=UTv[:, :, i, 0:256]), B_ub[j], w=[B_ub[j]])
            S.dma("sp", lambda e: e.dma_start(out=tl[j][:, :, 16:32], in_=UTv[:, :, i, 256:272]), B_tl[j], w=[B_tl[j]])
            if i > 0:
                S.dma("sp", lambda e: e.dma_start(out=tl[j][:, :, 0:16], in_=UTv[:, :, i - 1, 256:272]), B_tl[j], w=[B_tl[j]])
            else:
                S.op("dve", lambda e: e.memset(tl[j][:, :, 0:16], 0.0), w=[B_tl[j]])
            S.op("dve", lambda e: e.tensor_scalar(out=A[:, :, 0:16], in0=tl[j][:, :, 0:16], scalar1=halo[:, 0:1], scalar2=None, op0=ALU.mult),
                 r=[B_tl[j], B_ha], w=[B_ub[j]])
            S.op("dve", lambda e: e.scalar_tensor_tensor(out=A[:, :, 0:16], in0=tl[j][:, :, 16:32], scalar=halo[:, 1:2], in1=A[:, :, 0:16],
                                                         op0=ALU.mult, op1=ALU.add), r=[B_tl[j], B_ha, B_ub[j]], w=[B_ub[j]])
            S.op("dve", lambda e: e.tensor_tensor(out=Pb[:, :, 1:272], in0=A[:, :, 1:272], in1=A[:, :, 0:271], op=ALU.add),
                 r=[B_ub[j]], w=[B_P])
            S.op("dve", lambda e: e.tensor_tensor(out=Qb[:, 2:8, 3:272], in0=Pb[:, 2:8, 3:272], in1=Pb[:, 2:8, 1:270], op=ALU.add),
                 r=[B_P], w=[B_Q])
            S.op("dve", lambda e: e.tensor_tensor(out=Pb[:, 4:8, 7:272], in0=Qb[:, 4:8, 7:272], in1=Qb[:, 4:8, 3:268], op=ALU.add),
                 r=[B_Q], w=[B_P])
            S.op("dve", lambda e: e.tensor_tensor(out=Qb[:, 6:8, 15:272], in0=Pb[:, 6:8, 15:272], in1=Pb[:, 6:8, 7:264], op=ALU.add),
                 r=[B_P], w=[B_Q])
            for g in range(4):
                W = Pb if g % 2 == 0 else Qb
                B_Wb = B_P if g % 2 == 0 else B_Q
                cs = slice(2 * g, 2 * g + 2)
                if i == 0:
                    S.op("dve", lambda e: e.tensor_tensor(out=zt, in0=W[:, cs, 16:272],
                                                          in1=ptab[:, g, :].unsqueeze(1).to_broadcast([128, 2, 256]), op=ALU.mult),
                         r=[B_Wb, B_pt], w=[B_zt])
                    S.op("dve", lambda e: e.tensor_tensor(out=zb[:, cs, :], in0=zt, in1=A[:, cs, 16:272], op=ALU.subtract),
                         r=[B_zt, B_ub[j]], w=[B_z])
                else:
                    S.op("dve", lambda e: e.scalar_tensor_tensor(out=zb[:, cs, :], in0=W[:, cs, 16:272], scalar=1.0 / wins[g],
                                                                 in1=A[:, cs, 16:272], op0=ALU.mult, op1=ALU.subtract),
                         r=[B_Wb, B_ub[j]], w=[B_z])
            for t in range(2):
                jt = (i * 2 + t) % 2
                banks = [2 + 2 * jt, 3 + 2 * jt]
                for g in range(4):
                    bk = banks[g // 2]
                    for cc in range(2):
                        S.op("pe", lambda e: e.matmul(pb[bk][:, (g % 2) * 256:(g % 2 + 1) * 256], lhsT=zb[:, 2 * g + cc, t * 128:(t + 1) * 128],
                                                      rhs=wpb[:, g, cc, :], start=(cc == 0), stop=(cc == 1)),
                             r=[B_z, B_wp], w=[PB[bk]])
                S.op("act", lambda e: e.activation(out=tmpf[:, 0:512], in_=pb[banks[0]], func=AF.Square, accum_out=st[jt][:, 0:1]),
                     r=[PB[banks[0]]], w=[B_tmpf, B_st[jt]])
                S.op("act", lambda e: e.activation(out=tmpf[:, 512:1024], in_=pb[banks[1]], func=AF.Square, accum_out=st[jt][:, 3:4]),
                     r=[PB[banks[1]]], w=[B_tmpf, B_st[jt]])
                S.op("dve", lambda e: e.tensor_tensor(out=st[jt][:, 0:1], in0=st[jt][:, 0:1], in1=st[jt][:, 3:4], op=ALU.add),
                     r=[B_st[jt]], w=[B_st[jt]])
                S.op("act", lambda e: e.activation(out=st[jt][:, 1:2], in_=st[jt][:, 0:1], func=AF.Sqrt, scale=1.0 / PW, bias=EPS),
                     r=[B_st[jt]], w=[B_st[jt]])
                S.op("dve", lambda e: e.reciprocal(out=st[jt][:, 2:3], in_=st[jt][:, 1:2]), r=[B_st[jt]], w=[B_st[jt]])
                for hh in range(2):
                    S.op("dve", lambda e: e.scalar_tensor_tensor(out=mp[jt][:, hh * 512:(hh + 1) * 512], in0=pb[banks[hh]], scalar=st[jt][:, 2:3],
                                                                 in1=psc[:, hh * 512:(hh + 1) * 512], op0=ALU.mult, op1=ALU.mult),
                         r=[PB[banks[hh]], B_st[jt], B_psc], w=[B_mp[jt]])
                row0 = (i * 2 + t) * 128
                S.dma("sp", lambda e: e.dma_start(out=MP_d[row0:row0 + 128, :], in_=mp[jt]), B_mp[jt], r=[B_mp[jt]])
        S.end_phase()

    def phase_B():
        scale = HD ** -0.5
        kmf = sb("B_kmf", [128, H, NBP], F32)
        B_kmf = S.buf("B_kmf")
        S.dma("sp", lambda e: e.dma_start(out=kmf, in_=KM_d), B_kmf, w=[B_kmf])
        kmb = sb("B_kmb", [128, H, NBP], BF16)
        B_kmb = S.buf("B_kmb")
        S.op("dve", lambda e: e.tensor_copy(out=kmb, in_=kmf), r=[B_kmf], w=[B_kmb])
        gbias = sb("B_gbias", [128, NBo, NBP], F32)
        B_gb = S.buf("B_gbias")
        S.dma("sp", lambda e: e.dma_start(out=gbias, in_=IN('gbias').rearrange("i p s -> p i s")), B_gb, w=[B_gb])
        mtab = sb("B_mtab", [128, NBo, 2, H, NBP], F32)
        B_mt = S.buf("B_mtab")
        for i in range(NBo):
            S.dma("sp", lambda e: e.dma_start(out=mtab[:, i], in_=IN('mtab')[i].rearrange("t p h s -> p t h s")), B_mt, w=[B_mt])
        ctab = sb("B_ctab", [128, H, 2, 128], F32)
        B_ct = S.buf("B_ctab")
        S.dma("sp", lambda e: e.dma_start(out=ctab, in_=IN('ctab')), B_ct, w=[B_ct])
        KT = [sb("B_KT%d" % i, [128, S_len], BF16) for i in range(2)]
        VP = [sb("B_VP%d" % i, [128, NT, 129], BF16) for i in range(2)]
        QT = [sb("B_QT%d" % i, [128, To], BF16) for i in range(2)]
        V1 = [sb("B_V1%d" % i, [128, NTo, 129], BF16) for i in range(2)]
        B_KT = S.bufs("B_KT", 2)
        B_VP = S.bufs("B_VP", 2)
        B_QT = S.bufs("B_QT", 2)
        B_V1 = S.bufs("B_V1", 2)
        gs = sb("B_gs", [128, 2, NBP], F32)
        B_gs = S.buf("B_gs")
        top8 = sb("B_top8", [128, 2, 8], F32)
        B_t8 = S.buf("B_top8")
        sel = sb("B_sel", [128, 2, NBP], F32)
        B_sel = S.buf("B_sel")
        mm = [sb("B_m%d" % i, [128, 2, NBP], F32) for i in range(2)]
        B_m = S.bufs("B_m", 2)
        pT = [sb("B_pT%d" % i, [128, 512], BF16) for i in range(2)]
        B_pT = S.bufs("B_pT", 2)
        acc = [sb("B_acc%d" % i, [128, 2, 129], F32) for i in range(2)]
        B_acc = [S.bufs("B_acc%d_" % i, 2) for i in range(2)]
        rec = sb("B_rec", [128, 2], F32)
        B_rec = S.buf("B_rec")
        oa = [sb("B_oa%d" % i, [128, 2, 128], F32) for i in range(2)]
        B_oa = S.bufs("B_oa", 2)
        itc = [0]

        def load_head(h):
            j = h % 2
            S.dma("sp", lambda e: e.dma_start(out=KT[j], in_=KT_d[h]), B_KT[j], w=[B_KT[j]])
            S.dma("sp", lambda e: e.dma_start(out=QT[j], in_=QT_d[h]), B_QT[j], w=[B_QT[j]])
            S.dma("sp", lambda e: e.dma_start(out=VP[j], in_=VP_d[h]), B_VP[j], w=[B_VP[j]])
            S.dma("sp", lambda e: e.dma_start(out=V1[j], in_=V1_d[h]), B_V1[j], w=[B_V1[j]])

        items = []
        for h in range(H):
            for i in range(NBo):
                a = (h * NBo + i) % 2
                cands = list(range(2 * i)) + [2 * i + 1]
                items.append(dict(h=h, i=i, a=a, own=True, s=2 * i, last=False))
                for ci, s_ in enumerate(cands):
                    items.append(dict(h=h, i=i, a=a, own=False, s=s_, last=(ci == len(cands) - 1)))

        def emit_S(k):
            it = items[k]
            h, i, j = it["h"], it["i"], it["h"] % 2
            sbk = 1 + k % 2
            q0 = i * 256
            if it["own"]:
                for t in range(2):
                    S.op("pe", lambda e: e.matmul(pb[0][:, t * 256:t * 256 + NBP], lhsT=QT[j][:, q0 + t * 128:q0 + (t + 1) * 128],
                                                  rhs=kmb[:, h, :], start=True, stop=True), r=[B_QT[j], B_kmb], w=[PB[0]])
                S.op("pe", lambda e: e.matmul(pb[sbk][:, 0:256], lhsT=KT[j][:, (2 * i) * 256:(2 * i) * 256 + 128], rhs=QT[j][:, q0:q0 + 256],
                                              start=True, stop=True), r=[B_KT[j], B_QT[j]], w=[PB[sbk]])
                S.op("pe", lambda e: e.matmul(pb[sbk][:, 384:512], lhsT=KT[j][:, (2 * i) * 256 + 128:(2 * i) * 256 + 256],
                                              rhs=QT[j][:, q0 + 128:q0 + 256], start=True, stop=True), r=[B_KT[j], B_QT[j]], w=[PB[sbk]])
            else:
                s_ = it["s"]
                for kh in range(2):
                    S.op("pe", lambda e: e.matmul(pb[sbk][:, kh * 256:(kh + 1) * 256], lhsT=KT[j][:, s_ * 256 + kh * 128:s_ * 256 + (kh + 1) * 128],
                                                  rhs=QT[j][:, q0:q0 + 256], start=True, stop=True), r=[B_KT[j], B_QT[j]], w=[PB[sbk]])

        def emit_exp(k):
            it = items[k]
            h = it["h"]
            sbk = 1 + k % 2
            pt = pT[k % 2]
            B_pt_ = B_pT[k % 2]
            if it["own"]:
                S.op("act", lambda e: e.activation(out=pt[:, 0:256], in_=pb[sbk][:, 0:256], func=AF.Exp, scale=scale), r=[PB[sbk]], w=[B_pt_])
                S.op("act", lambda e: e.activation(out=pt[:, 384:512], in_=pb[sbk][:, 384:512], func=AF.Exp, scale=scale), r=[PB[sbk]], w=[B_pt_])
                S.op("dve", lambda e: e.tensor_tensor(out=pt[:, 0:256], in0=pt[:, 0:256], in1=ctab[:, h, :, :].rearrange("p a b -> p (a b)"), op=ALU.mult),
                     r=[B_pt_, B_ct], w=[B_pt_])
                S.op("dve", lambda e: e.tensor_tensor(out=pt[:, 384:512], in0=pt[:, 384:512], in1=ctab[:, h, 0, :], op=ALU.mult),
                     r=[B_pt_, B_ct], w=[B_pt_])
            else:
                S.op("act", lambda e: e.activation(out=pt, in_=pb[sbk], func=AF.Exp, scale=scale), r=[PB[sbk]], w=[B_pt_])

        def emit_rest(k):
            it = items[k]
            h, i, a, j = it["h"], it["i"], it["a"], it["h"] % 2
            obk = 3 + k % 2
            pt = pT[k % 2]
            B_pt_ = B_pT[k % 2]
            q0 = i * 256
            if it["own"]:
                if i == 0 and h + 1 < H:
                    load_head(h + 1)
                gv = pb[0].rearrange("p (t n) -> p t n", n=256)[:, :, 0:NBP]
                S.op("dve", lambda e: e.tensor_tensor(out=gs, in0=gv, in1=gbias[:, i, :].unsqueeze(1).to_broadcast([128, 2, NBP]), op=ALU.add),
                     r=[PB[0], B_gb], w=[B_gs])
                for t in range(2):
                    S.op("dve", lambda e: e.max(out=top8[:, t, :], in_=gs[:, t, :]), r=[B_gs], w=[B_t8])
                for t in range(2):
                    S.op("dve", lambda e: e.tensor_scalar(out=sel[:, t, :], in0=gs[:, t, :], scalar1=top8[:, t, 2:3], scalar2=None, op0=ALU.is_ge),
                         r=[B_gs, B_t8], w=[B_sel])
                S.op("dve", lambda e: e.tensor_tensor(out=mm[a], in0=sel, in1=mtab[:, i, :, h, :], op=ALU.mult), r=[B_sel, B_mt], w=[B_m[a]])
                S.op("pe", lambda e: e.matmul(pb[obk][:, 0:129], lhsT=pt[:, 0:128], rhs=V1[j][:, 2 * i, :], start=True, stop=True),
                     r=[B_pt_, B_V1[j]], w=[PB[obk]])
                S.op("pe", lambda e: e.matmul(pb[obk][:, 256:385], lhsT=pt[:, 128:256], rhs=V1[j][:, 2 * i, :], start=True, stop=False),
                     r=[B_pt_, B_V1[j]], w=[PB[obk]])
                S.op("pe", lambda e: e.matmul(pb[obk][:, 256:385], lhsT=pt[:, 384:512], rhs=V1[j][:, 2 * i + 1, :], start=False, stop=True),
                     r=[B_pt_, B_V1[j]], w=[PB[obk]])
                ov = pb[obk].rearrange("p (t n) -> p t n", n=256)[:, :, 0:129]
                S.op("dve", lambda e: e.tensor_copy(out=acc[a], in_=ov), r=[PB[obk]], w=[B_acc[a][0], B_acc[a][1]])
            else:
                s_ = it["s"]
                for t in range(2):
                    for kh in range(2):
                        S.op("pe", lambda e: e.matmul(pb[obk][:, t * 256:t * 256 + 129], lhsT=pt[:, kh * 256 + t * 128:kh * 256 + (t + 1) * 128],
                                                      rhs=VP[j][:, s_ * 2 + kh, :], start=(kh == 0), stop=(kh == 1)),
                             r=[B_pt_, B_VP[j]], w=[PB[obk]])
                for t in range(2):
                    S.op("dve", lambda e: e.scalar_tensor_tensor(out=acc[a][:, t, :], in0=pb[obk][:, t * 256:t * 256 + 129],
                                                                 scalar=mm[a][:, t, s_:s_ + 1], in1=acc[a][:, t, :],
                                                                 op0=ALU.mult, op1=ALU.add), r=[PB[obk], B_m[a], B_acc[a][t]], w=[B_acc[a][t]])
            if it["last"]:
                S.op("dve", lambda e: e.reciprocal(out=rec, in_=acc[a][:, :, 128]), r=[B_acc[a][0], B_acc[a][1]], w=[B_rec])
                for t in range(2):
                    S.op("dve", lambda e: e.tensor_scalar(out=oa[a][:, t, :], in0=acc[a][:, t, 0:128], scalar1=rec[:, t:t + 1], scalar2=None, op0=ALU.mult),
                         r=[B_acc[a][t], B_rec], w=[B_oa[a]])
                S.dma("sp", lambda e: e.dma_start(out=OA_d[q0:q0 + 256, h * 128:(h + 1) * 128].rearrange("(t p) c -> p t c", p=128), in_=oa[a]),
                      B_oa[a], r=[B_oa[a]])

        load_head(0)
        emit_S(0)
        for k in range(len(items)):
            emit_exp(k)
            if k + 1 < len(items):
                emit_S(k + 1)
            emit_rest(k)
        S.end_phase()

    WIDX = nc.alloc_sbuf_tensor("G_widx", [128, 128], I32).ap()
    B_widx = Buf("G_widx")
    DESTI = nc.alloc_sbuf_tensor("G_desti", [128, NTo, 2], I32).ap()
    B_desti = Buf("G_desti")

    def phase_C():
        Wo = sb("C_Wo", [128, 16, D], BF16)
        B_Wo = S.buf("C_Wo")
        for k in range(16):
            S.dma("pool", lambda e: e.dma_start(out=Wo[:, k, :], in_=IN('w_out')[k * 128:(k + 1) * 128, :]), B_Wo, w=[B_Wo])
        beta = sb("C_beta", [128, AW], F32)
        B_beta = S.buf("C_beta")
        S.dma("sp", lambda e: e.dma_start(out=beta, in_=IN('beta_attn').partition_broadcast(128)), B_beta, w=[B_beta])
        gffn = sb("C_gffn", [128, D], F32)
        B_gffn = S.buf("C_gffn")
        S.dma("sp", lambda e: e.dma_start(out=gffn, in_=IN('g_ffn').partition_broadcast(128)), B_gffn, w=[B_gffn])
        Wr32 = sb("C_Wr32", [128, 16, 72], F32)
        B_Wr32 = S.buf("C_Wr32")
        S.dma("sp", lambda e: e.dma_start(out=Wr32[:, :, 0:8], in_=IN('w_rg').rearrange("(k p) e -> p k e", p=128)), B_Wr32, w=[B_Wr32])
        for g in range(NG):
            S.dma("sp", lambda e: e.dma_start(out=Wr32[:, :, 8 + g * 8:16 + g * 8], in_=IN('w_re')[g].rearrange("(k p) e -> p k e", p=128)),
                  B_Wr32, w=[B_Wr32])
        Wr = sb("C_Wr", [128, 16, 72], BF16)
        B_Wr = S.buf("C_Wr")
        S.op("dve", lambda e: e.tensor_copy(out=Wr, in_=Wr32), r=[B_Wr32], w=[B_Wr])
        brb = sb("C_brb", [128, 72], F32)
        B_brb = S.buf("C_brb")
        S.dma("sp", lambda e: e.dma_start(out=brb[:, 0:8], in_=IN('b_rg').partition_broadcast(128)), B_brb, w=[B_brb])
        S.dma("sp", lambda e: e.dma_start(out=brb[:, 8:72], in_=IN('b_re').partition_broadcast(128)), B_brb, w=[B_brb])
        ltb = sb("C_ltb", [128, 128], BF16)
        B_ltb = S.buf("C_ltb")
        S.dma("pool", lambda e: e.dma_start(out=ltb, in_=IN('ltri')), B_ltb, w=[B_ltb])
        onesb = sb("C_ones", [128, 128], BF16)
        B_ones = S.buf("C_ones")
        S.op("dve", lambda e: e.memset(onesb, 1.0), w=[B_ones])
        pidx = sb("C_pidx", [128, 1], F32)
        B_pidx = S.buf("C_pidx")
        S.dma("sp", lambda e: e.dma_start(out=pidx, in_=IN('pidx')), B_pidx, w=[B_pidx])
        bpos = sb("C_bpos", [128, 1], F32)
        B_bpos = S.buf("C_bpos")
        S.dma("sp", lambda e: e.dma_start(out=bpos, in_=IN('bpos')), B_bpos, w=[B_bpos])
        zer = sb("C_zer", [128, (PL // 128) * 2], F32)
        B_zer = S.buf("C_zer")
        S.op("dve", lambda e: e.memset(zer, 0.0), w=[B_zer])
        B_slotd = S.buf("C_slotd")
        S.dma("sp", lambda e: e.dma_start(out=SLOT_d.rearrange("(p n) c -> p (n c)", p=128), in_=zer), B_zer, r=[B_zer], w=[B_slotd])
        OHK = sb("C_OHK", [128, NTo, 2, 64], F32)
        B_OHK = S.buf("C_OHK")
        RK = sb("C_RK", [128, NTo, 2], F32)
        B_RK = S.buf("C_RK")
        WK = sb("C_WK", [128, NTo, 2], F32)
        B_WK = S.buf("C_WK")
        ohacc = sb("C_ohacc", [128, 64], BF16)
        B_ohacc = S.buf("C_ohacc")
        S.op("dve", lambda e: e.memset(ohacc, 0.0), w=[B_ohacc])
        oa = [sb("C_oa%d" % i, [128, AW], F32) for i in range(2)]
        B_oa = S.bufs("C_oa", 2)
        mx = [sb("C_mx%d" % i, [128, D], BF16) for i in range(2)]
        B_mx = S.bufs("C_mx", 2)
        xt = [sb("C_x%d" % i, [128, D], F32) for i in range(2)]
        B_xt = S.bufs("C_x", 2)
        mTs = [sb("C_mT%d" % i, [128, 16, 128], BF16) for i in range(2)]
        B_mTs = S.bufs("C_mT", 2)
        h1 = [sb("C_h1%d" % i, [128, D], F32) for i in range(2)]
        B_h1 = S.bufs("C_h1", 2)
        fb = [sb("C_f%d" % i, [128, D], BF16) for i in range(2)]
        B_fb = S.bufs("C_f", 2)
        fT = sb("C_fT", [128, 16, 128], BF16)
        B_fT = S.buf("C_fT")
        st = [sb("C_st%d" % i, [128, 8], F32) for i in range(2)]
        B_st = S.bufs("C_st", 2)
        st2 = [sb("C_su%d" % i, [128, 8], F32) for i in range(2)]
        B_st2 = S.bufs("C_su", 2)
        lg = sb("C_lg", [128, 72], F32)
        B_lg = S.buf("C_lg")
        rs = sb("C_rs", [128, 32], F32)
        B_rs = S.buf("C_rs")
        ohg = sb("C_ohg", [128, 8], F32)
        B_ohg = S.buf("C_ohg")
        tmp88 = sb("C_tmp88", [128, 8, 8], F32)
        B_t88 = S.buf("C_tmp88")
        le = sb("C_le", [128, 8], F32)
        B_le = S.buf("C_le")
        t8 = sb("C_t8", [128, 8], F32)
        B_t8 = S.buf("C_t8")
        ohk = sb("C_ohk", [128, 2, 8], F32)
        B_ohk = S.buf("C_ohk")
        ohs = sb("C_ohs", [128, 64], BF16)
        B_ohs = S.buf("C_ohs")
        cum = sb("C_cum", [128, 64], F32)
        B_cum = S.buf("C_cum")
        tmp64 = sb("C_tmp64", [128, 64], F32)
        B_t64 = S.buf("C_tmp64")

        def load(j):
            b = j % 2
            i, t = j // 2, j % 2
            xr = i * 512 + t * 128
            S.dma("sp", lambda e: e.dma_start(out=oa[b], in_=OA_d[j * 128:(j + 1) * 128, :]), B_oa[b], w=[B_oa[b]])
            S.dma("sp", lambda e: e.dma_start(out=xt[b], in_=IN('x_perm')[xr:xr + 128, :]), B_xt[b], w=[B_xt[b]])
            S.dma("sp", lambda e: e.dma_start(out=mx[b][:, AW:D], in_=MP_d[j * 128:(j + 1) * 128, :]), B_mx[b], w=[B_mx[b]])

        def stageA(j):
            b = j % 2
            mT = mTs[b]
            B_mT = B_mTs[b]
            rmsnorm_rstd(oa[b], B_oa[b], mx[b][:, 0:AW], B_mx[b], st[b], B_st[b], AW)
            S.op("dve", lambda e: e.scalar_tensor_tensor(out=mx[b][:, 0:AW], in0=oa[b], scalar=st[b][:, 2:3], in1=beta, op0=ALU.mult, op1=ALU.mult),
                 r=[B_oa[b], B_st[b], B_beta], w=[B_mx[b]])
            transpose_to(mx[b], B_mx[b], 16, lambda k0, k1: mT[:, k0:k1, :], B_mT, [0, 1])

        load(0)
        if NTo > 1:
            load(1)
        stageA(0)
        for j in range(NTo):
            b = j % 2
            mT = mTs[b]
            B_mT = B_mTs[b]
            if j + 1 < NTo:
                stageA(j + 1)
            for n in range(4):
                bk = 2 + n
                for k in range(16):
                    S.op("pe", lambda e: e.matmul(pb[bk], lhsT=mT[:, k, :], rhs=Wo[:, k, n * 512:(n + 1) * 512], start=(k == 0), stop=(k == 15)),
                         r=[B_mT, B_Wo], w=[PB[bk]])
                S.op("dve", lambda e: e.tensor_tensor(out=h1[b][:, n * 512:(n + 1) * 512], in0=pb[bk], in1=xt[b][:, n * 512:(n + 1) * 512], op=ALU.add),
                     r=[PB[bk], B_xt[b]], w=[B_h1[b]])
            S.dma("sp", lambda e: e.dma_start(out=H1_d[j * 128:(j + 1) * 128, :], in_=h1[b]), B_h1[b], r=[B_h1[b]])
            if j + 2 < NTo:
                load(j + 2)
            rmsnorm_rstd(h1[b], B_h1[b], fb[b], B_fb[b], st2[b], B_st2[b], D)
            S.op("dve", lambda e: e.scalar_tensor_tensor(out=fb[b], in0=h1[b], scalar=st2[b][:, 2:3], in1=gffn, op0=ALU.mult, op1=ALU.mult),
                 r=[B_h1[b], B_st2[b], B_gffn], w=[B_fb[b]])
            S.dma("sp", lambda e: e.dma_start(out=F_d[j * 128:(j + 1) * 128, :], in_=fb[b]), B_fb[b], r=[B_fb[b]])
            transpose_to(fb[b], B_fb[b], 16, lambda k0, k1: fT[:, k0:k1, :], B_fT, [0, 1])
            bk = 6
            for k in range(16):
                S.op("pe", lambda e: e.matmul(pb[bk][:, 0:72], lhsT=fT[:, k, :], rhs=Wr[:, k, :], start=(k == 0), stop=(k == 15)),
                     r=[B_fT, B_Wr], w=[PB[bk]])
            S.op("dve", lambda e: e.tensor_tensor(out=lg, in0=pb[bk][:, 0:72], in1=brb, op=ALU.add), r=[PB[bk], B_brb], w=[B_lg])
            S.op("dve", lambda e: e.reduce_max(out=rs[:, 0:1], in_=lg[:, 0:8], axis=AX.X), r=[B_lg], w=[B_rs])
            S.op("dve", lambda e: e.tensor_scalar(out=ohg, in0=lg[:, 0:8], scalar1=rs[:, 0:1], scalar2=None, op0=ALU.is_equal), r=[B_lg, B_rs], w=[B_ohg])
            S.op("dve", lambda e: e.tensor_scalar(out=rs[:, 1:2], in0=rs[:, 0:1], scalar1=-1.0, scalar2=None, op0=ALU.mult), r=[B_rs], w=[B_rs])
            S.op("act", lambda e: e.activation(out=t8, in_=lg[:, 0:8], func=AF.Exp, bias=rs[:, 1:2], scale=1.0, accum_out=rs[:, 2:3]),
                 r=[B_lg, B_rs], w=[B_t8, B_rs])
            S.op("dve", lambda e: e.reciprocal(out=rs[:, 3:4], in_=rs[:, 2:3]), r=[B_rs], w=[B_rs])
            S.op("dve", lambda e: e.tensor_tensor(out=tmp88, in0=lg[:, 8:72].rearrange("p (g e) -> p g e", e=8),
                                                  in1=ohg.unsqueeze(2).to_broadcast([128, 8, 8]), op=ALU.mult), r=[B_lg, B_ohg], w=[B_t88])
            S.op("dve", lambda e: e.reduce_sum(out=le, in_=tmp88.rearrange("p g e -> p e g"), axis=AX.X), r=[B_t88], w=[B_le])
            S.op("dve", lambda e: e.max(out=t8, in_=le), r=[B_le], w=[B_t8])
            for k2 in range(2):
                S.op("dve", lambda e: e.tensor_scalar(out=ohk[:, k2, :], in0=le, scalar1=t8[:, k2:k2 + 1], scalar2=None, op0=ALU.is_equal),
                     r=[B_le, B_t8], w=[B_ohk])
            S.op("dve", lambda e: e.tensor_tensor(out=rs[:, 4:5], in0=t8[:, 1:2], in1=t8[:, 0:1], op=ALU.subtract), r=[B_t8], w=[B_rs])
            S.op("act", lambda e: e.activation(out=rs[:, 5:6], in_=rs[:, 4:5], func=AF.Exp), r=[B_rs], w=[B_rs])
            S.op("dve", lambda e: e.tensor_scalar(out=rs[:, 6:7], in0=rs[:, 5:6], scalar1=1.0, scalar2=None, op0=ALU.add), r=[B_rs], w=[B_rs])
            S.op("dve", lambda e: e.reciprocal(out=rs[:, 7:8], in_=rs[:, 6:7]), r=[B_rs], w=[B_rs])
            S.op("dve", lambda e: e.tensor_tensor(out=rs[:, 8:9], in0=rs[:, 5:6], in1=rs[:, 7:8], op=ALU.mult), r=[B_rs], w=[B_rs])
            S.op("dve", lambda e: e.tensor_tensor(out=WK[:, j, 0:1], in0=rs[:, 7:8], in1=rs[:, 3:4], op=ALU.mult), r=[B_rs], w=[B_WK])
            S.op("dve", lambda e: e.tensor_tensor(out=WK[:, j, 1:2], in0=rs[:, 8:9], in1=rs[:, 3:4], op=ALU.mult), r=[B_rs], w=[B_WK])
            for k2 in range(2):
                S.op("dve", lambda e: e.tensor_tensor(out=OHK[:, j, k2, :].rearrange("p (g e) -> p g e", e=8),
                                                      in0=ohg.unsqueeze(2).to_broadcast([128, 8, 8]),
                                                      in1=ohk[:, k2, :].unsqueeze(1).to_broadcast([128, 8, 8]), op=ALU.mult),
                     r=[B_ohg, B_ohk], w=[B_OHK])
            S.op("dve", lambda e: e.tensor_tensor(out=ohs, in0=OHK[:, j, 0, :], in1=OHK[:, j, 1, :], op=ALU.add), r=[B_OHK], w=[B_ohs])
            bk = 7
            S.op("pe", lambda e: e.matmul(pb[bk][:, 0:64], lhsT=ltb, rhs=ohs, start=True, stop=(j == 0)), r=[B_ltb, B_ohs], w=[PB[bk]])
            if j > 0:
                S.op("pe", lambda e: e.matmul(pb[bk][:, 0:64], lhsT=onesb, rhs=ohacc, start=False, stop=True), r=[B_ones, B_ohacc], w=[PB[bk]])
            S.op("dve", lambda e: e.tensor_copy(out=cum, in_=pb[bk][:, 0:64]), r=[PB[bk]], w=[B_cum])
            S.op("dve", lambda e: e.tensor_tensor(out=ohacc, in0=ohacc, in1=ohs, op=ALU.add), r=[B_ohacc, B_ohs], w=[B_ohacc])
            for k2 in range(2):
                S.op("dve", lambda e: e.tensor_tensor(out=tmp64, in0=OHK[:, j, k2, :], in1=cum, op=ALU.mult), r=[B_OHK, B_cum], w=[B_t64])
                S.op("dve", lambda e: e.reduce_sum(out=RK[:, j, k2:k2 + 1], in_=tmp64, axis=AX.X), r=[B_t64], w=[B_RK])
        bk = 7
        S.op("pe", lambda e: e.matmul(pb[bk][:, 0:64], lhsT=onesb, rhs=ohacc, start=True, stop=True), r=[B_ones, B_ohacc], w=[PB[bk]])
        cnt = sb("C_cnt", [128, 64], F32)
        B_cnt = S.buf("C_cnt")
        cnti = sb("C_cnti", [128, 64], I32)
        B_cnti = S.buf("C_cnti")
        padf = sb("C_padf", [128, 64], F32)
        B_padf = S.buf("C_padf")
        pend = sb("C_pend", [128, 64], F32)
        B_pend = S.buf("C_pend")
        pstart = sb("C_pstart", [128, 64], F32)
        B_pstart = S.buf("C_pstart")
        ones64 = sb("C_ones64", [128, 64], F32)
        B_o64 = S.buf("C_ones64")
        S.op("dve", lambda e: e.memset(ones64, 1.0), w=[B_o64])
        S.op("dve", lambda e: e.tensor_scalar(out=cnt, in0=pb[bk][:, 0:64], scalar1=127.0, scalar2=None, op0=ALU.add), r=[PB[bk]], w=[B_cnt])
        S.op("dve", lambda e: e.tensor_copy(out=cnti, in_=cnt), r=[B_cnt], w=[B_cnti])
        S.op("dve", lambda e: e.tensor_scalar(out=cnti, in0=cnti, scalar1=7, scalar2=7, op0=ALU.arith_shift_right, op1=ALU.logical_shift_left),
             r=[B_cnti], w=[B_cnti])
        S.op("dve", lambda e: e.tensor_copy(out=padf, in_=cnti), r=[B_cnti], w=[B_padf])
        S.op("dve", lambda e: e.tensor_tensor_scan(out=pend, data0=ones64, data1=padf, initial=0.0, op0=ALU.mult, op1=ALU.add),
             r=[B_o64, B_padf], w=[B_pend])
        S.op("dve", lambda e: e.tensor_tensor(out=pstart, in0=pend, in1=padf, op=ALU.subtract), r=[B_pend, B_padf], w=[B_pstart])
        S.op("dve", lambda e: e.tensor_scalar(out=tmp64, in0=pend, scalar1=bpos[:, 0:1], scalar2=None, op0=ALU.is_le), r=[B_pend, B_bpos], w=[B_t64])
        S.op("dve", lambda e: e.reduce_sum(out=rs[:, 10:11], in_=tmp64, axis=AX.X), r=[B_t64], w=[B_rs])
        S.op("dve", lambda e: e.tensor_scalar(out=rs[:, 11:12], in0=rs[:, 10:11], scalar1=63.0, scalar2=None, op0=ALU.min), r=[B_rs], w=[B_rs])
        S.op("dve", lambda e: e.tensor_scalar(out=rs[:, 13:14], in0=bpos, scalar1=-128.0, scalar2=None, op0=ALU.add), r=[B_bpos], w=[B_rs])
        S.op("dve", lambda e: e.tensor_scalar(out=tmp64, in0=pend, scalar1=rs[:, 13:14], scalar2=None, op0=ALU.is_le), r=[B_pend, B_rs], w=[B_t64])
        S.op("dve", lambda e: e.reduce_sum(out=rs[:, 14:15], in_=tmp64, axis=AX.X), r=[B_t64], w=[B_rs])
        S.op("dve", lambda e: e.tensor_scalar(out=rs[:, 15:16], in0=rs[:, 14:15], scalar1=63.0, scalar2=None, op0=ALU.min), r=[B_rs], w=[B_rs])
        S.op("dve", lambda e: e.tensor_tensor(out=rs[:, 16:17], in0=rs[:, 11:12], in1=rs[:, 15:16], op=ALU.not_equal), r=[B_rs], w=[B_rs])
        S.op("dve", lambda e: e.tensor_scalar(out=rs[:, 17:18], in0=pidx, scalar1=0.0, scalar2=None, op0=ALU.is_equal), r=[B_pidx], w=[B_rs])
        S.op("dve", lambda e: e.tensor_tensor(out=rs[:, 18:19], in0=rs[:, 16:17], in1=rs[:, 17:18], op=ALU.max), r=[B_rs], w=[B_rs])
        S.op("dve", lambda e: e.tensor_scalar(out=rs[:, 19:20], in0=rs[:, 11:12], scalar1=-64.0, scalar2=None, op0=ALU.add), r=[B_rs], w=[B_rs])
        S.op("dve", lambda e: e.tensor_tensor(out=rs[:, 20:21], in0=rs[:, 19:20], in1=rs[:, 18:19], op=ALU.mult), r=[B_rs], w=[B_rs])
        S.op("dve", lambda e: e.tensor_scalar(out=rs[:, 21:22], in0=rs[:, 20:21], scalar1=64.0, scalar2=None, op0=ALU.add), r=[B_rs], w=[B_rs])
        diag = sb("C_diag", [128, 128], BF16)
        B_diag = S.buf("C_diag")
        S.op("dve", lambda e: e.tensor_scalar(out=diag, in0=ident, scalar1=rs[:, 21:22], scalar2=None, op0=ALU.mult), r=[B_ident, B_rs], w=[B_diag])
        bk = 6
        S.op("pe", lambda e: e.matmul(pb[bk][:, 0:128], lhsT=onesb, rhs=diag, start=True, stop=True), r=[B_ones, B_diag], w=[PB[bk]])
        widf = sb("C_widf", [128, 128], F32)
        B_widf = S.buf("C_widf")
        S.op("dve", lambda e: e.tensor_scalar(out=widf, in0=pb[bk][:, 0:128], scalar1=128.0, scalar2=pidx[:, 0:1], op0=ALU.mult, op1=ALU.add),
             r=[PB[bk], B_pidx], w=[B_widf])
        S.op("dve", lambda e: e.tensor_copy(out=WIDX, in_=widf), r=[B_widf], w=[B_widx])
        destf = sb("C_destf", [128, NTo, 2], F32)
        B_destf = S.buf("C_destf")
        slt = [sb("C_slt%d" % i, [128, 2], F32) for i in range(4)]
        B_slt = S.bufs("C_slt", 4)
        for j in range(NTo):
            for k2 in range(2):
                S.op("dve", lambda e: e.tensor_tensor(out=tmp64, in0=OHK[:, j, k2, :], in1=pstart, op=ALU.mult), r=[B_OHK, B_pstart], w=[B_t64])
                S.op("dve", lambda e: e.reduce_sum(out=rs[:, 12:13], in_=tmp64, axis=AX.X), r=[B_t64], w=[B_rs])
                S.op("dve", lambda e: e.tensor_tensor(out=destf[:, j, k2:k2 + 1], in0=rs[:, 12:13], in1=RK[:, j, k2:k2 + 1], op=ALU.add),
                     r=[B_rs, B_RK], w=[B_destf])
        S.op("dve", lambda e: e.tensor_copy(out=DESTI, in_=destf), r=[B_destf], w=[B_desti])
        for j in range(NTo):
            for k2 in range(2):
                q = (j * 2 + k2) % 4
                S.op("dve", lambda e: e.tensor_scalar(out=slt[q][:, 0:1], in0=pidx, scalar1=float(j * 128), scalar2=None, op0=ALU.add),
                     r=[B_pidx], w=[B_slt[q]])
                S.op("dve", lambda e: e.tensor_copy(out=slt[q][:, 1:2], in_=WK[:, j, k2:k2 + 1]), r=[B_WK], w=[B_slt[q]])
                S.dma("pool", lambda e: e.indirect_dma_start(out=SLOT_d, out_offset=bass.IndirectOffsetOnAxis(ap=DESTI[:, j, k2:k2 + 1], axis=0),
                                                             in_=slt[q], in_offset=None),
                      B_slt[q], r=[B_slt[q], B_desti, B_slotd], w=[])
        if debug:
            S.dma("sp", lambda e: e.dma_start(out=RT_d.rearrange("(j p) c -> p j c", p=128)[:, :, 0:2], in_=destf), B_destf, r=[B_destf])
            S.dma("sp", lambda e: e.dma_start(out=RT_d.rearrange("(j p) c -> p j c", p=128)[:, :, 2:4], in_=WK), B_WK, r=[B_WK])
            S.dma("sp", lambda e: e.dma_start(out=RT_d[0:128, 4:6], in_=rs[:, 10:12]), B_rs, r=[B_rs])
        S.end_phase()

    def phase_D():
        wg = sb("D_wg", [128, 16 * DE], BF16)
        wu = sb("D_wu", [128, 16 * DE], BF16)
        wd = sb("D_wd", [128, 4 * D], BF16)
        B_wg = S.buf("D_wg")
        B_wu = S.buf("D_wu")
        B_wd = S.buf("D_wd")
        sl = [sb("D_sl%d" % i, [128, 2], F32) for i in range(2)]
        B_sl = S.bufs("D_sl", 2)
        ti = [sb("D_ti%d" % i, [128, 1], I32) for i in range(2)]
        B_ti = S.bufs("D_ti", 2)
        xg = [sb("D_xg%d" % i, [128, D], BF16) for i in range(2)]
        B_xg = S.bufs("D_xg", 2)
        xT = sb("D_xT", [128, 16, 128], BF16)
        B_xT = S.buf("D_xT")
        sg = sb("D_sg", [128, DE], F32)
        B_sg = S.buf("D_sg")
        hid = sb("D_hid", [128, DE], BF16)
        B_hid = S.buf("D_hid")
        hT = sb("D_hT", [128, 4, 128], BF16)
        B_hT = S.buf("D_hT")
        yb = [sb("D_y%d" % i, [128, D], F32) for i in range(2)]
        B_yb = S.bufs("D_y", 2)
        wgv = IN('w_eg').rearrange("e (p k) n -> (e p) (k n)", k=16)
        wuv = IN('w_eu').rearrange("e (p k) n -> (e p) (k n)", k=16)
        wdv = IN('w_ed').rearrange("e (p k) n -> (e p) (k n)", k=4)

        def load_x(b):
            j = b % 2
            S.dma("sp", lambda e: e.dma_start(out=sl[j], in_=SLOT_d[b * 128:(b + 1) * 128, :]), B_sl[j], w=[B_sl[j]])
            S.op("dve", lambda e: e.tensor_copy(out=ti[j], in_=sl[j][:, 0:1]), r=[B_sl[j]], w=[B_ti[j]])
            S.dma("pool", lambda e: e.indirect_dma_start(out=xg[j], out_offset=None, in_=F_d,
                                                         in_offset=bass.IndirectOffsetOnAxis(ap=ti[j][:, 0:1], axis=0)),
                  B_xg[j], r=[B_ti[j]], w=[B_xg[j]])

        def load_w(b, which):
            for (dst, B_dst, src) in which:
                S.dma("pool", lambda e: e.indirect_dma_start(out=dst, out_offset=None, in_=src,
                                                             in_offset=bass.IndirectOffsetOnAxis(ap=WIDX[:, b:b + 1], axis=0),
                                                             bounds_check=bc_reg, oob_is_err=False),
                      B_dst, r=[B_widx], w=[B_dst])

        bc_reg = nc.gpsimd.alloc_register("bc_reg")
        nc.gpsimd.reg_mov(bc_reg, NE * 128 - 1)
        GU = ((wg, B_wg, wgv), (wu, B_wu, wuv))
        DN = ((wd, B_wd, wdv),)
        load_x(0)
        load_w(0, GU)
        load_w(0, DN)
        for b in range(NBLK):
            j = b % 2
            if b + 1 < NBLK:
                load_x(b + 1)
            transpose_to(xg[j], B_xg[j], 16, lambda k0, k1: xT[:, k0:k1, :], B_xT, [0, 1], step=16)
            for (bk, wt, B_wt) in ((2, wg, B_wg), (3, wu, B_wu)):
                for k in range(16):
                    S.op("pe", lambda e: e.matmul(pb[bk], lhsT=xT[:, k, :], rhs=wt[:, k * DE:(k + 1) * DE], start=(k == 0), stop=(k == 15)),
                         r=[B_xT, B_wt], w=[PB[bk]])
            if b + 1 < NBLK:
                load_w(b + 1, GU)
            S.op("act", lambda e: e.activation(out=sg, in_=pb[2], func=AF.Silu), r=[PB[2]], w=[B_sg])
            S.op("dve", lambda e: e.tensor_tensor(out=hid, in0=pb[3], in1=sg, op=ALU.mult), r=[PB[3], B_sg], w=[B_hid])
            transpose_to(hid, B_hid, 4, lambda k0, k1: hT[:, k0:k1, :], B_hT, [0, 1], step=4)
            for n in range(4):
                bk = 4 + n
                for k in range(4):
                    S.op("pe", lambda e: e.matmul(pb[bk], lhsT=hT[:, k, :], rhs=wd[:, k * D + n * 512:k * D + (n + 1) * 512],
                                                  start=(k == 0), stop=(k == 3)), r=[B_hT, B_wd], w=[PB[bk]])
            if b + 1 < NBLK:
                load_w(b + 1, DN)
            for n in range(4):
                bk = 4 + n
                if n % 2 == 0:
                    S.op("act", lambda e: e.activation(out=yb[j][:, n * 512:(n + 1) * 512], in_=pb[bk], func=AF.Copy, scale=sl[j][:, 1:2]),
                         r=[PB[bk], B_sl[j]], w=[B_yb[j]])
                else:
                    S.op("dve", lambda e: e.tensor_scalar(out=yb[j][:, n * 512:(n + 1) * 512], in0=pb[bk], scalar1=sl[j][:, 1:2], scalar2=None, op0=ALU.mult),
                         r=[PB[bk], B_sl[j]], w=[B_yb[j]])
            S.dma("sp", lambda e: e.dma_start(out=Y_d[b * 128:(b + 1) * 128, :], in_=yb[j]), B_yb[j], r=[B_yb[j]])
        S.end_phase()

    def phase_E():
        Wpg = sb("E_Wpg", [128, 16, D], BF16)
        B_Wpg = S.buf("E_Wpg")
        for k in range(16):
            S.dma("pool", lambda e: e.dma_start(out=Wpg[:, k, :], in_=IN('w_pg')[k * 128:(k + 1) * 128, :]), B_Wpg, w=[B_Wpg])
        Wpl = sb("E_Wpl", [128, 2, D], BF16)
        B_Wpl = S.buf("E_Wpl")
        for k in range(2):
            S.dma("pool", lambda e: e.dma_start(out=Wpl[:, k, :], in_=IN('w_ple')[k * 128:(k + 1) * 128, :]), B_Wpl, w=[B_Wpl])
        gple = sb("E_gple", [128, D], F32)
        bpg = sb("E_bpg", [128, D], F32)
        gfin = sb("E_gfin", [128, D], F32)
        B_gple = S.buf("E_gple")
        B_bpg = S.buf("E_bpg")
        B_gfin = S.buf("E_gfin")
        S.dma("sp", lambda e: e.dma_start(out=gple, in_=IN('g_ple').partition_broadcast(128)), B_gple, w=[B_gple])
        S.dma("sp", lambda e: e.dma_start(out=bpg, in_=IN('b_pg').partition_broadcast(128)), B_bpg, w=[B_bpg])
        S.dma("sp", lambda e: e.dma_start(out=gfin, in_=IN('g_final').partition_broadcast(128)), B_gfin, w=[B_gfin])
        h2 = [sb("E_h2%d" % i, [128, D], F32) for i in range(2)]
        y1 = [sb("E_y1%d" % i, [128, D], F32) for i in range(2)]
        y2 = [sb("E_y2%d" % i, [128, D], F32) for i in range(2)]
        pbf = [sb("E_p%d" % i, [128, PLE], BF16) for i in range(2)]
        B_h2 = S.bufs("E_h2", 2)
        B_y1 = S.bufs("E_y1", 2)
        B_y2 = S.bufs("E_y2", 2)
        B_pbf = S.bufs("E_p", 2)
        hn = sb("E_hn", [128, D], BF16)
        B_hn = S.buf("E_hn")
        hTs = [sb("E_hT%d" % i, [128, 16, 128], BF16) for i in range(2)]
        B_hTs = S.bufs("E_hT", 2)
        pTs = [sb("E_pT%d" % i, [128, 2, 128], BF16) for i in range(2)]
        B_pTs = S.bufs("E_pT", 2)
        gl = sb("E_gl", [128, 512], F32)
        B_gl = S.buf("E_gl")
        sgm = sb("E_sg", [128, 512], F32)
        B_sgm = S.buf("E_sg")
        h3 = sb("E_h3", [128, D], F32)
        B_h3 = S.buf("E_h3")
        ob = [sb("E_o%d" % i, [128, D], F32) for i in range(2)]
        B_ob = S.bufs("E_o", 2)
        st = [sb("E_st%d" % i, [128, 8], F32) for i in range(2)]
        B_st = S.bufs("E_st", 2)
        st2 = [sb("E_su%d" % i, [128, 8], F32) for i in range(2)]
        B_st2 = S.bufs("E_su", 2)

        def load(j):
            b = j % 2
            S.dma("sp", lambda e: e.dma_start(out=h2[b], in_=H1_d[j * 128:(j + 1) * 128, :]), B_h2[b], w=[B_h2[b]])
            S.dma("pool", lambda e: e.dma_start(out=pbf[b], in_=IN('p_own')[j * 128:(j + 1) * 128, :]), B_pbf[b], w=[B_pbf[b]])
            S.dma("pool", lambda e: e.indirect_dma_start(out=y1[b], out_offset=None, in_=Y_d,
                                                         in_offset=bass.IndirectOffsetOnAxis(ap=DESTI[:, j, 0:1], axis=0)),
                  B_y1[b], r=[B_desti], w=[B_y1[b]])
            S.dma("pool", lambda e: e.indirect_dma_start(out=y2[b], out_offset=None, in_=Y_d,
                                                         in_offset=bass.IndirectOffsetOnAxis(ap=DESTI[:, j, 1:2], axis=0)),
                  B_y2[b], r=[B_desti], w=[B_y2[b]])

        def stage1(j):
            b = j % 2
            S.op("dve", lambda e: e.tensor_tensor(out=h2[b], in0=h2[b], in1=y1[b], op=ALU.add), r=[B_h2[b], B_y1[b]], w=[B_h2[b]])
            S.op("dve", lambda e: e.tensor_tensor(out=h2[b], in0=h2[b], in1=y2[b], op=ALU.add), r=[B_h2[b], B_y2[b]], w=[B_h2[b]])
            rmsnorm_rstd(h2[b], B_h2[b], hn, B_hn, st[b], B_st[b], D)
            S.op("dve", lambda e: e.scalar_tensor_tensor(out=hn, in0=h2[b], scalar=st[b][:, 2:3], in1=gple, op0=ALU.mult, op1=ALU.mult),
                 r=[B_h2[b], B_st[b], B_gple], w=[B_hn])
            transpose_to(hn, B_hn, 16, lambda k0, k1: hTs[b][:, k0:k1, :], B_hTs[b], [0, 1])
            transpose_to(pbf[b], B_pbf[b], 2, lambda k0, k1: pTs[b][:, k0:k1, :], B_pTs[b], [0, 1])

        load(0)
        if NTo > 1:
            load(1)
        stage1(0)
        for j in range(NTo):
            b = j % 2
            hT, B_hT, pT, B_pT = hTs[b], B_hTs[b], pTs[b], B_pTs[b]
            if j + 1 < NTo:
                stage1(j + 1)
            for n in range(4):
                bg = 2 + (n % 2) * 2
                bp_ = 3 + (n % 2) * 2
                for k in range(16):
                    S.op("pe", lambda e: e.matmul(pb[bg], lhsT=hT[:, k, :], rhs=Wpg[:, k, n * 512:(n + 1) * 512], start=(k == 0), stop=(k == 15)),
                         r=[B_hT, B_Wpg], w=[PB[bg]])
                for k in range(2):
                    S.op("pe", lambda e: e.matmul(pb[bp_], lhsT=pT[:, k, :], rhs=Wpl[:, k, n * 512:(n + 1) * 512], start=(k == 0), stop=(k == 1)),
                         r=[B_pT, B_Wpl], w=[PB[bp_]])
                cs = slice(n * 512, (n + 1) * 512)
                S.op("dve", lambda e: e.tensor_tensor(out=gl, in0=pb[bg], in1=bpg[:, cs], op=ALU.add), r=[PB[bg], B_bpg], w=[B_gl])
                S.op("act", lambda e: e.activation(out=sgm, in_=gl, func=AF.Sigmoid), r=[B_gl], w=[B_sgm])
                S.op("dve", lambda e: e.tensor_tensor(out=sgm, in0=pb[bp_], in1=sgm, op=ALU.mult), r=[PB[bp_], B_sgm], w=[B_sgm])
                S.op("dve", lambda e: e.tensor_tensor(out=h3[:, cs], in0=h2[b][:, cs], in1=sgm, op=ALU.add), r=[B_h2[b], B_sgm], w=[B_h3])
            if j + 2 < NTo:
                load(j + 2)
            rmsnorm_rstd(h3, B_h3, ob[b], B_ob[b], st2[b], B_st2[b], D)
            S.op("dve", lambda e: e.scalar_tensor_tensor(out=ob[b], in0=h3, scalar=st2[b][:, 2:3], in1=gfin, op0=ALU.mult, op1=ALU.mult),
                 r=[B_h3, B_st2[b], B_gfin], w=[B_ob[b]])
            S.dma("sp", lambda e: e.dma_start(out=out[j * 128:(j + 1) * 128, :], in_=ob[b]), B_ob[b], r=[B_ob[b]])
        S.end_phase()

    phases = [("A", phase_A), ("A2", phase_A2), ("B", phase_B), ("C", phase_C), ("D", phase_D), ("E", phase_E)]
    for name, fn in phases:
        fn()
        c.stack.close()
        c.stack = contextlib.ExitStack()
        if name == upto:
            break
    return nc, S, c


def make_in_maps(inputs, S_len, used=None):
    x = np.asarray(inputs["x"], np.float32)
    Bn = x.shape[0]
    NB = S_len // 256
    NBo = NB // 2
    p = np.asarray(inputs["p"], np.float32)[0]
    sq = lambda k: np.ascontiguousarray(np.asarray(inputs[k], np.float32)[0])
    row = lambda a: np.ascontiguousarray(a.reshape(1, -1))
    shared = dict(
        g_mix=row(sq("g_mix")), w_in=sq("w_in"), beta_attn=row(sq("beta_attn")), w_pool=sq("w_pool"),
        pool_scale=row(sq("pool_scale")), w_out=sq("w_out"), g_ffn=row(sq("g_ffn")),
        w_rg=sq("w_router_group"), b_rg=row(sq("b_router_group")), w_re=sq("w_router_expert"),
        b_re=row(sq("b_router_expert")), w_eg=sq("w_expert_gate"), w_eu=sq("w_expert_up"),
        w_ed=sq("w_expert_down"), g_ple=row(sq("g_ple")), w_ple=sq("w_ple"), w_pg=sq("w_ple_gate"),
        b_pg=row(sq("b_ple_gate")), g_final=row(np.asarray(inputs["g_final"], np.float32)),
    )
    tabs = [host_tables(S_len, r) for r in range(2)]
    in_maps = []
    orders = []
    for cidx in range(2 * Bn):
        b, r = cidx // 2, cidx % 2
        order = []
        for i in range(NBo):
            order += [2 * i + r, 2 * i + 1 - r]
        own = [2 * i + r for i in range(NBo)]
        xb = x[b].reshape(NB, 256, D)
        pb_ = p[b].reshape(NB, 256, PLE)
        m = dict(shared)
        m["x_perm"] = np.ascontiguousarray(xb[order].reshape(S_len, D))
        m["p_own"] = np.ascontiguousarray(pb_[own].reshape(-1, PLE))
        m.update(tabs[r])
        if used is not None:
            m = {k: v for k, v in m.items() if k in used}
        in_maps.append(m)
        orders.append(own)
    return in_maps, orders


def kernel(**inputs):
    x = np.asarray(inputs["x"])
    Bn, S_len, _ = x.shape
    nc, S, c = build(S_len, debug=False, upto="E")
    in_maps, orders = make_in_maps(inputs, S_len, used=set(c.used))
    ncores = 2 * Bn
    res = run_bass_kernel_spmd(nc, in_maps, core_ids=list(range(ncores)))
    outp = np.empty((Bn, S_len // 256, 256, D), np.float32)
    for cidx in range(ncores):
        b = cidx // 2
        o = np.asarray(res.results[cidx]["out"], np.float32).reshape(-1, 256, D)
        outp[b, orders[cidx]] = o
    return outp.reshape(Bn, S_len, D)
```

```python
import numpy as np
import concourse.bass as bass
import concourse.mybir as mybir
from concourse.bass_utils import run_bass_kernel_spmd

F32 = mybir.dt.float32
BF16 = mybir.dt.bfloat16
I32 = mybir.dt.int32
AF = mybir.ActivationFunctionType
ALU = mybir.AluOpType
AX = mybir.AxisListType

D = 2048
H = 8
HD = 128
AW = 1024
PW = 1024
INW = 4096
NE = 64
NG = 8
DE = 512
PLE = 256
EPS = 1e-6
import os
SKIP = os.environ.get('KSKIP', '')
NEG = -1.0e30


class Buf:
    __slots__ = ("name", "wev", "revs", "slot", "excl")

    def __init__(self, name, excl=False):
        self.name = name
        self.excl = excl
        self.wev = None
        self.revs = {}
        self.slot = None


class Sched:
    def __init__(self, nc):
        self.nc = nc
        self.eng = {"pe": nc.tensor, "act": nc.scalar, "dve": nc.vector, "pool": nc.gpsimd, "sp": nc.sync}
        self.esem = {k: nc.alloc_semaphore("es_" + k) for k in self.eng}
        self.ecnt = {k: 0 for k in self.eng}
        self.seen = {k: {} for k in self.eng}
        self.free_slots = []
        self.nslots = 0
        self.phase_bufs = []
        self.ninst = 0
        self.nwait = 0

    def buf(self, name, excl=False):
        b = Buf(name, excl)
        self.phase_bufs.append(b)
        return b

    def bufs(self, name, n, excl=False):
        return [self.buf("%s%d" % (name, i), excl) for i in range(n)]

    def _slot(self, b):
        if b.slot is None:
            if self.free_slots:
                b.slot = self.free_slots.pop()
            else:
                b.slot = [self.nc.alloc_semaphore("ds%d" % self.nslots), 0]
                self.nslots += 1
        return b.slot

    def _waits(self, e, r, w):
        need = {}

        def add(ev):
            s, v, src = ev
            if src == e and e == "pe":
                return
            k = id(s)
            if k not in need or need[k][1] < v:
                need[k] = (s, v)

        for b in r:
            if b.wev is not None:
                add(b.wev)
            if b.excl:
                for ev in b.revs.values():
                    if ev[2] != e:
                        add(ev)
        for b in w:
            if b.wev is not None:
                add(b.wev)
            for ev in b.revs.values():
                if ev[2] == e:
                    continue
                add(ev)
        seen = self.seen[e]
        for k, (s, v) in need.items():
            if seen.get(k, 0) >= v:
                continue
            self.eng[e].wait_ge(s, v)
            self.nwait += 1
            seen[k] = v

    def _record(self, ev, r, w):
        k = id(ev[0])
        for b in r:
            b.revs[k] = ev
        for b in w:
            b.wev = ev
            b.revs = {}

    def op(self, e, fn, r=(), w=()):
        self._waits(e, r, w)
        ins = fn(self.eng[e])
        self.ecnt[e] += 1
        ins.then_inc(self.esem[e], 1)
        self.ninst += 1
        self._record((self.esem[e], self.ecnt[e], e), r, w)
        return ins

    def dma(self, e, fn, sb, r=(), w=()):
        self._waits(e, r, w)
        slot = self._slot(sb)
        ins = fn(self.eng[e])
        slot[1] += 16
        ins.then_inc(slot[0], 16)
        self.ninst += 1
        self._record((slot[0], slot[1], "dma"), r, w)
        return ins

    def end_phase(self):
        for b in self.phase_bufs:
            if b.slot is not None:
                s, v = b.slot
                if self.seen["sp"].get(id(s), 0) < v:
                    self.eng["sp"].wait_ge(s, v)
                    self.seen["sp"][id(s)] = v
        self.ecnt["sp"] += 1
        self.eng["sp"].nop().then_inc(self.esem["sp"], 1)
        for e in self.eng:
            for f in self.eng:
                if f == e:
                    continue
                s, v = self.esem[f], self.ecnt[f]
                if v > 0 and self.seen[e].get(id(s), 0) < v:
                    self.eng[e].wait_ge(s, v)
                    self.seen[e][id(s)] = v
        for b in self.phase_bufs:
            if b.slot is not None:
                self.free_slots.append(b.slot)
                b.slot = None
        self.phase_bufs = []


class Ctx:
    pass


def alibi_slopes():
    return np.array([2.0 ** (-8.0 * (h + 1) / H) for h in range(H)], np.float64)


def host_tables(S_len, r):
    NB = S_len // 256
    NBo = NB // 2
    NBP = max(NB, 8)
    sl = alibi_slopes()
    seqblk = np.zeros(NB, np.int64)
    for i in range(NBo):
        seqblk[2 * i] = 2 * i + r
        seqblk[2 * i + 1] = 2 * i + 1 - r
    j = np.arange(256)
    vtab = np.exp(-sl[None, None, :] * (255 - (np.arange(2)[None, :, None] * 128 + np.arange(128)[:, None, None])))
    mtab = np.zeros((NBo, 2, 128, H, NBP), np.float64)
    gbias = np.full((NBo, 128, NBP), NEG, np.float64)
    for i in range(NBo):
        own = seqblk[2 * i]
        for s in range(NB):
            if seqblk[s] < own:
                gbias[i, :, s] = 0.0
                for t in range(2):
                    qpos = own * 256 + t * 128 + np.arange(128)
                    dist = qpos - (seqblk[s] * 256 + 255)
                    mtab[i, t, :, :, s] = np.exp(-sl[None, :] * dist[:, None])
    kj = np.arange(128)[:, None]
    qi = np.arange(128)[None, :]
    ctab = np.zeros((128, H, 2, 128), np.float64)
    for h in range(H):
        ctab[:, h, 0, :] = np.where(qi >= kj, np.exp(-sl[h] * np.maximum(qi - kj, 0)), 0.0)
        ctab[:, h, 1, :] = np.exp(-sl[h] * (128 + qi - kj))
    wins = np.array([2, 4, 8, 16])
    t = seqblk[0] * 256 + np.arange(256)
    cnt = np.minimum(t[None, :] + 1, wins[:, None]).astype(np.float64)
    ptab = np.broadcast_to((1.0 / cnt)[None], (128, 4, 256))
    halo = np.zeros((128, 2), np.float64)
    halo[:, 0] = 1.0 if r == 0 else 0.0
    halo[:, 1] = 0.0 if r == 0 else 1.0
    f = lambda a: np.ascontiguousarray(a, dtype=np.float32)
    lt = (np.arange(128)[:, None] < np.arange(128)[None, :]).astype(np.float32)
    return dict(ident=np.eye(128, dtype=np.float32), vtab=f(vtab), mtab=f(mtab), gbias=f(gbias),
                ctab=f(ctab), ptab=f(ptab), halo=f(halo), ltri=lt,
                bpos=f((np.arange(128) * 128.0).reshape(128, 1)),
                pidx=f(np.arange(128).reshape(128, 1)))


def build(S_len, debug=False, upto="E"):
    nc = bass.Bass("TRN2", target_bir_lowering=False)
    NB = S_len // 256
    NBo = NB // 2
    NBP = max(NB, 8)
    To = S_len // 2
    NTo = To // 128
    NT = S_len // 128
    PL = 2 * To + NE * 128
    NBLK = PL // 128
    skind = "ExternalOutput" if debug else "Internal"

    def din(name, shape, dt=F32):
        return nc.dram_tensor(name, list(shape), dt, kind="ExternalInput").ap()

    def dscr(name, shape, dt):
        return nc.dram_tensor(name, list(shape), dt, kind=skind).ap()

    c = Ctx()
    c.nc = nc
    in_shapes = dict(
        x_perm=[S_len, D], p_own=[To, PLE], g_mix=[1, D], w_in=[D, INW], beta_attn=[1, AW],
        w_pool=[4, 256, 256], pool_scale=[1, PW], w_out=[D, D], g_ffn=[1, D], w_rg=[D, NG], b_rg=[1, NG],
        w_re=[NG, D, 8], b_re=[1, NE], w_eg=[NE, D, DE], w_eu=[NE, D, DE], w_ed=[NE, DE, D], g_ple=[1, D],
        w_ple=[PLE, D], w_pg=[D, D], b_pg=[1, D], g_final=[1, D], ident=[128, 128], vtab=[128, 2, H],
        mtab=[NBo, 2, 128, H, NBP], gbias=[NBo, 128, NBP], ctab=[128, H, 2, 128], ptab=[128, 4, 256],
        halo=[128, 2], ltri=[128, 128], bpos=[128, 1], pidx=[128, 1])
    c.used = {}

    def IN(name):
        if name not in c.used:
            c.used[name] = din(name, in_shapes[name])
        return c.used[name]
    out = nc.dram_tensor("out", [To, D], F32, kind="ExternalOutput").ap()
    KT_d = dscr("KT_d", [H, 128, S_len], BF16)
    VP_d = dscr("VP_d", [H, 128, NT, 129], BF16)
    V1_d = dscr("V1_d", [H, 128, NTo, 129], BF16)
    QT_d = dscr("QT_d", [H, 128, To], BF16)
    UT_d = dscr("UT_d", [8, 128, NBo, 272], F32)
    KM_d = dscr("KM_d", [128, H, NBP], F32)
    OA_d = dscr("OA_d", [To, AW], F32)
    MP_d = dscr("MP_d", [To, PW], BF16)
    H1_d = dscr("H1_d", [To, D], F32)
    F_d = dscr("F_d", [To, D], BF16)
    SLOT_d = dscr("SLOT_d", [PL, 2], F32)
    Y_d = dscr("Y_d", [PL, D], F32)
    RT_d = dscr("RT_d", [To, 8], F32)

    S = Sched(nc)
    c.S = S
    import contextlib
    c.stack = contextlib.ExitStack()

    def sb(name, shape, dt):
        t = c.stack.enter_context(nc.sbuf_tensor(name, list(shape), dt))
        return t.ap() if hasattr(t, "ap") else t[:]
    pb = [nc.alloc_psum_tensor("pb%d" % i, [128, 512], F32).ap() for i in range(8)]
    PB = S.bufs("pbank", 8, excl=True)

    ident = nc.alloc_sbuf_tensor("identb", [128, 128], BF16).ap()
    B_ident = Buf("ident")
    S.dma("pool", lambda e: e.dma_start(out=ident, in_=IN('ident')), B_ident, w=[B_ident])

    def rmsnorm_rstd(e_xt, B_xt, junk, B_junk, st, B_st, width):
        S.op("act", lambda e: e.activation(out=junk, in_=e_xt, func=AF.Square, accum_out=st[:, 0:1]),
             r=[B_xt], w=[B_junk, B_st])
        S.op("act", lambda e: e.activation(out=st[:, 1:2], in_=st[:, 0:1], func=AF.Sqrt, scale=1.0 / width, bias=EPS),
             r=[B_st], w=[B_st])
        S.op("dve", lambda e: e.reciprocal(out=st[:, 2:3], in_=st[:, 1:2]), r=[B_st], w=[B_st])

    def transpose_to(src, B_src, nchunk, dst_fn, B_dst, tps, step=1):
        for g0 in range(0, nchunk, 8):
            n = min(8, nchunk - g0)
            bi = tps[(g0 // 8) % len(tps)]
            tpv = pb[bi].bitcast(BF16).rearrange("p (k n) -> p k n", n=128)
            for k in range(g0, g0 + n):
                if step == 1:
                    sv = src[:, k * 128:(k + 1) * 128]
                else:
                    sv = src[:, k:k + 127 * step + 1:step]
                S.op("pe", lambda e: e.transpose(tpv[:, k - g0, :], sv, ident), r=[B_src, B_ident], w=[PB[bi]])
            eng = "act" if (g0 // 8) % 2 == 0 else "dve"
            if eng == "act":
                S.op("act", lambda e: e.copy(out=dst_fn(g0, g0 + n), in_=tpv[:, 0:n, :]), r=[PB[bi]], w=[B_dst])
            else:
                S.op("dve", lambda e: e.tensor_copy(out=dst_fn(g0, g0 + n), in_=tpv[:, 0:n, :]), r=[PB[bi]], w=[B_dst])

    def phase_A():
        Wb = sb("A_Wb", [128, 16, INW], BF16)
        B_W = S.buf("A_Wb")
        for k in range(16):
            S.dma("pool", lambda e: e.dma_start(out=Wb[:, k, :], in_=IN('w_in')[k * 128:(k + 1) * 128, :]), B_W, w=[B_W])
        gmix = sb("A_gmix", [128, D], F32)
        B_g = S.buf("A_gmix")
        S.dma("sp", lambda e: e.dma_start(out=gmix, in_=IN('g_mix').partition_broadcast(128)), B_g, w=[B_g])
        vtab = sb("A_vtab", [128, 2, H], F32)
        B_vt = S.buf("A_vtab")
        S.dma("sp", lambda e: e.dma_start(out=vtab, in_=IN('vtab')), B_vt, w=[B_vt])
        xb = [sb("A_x%d" % i, [128, D], F32) for i in range(2)]
        B_x = S.bufs("A_x", 2)
        ab = [sb("A_a%d" % i, [128, D], BF16) for i in range(4)]
        B_a = S.bufs("A_a", 4)
        stt = [sb("A_st%d" % i, [128, 4], F32) for i in range(2)]
        B_st = S.bufs("A_st", 2)
        aT = sb("A_aT", [128, 16, 512], BF16)
        B_aT = S.buf("A_aT")
        NSTG = 3
        fst = [sb("A_fs%d" % i, [128, 512], BF16) for i in range(NSTG)]
        B_fs = S.bufs("A_fs", NSTG)
        ust = [sb("A_us%d" % i, [128, 272], F32) for i in range(2)]
        B_us = S.bufs("A_us", 2)
        vp = [sb("A_vp%d" % i, [128, H, 129], BF16) for i in range(2)]
        B_vp = S.bufs("A_vp", 2)
        v1 = [sb("A_v1%d" % i, [128, H, 129], BF16) for i in range(2)]
        B_v1 = S.bufs("A_v1", 2)
        km = sb("A_km", [128, H, NBP], F32)
        B_km = S.buf("A_km")
        kms = sb("A_kms", [128, 2], F32)
        B_kms = S.buf("A_kms")
        S.op("dve", lambda e: e.memset(km, 0.0), w=[B_km])
        for i in range(2):
            S.op("dve", lambda e: e.memset(v1[i][:, :, 128:129], 1.0), w=[B_v1[i]])
        mmb = [2, 3, 4, 5, 6, 7]
        mmi = [0]

        def nextbank():
            b = mmb[mmi[0] % len(mmb)]
            mmi[0] += 1
            return b

        fsi = [0]
        tcount = [0]

        def norm_tile(i, t):
            j = tcount[0] % 2
            tcount[0] += 1
            row0 = (i * 4 + t) * 128
            S.dma("sp", lambda e: e.dma_start(out=xb[j], in_=IN('x_perm')[row0:row0 + 128, :]), B_x[j], w=[B_x[j]])
            rmsnorm_rstd(xb[j], B_x[j], ab[t], B_a[t], stt[j], B_st[j], D)
            S.op("dve", lambda e: e.scalar_tensor_tensor(out=ab[t], in0=xb[j], scalar=stt[j][:, 2:3], in1=gmix,
                                                         op0=ALU.mult, op1=ALU.mult),
                 r=[B_x[j], B_st[j], B_g], w=[B_a[t]])

        def transposes(i):
            for t in range(4):
                transpose_to(ab[t], B_a[t], 16, lambda k0, k1: aT[:, k0:k1, t * 128:(t + 1) * 128], B_aT, [0, 1])

        for t in range(4):
            norm_tile(0, t)
        transposes(0)
        for i in range(NBo):
            for ch in range(H if 'k' not in SKIP else 0):
                bk = nextbank()
                for k in range(16):
                    S.op("pe", lambda e: e.matmul(pb[bk], lhsT=Wb[:, k, AW + ch * 128:AW + (ch + 1) * 128], rhs=aT[:, k, :],
                                                  start=(k == 0), stop=(k == 15)), r=[B_W, B_aT], w=[PB[bk]])
                f = fsi[0] % NSTG
                fsi[0] += 1
                S.op("act", lambda e: e.copy(out=fst[f], in_=pb[bk]), r=[PB[bk]], w=[B_fs[f]])
                S.op("dve", lambda e: e.reduce_sum(out=kms, in_=pb[bk].rearrange("p (b n) -> p b n", n=256), axis=AX.X),
                     r=[PB[bk], B_fs[f]], w=[B_kms])
                S.op("dve", lambda e: e.tensor_scalar(out=km[:, ch, 2 * i:2 * i + 2], in0=kms, scalar1=1.0 / 256, scalar2=None,
                                                      op0=ALU.mult), r=[B_kms], w=[B_km])
                S.dma("sp", lambda e: e.dma_start(out=KT_d[ch, :, i * 512:(i + 1) * 512], in_=fst[f]), B_fs[f], r=[B_fs[f]])
            if i + 1 < NBo:
                norm_tile(i + 1, 0)
            for ch in range(H if 'q' not in SKIP else 0):
                bk = nextbank()
                for k in range(16):
                    S.op("pe", lambda e: e.matmul(pb[bk][:, 0:256], lhsT=Wb[:, k, ch * 128:(ch + 1) * 128], rhs=aT[:, k, 0:256],
                                                  start=(k == 0), stop=(k == 15)), r=[B_W, B_aT], w=[PB[bk]])
                f = fsi[0] % NSTG
                fsi[0] += 1
                S.op("act", lambda e: e.copy(out=fst[f][:, 0:256], in_=pb[bk][:, 0:256]), r=[PB[bk]], w=[B_fs[f]])
                S.dma("sp", lambda e: e.dma_start(out=QT_d[ch, :, i * 256:(i + 1) * 256], in_=fst[f][:, 0:256]), B_fs[f], r=[B_fs[f]])
            if i + 1 < NBo:
                norm_tile(i + 1, 1)
            for ch in range(8 if 'u' not in SKIP else 0):
                bk = nextbank()
                for k in range(16):
                    S.op("pe", lambda e: e.matmul(pb[bk], lhsT=Wb[:, k, 3 * AW + ch * 128:3 * AW + (ch + 1) * 128], rhs=aT[:, k, :],
                                                  start=(k == 0), stop=(k == 15)), r=[B_W, B_aT], w=[PB[bk]])
                f = ch % 2
                S.op("act", lambda e: e.copy(out=ust[f][:, 0:256], in_=pb[bk][:, 0:256]), r=[PB[bk]], w=[B_us[f]])
                S.op("dve", lambda e: e.tensor_copy(out=ust[f][:, 256:272], in_=pb[bk][:, 496:512]), r=[PB[bk]], w=[B_us[f]])
                S.dma("sp", lambda e: e.dma_start(out=UT_d[ch, :, i, :], in_=ust[f]), B_us[f], r=[B_us[f]])
            if i + 1 < NBo:
                norm_tile(i + 1, 2)
            for t in range(4 if 'v' not in SKIP else 0):
                par = t % 2
                tile_g = i * 4 + t
                jj = tile_g % 2
                for half in range(2):
                    bk = nextbank()
                    for k in range(16):
                        S.op("pe", lambda e: e.matmul(pb[bk], lhsT=aT[:, k, t * 128:(t + 1) * 128],
                                                      rhs=Wb[:, k, 2 * AW + half * 512:2 * AW + (half + 1) * 512],
                                                      start=(k == 0), stop=(k == 15)), r=[B_W, B_aT], w=[PB[bk]])
                    pv = pb[bk].rearrange("p (h n) -> p h n", n=128)
                    hs = slice(half * 4, (half + 1) * 4)
                    S.op("dve", lambda e: e.tensor_tensor(out=vp[jj][:, hs, 0:128], in0=pv,
                                                          in1=vtab[:, par, hs].unsqueeze(2).to_broadcast([128, 4, 128]),
                                                          op=ALU.mult), r=[PB[bk], B_vt], w=[B_vp[jj]])
                    if t < 2:
                        S.op("act", lambda e: e.copy(out=v1[jj][:, hs, 0:128], in_=pv), r=[PB[bk], B_vp[jj]], w=[B_v1[jj]])
                S.op("dve", lambda e: e.tensor_copy(out=vp[jj][:, :, 128:129], in_=vtab[:, par, :].unsqueeze(2)),
                     r=[B_vt], w=[B_vp[jj]])
                S.dma("sp", lambda e: e.dma_start(out=VP_d[:, :, tile_g, :].rearrange("h p c -> p h c"), in_=vp[jj]),
                      B_vp[jj], r=[B_vp[jj]])
                if t < 2:
                    S.dma("sp", lambda e: e.dma_start(out=V1_d[:, :, i * 2 + t, :].rearrange("h p c -> p h c"), in_=v1[jj]),
                          B_v1[jj], r=[B_v1[jj]])
            if i + 1 < NBo:
                norm_tile(i + 1, 3)
                transposes(i + 1)
        S.dma("sp", lambda e: e.dma_start(out=KM_d, in_=km), B_km, r=[B_km])
        S.end_phase()


    def phase_A2():
        wpb = sb("P_wp", [128, 4, 2, 256], BF16)
        B_wp = S.buf("P_wp")
        S.dma("pool", lambda e: e.dma_start(out=wpb, in_=IN('w_pool').rearrange("g (cc p) d -> p g cc d", p=128)), B_wp, w=[B_wp])
        psc = sb("P_psc", [128, PW], F32)
        B_psc = S.buf("P_psc")
        S.dma("sp", lambda e: e.dma_start(out=psc, in_=IN('pool_scale').partition_broadcast(128)), B_psc, w=[B_psc])
        ptab = sb("P_ptab", [128, 4, 256], F32)
        B_pt = S.buf("P_ptab")
        S.dma("sp", lambda e: e.dma_start(out=ptab, in_=IN('ptab')), B_pt, w=[B_pt])
        halo = sb("P_halo", [128, 2], F32)
        B_ha = S.buf("P_halo")
        S.dma("sp", lambda e: e.dma_start(out=halo, in_=IN('halo')), B_ha, w=[B_ha])
        ub = [sb("P_ub%d" % i, [128, 8, 272], F32) for i in range(2)]
        B_ub = S.bufs("P_ub", 2)
        tl = [sb("P_tl%d" % i, [128, 8, 32], F32) for i in range(2)]
        B_tl = S.bufs("P_tl", 2)
        Pb = sb("P_P", [128, 8, 272], F32)
        B_P = S.buf("P_P")
        Qb = sb("P_Q", [128, 8, 272], F32)
        B_Q = S.buf("P_Q")
        zb = sb("P_z", [128, 8, 256], BF16)
        B_z = S.buf("P_z")
        zt = sb("P_zt", [128, 2, 256], F32)
        B_zt = S.buf("P_zt")
        mp = [sb("P_mp%d" % i, [128, PW], BF16) for i in range(2)]
        B_mp = S.bufs("P_mp", 2)
        tmpf = sb("P_tmpf", [128, PW], F32)
        B_tmpf = S.buf("P_tmpf")
        st = [sb("P_st%d" % i, [128, 8], F32) for i in range(2)]
        B_st = S.bufs("P_st", 2)
        wins = [2, 4, 8, 16]
        UTv = UT_d.rearrange("c p i n -> p c i n")
        for i in range(NBo):
            j = i % 2
            A = ub[j]
            S.dma("sp", lambda e: e.dma_start(out=A[:, :, 16:272], in_=UTv[:, :, i, 0:256]), B_ub[j], w=[B_ub[j]])
            S.dma("sp", lambda e: e.dma_start(out=tl[j][:, :, 16:32], in_=UTv[:, :, i, 256:272]), B_tl[j], w=[B_tl[j]])
            if i > 0:
                S.dma("sp", lambda e: e.dma_start(out=tl[j][:, :, 0:16], in_=UTv[:, :, i - 1, 256:272]), B_tl[j], w=[B_tl[j]])
            else:
                S.op("dve", lambda e: e.memset(tl[j][:, :, 0:16], 0.0), w=[B_tl[j]])
            S.op("dve", lambda e: e.tensor_scalar(out=A[:, :, 0:16], in0=tl[j][:, :, 0:16], scalar1=halo[:, 0:1], scalar2=None, op0=ALU.mult),
                 r=[B_tl[j], B_ha], w=[B_ub[j]])
            S.op("dve", lambda e: e.scalar_tensor_tensor(out=A[:, :, 0:16], in0=tl[j][:, :, 16:32], scalar=halo[:, 1:2], in1=A[:, :, 0:16],
                                                         op0=ALU.mult, op1=ALU.add), r=[B_tl[j], B_ha, B_ub[j]], w=[B_ub[j]])
            S.op("dve", lambda e: e.tensor_tensor(out=Pb[:, :, 1:272], in0=A[:, :, 1:272], in1=A[:, :, 0:271], op=ALU.add),
                 r=[B_ub[j]], w=[B_P])
            S.op("dve", lambda e: e.tensor_tensor(out=Qb[:, 2:8, 3:272], in0=Pb[:, 2:8, 3:272], in1=Pb[:, 2:8, 1:270], op=ALU.add),
                 r=[B_P], w=[B_Q])
            S.op("dve", lambda e: e.tensor_tensor(out=Pb[:, 4:8, 7:272], in0=Qb[:, 4:8, 7:272], in1=Qb[:, 4:8, 3:268], op=ALU.add),
                 r=[B_Q], w=[B_P])
            S.op("dve", lambda e: e.tensor_tensor(out=Qb[:, 6:8, 15:272], in0=Pb[:, 6:8, 15:272], in1=Pb[:, 6:8, 7:264], op=ALU.add),
                 r=[B_P], w=[B_Q])
            for g in range(4):
                W = Pb if g % 2 == 0 else Qb
                B_Wb = B_P if g % 2 == 0 else B_Q
                cs = slice(2 * g, 2 * g + 2)
                if i == 0:
                    S.op("dve", lambda e: e.tensor_tensor(out=zt, in0=W[:, cs, 16:272],
                                                          in1=ptab[:, g, :].unsqueeze(1).to_broadcast([128, 2, 256]), op=ALU.mult),
                         r=[B_Wb, B_pt], w=[B_zt])
                    S.op("dve", lambda e: e.tensor_tensor(out=zb[:, cs, :], in0=zt, in1=A[:, cs, 16:272], op=ALU.subtract),
                         r=[B_zt, B_ub[j]], w=[B_z])
                else:
                    S.op("dve", lambda e: e.scalar_tensor_tensor(out=zb[:, cs, :], in0=W[:, cs, 16:272], scalar=1.0 / wins[g],
                                                                 in1=A[:, cs, 16:272], op0=ALU.mult, op1=ALU.subtract),
                         r=[B_Wb, B_ub[j]], w=[B_z])
            for t in range(2):
                jt = (i * 2 + t) % 2
                banks = [2 + 2 * jt, 3 + 2 * jt]
                for g in range(4):
                    bk = banks[g // 2]
                    for cc in range(2):
                        S.op("pe", lambda e: e.matmul(pb[bk][:, (g % 2) * 256:(g % 2 + 1) * 256], lhsT=zb[:, 2 * g + cc, t * 128:(t + 1) * 128],
                                                      rhs=wpb[:, g, cc, :], start=(cc == 0), stop=(cc == 1)),
                             r=[B_z, B_wp], w=[PB[bk]])
                S.op("act", lambda e: e.activation(out=tmpf[:, 0:512], in_=pb[banks[0]], func=AF.Square, accum_out=st[jt][:, 0:1]),
                     r=[PB[banks[0]]], w=[B_tmpf, B_st[jt]])
                S.op("act", lambda e: e.activation(out=tmpf[:, 512:1024], in_=pb[banks[1]], func=AF.Square, accum_out=st[jt][:, 3:4]),
                     r=[PB[banks[1]]], w=[B_tmpf, B_st[jt]])
                S.op("dve", lambda e: e.tensor_tensor(out=st[jt][:, 0:1], in0=st[jt][:, 0:1], in1=st[jt][:, 3:4], op=ALU.add),
                     r=[B_st[jt]], w=[B_st[jt]])
                S.op("act", lambda e: e.activation(out=st[jt][:, 1:2], in_=st[jt][:, 0:1], func=AF.Sqrt, scale=1.0 / PW, bias=EPS),
                     r=[B_st[jt]], w=[B_st[jt]])
                S.op("dve", lambda e: e.reciprocal(out=st[jt][:, 2:3], in_=st[jt][:, 1:2]), r=[B_st[jt]], w=[B_st[jt]])
                for hh in range(2):
                    S.op("dve", lambda e: e.scalar_tensor_tensor(out=mp[jt][:, hh * 512:(hh + 1) * 512], in0=pb[banks[hh]], scalar=st[jt][:, 2:3],
                                                                 in1=psc[:, hh * 512:(hh + 1) * 512], op0=ALU.mult, op1=ALU.mult),
                         r=[PB[banks[hh]], B_st[jt], B_psc], w=[B_mp[jt]])
                row0 = (i * 2 + t) * 128
                S.dma("sp", lambda e: e.dma_start(out=MP_d[row0:row0 + 128, :], in_=mp[jt]), B_mp[jt], r=[B_mp[jt]])
        S.end_phase()

    def phase_B():
        scale = HD ** -0.5
        kmf = sb("B_kmf", [128, H, NBP], F32)
        B_kmf = S.buf("B_kmf")
        S.dma("sp", lambda e: e.dma_start(out=kmf, in_=KM_d), B_kmf, w=[B_kmf])
        kmb = sb("B_kmb", [128, H, NBP], BF16)
        B_kmb = S.buf("B_kmb")
        S.op("dve", lambda e: e.tensor_copy(out=kmb, in_=kmf), r=[B_kmf], w=[B_kmb])
        gbias = sb("B_gbias", [128, NBo, NBP], F32)
        B_gb = S.buf("B_gbias")
        S.dma("sp", lambda e: e.dma_start(out=gbias, in_=IN('gbias').rearrange("i p s -> p i s")), B_gb, w=[B_gb])
        mtab = sb("B_mtab", [128, NBo, 2, H, NBP], F32)
        B_mt = S.buf("B_mtab")
        for i in range(NBo):
            S.dma("sp", lambda e: e.dma_start(out=mtab[:, i], in_=IN('mtab')[i].rearrange("t p h s -> p t h s")), B_mt, w=[B_mt])
        ctab = sb("B_ctab", [128, H, 2, 128], F32)
        B_ct = S.buf("B_ctab")
        S.dma("sp", lambda e: e.dma_start(out=ctab, in_=IN('ctab')), B_ct, w=[B_ct])
        KT = [sb("B_KT%d" % i, [128, S_len], BF16) for i in range(2)]
        VP = [sb("B_VP%d" % i, [128, NT, 129], BF16) for i in range(2)]
        QT = [sb("B_QT%d" % i, [128, To], BF16) for i in range(2)]
        V1 = [sb("B_V1%d" % i, [128, NTo, 129], BF16) for i in range(2)]
        B_KT = S.bufs("B_KT", 2)
        B_VP = S.bufs("B_VP", 2)
        B_QT = S.bufs("B_QT", 2)
        B_V1 = S.bufs("B_V1", 2)
        gs = sb("B_gs", [128, 2, NBP], F32)
        B_gs = S.buf("B_gs")
        top8 = sb("B_top8", [128, 2, 8], F32)
        B_t8 = S.buf("B_top8")
        sel = sb("B_sel", [128, 2, NBP], F32)
        B_sel = S.buf("B_sel")
        mm = [sb("B_m%d" % i, [128, 2, NBP], F32) for i in range(2)]
        B_m = S.bufs("B_m", 2)
        pT = [sb("B_pT%d" % i, [128, 512], BF16) for i in range(2)]
        B_pT = S.bufs("B_pT", 2)
        acc = [sb("B_acc%d" % i, [128, 2, 129], F32) for i in range(2)]
        B_acc = [S.bufs("B_acc%d_" % i, 2) for i in range(2)]
        rec = sb("B_rec", [128, 2], F32)
        B_rec = S.buf("B_rec")
        oa = [sb("B_oa%d" % i, [128, 2, 128], F32) for i in range(2)]
        B_oa = S.bufs("B_oa", 2)
        itc = [0]

        def load_head(h):
            j = h % 2
            S.dma("sp", lambda e: e.dma_start(out=KT[j], in_=KT_d[h]), B_KT[j], w=[B_KT[j]])
            S.dma("sp", lambda e: e.dma_start(out=QT[j], in_=QT_d[h]), B_QT[j], w=[B_QT[j]])
            S.dma("sp", lambda e: e.dma_start(out=VP[j], in_=VP_d[h]), B_VP[j], w=[B_VP[j]])
            S.dma("sp", lambda e: e.dma_start(out=V1[j], in_=V1_d[h]), B_V1[j], w=[B_V1[j]])

        items = []
        for h in range(H):
            for i in range(NBo):
                a = (h * NBo + i) % 2
                cands = list(range(2 * i)) + [2 * i + 1]
                items.append(dict(h=h, i=i, a=a, own=True, s=2 * i, last=False))
                for ci, s_ in enumerate(cands):
                    items.append(dict(h=h, i=i, a=a, own=False, s=s_, last=(ci == len(cands) - 1)))

        def emit_S(k):
            it = items[k]
            h, i, j = it["h"], it["i"], it["h"] % 2
            sbk = 1 + k % 2
            q0 = i * 256
            if it["own"]:
                for t in range(2):
                    S.op("pe", lambda e: e.matmul(pb[0][:, t * 256:t * 256 + NBP], lhsT=QT[j][:, q0 + t * 128:q0 + (t + 1) * 128],
                                                  rhs=kmb[:, h, :], start=True, stop=True), r=[B_QT[j], B_kmb], w=[PB[0]])
                S.op("pe", lambda e: e.matmul(pb[sbk][:, 0:256], lhsT=KT[j][:, (2 * i) * 256:(2 * i) * 256 + 128], rhs=QT[j][:, q0:q0 + 256],
                                              start=True, stop=True), r=[B_KT[j], B_QT[j]], w=[PB[sbk]])
                S.op("pe", lambda e: e.matmul(pb[sbk][:, 384:512], lhsT=KT[j][:, (2 * i) * 256 + 128:(2 * i) * 256 + 256],
                                              rhs=QT[j][:, q0 + 128:q0 + 256], start=True, stop=True), r=[B_KT[j], B_QT[j]], w=[PB[sbk]])
            else:
                s_ = it["s"]
                for kh in range(2):
                    S.op("pe", lambda e: e.matmul(pb[sbk][:, kh * 256:(kh + 1) * 256], lhsT=KT[j][:, s_ * 256 + kh * 128:s_ * 256 + (kh + 1) * 128],
                                                  rhs=QT[j][:, q0:q0 + 256], start=True, stop=True), r=[B_KT[j], B_QT[j]], w=[PB[sbk]])

        def emit_exp(k):
            it = items[k]
            h = it["h"]
            sbk = 1 + k % 2
            pt = pT[k % 2]
            B_pt_ = B_pT[k % 2]
            if it["own"]:
                S.op("act", lambda e: e.activation(out=pt[:, 0:256], in_=pb[sbk][:, 0:256], func=AF.Exp, scale=scale), r=[PB[sbk]], w=[B_pt_])
                S.op("act", lambda e: e.activation(out=pt[:, 384:512], in_=pb[sbk][:, 384:512], func=AF.Exp, scale=scale), r=[PB[sbk]], w=[B_pt_])
                S.op("dve", lambda e: e.tensor_tensor(out=pt[:, 0:256], in0=pt[:, 0:256], in1=ctab[:, h, :, :].rearrange("p a b -> p (a b)"), op=ALU.mult),
                     r=[B_pt_, B_ct], w=[B_pt_])
                S.op("dve", lambda e: e.tensor_tensor(out=pt[:, 384:512], in0=pt[:, 384:512], in1=ctab[:, h, 0, :], op=ALU.mult),
                     r=[B_pt_, B_ct], w=[B_pt_])
            else:
                S.op("act", lambda e: e.activation(out=pt, in_=pb[sbk], func=AF.Exp, scale=scale), r=[PB[sbk]], w=[B_pt_])

        def emit_rest(k):
            it = items[k]
            h, i, a, j = it["h"], it["i"], it["a"], it["h"] % 2
            obk = 3 + k % 2
            pt = pT[k % 2]
            B_pt_ = B_pT[k % 2]
            q0 = i * 256
            if it["own"]:
                if i == 0 and h + 1 < H:
                    load_head(h + 1)
                gv = pb[0].rearrange("p (t n) -> p t n", n=256)[:, :, 0:NBP]
                S.op("dve", lambda e: e.tensor_tensor(out=gs, in0=gv, in1=gbias[:, i, :].unsqueeze(1).to_broadcast([128, 2, NBP]), op=ALU.add),
                     r=[PB[0], B_gb], w=[B_gs])
                for t in range(2):
                    S.op("dve", lambda e: e.max(out=top8[:, t, :], in_=gs[:, t, :]), r=[B_gs], w=[B_t8])
                for t in range(2):
                    S.op("dve", lambda e: e.tensor_scalar(out=sel[:, t, :], in0=gs[:, t, :], scalar1=top8[:, t, 2:3], scalar2=None, op0=ALU.is_ge),
                         r=[B_gs, B_t8], w=[B_sel])
                S.op("dve", lambda e: e.tensor_tensor(out=mm[a], in0=sel, in1=mtab[:, i, :, h, :], op=ALU.mult), r=[B_sel, B_mt], w=[B_m[a]])
                S.op("pe", lambda e: e.matmul(pb[obk][:, 0:129], lhsT=pt[:, 0:128], rhs=V1[j][:, 2 * i, :], start=True, stop=True),
                     r=[B_pt_, B_V1[j]], w=[PB[obk]])
                S.op("pe", lambda e: e.matmul(pb[obk][:, 256:385], lhsT=pt[:, 128:256], rhs=V1[j][:, 2 * i, :], start=True, stop=False),
                     r=[B_pt_, B_V1[j]], w=[PB[obk]])
                S.op("pe", lambda e: e.matmul(pb[obk][:, 256:385], lhsT=pt[:, 384:512], rhs=V1[j][:, 2 * i + 1, :], start=False, stop=True),
                     r=[B_pt_, B_V1[j]], w=[PB[obk]])
                ov = pb[obk].rearrange("p (t n) -> p t n", n=256)[:, :, 0:129]
                S.op("dve", lambda e: e.tensor_copy(out=acc[a], in_=ov), r=[PB[obk]], w=[B_acc[a][0], B_acc[a][1]])
            else:
                s_ = it["s"]
                for t in range(2):
                    for kh in range(2):
                        S.op("pe", lambda e: e.matmul(pb[obk][:, t * 256:t * 256 + 129], lhsT=pt[:, kh * 256 + t * 128:kh * 256 + (t + 1) * 128],
                                                      rhs=VP[j][:, s_ * 2 + kh, :], start=(kh == 0), stop=(kh == 1)),
                             r=[B_pt_, B_VP[j]], w=[PB[obk]])
                for t in range(2):
                    S.op("dve", lambda e: e.scalar_tensor_tensor(out=acc[a][:, t, :], in0=pb[obk][:, t * 256:t * 256 + 129],
                                                                 scalar=mm[a][:, t, s_:s_ + 1], in1=acc[a][:, t, :],
                                                                 op0=ALU.mult, op1=ALU.add), r=[PB[obk], B_m[a], B_acc[a][t]], w=[B_acc[a][t]])
            if it["last"]:
                S.op("dve", lambda e: e.reciprocal(out=rec, in_=acc[a][:, :, 128]), r=[B_acc[a][0], B_acc[a][1]], w=[B_rec])
                for t in range(2):
                    S.op("dve", lambda e: e.tensor_scalar(out=oa[a][:, t, :], in0=acc[a][:, t, 0:128], scalar1=rec[:, t:t + 1], scalar2=None, op0=ALU.mult),
                         r=[B_acc[a][t], B_rec], w=[B_oa[a]])
                S.dma("sp", lambda e: e.dma_start(out=OA_d[q0:q0 + 256, h * 128:(h + 1) * 128].rearrange("(t p) c -> p t c", p=128), in_=oa[a]),
                      B_oa[a], r=[B_oa[a]])

        load_head(0)
        emit_S(0)
        for k in range(len(items)):
            emit_exp(k)
            if k + 1 < len(items):
                emit_S(k + 1)
            emit_rest(k)
        S.end_phase()

    WIDX = nc.alloc_sbuf_tensor("G_widx", [128, 128], I32).ap()
    B_widx = Buf("G_widx")
    DESTI = nc.alloc_sbuf_tensor("G_desti", [128, NTo, 2], I32).ap()
    B_desti = Buf("G_desti")

    def phase_C():
        Wo = sb("C_Wo", [128, 16, D], BF16)
        B_Wo = S.buf("C_Wo")
        for k in range(16):
            S.dma("pool", lambda e: e.dma_start(out=Wo[:, k, :], in_=IN('w_out')[k * 128:(k + 1) * 128, :]), B_Wo, w=[B_Wo])
        beta = sb("C_beta", [128, AW], F32)
        B_beta = S.buf("C_beta")
        S.dma("sp", lambda e: e.dma_start(out=beta, in_=IN('beta_attn').partition_broadcast(128)), B_beta, w=[B_beta])
        gffn = sb("C_gffn", [128, D], F32)
        B_gffn = S.buf("C_gffn")
        S.dma("sp", lambda e: e.dma_start(out=gffn, in_=IN('g_ffn').partition_broadcast(128)), B_gffn, w=[B_gffn])
        Wr32 = sb("C_Wr32", [128, 16, 72], F32)
        B_Wr32 = S.buf("C_Wr32")
        S.dma("sp", lambda e: e.dma_start(out=Wr32[:, :, 0:8], in_=IN('w_rg').rearrange("(k p) e -> p k e", p=128)), B_Wr32, w=[B_Wr32])
        for g in range(NG):
            S.dma("sp", lambda e: e.dma_start(out=Wr32[:, :, 8 + g * 8:16 + g * 8], in_=IN('w_re')[g].rearrange("(k p) e -> p k e", p=128)),
                  B_Wr32, w=[B_Wr32])
        Wr = sb("C_Wr", [128, 16, 72], BF16)
        B_Wr = S.buf("C_Wr")
        S.op("dve", lambda e: e.tensor_copy(out=Wr, in_=Wr32), r=[B_Wr32], w=[B_Wr])
        brb = sb("C_brb", [128, 72], F32)
        B_brb = S.buf("C_brb")
        S.dma("sp", lambda e: e.dma_start(out=brb[:, 0:8], in_=IN('b_rg').partition_broadcast(128)), B_brb, w=[B_brb])
        S.dma("sp", lambda e: e.dma_start(out=brb[:, 8:72], in_=IN('b_re').partition_broadcast(128)), B_brb, w=[B_brb])
        ltb = sb("C_ltb", [128, 128], BF16)
        B_ltb = S.buf("C_ltb")
        S.dma("pool", lambda e: e.dma_start(out=ltb, in_=IN('ltri')), B_ltb, w=[B_ltb])
        onesb = sb("C_ones", [128, 128], BF16)
        B_ones = S.buf("C_ones")
        S.op("dve", lambda e: e.memset(onesb, 1.0), w=[B_ones])
        pidx = sb("C_pidx", [128, 1], F32)
        B_pidx = S.buf("C_pidx")
        S.dma("sp", lambda e: e.dma_start(out=pidx, in_=IN('pidx')), B_pidx, w=[B_pidx])
        bpos = sb("C_bpos", [128, 1], F32)
        B_bpos = S.buf("C_bpos")
        S.dma("sp", lambda e: e.dma_start(out=bpos, in_=IN('bpos')), B_bpos, w=[B_bpos])
        zer = sb("C_zer", [128, (PL // 128) * 2], F32)
        B_zer = S.buf("C_zer")
        S.op("dve", lambda e: e.memset(zer, 0.0), w=[B_zer])
        B_slotd = S.buf("C_slotd")
        S.dma("sp", lambda e: e.dma_start(out=SLOT_d.rearrange("(p n) c -> p (n c)", p=128), in_=zer), B_zer, r=[B_zer], w=[B_slotd])
        OHK = sb("C_OHK", [128, NTo, 2, 64], F32)
        B_OHK = S.buf("C_OHK")
        RK = sb("C_RK", [128, NTo, 2], F32)
        B_RK = S.buf("C_RK")
        WK = sb("C_WK", [128, NTo, 2], F32)
        B_WK = S.buf("C_WK")
        ohacc = sb("C_ohacc", [128, 64], BF16)
        B_ohacc = S.buf("C_ohacc")
        S.op("dve", lambda e: e.memset(ohacc, 0.0), w=[B_ohacc])
        oa = [sb("C_oa%d" % i, [128, AW], F32) for i in range(2)]
        B_oa = S.bufs("C_oa", 2)
        mx = [sb("C_mx%d" % i, [128, D], BF16) for i in range(2)]
        B_mx = S.bufs("C_mx", 2)
        xt = [sb("C_x%d" % i, [128, D], F32) for i in range(2)]
        B_xt = S.bufs("C_x", 2)
        mTs = [sb("C_mT%d" % i, [128, 16, 128], BF16) for i in range(2)]
        B_mTs = S.bufs("C_mT", 2)
        h1 = [sb("C_h1%d" % i, [128, D], F32) for i in range(2)]
        B_h1 = S.bufs("C_h1", 2)
        fb = [sb("C_f%d" % i, [128, D], BF16) for i in range(2)]
        B_fb = S.bufs("C_f", 2)
        fT = sb("C_fT", [128, 16, 128], BF16)
        B_fT = S.buf("C_fT")
        st = [sb("C_st%d" % i, [128, 8], F32) for i in range(2)]
        B_st = S.bufs("C_st", 2)
        st2 = [sb("C_su%d" % i, [128, 8], F32) for i in range(2)]
        B_st2 = S.bufs("C_su", 2)
        lg = sb("C_lg", [128, 72], F32)
        B_lg = S.buf("C_lg")
        rs = sb("C_rs", [128, 32], F32)
        B_rs = S.buf("C_rs")
        ohg = sb("C_ohg", [128, 8], F32)
        B_ohg = S.buf("C_ohg")
        tmp88 = sb("C_tmp88", [128, 8, 8], F32)
        B_t88 = S.buf("C_tmp88")
        le = sb("C_le", [128, 8], F32)
        B_le = S.buf("C_le")
        t8 = sb("C_t8", [128, 8], F32)
        B_t8 = S.buf("C_t8")
        ohk = sb("C_ohk", [128, 2, 8], F32)
        B_ohk = S.buf("C_ohk")
        ohs = sb("C_ohs", [128, 64], BF16)
        B_ohs = S.buf("C_ohs")
        cum = sb("C_cum", [128, 64], F32)
        B_cum = S.buf("C_cum")
        tmp64 = sb("C_tmp64", [128, 64], F32)
        B_t64 = S.buf("C_tmp64")

        def load(j):
            b = j % 2
            i, t = j // 2, j % 2
            xr = i * 512 + t * 128
            S.dma("sp", lambda e: e.dma_start(out=oa[b], in_=OA_d[j * 128:(j + 1) * 128, :]), B_oa[b], w=[B_oa[b]])
            S.dma("sp", lambda e: e.dma_start(out=xt[b], in_=IN('x_perm')[xr:xr + 128, :]), B_xt[b], w=[B_xt[b]])
            S.dma("sp", lambda e: e.dma_start(out=mx[b][:, AW:D], in_=MP_d[j * 128:(j + 1) * 128, :]), B_mx[b], w=[B_mx[b]])

        def stageA(j):
            b = j % 2
            mT = mTs[b]
            B_mT = B_mTs[b]
            rmsnorm_rstd(oa[b], B_oa[b], mx[b][:, 0:AW], B_mx[b], st[b], B_st[b], AW)
            S.op("dve", lambda e: e.scalar_tensor_tensor(out=mx[b][:, 0:AW], in0=oa[b], scalar=st[b][:, 2:3], in1=beta, op0=ALU.mult, op1=ALU.mult),
                 r=[B_oa[b], B_st[b], B_beta], w=[B_mx[b]])
            transpose_to(mx[b], B_mx[b], 16, lambda k0, k1: mT[:, k0:k1, :], B_mT, [0, 1])

        load(0)
        if NTo > 1:
            load(1)
        stageA(0)
        for j in range(NTo):
            b = j % 2
            mT = mTs[b]
            B_mT = B_mTs[b]
            if j + 1 < NTo:
                stageA(j + 1)
            for n in range(4):
                bk = 2 + n
                for k in range(16):
                    S.op("pe", lambda e: e.matmul(pb[bk], lhsT=mT[:, k, :], rhs=Wo[:, k, n * 512:(n + 1) * 512], start=(k == 0), stop=(k == 15)),
                         r=[B_mT, B_Wo], w=[PB[bk]])
                S.op("dve", lambda e: e.tensor_tensor(out=h1[b][:, n * 512:(n + 1) * 512], in0=pb[bk], in1=xt[b][:, n * 512:(n + 1) * 512], op=ALU.add),
                     r=[PB[bk], B_xt[b]], w=[B_h1[b]])
            S.dma("sp", lambda e: e.dma_start(out=H1_d[j * 128:(j + 1) * 128, :], in_=h1[b]), B_h1[b], r=[B_h1[b]])
            if j + 2 < NTo:
                load(j + 2)
            rmsnorm_rstd(h1[b], B_h1[b], fb[b], B_fb[b], st2[b], B_st2[b], D)
            S.op("dve", lambda e: e.scalar_tensor_tensor(out=fb[b], in0=h1[b], scalar=st2[b][:, 2:3], in1=gffn, op0=ALU.mult, op1=ALU.mult),
                 r=[B_h1[b], B_st2[b], B_gffn], w=[B_fb[b]])
            S.dma("sp", lambda e: e.dma_start(out=F_d[j * 128:(j + 1) * 128, :], in_=fb[b]), B_fb[b], r=[B_fb[b]])
            transpose_to(fb[b], B_fb[b], 16, lambda k0, k1: fT[:, k0:k1, :], B_fT, [0, 1])
            bk = 6
            for k in range(16):
                S.op("pe", lambda e: e.matmul(pb[bk][:, 0:72], lhsT=fT[:, k, :], rhs=Wr[:, k, :], start=(k == 0), stop=(k == 15)),
                     r=[B_fT, B_Wr], w=[PB[bk]])
            S.op("dve", lambda e: e.tensor_tensor(out=lg, in0=pb[bk][:, 0:72], in1=brb, op=ALU.add), r=[PB[bk], B_brb], w=[B_lg])
            S.op("dve", lambda e: e.reduce_max(out=rs[:, 0:1], in_=lg[:, 0:8], axis=AX.X), r=[B_lg], w=[B_rs])
            S.op("dve", lambda e: e.tensor_scalar(out=ohg, in0=lg[:, 0:8], scalar1=rs[:, 0:1], scalar2=None, op0=ALU.is_equal), r=[B_lg, B_rs], w=[B_ohg])
            S.op("dve", lambda e: e.tensor_scalar(out=rs[:, 1:2], in0=rs[:, 0:1], scalar1=-1.0, scalar2=None, op0=ALU.mult), r=[B_rs], w=[B_rs])
            S.op("act", lambda e: e.activation(out=t8, in_=lg[:, 0:8], func=AF.Exp, bias=rs[:, 1:2], scale=1.0, accum_out=rs[:, 2:3]),
                 r=[B_lg, B_rs], w=[B_t8, B_rs])
            S.op("dve", lambda e: e.reciprocal(out=rs[:, 3:4], in_=rs[:, 2:3]), r=[B_rs], w=[B_rs])
            S.op("dve", lambda e: e.tensor_tensor(out=tmp88, in0=lg[:, 8:72].rearrange("p (g e) -> p g e", e=8),
                                                  in1=ohg.unsqueeze(2).to_broadcast([128, 8, 8]), op=ALU.mult), r=[B_lg, B_ohg], w=[B_t88])
            S.op("dve", lambda e: e.reduce_sum(out=le, in_=tmp88.rearrange("p g e -> p e g"), axis=AX.X), r=[B_t88], w=[B_le])
            S.op("dve", lambda e: e.max(out=t8, in_=le), r=[B_le], w=[B_t8])
            for k2 in range(2):
                S.op("dve", lambda e: e.tensor_scalar(out=ohk[:, k2, :], in0=le, scalar1=t8[:, k2:k2 + 1], scalar2=None, op0=ALU.is_equal),
                     r=[B_le, B_t8], w=[B_ohk])
            S.op("dve", lambda e: e.tensor_tensor(out=rs[:, 4:5], in0=t8[:, 1:2], in1=t8[:, 0:1], op=ALU.subtract), r=[B_t8], w=[B_rs])
            S.op("act", lambda e: e.activation(out=rs[:, 5:6], in_=rs[:, 4:5], func=AF.Exp), r=[B_rs], w=[B_rs])
            S.op("dve", lambda e: e.tensor_scalar(out=rs[:, 6:7], in0=rs[:, 5:6], scalar1=1.0, scalar2=None, op0=ALU.add), r=[B_rs], w=[B_rs])
            S.op("dve", lambda e: e.reciprocal(out=rs[:, 7:8], in_=rs[:, 6:7]), r=[B_rs], w=[B_rs])
            S.op("dve", lambda e: e.tensor_tensor(out=rs[:, 8:9], in0=rs[:, 5:6], in1=rs[:, 7:8], op=ALU.mult), r=[B_rs], w=[B_rs])
            S.op("dve", lambda e: e.tensor_tensor(out=WK[:, j, 0:1], in0=rs[:, 7:8], in1=rs[:, 3:4], op=ALU.mult), r=[B_rs], w=[B_WK])
            S.op("dve", lambda e: e.tensor_tensor(out=WK[:, j, 1:2], in0=rs[:, 8:9], in1=rs[:, 3:4], op=ALU.mult), r=[B_rs], w=[B_WK])
            for k2 in range(2):
                S.op("dve", lambda e: e.tensor_tensor(out=OHK[:, j, k2, :].rearrange("p (g e) -> p g e", e=8),
                                                      in0=ohg.unsqueeze(2).to_broadcast([128, 8, 8]),
                                                      in1=ohk[:, k2, :].unsqueeze(1).to_broadcast([128, 8, 8]), op=ALU.mult),
                     r=[B_ohg, B_ohk], w=[B_OHK])
            S.op("dve", lambda e: e.tensor_tensor(out=ohs, in0=OHK[:, j, 0, :], in1=OHK[:, j, 1, :], op=ALU.add), r=[B_OHK], w=[B_ohs])
            bk = 7
            S.op("pe", lambda e: e.matmul(pb[bk][:, 0:64], lhsT=ltb, rhs=ohs, start=True, stop=(j == 0)), r=[B_ltb, B_ohs], w=[PB[bk]])
            if j > 0:
                S.op("pe", lambda e: e.matmul(pb[bk][:, 0:64], lhsT=onesb, rhs=ohacc, start=False, stop=True), r=[B_ones, B_ohacc], w=[PB[bk]])
            S.op("dve", lambda e: e.tensor_copy(out=cum, in_=pb[bk][:, 0:64]), r=[PB[bk]], w=[B_cum])
            S.op("dve", lambda e: e.tensor_tensor(out=ohacc, in0=ohacc, in1=ohs, op=ALU.add), r=[B_ohacc, B_ohs], w=[B_ohacc])
            for k2 in range(2):
                S.op("dve", lambda e: e.tensor_tensor(out=tmp64, in0=OHK[:, j, k2, :], in1=cum, op=ALU.mult), r=[B_OHK, B_cum], w=[B_t64])
                S.op("dve", lambda e: e.reduce_sum(out=RK[:, j, k2:k2 + 1], in_=tmp64, axis=AX.X), r=[B_t64], w=[B_RK])
        bk = 7
        S.op("pe", lambda e: e.matmul(pb[bk][:, 0:64], lhsT=onesb, rhs=ohacc, start=True, stop=True), r=[B_ones, B_ohacc], w=[PB[bk]])
        cnt = sb("C_cnt", [128, 64], F32)
        B_cnt = S.buf("C_cnt")
        cnti = sb("C_cnti", [128, 64], I32)
        B_cnti = S.buf("C_cnti")
        padf = sb("C_padf", [128, 64], F32)
        B_padf = S.buf("C_padf")
        pend = sb("C_pend", [128, 64], F32)
        B_pend = S.buf("C_pend")
        pstart = sb("C_pstart", [128, 64], F32)
        B_pstart = S.buf("C_pstart")
        ones64 = sb("C_ones64", [128, 64], F32)
        B_o64 = S.buf("C_ones64")
        S.op("dve", lambda e: e.memset(ones64, 1.0), w=[B_o64])
        S.op("dve", lambda e: e.tensor_scalar(out=cnt, in0=pb[bk][:, 0:64], scalar1=127.0, scalar2=None, op0=ALU.add), r=[PB[bk]], w=[B_cnt])
        S.op("dve", lambda e: e.tensor_copy(out=cnti, in_=cnt), r=[B_cnt], w=[B_cnti])
        S.op("dve", lambda e: e.tensor_scalar(out=cnti, in0=cnti, scalar1=7, scalar2=7, op0=ALU.arith_shift_right, op1=ALU.logical_shift_left),
             r=[B_cnti], w=[B_cnti])
        S.op("dve", lambda e: e.tensor_copy(out=padf, in_=cnti), r=[B_cnti], w=[B_padf])
        S.op("dve", lambda e: e.tensor_tensor_scan(out=pend, data0=ones64, data1=padf, initial=0.0, op0=ALU.mult, op1=ALU.add),
             r=[B_o64, B_padf], w=[B_pend])
        S.op("dve", lambda e: e.tensor_tensor(out=pstart, in0=pend, in1=padf, op=ALU.subtract), r=[B_pend, B_padf], w=[B_pstart])
        S.op("dve", lambda e: e.tensor_scalar(out=tmp64, in0=pend, scalar1=bpos[:, 0:1], scalar2=None, op0=ALU.is_le), r=[B_pend, B_bpos], w=[B_t64])
        S.op("dve", lambda e: e.reduce_sum(out=rs[:, 10:11], in_=tmp64, axis=AX.X), r=[B_t64], w=[B_rs])
        S.op("dve", lambda e: e.tensor_scalar(out=rs[:, 11:12], in0=rs[:, 10:11], scalar1=63.0, scalar2=None, op0=ALU.min), r=[B_rs], w=[B_rs])
        S.op("dve", lambda e: e.tensor_scalar(out=rs[:, 13:14], in0=bpos, scalar1=-128.0, scalar2=None, op0=ALU.add), r=[B_bpos], w=[B_rs])
        S.op("dve", lambda e: e.tensor_scalar(out=tmp64, in0=pend, scalar1=rs[:, 13:14], scalar2=None, op0=ALU.is_le), r=[B_pend, B_rs], w=[B_t64])
        S.op("dve", lambda e: e.reduce_sum(out=rs[:, 14:15], in_=tmp64, axis=AX.X), r=[B_t64], w=[B_rs])
        S.op("dve", lambda e: e.tensor_scalar(out=rs[:, 15:16], in0=rs[:, 14:15], scalar1=63.0, scalar2=None, op0=ALU.min), r=[B_rs], w=[B_rs])
        S.op("dve", lambda e: e.tensor_tensor(out=rs[:, 16:17], in0=rs[:, 11:12], in1=rs[:, 15:16], op=ALU.not_equal), r=[B_rs], w=[B_rs])
        S.op("dve", lambda e: e.tensor_scalar(out=rs[:, 17:18], in0=pidx, scalar1=0.0, scalar2=None, op0=ALU.is_equal), r=[B_pidx], w=[B_rs])
        S.op("dve", lambda e: e.tensor_tensor(out=rs[:, 18:19], in0=rs[:, 16:17], in1=rs[:, 17:18], op=ALU.max), r=[B_rs], w=[B_rs])
        S.op("dve", lambda e: e.tensor_scalar(out=rs[:, 19:20], in0=rs[:, 11:12], scalar1=-64.0, scalar2=None, op0=ALU.add), r=[B_rs], w=[B_rs])
        S.op("dve", lambda e: e.tensor_tensor(out=rs[:, 20:21], in0=rs[:, 19:20], in1=rs[:, 18:19], op=ALU.mult), r=[B_rs], w=[B_rs])
        S.op("dve", lambda e: e.tensor_scalar(out=rs[:, 21:22], in0=rs[:, 20:21], scalar1=64.0, scalar2=None, op0=ALU.add), r=[B_rs], w=[B_rs])
        diag = sb("C_diag", [128, 128], BF16)
        B_diag = S.buf("C_diag")
        S.op("dve", lambda e: e.tensor_scalar(out=diag, in0=ident, scalar1=rs[:, 21:22], scalar2=None, op0=ALU.mult), r=[B_ident, B_rs], w=[B_diag])
        bk = 6
        S.op("pe", lambda e: e.matmul(pb[bk][:, 0:128], lhsT=onesb, rhs=diag, start=True, stop=True), r=[B_ones, B_diag], w=[PB[bk]])
        widf = sb("C_widf", [128, 128], F32)
        B_widf = S.buf("C_widf")
        S.op("dve", lambda e: e.tensor_scalar(out=widf, in0=pb[bk][:, 0:128], scalar1=128.0, scalar2=pidx[:, 0:1], op0=ALU.mult, op1=ALU.add),
             r=[PB[bk], B_pidx], w=[B_widf])
        S.op("dve", lambda e: e.tensor_copy(out=WIDX, in_=widf), r=[B_widf], w=[B_widx])
        destf = sb("C_destf", [128, NTo, 2], F32)
        B_destf = S.buf("C_destf")
        slt = [sb("C_slt%d" % i, [128, 2], F32) for i in range(4)]
        B_slt = S.bufs("C_slt", 4)
        for j in range(NTo):
            for k2 in range(2):
                S.op("dve", lambda e: e.tensor_tensor(out=tmp64, in0=OHK[:, j, k2, :], in1=pstart, op=ALU.mult), r=[B_OHK, B_pstart], w=[B_t64])
                S.op("dve", lambda e: e.reduce_sum(out=rs[:, 12:13], in_=tmp64, axis=AX.X), r=[B_t64], w=[B_rs])
                S.op("dve", lambda e: e.tensor_tensor(out=destf[:, j, k2:k2 + 1], in0=rs[:, 12:13], in1=RK[:, j, k2:k2 + 1], op=ALU.add),
                     r=[B_rs, B_RK], w=[B_destf])
        S.op("dve", lambda e: e.tensor_copy(out=DESTI, in_=destf), r=[B_destf], w=[B_desti])
        for j in range(NTo):
            for k2 in range(2):
                q = (j * 2 + k2) % 4
                S.op("dve", lambda e: e.tensor_scalar(out=slt[q][:, 0:1], in0=pidx, scalar1=float(j * 128), scalar2=None, op0=ALU.add),
                     r=[B_pidx], w=[B_slt[q]])
                S.op("dve", lambda e: e.tensor_copy(out=slt[q][:, 1:2], in_=WK[:, j, k2:k2 + 1]), r=[B_WK], w=[B_slt[q]])
                S.dma("pool", lambda e: e.indirect_dma_start(out=SLOT_d, out_offset=bass.IndirectOffsetOnAxis(ap=DESTI[:, j, k2:k2 + 1], axis=0),
                                                             in_=slt[q], in_offset=None),
                      B_slt[q], r=[B_slt[q], B_desti, B_slotd], w=[])
        if debug:
            S.dma("sp", lambda e: e.dma_start(out=RT_d.rearrange("(j p) c -> p j c", p=128)[:, :, 0:2], in_=destf), B_destf, r=[B_destf])
            S.dma("sp", lambda e: e.dma_start(out=RT_d.rearrange("(j p) c -> p j c", p=128)[:, :, 2:4], in_=WK), B_WK, r=[B_WK])
            S.dma("sp", lambda e: e.dma_start(out=RT_d[0:128, 4:6], in_=rs[:, 10:12]), B_rs, r=[B_rs])
        S.end_phase()

    def phase_D():
        wg = sb("D_wg", [128, 16 * DE], BF16)
        wu = sb("D_wu", [128, 16 * DE], BF16)
        wd = sb("D_wd", [128, 4 * D], BF16)
        B_wg = S.buf("D_wg")
        B_wu = S.buf("D_wu")
        B_wd = S.buf("D_wd")
        sl = [sb("D_sl%d" % i, [128, 2], F32) for i in range(2)]
        B_sl = S.bufs("D_sl", 2)
        ti = [sb("D_ti%d" % i, [128, 1], I32) for i in range(2)]
        B_ti = S.bufs("D_ti", 2)
        xg = [sb("D_xg%d" % i, [128, D], BF16) for i in range(2)]
        B_xg = S.bufs("D_xg", 2)
        xT = sb("D_xT", [128, 16, 128], BF16)
        B_xT = S.buf("D_xT")
        sg = sb("D_sg", [128, DE], F32)
        B_sg = S.buf("D_sg")
        hid = sb("D_hid", [128, DE], BF16)
        B_hid = S.buf("D_hid")
        hT = sb("D_hT", [128, 4, 128], BF16)
        B_hT = S.buf("D_hT")
        yb = [sb("D_y%d" % i, [128, D], F32) for i in range(2)]
        B_yb = S.bufs("D_y", 2)
        wgv = IN('w_eg').rearrange("e (p k) n -> (e p) (k n)", k=16)
        wuv = IN('w_eu').rearrange("e (p k) n -> (e p) (k n)", k=16)
        wdv = IN('w_ed').rearrange("e (p k) n -> (e p) (k n)", k=4)

        def load_x(b):
            j = b % 2
            S.dma("sp", lambda e: e.dma_start(out=sl[j], in_=SLOT_d[b * 128:(b + 1) * 128, :]), B_sl[j], w=[B_sl[j]])
            S.op("dve", lambda e: e.tensor_copy(out=ti[j], in_=sl[j][:, 0:1]), r=[B_sl[j]], w=[B_ti[j]])
            S.dma("pool", lambda e: e.indirect_dma_start(out=xg[j], out_offset=None, in_=F_d,
                                                         in_offset=bass.IndirectOffsetOnAxis(ap=ti[j][:, 0:1], axis=0)),
                  B_xg[j], r=[B_ti[j]], w=[B_xg[j]])

        def load_w(b, which):
            for (dst, B_dst, src) in which:
                S.dma("pool", lambda e: e.indirect_dma_start(out=dst, out_offset=None, in_=src,
                                                             in_offset=bass.IndirectOffsetOnAxis(ap=WIDX[:, b:b + 1], axis=0),
                                                             bounds_check=bc_reg, oob_is_err=False),
                      B_dst, r=[B_widx], w=[B_dst])

        bc_reg = nc.gpsimd.alloc_register("bc_reg")
        nc.gpsimd.reg_mov(bc_reg, NE * 128 - 1)
        GU = ((wg, B_wg, wgv), (wu, B_wu, wuv))
        DN = ((wd, B_wd, wdv),)
        load_x(0)
        load_w(0, GU)
        load_w(0, DN)
        for b in range(NBLK):
            j = b % 2
            if b + 1 < NBLK:
                load_x(b + 1)
            transpose_to(xg[j], B_xg[j], 16, lambda k0, k1: xT[:, k0:k1, :], B_xT, [0, 1], step=16)
            for (bk, wt, B_wt, nxt) in ((2, wg, B_wg, GU[0:1]), (3, wu, B_wu, GU[1:2])):
                for k in range(16):
                    S.op("pe", lambda e: e.matmul(pb[bk], lhsT=xT[:, k, :], rhs=wt[:, k * DE:(k + 1) * DE], start=(k == 0), stop=(k == 15)),
                         r=[B_xT, B_wt], w=[PB[bk]])
                if b + 1 < NBLK:
                    load_w(b + 1, nxt)
            S.op("act", lambda e: e.activation(out=sg, in_=pb[2], func=AF.Silu), r=[PB[2]], w=[B_sg])
            S.op("dve", lambda e: e.tensor_tensor(out=hid, in0=pb[3], in1=sg, op=ALU.mult), r=[PB[3], B_sg], w=[B_hid])
            transpose_to(hid, B_hid, 4, lambda k0, k1: hT[:, k0:k1, :], B_hT, [0, 1], step=4)
            for n in range(4):
                bk = 4 + n
                for k in range(4):
                    S.op("pe", lambda e: e.matmul(pb[bk], lhsT=hT[:, k, :], rhs=wd[:, k * D + n * 512:k * D + (n + 1) * 512],
                                                  start=(k == 0), stop=(k == 3)), r=[B_hT, B_wd], w=[PB[bk]])
            if b + 1 < NBLK:
                load_w(b + 1, DN)
            for n in range(4):
                bk = 4 + n
                if n % 2 == 0:
                    S.op("act", lambda e: e.activation(out=yb[j][:, n * 512:(n + 1) * 512], in_=pb[bk], func=AF.Copy, scale=sl[j][:, 1:2]),
                         r=[PB[bk], B_sl[j]], w=[B_yb[j]])
                else:
                    S.op("dve", lambda e: e.tensor_scalar(out=yb[j][:, n * 512:(n + 1) * 512], in0=pb[bk], scalar1=sl[j][:, 1:2], scalar2=None, op0=ALU.mult),
                         r=[PB[bk], B_sl[j]], w=[B_yb[j]])
            S.dma("sp", lambda e: e.dma_start(out=Y_d[b * 128:(b + 1) * 128, :], in_=yb[j]), B_yb[j], r=[B_yb[j]])
        S.end_phase()

    def phase_E():
        Wpg = sb("E_Wpg", [128, 16, D], BF16)
        B_Wpg = S.buf("E_Wpg")
        for k in range(16):
            S.dma("pool", lambda e: e.dma_start(out=Wpg[:, k, :], in_=IN('w_pg')[k * 128:(k + 1) * 128, :]), B_Wpg, w=[B_Wpg])
        Wpl = sb("E_Wpl", [128, 2, D], BF16)
        B_Wpl = S.buf("E_Wpl")
        for k in range(2):
            S.dma("pool", lambda e: e.dma_start(out=Wpl[:, k, :], in_=IN('w_ple')[k * 128:(k + 1) * 128, :]), B_Wpl, w=[B_Wpl])
        gple = sb("E_gple", [128, D], F32)
        bpg = sb("E_bpg", [128, D], F32)
        gfin = sb("E_gfin", [128, D], F32)
        B_gple = S.buf("E_gple")
        B_bpg = S.buf("E_bpg")
        B_gfin = S.buf("E_gfin")
        S.dma("sp", lambda e: e.dma_start(out=gple, in_=IN('g_ple').partition_broadcast(128)), B_gple, w=[B_gple])
        S.dma("sp", lambda e: e.dma_start(out=bpg, in_=IN('b_pg').partition_broadcast(128)), B_bpg, w=[B_bpg])
        S.dma("sp", lambda e: e.dma_start(out=gfin, in_=IN('g_final').partition_broadcast(128)), B_gfin, w=[B_gfin])
        h2 = [sb("E_h2%d" % i, [128, D], F32) for i in range(2)]
        y1 = [sb("E_y1%d" % i, [128, D], F32) for i in range(2)]
        y2 = [sb("E_y2%d" % i, [128, D], F32) for i in range(2)]
        pbf = [sb("E_p%d" % i, [128, PLE], BF16) for i in range(2)]
        B_h2 = S.bufs("E_h2", 2)
        B_y1 = S.bufs("E_y1", 2)
        B_y2 = S.bufs("E_y2", 2)
        B_pbf = S.bufs("E_p", 2)
        hn = sb("E_hn", [128, D], BF16)
        B_hn = S.buf("E_hn")
        hTs = [sb("E_hT%d" % i, [128, 16, 128], BF16) for i in range(2)]
        B_hTs = S.bufs("E_hT", 2)
        pTs = [sb("E_pT%d" % i, [128, 2, 128], BF16) for i in range(2)]
        B_pTs = S.bufs("E_pT", 2)
        gl = sb("E_gl", [128, 512], F32)
        B_gl = S.buf("E_gl")
        sgm = sb("E_sg", [128, 512], F32)
        B_sgm = S.buf("E_sg")
        h3 = sb("E_h3", [128, D], F32)
        B_h3 = S.buf("E_h3")
        ob = [sb("E_o%d" % i, [128, D], F32) for i in range(2)]
        B_ob = S.bufs("E_o", 2)
        st = [sb("E_st%d" % i, [128, 8], F32) for i in range(2)]
        B_st = S.bufs("E_st", 2)
        st2 = [sb("E_su%d" % i, [128, 8], F32) for i in range(2)]
        B_st2 = S.bufs("E_su", 2)

        def load(j):
            b = j % 2
            S.dma("sp", lambda e: e.dma_start(out=h2[b], in_=H1_d[j * 128:(j + 1) * 128, :]), B_h2[b], w=[B_h2[b]])
            S.dma("pool", lambda e: e.dma_start(out=pbf[b], in_=IN('p_own')[j * 128:(j + 1) * 128, :]), B_pbf[b], w=[B_pbf[b]])
            S.dma("pool", lambda e: e.indirect_dma_start(out=y1[b], out_offset=None, in_=Y_d,
                                                         in_offset=bass.IndirectOffsetOnAxis(ap=DESTI[:, j, 0:1], axis=0)),
                  B_y1[b], r=[B_desti], w=[B_y1[b]])
            S.dma("pool", lambda e: e.indirect_dma_start(out=y2[b], out_offset=None, in_=Y_d,
                                                         in_offset=bass.IndirectOffsetOnAxis(ap=DESTI[:, j, 1:2], axis=0)),
                  B_y2[b], r=[B_desti], w=[B_y2[b]])

        def stage1(j):
            b = j % 2
            S.op("dve", lambda e: e.tensor_tensor(out=h2[b], in0=h2[b], in1=y1[b], op=ALU.add), r=[B_h2[b], B_y1[b]], w=[B_h2[b]])
            S.op("dve", lambda e: e.tensor_tensor(out=h2[b], in0=h2[b], in1=y2[b], op=ALU.add), r=[B_h2[b], B_y2[b]], w=[B_h2[b]])
            rmsnorm_rstd(h2[b], B_h2[b], hn, B_hn, st[b], B_st[b], D)
            S.op("dve", lambda e: e.scalar_tensor_tensor(out=hn, in0=h2[b], scalar=st[b][:, 2:3], in1=gple, op0=ALU.mult, op1=ALU.mult),
                 r=[B_h2[b], B_st[b], B_gple], w=[B_hn])
            transpose_to(hn, B_hn, 16, lambda k0, k1: hTs[b][:, k0:k1, :], B_hTs[b], [0, 1])
            transpose_to(pbf[b], B_pbf[b], 2, lambda k0, k1: pTs[b][:, k0:k1, :], B_pTs[b], [0, 1])

        load(0)
        if NTo > 1:
            load(1)
        stage1(0)
        for j in range(NTo):
            b = j % 2
            hT, B_hT, pT, B_pT = hTs[b], B_hTs[b], pTs[b], B_pTs[b]
            if j + 1 < NTo:
                stage1(j + 1)
            for n in range(4):
                bg = 2 + (n % 2) * 2
                bp_ = 3 + (n % 2) * 2
                for k in range(16):
                    S.op("pe", lambda e: e.matmul(pb[bg], lhsT=hT[:, k, :], rhs=Wpg[:, k, n * 512:(n + 1) * 512], start=(k == 0), stop=(k == 15)),
                         r=[B_hT, B_Wpg], w=[PB[bg]])
                for k in range(2):
                    S.op("pe", lambda e: e.matmul(pb[bp_], lhsT=pT[:, k, :], rhs=Wpl[:, k, n * 512:(n + 1) * 512], start=(k == 0), stop=(k == 1)),
                         r=[B_pT, B_Wpl], w=[PB[bp_]])
                cs = slice(n * 512, (n + 1) * 512)
                S.op("dve", lambda e: e.tensor_tensor(out=gl, in0=pb[bg], in1=bpg[:, cs], op=ALU.add), r=[PB[bg], B_bpg], w=[B_gl])
                S.op("act", lambda e: e.activation(out=sgm, in_=gl, func=AF.Sigmoid), r=[B_gl], w=[B_sgm])
                S.op("dve", lambda e: e.tensor_tensor(out=sgm, in0=pb[bp_], in1=sgm, op=ALU.mult), r=[PB[bp_], B_sgm], w=[B_sgm])
                S.op("dve", lambda e: e.tensor_tensor(out=h3[:, cs], in0=h2[b][:, cs], in1=sgm, op=ALU.add), r=[B_h2[b], B_sgm], w=[B_h3])
            if j + 2 < NTo:
                load(j + 2)
            rmsnorm_rstd(h3, B_h3, ob[b], B_ob[b], st2[b], B_st2[b], D)
            S.op("dve", lambda e: e.scalar_tensor_tensor(out=ob[b], in0=h3, scalar=st2[b][:, 2:3], in1=gfin, op0=ALU.mult, op1=ALU.mult),
                 r=[B_h3, B_st2[b], B_gfin], w=[B_ob[b]])
            S.dma("sp", lambda e: e.dma_start(out=out[j * 128:(j + 1) * 128, :], in_=ob[b]), B_ob[b], r=[B_ob[b]])
        S.end_phase()

    phases = [("A", phase_A), ("A2", phase_A2), ("B", phase_B), ("C", phase_C), ("D", phase_D), ("E", phase_E)]
    for name, fn in phases:
        fn()
        c.stack.close()
        c.stack = contextlib.ExitStack()
        if name == upto:
            break
    return nc, S, c


def make_in_maps(inputs, S_len, used=None):
    x = np.asarray(inputs["x"], np.float32)
    Bn = x.shape[0]
    NB = S_len // 256
    NBo = NB // 2
    p = np.asarray(inputs["p"], np.float32)[0]
    sq = lambda k: np.ascontiguousarray(np.asarray(inputs[k], np.float32)[0])
    row = lambda a: np.ascontiguousarray(a.reshape(1, -1))
    shared = dict(
        g_mix=row(sq("g_mix")), w_in=sq("w_in"), beta_attn=row(sq("beta_attn")), w_pool=sq("w_pool"),
        pool_scale=row(sq("pool_scale")), w_out=sq("w_out"), g_ffn=row(sq("g_ffn")),
        w_rg=sq("w_router_group"), b_rg=row(sq("b_router_group")), w_re=sq("w_router_expert"),
        b_re=row(sq("b_router_expert")), w_eg=sq("w_expert_gate"), w_eu=sq("w_expert_up"),
        w_ed=sq("w_expert_down"), g_ple=row(sq("g_ple")), w_ple=sq("w_ple"), w_pg=sq("w_ple_gate"),
        b_pg=row(sq("b_ple_gate")), g_final=row(np.asarray(inputs["g_final"], np.float32)),
    )
    tabs = [host_tables(S_len, r) for r in range(2)]
    in_maps = []
    orders = []
    for cidx in range(2 * Bn):
        b, r = cidx // 2, cidx % 2
        order = []
        for i in range(NBo):
            order += [2 * i + r, 2 * i + 1 - r]
        own = [2 * i + r for i in range(NBo)]
        xb = x[b].reshape(NB, 256, D)
        pb_ = p[b].reshape(NB, 256, PLE)
        m = dict(shared)
        m["x_perm"] = np.ascontiguousarray(xb[order].reshape(S_len, D))
        m["p_own"] = np.ascontiguousarray(pb_[own].reshape(-1, PLE))
        m.update(tabs[r])
        if used is not None:
            m = {k: v for k, v in m.items() if k in used}
        in_maps.append(m)
        orders.append(own)
    return in_maps, orders


def kernel(**inputs):
    x = np.asarray(inputs["x"])
    Bn, S_len, _ = x.shape
    nc, S, c = build(S_len, debug=False, upto="E")
    in_maps, orders = make_in_maps(inputs, S_len, used=set(c.used))
    ncores = 2 * Bn
    res = run_bass_kernel_spmd(nc, in_maps, core_ids=list(range(ncores)))
    outp = np.empty((Bn, S_len // 256, 256, D), np.float32)
    for cidx in range(ncores):
        b = cidx // 2
        o = np.asarray(res.results[cidx]["out"], np.float32).reshape(-1, 256, D)
        outp[b, orders[cidx]] = o
    return outp.reshape(Bn, S_len, D)
```
